# Optimizing a Trainium2 kernel written in Bass

```python
import math
import jax, jax.numpy as jnp
from jax import lax
import numpy as np

D_MODEL = 1024
BATCH = 16
SEQ = 4096
DEPTH = 2

HEAD_DIM = 64
Q_BLOCK = 128
A_HEADS = 4
IDX_HEADS = 4
IDX_DIM = 32
DSA_TOPK = 256
B_HEADS = 4
N_NSA_KV = 6
CMP_BLOCK = 32
CMP_STRIDE = 16
CMP_HIDDEN = 256
SLC_BLOCK = 64
SLC_TOPN = 16
WINDOW = 512
FORCE_SCORE = 1e9
C_HEADS = 4
C_VDIM = 2 * HEAD_DIM
N_BUCKETS = 32
MAX_DISTANCE = 128
N_BIAS_HEADS = A_HEADS + B_HEADS + C_HEADS
N_EXPERTS = 32
TOP_K = 4
D_FF = D_MODEL
SWIGLU_LIMIT = 7.0
SWIGLU_ALPHA = 1.702
DN_ALPHA = (2 * DEPTH) ** 0.25
DN_BETA = (8 * DEPTH) ** -0.25
LN_EPS = 1e-5
A_W = A_HEADS * HEAD_DIM
B_W = B_HEADS * HEAD_DIM
C_W = C_HEADS * C_VDIM
IN_SIZES = (A_HEADS * HEAD_DIM, HEAD_DIM, HEAD_DIM,
            IDX_HEADS * IDX_DIM, IDX_DIM, IDX_HEADS,
            B_HEADS * HEAD_DIM, N_NSA_KV * HEAD_DIM, 3 * B_HEADS,
            C_HEADS * 2 * HEAD_DIM, C_HEADS * 2 * HEAD_DIM, C_W,
            3 * D_MODEL)
D_IN = sum(IN_SIZES)

kernel_name = "hybrid_dsa_nsa_diff_moe_deepnorm_adaln"


def _split_points():
    return [int(v) for v in np.cumsum(IN_SIZES)[:-1]]


def _normal(key, shape, scale):
    return jax.random.normal(key, shape, jnp.float32) * scale


def layer_norm(x, g, b):
    xf = x.astype(jnp.float32)
    mu = jnp.mean(xf, axis=-1, keepdims=True)
    var = jnp.mean(jnp.square(xf - mu), axis=-1, keepdims=True)
    return ((xf - mu) * lax.rsqrt(var + LN_EPS) * g + b).astype(x.dtype)


def rms_norm(x, g):
    xf = x.astype(jnp.float32)
    return xf * lax.rsqrt(jnp.mean(jnp.square(xf), axis=-1, keepdims=True) + LN_EPS) * g


def masked_softmax(logits, mask):
    lg = jnp.where(mask, logits.astype(jnp.float32), -jnp.inf)
    m = jnp.max(lg, axis=-1, keepdims=True)
    m = jnp.where(jnp.isfinite(m), m, 0.0)
    p = jnp.exp(lg - m)
    return p / jnp.maximum(jnp.sum(p, axis=-1, keepdims=True), 1e-30)


def rel_bucket(dist):
    exact = N_BUCKETS // 2
    n = jnp.maximum(dist, 0)
    nf = jnp.maximum(n, 1).astype(jnp.float32)
    large = exact + (jnp.log(nf / exact) / math.log(MAX_DISTANCE / exact)
                     * (N_BUCKETS - exact)).astype(jnp.int32)
    large = jnp.minimum(large, N_BUCKETS - 1)
    return jnp.where(n < exact, n, large)


def _gather_rows(table, idx):
    return jax.vmap(lambda tb, ib: tb[ib])(table, idx)


def _slc_overlap(n_cmp, n_slc):
    start = np.arange(n_cmp) * CMP_STRIDE
    end = start + CMP_BLOCK
    bs = np.arange(n_slc) * SLC_BLOCK
    ov = (start[:, None] < bs[None, :] + SLC_BLOCK) & (end[:, None] > bs[None, :])
    return ov.astype(np.float32)


def adaln(c, w, b):
    mod = jax.nn.silu(c) @ w + b
    shift, scale, gate = jnp.split(mod, 3, axis=-1)
    return shift[:, None, :], scale[:, None, :], gate[:, None, :]


def hybrid_mixer(h, w_in, cmp_pos, cmp_w1, cmp_w2, diff_lambda, diff_norm_g,
                 w_branch_a, w_branch_b, w_branch_c, w_out, rel_bias, lam_init):
    Bn, S, _ = h.shape
    f32 = jnp.float32
    scale = HEAD_DIM ** -0.5
    proj = h @ w_in
    (aq, ak, av, iq, ik, iw, bq, bkv, bg, cq, ck, cv, mg) = jnp.split(proj, _split_points(), axis=-1)
    aq = aq.reshape(Bn, S, A_HEADS, HEAD_DIM)
    iq = iq.reshape(Bn, S, IDX_HEADS, IDX_DIM)
    bq = bq.reshape(Bn, S, B_HEADS, HEAD_DIM)
    bkv = bkv.reshape(Bn, S, N_NSA_KV, HEAD_DIM)
    k_cmp_raw, v_cmp_raw, k_slc, v_slc, k_win, v_win = [bkv[:, :, i] for i in range(N_NSA_KV)]
    bg = jax.nn.sigmoid(bg.reshape(Bn, S, B_HEADS, 3).astype(f32))
    cq = cq.reshape(Bn, S, C_HEADS, 2, HEAD_DIM)
    ck = ck.reshape(Bn, S, C_HEADS, 2, HEAD_DIM)
    cv = cv.reshape(Bn, S, C_HEADS, C_VDIM)

    n_cmp = (S - CMP_BLOCK) // CMP_STRIDE + 1
    cmp_idx = np.arange(n_cmp)[:, None] * CMP_STRIDE + np.arange(CMP_BLOCK)[None, :]
    cmp_last = jnp.asarray(cmp_idx[:, -1], jnp.int32)

    def compress(raw, j):
        blocks = raw[:, cmp_idx] + cmp_pos[j]
        hid = jax.nn.gelu(blocks.reshape(Bn, n_cmp, CMP_BLOCK * HEAD_DIM) @ cmp_w1[j])
        return hid @ cmp_w2[j]

    k_cmp = compress(k_cmp_raw, 0)
    v_cmp = compress(v_cmp_raw, 1)
    n_slc = S // SLC_BLOCK
    overlap = jnp.asarray(_slc_overlap(n_cmp, n_slc))
    k_slc_blk = k_slc.reshape(Bn, n_slc, SLC_BLOCK, HEAD_DIM)
    v_slc_blk = v_slc.reshape(Bn, n_slc, SLC_BLOCK, HEAD_DIM)
    k_win_pad = jnp.pad(k_win, ((0, 0), (WINDOW, 0), (0, 0)))
    v_win_pad = jnp.pad(v_win, ((0, 0), (WINDOW, 0), (0, 0)))

    k_dsa = min(DSA_TOPK, S // 4)
    n_sel = min(SLC_TOPN, n_slc)
    tab_a = rel_bias[:, :A_HEADS]
    tab_b = rel_bias[:, A_HEADS:A_HEADS + B_HEADS]
    tab_c = rel_bias[:, A_HEADS + B_HEADS:]
    dl = diff_lambda.astype(f32)
    lam = jnp.exp(jnp.sum(dl[0] * dl[1])) - jnp.exp(jnp.sum(dl[2] * dl[3])) + lam_init
    keys = jnp.arange(S, dtype=jnp.int32)

    def block_fn(qi):
        q0 = qi * Q_BLOCK
        t = q0 + jnp.arange(Q_BLOCK, dtype=jnp.int32)
        sl = lambda a: lax.dynamic_slice_in_dim(a, q0, Q_BLOCK, axis=1)
        causal = keys[None, :] <= t[:, None]

        isc = jnp.einsum('bthd,bsd->bths', sl(iq), ik) * (IDX_DIM ** -0.5)
        iscore = jnp.einsum('bth,bths->bts', sl(iw) * (IDX_HEADS ** -0.5), jax.nn.relu(isc))
        iscore = jnp.where(causal[None], iscore.astype(f32), -jnp.inf)
        top_val, top_idx = lax.top_k(iscore, k_dsa)
        valid_a = jnp.isfinite(top_val)
        ka = _gather_rows(ak, top_idx)
        va = _gather_rows(av, top_idx)
        la = jnp.einsum('bthd,btkd->bhtk', sl(aq), ka) * scale
        la = la + jnp.transpose(tab_a[rel_bucket(t[None, :, None] - top_idx)], (0, 3, 1, 2))
        pa = masked_softmax(la, valid_a[:, None])
        oa = jnp.einsum('bhtk,btkd->bthd', pa, va).reshape(Bn, Q_BLOCK, A_W)

        qb = sl(bq)
        lc = jnp.einsum('bthd,bnd->bhtn', qb, k_cmp) * scale
        cmask = cmp_last[None, :] <= t[:, None]
        p_cmp = masked_softmax(lc, cmask[None, None])
        o_cmp = jnp.einsum('bhtn,bnd->bthd', p_cmp, v_cmp)
        imp = jnp.einsum('bhtn,nj->btj', p_cmp, overlap)
        blk = jnp.arange(n_slc, dtype=jnp.int32)
        cur = t // SLC_BLOCK
        forced = (blk[None] == 0) | (blk[None] == cur[:, None]) | (blk[None] == cur[:, None] - 1)
        allowed = blk[None] <= cur[:, None]
        imp = jnp.where(allowed[None], jnp.where(forced[None], FORCE_SCORE, imp), -jnp.inf)
        sv, sidx = lax.top_k(imp, n_sel)
        ks = _gather_rows(k_slc_blk, sidx).reshape(Bn, Q_BLOCK, n_sel * SLC_BLOCK, HEAD_DIM)
        vs = _gather_rows(v_slc_blk, sidx).reshape(Bn, Q_BLOCK, n_sel * SLC_BLOCK, HEAD_DIM)
        tok = (sidx[..., None] * SLC_BLOCK + jnp.arange(SLC_BLOCK, dtype=jnp.int32)).reshape(Bn, Q_BLOCK, -1)
        smask = jnp.repeat(jnp.isfinite(sv), SLC_BLOCK, axis=-1) & (tok <= t[None, :, None])
        ls = jnp.einsum('bthd,btmd->bhtm', qb, ks) * scale
        ls = ls + jnp.transpose(tab_b[rel_bucket(t[None, :, None] - tok)], (0, 3, 1, 2))
        o_slc = jnp.einsum('bhtm,btmd->bthd', masked_softmax(ls, smask[:, None]), vs)
        kw = lax.dynamic_slice_in_dim(k_win_pad, q0, Q_BLOCK + WINDOW, axis=1)
        vw = lax.dynamic_slice_in_dim(v_win_pad, q0, Q_BLOCK + WINDOW, axis=1)
        pos = q0 - WINDOW + jnp.arange(Q_BLOCK + WINDOW, dtype=jnp.int32)
        dw = t[:, None] - pos[None, :]
        wmask = (dw >= 0) & (dw < WINDOW) & (pos[None, :] >= 0)
        lw = jnp.einsum('bthd,bsd->bhts', qb, kw) * scale
        lw = lw + jnp.transpose(tab_b[rel_bucket(dw)], (2, 0, 1))[None]
        o_win = jnp.einsum('bhts,bsd->bthd', masked_softmax(lw, wmask[None, None]), vw)
        g = sl(bg)
        ob = (g[..., 0:1] * o_cmp + g[..., 1:2] * o_slc + g[..., 2:3] * o_win).reshape(Bn, Q_BLOCK, B_W)

        lcd = jnp.einsum('bthjd,bshjd->bjhts', sl(cq), ck) * scale
        lcd = lcd + jnp.transpose(tab_c[rel_bucket(t[:, None] - keys[None, :])], (2, 0, 1))[None, None]
        pc = masked_softmax(lcd, causal[None, None, None])
        attn = pc[:, 0] - lam * pc[:, 1]
        oc = jnp.einsum('bhts,bshd->bthd', attn, cv)
        oc = (rms_norm(oc, diff_norm_g) * (1.0 - lam_init)).reshape(Bn, Q_BLOCK, C_W)
        return oa, ob, oc

    oa, ob, oc = lax.map(block_fn, jnp.arange(S // Q_BLOCK))
    unblock = lambda o: jnp.swapaxes(o, 0, 1).reshape(Bn, S, -1).astype(h.dtype)
    ya = unblock(oa) @ w_branch_a
    yb = unblock(ob) @ w_branch_b
    yc = unblock(oc) @ w_branch_c
    gates = jax.nn.sigmoid(mg.reshape(Bn, S, 3, D_MODEL))
    merged = gates[:, :, 0] * ya + gates[:, :, 1] * yb + gates[:, :, 2] * yc
    return merged @ w_out


def moe_ffn(h, router_w, router_b, w_gu, b_gu, w_down, b_down):
    Bn, S, D = h.shape
    tok = h.reshape(Bn * S, D)
    logits = (tok @ router_w + router_b).astype(jnp.float32)
    top_v, top_i = lax.top_k(logits, TOP_K)
    top_w = jax.nn.softmax(top_v, axis=-1)
    combine = jnp.sum(jax.nn.one_hot(top_i, N_EXPERTS, dtype=jnp.float32) * top_w[..., None], axis=1)
    out = jnp.zeros((Bn * S, D), jnp.float32)
    for e in range(N_EXPERTS):
        gu = tok @ w_gu[e] + b_gu[e]
        gate = jnp.minimum(gu[:, 0::2], SWIGLU_LIMIT)
        up = jnp.clip(gu[:, 1::2], -SWIGLU_LIMIT, SWIGLU_LIMIT)
        act = (up + 1.0) * (gate * jax.nn.sigmoid(SWIGLU_ALPHA * gate))
        out = out + combine[:, e:e + 1] * (act @ w_down[e] + b_down[e])
    return out.reshape(Bn, S, D).astype(h.dtype)


def setup_inputs(seed: int = 0) -> dict:
    key = jax.random.key(seed)
    ks = jax.random.split(key, 32)
    L, D = DEPTH, D_MODEL
    cmp_in = CMP_BLOCK * HEAD_DIM
    return {
        "x": _normal(ks[0], (BATCH, SEQ, D), 1.0),
        "c": _normal(ks[1], (BATCH, D), 1.0),
        "rel_bias": _normal(ks[2], (N_BUCKETS, N_BIAS_HEADS), 0.5),
        "mod_attn_w": _normal(ks[3], (L, D, 3 * D), 0.1 * D ** -0.5),
        "mod_attn_b": _normal(ks[4], (L, 3 * D), 0.02),
        "w_in": _normal(ks[5], (L, D, D_IN), D ** -0.5),
        "cmp_pos": _normal(ks[6], (L, 2, CMP_BLOCK, HEAD_DIM), 0.1),
        "cmp_w1": _normal(ks[7], (L, 2, cmp_in, CMP_HIDDEN), cmp_in ** -0.5),
        "cmp_w2": _normal(ks[8], (L, 2, CMP_HIDDEN, HEAD_DIM), CMP_HIDDEN ** -0.5),
        "diff_lambda": _normal(ks[9], (L, 4, HEAD_DIM), 0.1),
        "diff_norm_g": 1.0 + _normal(ks[10], (L, C_VDIM), 0.02),
        "w_branch_a": _normal(ks[11], (L, A_W, D), A_W ** -0.5),
        "w_branch_b": _normal(ks[12], (L, B_W, D), B_W ** -0.5),
        "w_branch_c": _normal(ks[13], (L, C_W, D), C_W ** -0.5),
        "w_out": _normal(ks[14], (L, D, D), DN_BETA * D ** -0.5),
        "ln1_g": 1.0 + _normal(ks[15], (L, D), 0.02),
        "ln1_b": _normal(ks[16], (L, D), 0.02),
        "mod_ffn_w": _normal(ks[17], (L, D, 3 * D), 0.1 * D ** -0.5),
        "mod_ffn_b": _normal(ks[18], (L, 3 * D), 0.02),
        "router_w": _normal(ks[19], (L, D, N_EXPERTS), D ** -0.5),
        "router_b": _normal(ks[20], (L, N_EXPERTS), 0.01),
        "exp_w_gu": _normal(ks[21], (L, N_EXPERTS, D, 2 * D_FF), D ** -0.5),
        "exp_b_gu": _normal(ks[22], (L, N_EXPERTS, 2 * D_FF), 0.02),
        "exp_w_down": _normal(ks[23], (L, N_EXPERTS, D_FF, D), DN_BETA * D_FF ** -0.5),
        "exp_b_down": _normal(ks[24], (L, N_EXPERTS, D), 0.02),
        "ln2_g": 1.0 + _normal(ks[25], (L, D), 0.02),
        "ln2_b": _normal(ks[26], (L, D), 0.02),
    }


def reference(x, c, rel_bias, mod_attn_w, mod_attn_b, w_in, cmp_pos, cmp_w1, cmp_w2,
              diff_lambda, diff_norm_g, w_branch_a, w_branch_b, w_branch_c, w_out,
              ln1_g, ln1_b, mod_ffn_w, mod_ffn_b, router_w, router_b,
              exp_w_gu, exp_b_gu, exp_w_down, exp_b_down, ln2_g, ln2_b):
    for l in range(DEPTH):
        lam_init = 0.8 - 0.6 * math.exp(-0.3 * l)
        shift, scale, gate = adaln(c, mod_attn_w[l], mod_attn_b[l])
        h = x * (1.0 + scale) + shift
        y = hybrid_mixer(h, w_in[l], cmp_pos[l], cmp_w1[l], cmp_w2[l], diff_lambda[l], diff_norm_g[l],
                         w_branch_a[l], w_branch_b[l], w_branch_c[l], w_out[l], rel_bias, lam_init)
        x = layer_norm(DN_ALPHA * x + (1.0 + gate) * y, ln1_g[l], ln1_b[l])
        shift, scale, gate = adaln(c, mod_ffn_w[l], mod_ffn_b[l])
        h = x * (1.0 + scale) + shift
        y = moe_ffn(h, router_w[l], router_b[l], exp_w_gu[l], exp_b_gu[l], exp_w_down[l], exp_b_down[l])
        x = layer_norm(DN_ALPHA * x + (1.0 + gate) * y, ln2_g[l], ln2_b[l])
    return x
```

```python
import math
from contextlib import ExitStack
import numpy as np
import concourse.bass as bass
import concourse.mybir as mybir
from concourse.bass_utils import run_bass_kernel_spmd

F32 = mybir.dt.float32
BF16 = mybir.dt.bfloat16
I32 = mybir.dt.int32
AF = mybir.ActivationFunctionType
ALU = mybir.AluOpType
AX = mybir.AxisListType

D = 1024
S = 4096
NSEQ = 2
T = NSEQ * S
DEPTH = 2
D_IN = 5808
NEG = -30000.0


class Buf:
    def __init__(self, name, t):
        self.name = name
        self.t = t
        self.wr = {}
        self.rd = {}
        self.dsem = None
        self.dcount = 0

    def __getitem__(self, idx):
        return self.t[idx]


class Sched:
    def __init__(self, nc, es):
        self.nc = nc
        self.es = es
        self.eng = {"pe": nc.tensor, "dve": nc.vector, "act": nc.scalar,
                    "pool": nc.gpsimd, "sp": nc.sync}
        self.sem = {}
        self.cnt = {}
        for k in ("pe", "dve", "act", "pool"):
            self.sem[k] = es.enter_context(nc.semaphore("s_" + k))
            self.cnt[k] = 0
        self.known = {k: {} for k in self.eng}
        self.nbuf = 0
        self.n_inst = 0
        self.sempool = []

    def sbuf(self, name, shape, dt, es=None):
        es = es or self.es
        self.nbuf += 1
        name = "%s_%d" % (name, self.nbuf)
        t = es.enter_context(self.nc.sbuf_tensor(name, list(shape), dt))
        b = Buf(name, t)
        b.es = es
        return b

    def psum(self, name, shape, dt=F32, es=None):
        es = es or self.es
        self.nbuf += 1
        name = "%s_%d" % (name, self.nbuf)
        t = es.enter_context(self.nc.psum_tensor(name, list(shape), dt))
        b = Buf(name, t)
        b.is_psum = True
        b.es = es
        return b

    def dram(self, name, shape, dt, kind=None):
        if kind is None:
            t = self.nc.dram_tensor(name, list(shape), dt)
            b = Buf(name, t.ap())
            b.es = self.es
            return b
        else:
            t = self.nc.dram_tensor(name, list(shape), dt, kind=kind)
        b = Buf(name, t.ap())
        b.es = self.es
        return b

    def _semobj(self, key):
        if isinstance(key, str):
            return self.sem[key]
        return key.dsem

    def _wait(self, ekey, deps):
        eng = self.eng[ekey]
        kn = self.known[ekey]
        for key, val in deps.items():
            if isinstance(key, str):
                if key == "pe" and ekey == "pe":
                    continue
                v = val
            else:
                v = key.dcount * 16
            if kn.get(key, 0) >= v:
                continue
            eng.wait_ge(self._semobj(key), v)
            kn[key] = v

    def _collect(self, reads, writes, nowaw):
        deps = {}
        def add(d):
            for k, v in d.items():
                if deps.get(k, 0) < v:
                    deps[k] = v
        for b in reads:
            add(b.wr)
            if getattr(b, "is_psum", False):
                add(b.rd)
        for b in writes:
            if not nowaw:
                add(b.wr)
            add(b.rd)
        return deps

    def op(self, ekey, fn, reads=(), writes=(), nowaw=False):
        deps = self._collect(reads, writes, nowaw)
        self._wait(ekey, deps)
        ins = fn()
        self.cnt[ekey] += 1
        ins.then_inc(self.sem[ekey], 1)
        tk = self.cnt[ekey]
        self.n_inst += 1
        for b in reads:
            if b.rd.get(ekey, 0) < tk:
                b.rd[ekey] = tk
        for b in writes:
            if nowaw:
                b.wr[ekey] = tk
            else:
                b.wr = {ekey: tk}
                b.rd = {}
        return ins

    def dma(self, qkey, out_ap, in_ap, reads=(), writes=(), track=None, nowaw=True, **kw):
        assert track is not None
        if track.dsem is None:
            if self.sempool:
                track.dsem, track.dcount = self.sempool.pop()
            else:
                track.dsem = self.es.enter_context(self.nc.semaphore("d_" + track.name))
        deps = self._collect(reads, writes, nowaw)
        self._wait(qkey, deps)
        ins = self.eng[qkey].dma_start(out=out_ap, in_=in_ap, **kw)
        ins.then_inc(track.dsem, 16)
        track.dcount += 1
        self.n_inst += 1
        for b in reads:
            b.rd[track] = track.dcount
        for b in writes:
            if nowaw:
                b.wr[track] = track.dcount
            else:
                b.wr = {track: track.dcount}
                b.rd = {}
        return ins

    def barrier_on(self, bufs):
        deps = {}
        for b in bufs:
            for d in (b.wr, b.rd):
                for k, v in d.items():
                    if deps.get(k, 0) < v:
                        deps[k] = v
        for e in self.eng:
            self._wait(e, deps)

    def finish(self, bufs):
        deps = {}
        for b in bufs:
            for k, v in b.wr.items():
                if deps.get(k, 0) < v:
                    deps[k] = v
        self._wait("sp", deps)


C_AQ, C_AK, C_AV, C_IQ, C_IK, C_IW = 0, 256, 320, 384, 512, 544
C_BQ, C_KCR, C_VCR, C_KS, C_VS, C_KW, C_VW, C_BG = 548, 804, 868, 932, 996, 1060, 1124, 1188
C_CQ, C_CK, C_CV, C_MG = 1200, 1712, 2224, 2736
FM_CHUNKS = [("aq0", 0, 128), ("aq1", 128, 128), ("ak", 256, 64), ("iq", 384, 128), ("ik", 512, 32),
             ("bq0", 548, 128), ("bq1", 676, 128), ("kvcr", 804, 128), ("ks", 932, 64), ("kw", 1060, 64),
             ("cq0", 1200, 128), ("cq1", 1328, 128), ("cq2", 1456, 128), ("cq3", 1584, 128),
             ("ck0", 1712, 128), ("ck1", 1840, 128), ("ck2", 1968, 128), ("ck3", 2096, 128)]
FM_ROW = {}
_r = 0
for _n, _c, _w in FM_CHUNKS:
    FM_ROW[_n] = _r
    _r += 128
FM_ROWS = _r
VV_W = 704


def sched_release(sc, bufs):
    for b in bufs:
        if b.dsem is not None:
            sc.sempool.append((b.dsem, b.dcount))
            b.dsem = None


def load_cast(sc, es, dst, dst_ap, src, src_ap):
    sc.dma("pool", dst_ap, src_ap, reads=[src], writes=[dst], track=dst)


def phase_adaln(sc, cfg, l, modw, modbT, modb, which, siluT, out_opsc, out_shf, out_gate, want):
    nc = sc.nc
    nseq = cfg["nseq"]
    with ExitStack() as es:
        wk = [sc.sbuf("adw", [128, 3072], F32, es) for _ in range(2)]
        bT = sc.sbuf("adbT", [128, 24], F32, es)
        gb = sc.sbuf("adgb", [128, 1024], F32, es)
        psA = sc.psum("adpsA", [128, 512], F32, es)
        psG = [[sc.psum("adpsG", [128, 512], F32, es) for _ in range(2)] for _ in range(nseq)]
        silu_bc = []
        if want == "gate":
            for b in range(nseq):
                t = sc.sbuf("silubc", [128, 8, 128], F32, es)
                sc.op("dve", lambda: nc.vector.tensor_copy(t[:], siluT[:, :, b:b + 1].to_broadcast([128, 8, 128])), reads=[siluT], writes=[t])
                silu_bc.append(t)
        sc.dma("sp", bT[:], modbT[l, which], reads=[modbT], writes=[bT], track=bT)
        sc.dma("sp", gb[:], modb[l:l + 1, 2048:3072].partition_broadcast(128), reads=[modb], writes=[gb], track=gb)
        for kc in range(8):
            w = wk[kc % 2]
            sc.dma("sp", w[:], modw[l, kc * 128:(kc + 1) * 128, :], reads=[modw], writes=[w], track=w, nowaw=False)
            for j in (range(16) if want == "fm" else []):
                sc.op("pe", lambda: nc.tensor.matmul(psA[:, j * nseq:(j + 1) * nseq], lhsT=w[:, j * 128:(j + 1) * 128],
                                                     rhs=siluT[:, kc, :], start=(kc == 0 and j == 0), stop=(kc == 7),
                                                     skip_group_check=True),
                      reads=[w, siluT], writes=[psA], nowaw=True)
            for b in (range(nseq) if want == "gate" else []):
                for hf in range(2):
                    sc.op("pe", lambda: nc.tensor.matmul(psG[b][hf][:, :], lhsT=silu_bc[b][:, kc, :],
                                                         rhs=w[:, 2048 + hf * 512:2048 + (hf + 1) * 512],
                                                         start=(kc == 0), stop=(kc == 7)),
                          reads=[w, silu_bc[b]], writes=[psG[b][hf]], nowaw=True)
        for j in (range(8) if want == "fm" else []):
            sc.op("dve", lambda: nc.vector.tensor_scalar(out=out_shf[:, j, :], in0=psA[:, j * nseq:(j + 1) * nseq],
                                                        scalar1=bT[:, j:j + 1], scalar2=None, op0=ALU.add),
                  reads=[psA, bT], writes=[out_shf], nowaw=True)
            sc.op("dve", lambda: nc.vector.tensor_scalar(out=out_opsc[:, j, :], in0=psA[:, (j + 8) * nseq:(j + 9) * nseq],
                                                        scalar1=bT[:, j + 8:j + 9], scalar2=1.0, op0=ALU.add, op1=ALU.add),
                  reads=[psA, bT], writes=[out_opsc], nowaw=True)
        for b in (range(nseq) if want == "gate" else []):
            for hf in range(2):
                sc.op("dve", lambda: nc.vector.scalar_tensor_tensor(out=out_gate[:, b, hf * 512:(hf + 1) * 512],
                                                                   in0=psG[b][hf][:, :], scalar=1.0,
                                                                   in1=gb[:, hf * 512:(hf + 1) * 512],
                                                                   op0=ALU.add, op1=ALU.add),
                      reads=[psG[b][hf], gb], writes=[out_gate], nowaw=True)
        sc.barrier_on([wk[0], wk[1], bT, gb, psA] + [p for q in psG for p in q] + silu_bc)
        sched_release(sc, [wk[0], wk[1], bT, gb])


def phase_proj(sc, cfg, l, xsrc, w_in, ident, opsc, shf, FM, VV, SM, MG):
    nc = sc.nc
    nseq, seq = cfg["nseq"], cfg["seq"]
    ntok = nseq * seq
    with ExitStack() as es:
        wb = sc.sbuf("w_in_bf", [128, 8, D_IN], BF16, es)
        for kc in range(8):
            sc.dma("pool", wb[:, kc, :], w_in[l, kc * 128:(kc + 1) * 128, :], reads=[w_in], writes=[wb], track=wb)
        xs = [sc.sbuf("xs", [128, 4, D], F32, es) for _ in range(2)]
        hT = [sc.sbuf("hT", [128, 8, 512], BF16, es) for _ in range(2)]
        fst = [sc.sbuf("fst", [128, 512], BF16, es) for _ in range(4)]
        vst = [sc.sbuf("vst", [128, VV_W], BF16, es) for _ in range(2)]
        sst = [sc.sbuf("sst", [128, 16], F32, es) for _ in range(2)]
        mst = [sc.sbuf("mst", [128, 3072], F32, es) for _ in range(2)]
        ps = [sc.psum("pps", [128, 512], F32, es) for _ in range(8)]
        pi = [0]
        def nextps():
            p = ps[pi[0] % 8]
            pi[0] += 1
            return p
        nblk = ntok // 512
        fi = 0
        for blk in range(nblk):
            b = (blk * 512) // seq
            x_t = xs[blk % 2]
            h_t = hT[blk % 2]
            sc.dma("sp", x_t[:], xsrc[blk * 512:(blk + 1) * 512, :].rearrange("(j p) d -> p j d", p=128),
                   reads=[xsrc], writes=[x_t], track=x_t, nowaw=False)
            for c in range(8):
                p = nextps()
                for j in range(4):
                    sc.op("pe", lambda: nc.tensor.transpose(p[:, j * 128:(j + 1) * 128], x_t[:, j, c * 128:(c + 1) * 128], ident[:]),
                          reads=[x_t, ident], writes=[p], nowaw=(j > 0))
                sc.op("act", lambda: nc.scalar.activation(out=h_t[:, c, :], in_=p[:, :], func=AF.Identity,
                                                          bias=shf[:, c, b:b + 1], scale=opsc[:, c, b:b + 1]),
                      reads=[p, shf, opsc], writes=[h_t], nowaw=(c > 0))
            for (nm, c0, wd) in FM_CHUNKS:
                p = nextps()
                for kc in range(8):
                    sc.op("pe", lambda: nc.tensor.matmul(p[:wd, :], lhsT=wb[:, kc, c0:c0 + wd], rhs=h_t[:, kc, :],
                                                         start=(kc == 0), stop=(kc == 7)),
                          reads=[wb, h_t], writes=[p], nowaw=(kc > 0))
                f = fst[fi % 4]
                ek = "dve" if fi % 2 == 0 else "act"
                if ek == "dve":
                    sc.op("dve", lambda: nc.vector.tensor_copy(f[:wd, :], p[:wd, :]), reads=[p], writes=[f])
                else:
                    sc.op("act", lambda: nc.scalar.copy(f[:wd, :], p[:wd, :]), reads=[p], writes=[f])
                r0 = FM_ROW[nm]
                sc.dma("sp", FM[r0:r0 + wd, blk * 512:(blk + 1) * 512], f[:wd, :], reads=[f], writes=[FM], track=f)
                fi += 1
            for j in range(4):
                tk = blk * 4 + j
                v_t, s_t, m_t = vst[tk % 2], sst[tk % 2], mst[tk % 2]
                tok0 = blk * 512 + j * 128
                def tm(c0, wd):
                    p = nextps()
                    for kc in range(8):
                        sc.op("pe", lambda: nc.tensor.matmul(p[:, :wd], lhsT=h_t[:, kc, j * 128:(j + 1) * 128],
                                                             rhs=wb[:, kc, c0:c0 + wd], start=(kc == 0), stop=(kc == 7)),
                              reads=[wb, h_t], writes=[p], nowaw=(kc > 0))
                    return p
                p = tm(C_AV, 64)
                sc.op("dve", lambda: nc.vector.tensor_copy(v_t[:, 0:64], p[:, 0:64]), reads=[p], writes=[v_t])
                p = tm(C_VS, 64)
                sc.op("dve", lambda: nc.vector.tensor_copy(v_t[:, 64:128], p[:, 0:64]), reads=[p], writes=[v_t], nowaw=True)
                p = tm(C_VW, 76)
                sc.op("dve", lambda: nc.vector.tensor_copy(v_t[:, 128:192], p[:, 0:64]), reads=[p], writes=[v_t], nowaw=True)
                sc.op("act", lambda: nc.scalar.activation(out=s_t[:, 4:16], in_=p[:, 64:76], func=AF.Sigmoid),
                      reads=[p], writes=[s_t])
                p = tm(C_IW, 4)
                sc.op("dve", lambda: nc.vector.tensor_copy(s_t[:, 0:4], p[:, 0:4]), reads=[p], writes=[s_t], nowaw=True)
                p = tm(C_CV, 512)
                sc.op("dve", lambda: nc.vector.tensor_copy(v_t[:, 192:704], p[:, :]), reads=[p], writes=[v_t], nowaw=True)
                sc.dma("sp", VV[tok0:tok0 + 128, :], v_t[:], reads=[v_t], writes=[VV], track=v_t)
                sc.dma("sp", SM[tok0:tok0 + 128, :], s_t[:], reads=[s_t], writes=[SM], track=s_t)
                for g in range(6):
                    p = tm(C_MG + g * 512, 512)
                    sc.op("act", lambda: nc.scalar.activation(out=m_t[:, g * 512:(g + 1) * 512], in_=p[:, :], func=AF.Sigmoid),
                          reads=[p], writes=[m_t], nowaw=(g > 0))
                sc.dma("sp", MG[tok0:tok0 + 128, :], m_t[:], reads=[m_t], writes=[MG], track=m_t)
        allb = [wb] + xs + hT + fst + vst + sst + mst + ps
        sc.barrier_on(allb)
        sched_release(sc, allb)


def rel_bucket_np(dist):
    n = np.maximum(dist, 0)
    nf = np.maximum(n, 1).astype(np.float32)
    large = 16 + (np.log(nf / 16) / np.float32(math.log(128 / 16)) * 16).astype(np.int32)
    large = np.minimum(large, 31)
    return np.where(n < 16, n, large)


def make_consts():
    import ml_dtypes
    c = {}
    r = np.arange(128)[:, None]
    s = np.arange(256)[None, :]
    dist = r + 128 - s
    bk = rel_bucket_np(dist)
    oh = np.zeros((128, 256, 32), np.float32)
    oh[np.arange(128)[:, None], np.arange(256)[None, :], bk] = 1.0
    c["c_oh"] = oh.reshape(128, 256 * 32).astype(ml_dtypes.bfloat16)
    c["c_cneg"] = np.where(dist >= 0, 0.0, NEG).astype(np.float32)
    cm = np.where(np.arange(128)[None, :] <= np.arange(128)[:, None], 0.0, -1e30).astype(np.float32)
    c["c_cm"] = cm
    cc = np.arange(384)[None, :]
    c["c_wneg"] = np.where(cc > r, 0.0, NEG).astype(np.float32)
    c["c_iota"] = np.broadcast_to(np.arange(512, dtype=np.float32)[None, :], (128, 512)).copy()
    qi = np.arange(32)[None, :]
    c["c_tc"] = ((r + qi * 128 - 31) / 16.0).astype(np.float32)
    n_cmp, n_slc = 255, 64
    start = np.arange(n_cmp) * 16
    end = start + 32
    bs = np.arange(n_slc) * 64
    ov = ((start[:, None] < bs[None, :] + 64) & (end[:, None] > bs[None, :])).astype(np.float32)
    ovp = np.zeros((256, 64), np.float32)
    ovp[:255] = ov
    c["c_ovl"] = np.ascontiguousarray(ovp.reshape(2, 128, 64).transpose(1, 0, 2)).astype(ml_dtypes.bfloat16)
    sel = np.zeros((32, 128, 192), np.float32)
    blk = np.arange(64)[None, :]
    for q in range(32):
        t = q * 128 + np.arange(128)[:, None]
        cur = t // 64
        forced = (blk == 0) | (blk == cur) | (blk == cur - 1)
        allowed = blk <= cur
        sel[q, :, 0:64] = (allowed & ~forced).astype(np.float32)
        sel[q, :, 64:128] = np.where(allowed, np.where(forced, 1e9 + 1024.0 * blk, 0.0), -1e30)
        sel[q, :, 128:192] = allowed.astype(np.float32)
    c["c_sel"] = sel
    c["c_pow2"] = np.broadcast_to((2.0 ** -np.arange(40, dtype=np.float32))[None, :], (128, 40)).copy()
    c["c_ident"] = np.eye(128, dtype=np.float32)
    return c


CONST_SPECS = [("c_oh", [128, 8192], BF16), ("c_cneg", [128, 256], F32), ("c_cm", [128, 128], F32),
               ("c_wneg", [128, 384], F32), ("c_iota", [128, 512], F32), ("c_tc", [128, 32], F32),
               ("c_ovl", [128, 2, 64], BF16), ("c_sel", [32, 128, 192], F32), ("c_pow2", [128, 40], F32),
               ("c_ident", [128, 128], F32)]
NBIS = 38
TIE_EPS = 2.0 ** -34


def setup_attn_consts(sc, cd, rel_bias, es):
    nc = sc.nc
    K = {}
    def ld(name, shape, dt, src_ap, srcbuf):
        b = sc.sbuf(name, shape, dt, es)
        sc.dma("sp", b[:], src_ap, reads=[srcbuf], writes=[b], track=b)
        return b
    K["cneg"] = ld("cneg", [128, 256], F32, cd["c_cneg"][:, :], cd["c_cneg"])
    K["cm"] = ld("cm", [128, 128], F32, cd["c_cm"][:, :], cd["c_cm"])
    K["wneg"] = ld("wneg", [128, 384], F32, cd["c_wneg"][:, :], cd["c_wneg"])
    K["iota"] = ld("iota", [128, 512], F32, cd["c_iota"][:, :], cd["c_iota"])
    K["tc"] = ld("tc", [128, 32], F32, cd["c_tc"][:, :], cd["c_tc"])
    K["ovl"] = ld("ovl", [128, 2, 64], BF16, cd["c_ovl"][:, :, :], cd["c_ovl"])
    K["pow2"] = ld("pow2", [128, 40], F32, cd["c_pow2"][:, :], cd["c_pow2"])
    K["ident"] = ld("identf", [128, 128], F32, cd["c_ident"][:, :], cd["c_ident"])
    K["tabb"] = ld("tabb", [128, 384], F32,
                   rel_bias.t.rearrange("b h -> (b h)").unsqueeze(0).partition_broadcast(128), rel_bias)
    K["identb"] = sc.sbuf("identb", [128, 128], BF16, es)
    sc.op("dve", lambda: nc.vector.tensor_copy(K["identb"][:], K["ident"][:]), reads=[K["ident"]], writes=[K["identb"]])
    K["near"] = sc.sbuf("near", [128, 12, 256], F32, es)
    with ExitStack() as tes:
        oh = sc.sbuf("oh", [128, 8192], BF16, tes)
        tmp = sc.sbuf("ohtmp", [128, 8192], F32, tes)
        sc.dma("sp", oh[:], cd["c_oh"][:, :], reads=[cd["c_oh"]], writes=[oh], track=oh)
        for hd in range(12):
            tb = K["tabb"][:, :].rearrange("p (b h) -> p h b", h=12)[:, hd, :]
            sc.op("dve", lambda: nc.vector.tensor_tensor(out=tmp[:].rearrange("p (s b) -> p s b", b=32),
                                                        in0=oh[:].rearrange("p (s b) -> p s b", b=32),
                                                        in1=tb.unsqueeze(1).to_broadcast([128, 256, 32]), op=ALU.mult),
                  reads=[oh, K["tabb"]], writes=[tmp])
            sc.op("dve", lambda: nc.vector.tensor_reduce(out=K["near"][:, hd, :], in_=tmp[:].rearrange("p (s b) -> p s b", b=32),
                                                        axis=AX.X, op=ALU.add), reads=[tmp], writes=[K["near"]], nowaw=True)
            sc.op("dve", lambda: nc.vector.scalar_tensor_tensor(out=K["near"][:, hd, :], in0=K["near"][:, hd, :],
                                                               scalar=K["tabb"][:, 31 * 12 + hd:31 * 12 + hd + 1], in1=K["cneg"][:],
                                                               op0=ALU.subtract, op1=ALU.add), reads=[K["near"], K["cneg"], K["tabb"]], writes=[K["near"]])
        sc.barrier_on([oh, tmp])
        sched_release(sc, [oh, tmp])
    return K


def phase_compress(sc, cfg, l, FM, cmp_w1, cmp_w2, cposT, KC, VC):
    nc = sc.nc
    nseq, seq = cfg["nseq"], cfg["seq"]
    ncmp = (seq - 32) // 16 + 1
    with ExitStack() as es:
        w1 = sc.sbuf("cw1", [128, 32, 256], BF16, es)
        w2 = sc.sbuf("cw2", [128, 2, 2, 64], BF16, es)
        posT = sc.sbuf("cposT", [128, 32], F32, es)
        raw = sc.sbuf("craw", [128, seq], BF16, es)
        rawp = sc.sbuf("crawp", [128, 32, 256], BF16, es)
        hidT = [sc.sbuf("chid", [128, 2, 256], BF16, es) for _ in range(2)]
        t1 = sc.sbuf("ct1", [128, 256], F32, es)
        t2 = sc.sbuf("ct2", [128, 256], F32, es)
        ps = [sc.psum("cps", [128, 512], F32, es) for _ in range(2)]
        for j in range(2):
            sc.dma("pool", w1[j * 64:(j + 1) * 64, :, :], cmp_w1[l, j].rearrange("(p d) h -> d p h", d=64),
                   reads=[cmp_w1], writes=[w1], track=w1)
        sc.dma("pool", w2[:], cmp_w2[l].rearrange("j (hc p) d -> p j hc d", p=128), reads=[cmp_w2], writes=[w2], track=w2)
        sc.dma("sp", posT[:], cposT[l], reads=[cposT], writes=[posT], track=posT)
        sc.op("dve", lambda: nc.vector.memset(VC[:], 0.0), writes=[VC])
        sc.op("dve", lambda: nc.vector.memset(KC[:], 0.0), writes=[KC])
        r0 = FM_ROW["kvcr"]
        pi = 0
        for b in range(nseq):
            sc.dma("sp", raw[:], FM[r0:r0 + 128, b * seq:(b + 1) * seq], reads=[FM], writes=[raw], track=raw, nowaw=False)
            r3 = raw[:].rearrange("p (n s) -> p n s", s=16)
            for p in range(32):
                src = r3[:, 0:ncmp, p] if p < 16 else r3[:, 1:ncmp + 1, p - 16]
                sc.op("dve", lambda: nc.vector.tensor_scalar(out=rawp[:, p, :ncmp], in0=src, scalar1=posT[:, p:p + 1],
                                                            scalar2=None, op0=ALU.add),
                      reads=[raw, posT], writes=[rawp], nowaw=(p > 0))
            for j in range(2):
                for hc in range(2):
                    ph = ps[pi % 2]; pi += 1
                    for p in range(32):
                        sc.op("pe", lambda: nc.tensor.matmul(ph[:, :ncmp], lhsT=w1[j * 64:(j + 1) * 64, p, hc * 128:(hc + 1) * 128],
                                                             rhs=rawp[j * 64:(j + 1) * 64, p, :ncmp], start=(p == 0), stop=(p == 31)),
                              reads=[w1, rawp], writes=[ph], nowaw=(p > 0))
                    sc.op("act", lambda: nc.scalar.activation(out=t1[:, :ncmp], in_=ph[:, :ncmp], func=AF.Square), reads=[ph], writes=[t1])
                    sc.op("dve", lambda: nc.vector.tensor_scalar(out=t1[:, :ncmp], in0=t1[:, :ncmp], scalar1=0.044715, scalar2=1.0,
                                                                op0=ALU.mult, op1=ALU.add), reads=[t1], writes=[t1])
                    sc.op("dve", lambda: nc.vector.tensor_tensor(out=t1[:, :ncmp], in0=t1[:, :ncmp], in1=ph[:, :ncmp], op=ALU.mult),
                          reads=[t1, ph], writes=[t1])
                    sc.op("act", lambda: nc.scalar.activation(out=t2[:, :ncmp], in_=t1[:, :ncmp], func=AF.Sigmoid, scale=1.5957691216057308),
                          reads=[t1], writes=[t2])
                    sc.op("dve", lambda: nc.vector.tensor_tensor(out=hidT[j][:, hc, :ncmp], in0=t2[:, :ncmp], in1=ph[:, :ncmp], op=ALU.mult),
                          reads=[t2, ph], writes=[hidT[j]], nowaw=(hc > 0))
            pk = ps[pi % 2]; pi += 1
            for hc in range(2):
                sc.op("pe", lambda: nc.tensor.matmul(pk[:64, :ncmp], lhsT=w2[:, 0, hc, :], rhs=hidT[0][:, hc, :ncmp],
                                                     start=(hc == 0), stop=(hc == 1)), reads=[w2, hidT[0]], writes=[pk], nowaw=(hc > 0))
            sc.op("dve", lambda: nc.vector.tensor_copy(KC[:, b, :ncmp], pk[:64, :ncmp]), reads=[pk], writes=[KC], nowaw=True)
            for nch in range(2):
                n0 = nch * 128
                nsz = min(128, ncmp - n0)
                if nsz <= 0:
                    continue
                pv = ps[pi % 2]; pi += 1
                for hc in range(2):
                    sc.op("pe", lambda: nc.tensor.matmul(pv[:nsz, 0:64], lhsT=hidT[1][:, hc, n0:n0 + nsz], rhs=w2[:, 1, hc, :],
                                                         start=(hc == 0), stop=(hc == 1)), reads=[w2, hidT[1]], writes=[pv], nowaw=(hc > 0))
                sc.op("dve", lambda: nc.vector.tensor_copy(VC[:nsz, b, nch, :], pv[:nsz, 0:64]), reads=[pv], writes=[VC], nowaw=True)
        allb = [w1, w2, posT, raw, rawp, t1, t2] + hidT + ps
        sc.barrier_on(allb)
        sched_release(sc, allb)


def phase_attn(sc, cfg, l, K, FM, VV, SM, KC, VC, csel, diff_lambda, diff_norm_g, lam_init, OABC, qtiles=None):
    nc = sc.nc
    nseq, seq = cfg["nseq"], cfg["seq"]
    nqt = seq // 128
    IDXC = 0.5 * (32 ** -0.5)
    with ExitStack() as es:
        KT = sc.sbuf("KT", [128, 2, seq], BF16, es)
        CK = sc.sbuf("CK", [128, 4, seq], BF16, es)
        VT = sc.sbuf("VT", [128, nqt, VV_W], BF16, es)
        ISC = sc.sbuf("ISC", [128, seq], F32, es)
        JUNK = sc.sbuf("JUNK", [128, seq], BF16, es)
        QI = [sc.sbuf("QI", [128, 4, 128], BF16, es) for _ in range(2)]
        QA = [sc.sbuf("QA", [64, 4, 128], BF16, es) for _ in range(2)]
        QB = [sc.sbuf("QB", [128, 4, 128], BF16, es) for _ in range(2)]
        QC = [sc.sbuf("QC", [128, 4, 128], BF16, es) for _ in range(2)]
        SMt = [sc.sbuf("SMt", [128, 16], F32, es) for _ in range(2)]
        SELt = [sc.sbuf("SELt", [128, 192], F32, es) for _ in range(2)]
        OUT = [sc.sbuf("OUT", [128, 1024], F32, es) for _ in range(1)]
        AW = sc.sbuf("AW", [128, 4], F32, es)
        SGN = sc.sbuf("SGN", [128, 4], F32, es)
        RT = [sc.sbuf("RT", [128, 512], F32, es) for _ in range(2)]
        ST = sc.sbuf("STAT", [128, 64], F32, es)
        W = sc.sbuf("Wb", [128, 40], F32, es)
        CMK = sc.sbuf("CMK", [128, 256], F32, es)
        PCf = sc.sbuf("PCf", [128, 256], F32, es)
        PCm = sc.sbuf("PCm", [128, 4, 256], F32, es)
        PCn = sc.sbuf("PCn", [128, 5, 256], BF16, es)
        PCT = sc.sbuf("PCT", [128, 10, 128], BF16, es)
        CS = sc.sbuf("CS", [128, 8], F32, es)
        OCMP = sc.sbuf("OCMP", [128, 256], F32, es)
        OWIN = sc.sbuf("OWIN", [128, 256], F32, es)
        IMP = sc.sbuf("IMP", [128, 64], F32, es)
        RANK = sc.sbuf("RANK", [128, 64], F32, es)
        BM = sc.sbuf("BM", [128, 64], F32, es)
        PW = sc.sbuf("PW", [128, 640], BF16, es)
        TMPN = [sc.sbuf("TMPN", [128, 384], F32, es) for _ in range(2)]
        WS = sc.sbuf("WS", [128, 8], F32, es)
        P = [sc.sbuf("P", [128, 512], BF16, es) for _ in range(3)]
        PM = [sc.sbuf("PM", [128, 512], BF16, es) for _ in range(2)]
        MAc = sc.sbuf("MAc", [128, 512], BF16, es)
        PT = [sc.sbuf("PT", [128, 8, 128], BF16, es) for _ in range(2)]
        SUMS = sc.sbuf("SUMS", [128, 16, 16], F32, es)
        TOT = sc.sbuf("TOT", [128, 16], F32, es)
        RCP = sc.sbuf("RCP", [128, 16], F32, es)
        LAM = sc.sbuf("LAM", [128, 4], F32, es)
        DL = sc.sbuf("DL", [128, 256], F32, es)
        DNG = sc.sbuf("DNG", [128, 128], F32, es)
        OCt = sc.sbuf("OCt", [128, 128], F32, es)
        OCj = sc.sbuf("OCj", [128, 128], F32, es)
        TAB31 = sc.sbuf("TAB31", [128, 12], F32, es)
        psS = [sc.psum("psS", [128, 512], F32, es) for _ in range(3)]
        psT = [sc.psum("psT", [128, 8, 128], BF16, es) for _ in range(2)]
        psOab = sc.psum("psOab", [128, 512], F32, es)
        psOc = [sc.psum("psOc", [128, 512], F32, es) for _ in range(2)]
        cnt = {"s": 0, "t": 0, "p": 0, "pm": 0, "pt": 0, "rt": 0, "tn": 0, "ev": 0}
        def nxt(lst, key):
            b = lst[cnt[key] % len(lst)]
            cnt[key] += 1
            return b

        sc.op("dve", lambda: nc.vector.tensor_copy(TAB31[:], K["tabb"][:, 31 * 12:32 * 12]), reads=[K["tabb"]], writes=[TAB31])
        sc.dma("sp", DL[:], diff_lambda[l].rearrange("a d -> (a d)").unsqueeze(0).partition_broadcast(128),
               reads=[diff_lambda], writes=[DL], track=DL)
        sc.dma("sp", DNG[:], diff_norm_g[l:l + 1, :].partition_broadcast(128), reads=[diff_norm_g], writes=[DNG], track=DNG)
        sc.op("dve", lambda: nc.vector.tensor_scalar(out=DNG[:], in0=DNG[:], scalar1=float(1.0 - lam_init), scalar2=None, op0=ALU.mult),
              reads=[DNG], writes=[DNG])
        for a in range(2):
            sc.op("dve", lambda: nc.vector.scalar_tensor_tensor(out=OCt[:, 0:64], in0=DL[:, (2 * a) * 64:(2 * a + 1) * 64], scalar=1.0,
                                                               in1=DL[:, (2 * a + 1) * 64:(2 * a + 2) * 64], op0=ALU.mult, op1=ALU.mult,
                                                               accum_out=LAM[:, 2 + a:3 + a]), reads=[DL], writes=[OCt, LAM])
        sc.op("act", lambda: nc.scalar.activation(out=LAM[:, 2:4], in_=LAM[:, 2:4], func=AF.Exp), reads=[LAM], writes=[LAM])
        sc.op("dve", lambda: nc.vector.scalar_tensor_tensor(out=LAM[:, 0:1], in0=LAM[:, 2:3], scalar=float(lam_init), in1=LAM[:, 3:4],
                                                           op0=ALU.add, op1=ALU.subtract), reads=[LAM], writes=[LAM])
        sc.op("dve", lambda: nc.vector.tensor_scalar(out=LAM[:, 1:2], in0=LAM[:, 0:1], scalar1=-1.0, scalar2=None, op0=ALU.mult),
              reads=[LAM], writes=[LAM])
        sc.op("dve", lambda: nc.vector.memset(PCn[:], 0.0), writes=[PCn])

        qn = 0
        for b in range(nseq):
            c0 = b * seq
            sc.dma("sp", KT[64:96, 1, :], FM[FM_ROW["ik"]:FM_ROW["ik"] + 32, c0:c0 + seq], reads=[FM], writes=[KT], track=KT, nowaw=False)
            for nm, (p0, sl) in (("ak", (0, 0)), ("ks", (64, 0)), ("kw", (0, 1))):
                sc.dma("sp", KT[p0:p0 + 64, sl, :], FM[FM_ROW[nm]:FM_ROW[nm] + 64, c0:c0 + seq], reads=[FM], writes=[KT], track=KT)
            for h in range(4):
                r = FM_ROW["ck%d" % h]
                sc.dma("sp", CK[:, h, :], FM[r:r + 128, c0:c0 + seq], reads=[FM], writes=[CK], track=CK, nowaw=(h > 0))
            for n4 in range(0, nqt, 8):
                sc.dma("sp", VT[:, n4:n4 + 8, :], VV[c0 + n4 * 128:c0 + (n4 + 8) * 128, :].rearrange("(n p) c -> p n c", p=128),
                       reads=[VV], writes=[VT], track=VT, nowaw=(n4 > 0))
            for qi in (qtiles if qtiles is not None else range(nqt)):
                tok0 = c0 + qi * 128
                nk = qi + 1
                Nk = nk * 128
                qI, qA, qB, qC, smt, selt = (x[qn % 2] for x in (QI, QA, QB, QC, SMt, SELt))
                out = OUT[0]
                qn += 1
                r = FM_ROW["iq"]
                sc.dma("sp", qI[64:96, :, :], FM[r:r + 128, tok0:tok0 + 128].rearrange("(h d) q -> d h q", d=32), reads=[FM], writes=[qI], track=qI, nowaw=False)
                r = FM_ROW["aq0"]
                sc.dma("sp", qA[:], FM[r:r + 256, tok0:tok0 + 128].rearrange("(h d) q -> d h q", d=64), reads=[FM], writes=[qA], track=qA, nowaw=False)
                r = FM_ROW["bq0"]
                sc.dma("sp", qB[0:64, :, :], FM[r:r + 256, tok0:tok0 + 128].rearrange("(h d) q -> d h q", d=64), reads=[FM], writes=[qB], track=qB, nowaw=False)
                sc.dma("sp", qB[64:128, :, :], FM[r:r + 256, tok0:tok0 + 128].rearrange("(h d) q -> d h q", d=64), reads=[FM], writes=[qB], track=qB)
                r = FM_ROW["cq0"]
                sc.dma("sp", qC[:], FM[r:r + 512, tok0:tok0 + 128].rearrange("(h r) q -> r h q", r=128), reads=[FM], writes=[qC], track=qC, nowaw=False)
                sc.dma("sp", smt[:], SM[tok0:tok0 + 128, :], reads=[SM], writes=[smt], track=smt, nowaw=False)
                sc.dma("sp", selt[:], csel[qi], reads=[csel], writes=[selt], track=selt, nowaw=False)
                chunks = [(k0, min(512, Nk - k0)) for k0 in range(0, Nk, 512)]

                sc.op("act", lambda: nc.scalar.activation(out=AW[:], in_=smt[:, 0:4], func=AF.Abs, scale=IDXC), reads=[smt], writes=[AW])
                sc.op("act", lambda: nc.scalar.activation(out=SGN[:], in_=smt[:, 0:4], func=AF.Sign), reads=[smt], writes=[SGN])
                for (k0, w) in chunks:
                    for h in range(4):
                        ps = nxt(psS, "s")
                        sc.op("pe", lambda: nc.tensor.matmul(ps[:, :w], lhsT=qI[64:96, h, :], rhs=KT[64:96, 1, k0:k0 + w], start=True, stop=True),
                              reads=[qI, KT], writes=[ps])
                        rt = nxt(RT, "rt")
                        sc.op("act", lambda: nc.scalar.activation(out=rt[:, :w], in_=ps[:, :w], func=AF.Relu, scale=AW[:, h:h + 1]),
                              reads=[ps, AW], writes=[rt])
                        if h == 0:
                            sc.op("dve", lambda: nc.vector.tensor_scalar(out=ISC[:, k0:k0 + w], in0=rt[:, :w], scalar1=SGN[:, 0:1], scalar2=None,
                                                                        op0=ALU.mult), reads=[rt, SGN], writes=[ISC], nowaw=True)
                        else:
                            sc.op("dve", lambda: nc.vector.scalar_tensor_tensor(out=ISC[:, k0:k0 + w], in0=rt[:, :w], scalar=SGN[:, h:h + 1],
                                                                               in1=ISC[:, k0:k0 + w], op0=ALU.mult, op1=ALU.add),
                                  reads=[rt, SGN, ISC], writes=[ISC])
                    rt = nxt(RT, "rt")
                    sc.op("dve", lambda: nc.vector.tensor_scalar(out=rt[:, :w], in0=K["iota"][:, :w], scalar1=float(k0), scalar2=-TIE_EPS,
                                                                op0=ALU.add, op1=ALU.mult), reads=[K["iota"]], writes=[rt])
                    sc.op("dve", lambda: nc.vector.scalar_tensor_tensor(out=rt[:, :w], in0=ISC[:, k0:k0 + w], scalar=0.0, in1=rt[:, :w],
                                                                       op0=ALU.is_equal, op1=ALU.mult), reads=[ISC, rt], writes=[rt])
                    sc.op("dve", lambda: nc.vector.tensor_tensor(out=ISC[:, k0:k0 + w], in0=ISC[:, k0:k0 + w], in1=rt[:, :w], op=ALU.add),
                          reads=[ISC, rt], writes=[ISC])
                sc.op("dve", lambda: nc.vector.tensor_tensor(out=ISC[:, Nk - 128:Nk], in0=ISC[:, Nk - 128:Nk], in1=K["cm"][:], op=ALU.add),
                      reads=[ISC, K["cm"]], writes=[ISC])
                if cfg.get('stop_after', 9) <= 1:
                    continue
                THR = ST[:, 7:8]
                if qi >= 2:
                    sc.op("dve", lambda: nc.vector.tensor_reduce(out=ST[:, 0:1], in_=ISC[:, :Nk], axis=AX.X, op=ALU.max), reads=[ISC], writes=[ST])
                    sc.op("dve", lambda: nc.vector.tensor_reduce(out=ST[:, 1:2], in_=ISC[:, :Nk - 128], axis=AX.X, op=ALU.min), reads=[ISC], writes=[ST])
                    sc.op("dve", lambda: nc.vector.scalar_tensor_tensor(out=ST[:, 2:3], in0=ST[:, 0:1], scalar=1.0, in1=ST[:, 1:2],
                                                                       op0=ALU.add, op1=ALU.subtract), reads=[ST], writes=[ST])
                    sc.op("dve", lambda: nc.vector.tensor_scalar(out=W[:], in0=K["pow2"][:], scalar1=ST[:, 2:3], scalar2=None, op0=ALU.mult),
                          reads=[K["pow2"], ST], writes=[W])
                    sc.op("dve", lambda: nc.vector.tensor_tensor(out=ST[:, 3:4], in0=ST[:, 1:2], in1=W[:, 1:2], op=ALU.add), reads=[ST, W], writes=[ST])
                    for k in range(1, NBIS + 1):
                        sc.op("dve", lambda: nc.vector.tensor_scalar(out=JUNK[:, :Nk], in0=ISC[:, :Nk], scalar1=ST[:, 3:4], scalar2=0.0,
                                                                    op0=ALU.is_ge, op1=ALU.add, accum_out=ST[:, 4:5]),
                              reads=[ISC, ST], writes=[JUNK, ST])
                        sc.op("dve", lambda: nc.vector.tensor_scalar(out=ST[:, 5:6], in0=ST[:, 4:5], scalar1=255.5, scalar2=0.5,
                                                                    op0=ALU.is_ge, op1=ALU.subtract), reads=[ST], writes=[ST])
                        if k < NBIS:
                            sc.op("dve", lambda: nc.vector.scalar_tensor_tensor(out=ST[:, 3:4], in0=ST[:, 5:6], scalar=W[:, k:k + 1], in1=ST[:, 3:4],
                                                                               op0=ALU.mult, op1=ALU.add), reads=[ST, W], writes=[ST])
                    sc.op("dve", lambda: nc.vector.tensor_scalar(out=ST[:, 6:7], in0=ST[:, 5:6], scalar1=0.5, scalar2=W[:, NBIS:NBIS + 1],
                                                                op0=ALU.subtract, op1=ALU.mult), reads=[ST, W], writes=[ST])
                    sc.op("dve", lambda: nc.vector.tensor_tensor(out=THR, in0=ST[:, 3:4], in1=ST[:, 6:7], op=ALU.add), reads=[ST], writes=[ST])
                else:
                    sc.op("dve", lambda: nc.vector.memset(THR, -1e29), reads=[], writes=[ST])

                if cfg.get('stop_after', 9) <= 2:
                    continue
                sc.op("dve", lambda: nc.vector.tensor_scalar(out=CMK[:], in0=K["iota"][:, 0:256], scalar1=K["tc"][:, qi:qi + 1], scalar2=1.0, op0=ALU.is_gt, op1=ALU.subtract),
                      reads=[K["iota"], K["tc"]], writes=[CMK])
                for h in range(4):
                    ps = nxt(psS, "s")
                    sc.op("pe", lambda: nc.tensor.matmul(ps[:, :256], lhsT=qB[0:64, h, :], rhs=KC[:, b, :], start=True, stop=True),
                          reads=[qB, KC], writes=[ps])
                    sc.op("act", lambda: nc.scalar.activation(out=PCf[:, :255], in_=ps[:, :255], func=AF.Exp, scale=0.125), reads=[ps], writes=[PCf])
                    sc.op("dve", lambda: nc.vector.scalar_tensor_tensor(out=PCm[:, h, :255], in0=PCf[:, :255], scalar=-1.0, in1=CMK[:, :255],
                                                                       op0=ALU.mult, op1=ALU.mult, accum_out=CS[:, h:h + 1]),
                          reads=[PCf, CMK], writes=[PCm, CS], nowaw=True)
                sc.op("dve", lambda: nc.vector.tensor_scalar(out=CS[:, 4:8], in0=CS[:, 0:4], scalar1=1e-30, scalar2=None, op0=ALU.max), reads=[CS], writes=[CS])
                sc.op("dve", lambda: nc.vector.reciprocal(out=CS[:, 4:8], in_=CS[:, 4:8]), reads=[CS], writes=[CS])
                for h in range(4):
                    sc.op("dve", lambda: nc.vector.tensor_scalar(out=PCn[:, h, :255], in0=PCm[:, h, :255], scalar1=CS[:, 4 + h:5 + h], scalar2=None,
                                                                op0=ALU.mult), reads=[PCm, CS], writes=[PCn], nowaw=True)
                    if h == 0:
                        sc.op("dve", lambda: nc.vector.tensor_scalar(out=PCf[:, :255], in0=PCm[:, 0, :255], scalar1=CS[:, 4:5], scalar2=None,
                                                                    op0=ALU.mult), reads=[PCm, CS], writes=[PCf])
                    else:
                        sc.op("dve", lambda: nc.vector.scalar_tensor_tensor(out=PCf[:, :255], in0=PCm[:, h, :255], scalar=CS[:, 4 + h:5 + h],
                                                                           in1=PCf[:, :255], op0=ALU.mult, op1=ALU.add),
                              reads=[PCm, CS, PCf], writes=[PCf])
                sc.op("dve", lambda: nc.vector.tensor_copy(PCn[:, 4, :255], PCf[:, :255]), reads=[PCf], writes=[PCn], nowaw=True)
                for g in range(2):
                    pt = nxt(psT, "t")
                    lo, hi = (0, 8) if g == 0 else (8, 10)
                    for i in range(lo, hi):
                        h, nch = i // 2, i % 2
                        sc.op("pe", lambda: nc.tensor.transpose(pt[:, i - lo, :], PCn[:, h, nch * 128:(nch + 1) * 128], K["identb"][:]),
                              reads=[PCn, K["identb"]], writes=[pt], nowaw=(i > lo))
                    sc.op("act", lambda: nc.scalar.copy(PCT[:, lo:hi, :], pt[:, 0:hi - lo, :]), reads=[pt], writes=[PCT], nowaw=True)
                first = True
                for h in range(4):
                    for nch in range(2):
                        sc.op("pe", lambda: nc.tensor.matmul(psOab[:, h * 64:(h + 1) * 64], lhsT=PCT[:, h * 2 + nch, :], rhs=VC[:, b, nch, :],
                                                             start=first, stop=(nch == 1), skip_group_check=True),
                              reads=[PCT, VC], writes=[psOab], nowaw=(not first))
                        first = False
                psI = nxt(psS, "s")
                for nch in range(2):
                    sc.op("pe", lambda: nc.tensor.matmul(psI[:, 0:64], lhsT=PCT[:, 8 + nch, :], rhs=K["ovl"][:, nch, :], start=(nch == 0), stop=(nch == 1)),
                          reads=[PCT, K["ovl"]], writes=[psI], nowaw=(nch > 0))
                sc.op("act", lambda: nc.scalar.copy(OCMP[:], psOab[:, 0:256]), reads=[psOab], writes=[OCMP])
                sc.op("dve", lambda: nc.vector.tensor_tensor(out=IMP[:], in0=psI[:, 0:64], in1=selt[:, 0:64], op=ALU.mult), reads=[psI, selt], writes=[IMP])
                sc.op("dve", lambda: nc.vector.tensor_tensor(out=IMP[:], in0=IMP[:], in1=selt[:, 64:128], op=ALU.add), reads=[IMP, selt], writes=[IMP])
                J3 = JUNK[:, 0:4096].rearrange("p (j i) -> p j i", i=64)
                sc.op("dve", lambda: nc.vector.tensor_tensor(out=J3, in0=IMP[:].unsqueeze(1).to_broadcast([128, 64, 64]),
                                                            in1=IMP[:].unsqueeze(2).to_broadcast([128, 64, 64]), op=ALU.is_gt),
                      reads=[IMP], writes=[JUNK])
                sc.op("dve", lambda: nc.vector.tensor_reduce(out=RANK[:], in_=J3, axis=AX.X, op=ALU.add), reads=[JUNK], writes=[RANK])
                sc.op("dve", lambda: nc.vector.scalar_tensor_tensor(out=BM[:], in0=RANK[:], scalar=15.5, in1=selt[:, 128:192], op0=ALU.is_lt, op1=ALU.mult),
                      reads=[RANK, selt], writes=[BM])

                if cfg.get('stop_after', 9) <= 3:
                    continue
                nwin = min(qi, 4) + 1
                nnear = min(nwin, 2)
                nfar = nwin - nnear
                kt0 = qi - (nwin - 1)
                for h in range(4):
                    if nfar > 0:
                        ps = nxt(psS, "s")
                        wf = nfar * 128
                        sc.op("pe", lambda: nc.tensor.matmul(ps[:, :wf], lhsT=qB[0:64, h, :], rhs=KT[0:64, 1, kt0 * 128:kt0 * 128 + wf], start=True, stop=True),
                              reads=[qB, KT], writes=[ps])
                        tn = nxt(TMPN, "tn")
                        sc.op("dve", lambda: nc.vector.scalar_tensor_tensor(out=tn[:, :wf], in0=ps[:, :wf], scalar=0.125, in1=K["wneg"][:, 384 - wf:384],
                                                                           op0=ALU.mult, op1=ALU.add), reads=[ps, K["wneg"]], writes=[tn])
                        sc.op("act", lambda: nc.scalar.activation(out=PW[:, 0:wf], in_=tn[:, :wf], func=AF.Exp,
                                                                  accum_out=WS[:, h:h + 1]), reads=[tn], writes=[PW, WS], nowaw=True)
                    else:
                        wf = 0
                        sc.op("dve", lambda: nc.vector.memset(WS[:, h:h + 1], 0.0), writes=[WS], nowaw=True)
                    ps = nxt(psS, "s")
                    wn = nnear * 128
                    kn0 = (qi - (nnear - 1)) * 128
                    sc.op("pe", lambda: nc.tensor.matmul(ps[:, :wn], lhsT=qB[0:64, h, :], rhs=KT[0:64, 1, kn0:kn0 + wn], start=True, stop=True),
                          reads=[qB, KT], writes=[ps])
                    tn = nxt(TMPN, "tn")
                    sc.op("dve", lambda: nc.vector.scalar_tensor_tensor(out=tn[:, :wn], in0=ps[:, :wn], scalar=0.125, in1=K["near"][:, 4 + h, 256 - wn:256],
                                                                       op0=ALU.mult, op1=ALU.add), reads=[ps, K["near"]], writes=[tn])
                    sc.op("act", lambda: nc.scalar.activation(out=PW[:, wf:wf + wn], in_=tn[:, :wn], func=AF.Exp, accum_out=WS[:, 4 + h:5 + h]),
                          reads=[tn], writes=[PW, WS], nowaw=True)
                    pt = nxt(psT, "t")
                    for i in range(nwin):
                        sc.op("pe", lambda: nc.tensor.transpose(pt[:, i, :], PW[:, i * 128:(i + 1) * 128], K["identb"][:]),
                              reads=[PW, K["identb"]], writes=[pt], nowaw=(i > 0))
                    ptb = nxt(PT, "pt")
                    sc.op("act", lambda: nc.scalar.copy(ptb[:, 0:nwin, :], pt[:, 0:nwin, :]), reads=[pt], writes=[ptb])
                    for i in range(nwin):
                        fst = (h == 0 and i == 0)
                        sc.op("pe", lambda: nc.tensor.matmul(psOab[:, 256 + h * 64:256 + (h + 1) * 64], lhsT=ptb[:, i, :], rhs=VT[:, kt0 + i, 128:192],
                                                             start=fst, stop=(i == nwin - 1), skip_group_check=True),
                              reads=[ptb, VT], writes=[psOab], nowaw=(not fst))
                sc.op("act", lambda: nc.scalar.copy(OWIN[:], psOab[:, 256:512]), reads=[psOab], writes=[OWIN])
                sc.op("dve", lambda: nc.vector.tensor_tensor(out=WS[:, 0:4], in0=WS[:, 0:4], in1=WS[:, 4:8], op=ALU.add), reads=[WS], writes=[WS])

                if cfg.get('stop_after', 9) <= 4:
                    continue
                sc.op("dve", lambda: nc.vector.memset(SUMS[:], 0.0), writes=[SUMS])
                firstO = {"ab": True, 0: True, 1: True}
                nch_ = len(chunks)
                for ci, (k0, w) in enumerate(chunks):
                    ntile = w // 128
                    wf = min(max(Nk - 256 - k0, 0), w)
                    wn = w - wf
                    no = (k0 + wf) - (Nk - 256)
                    sc.op("dve", lambda: nc.vector.tensor_scalar(out=MAc[:, :w], in0=ISC[:, k0:k0 + w], scalar1=THR, scalar2=None, op0=ALU.is_ge),
                          reads=[ISC, ST], writes=[MAc])
                    maps = [("a", h, 0) for h in range(4)] + [("s", h, 0) for h in range(4)] + [("c", h, j) for h in range(4) for j in range(2)]
                    if "kinds" in cfg:
                        maps = [m_ for m_ in maps if m_[0] in cfg["kinds"]]
                    pend = []
                    for mi, (kind, h, j) in enumerate(maps):
                        ps = nxt(psS, "s")
                        if kind == "a":
                            lhsT, rhs, hd = qA[:, h, :], KT[0:64, 0, k0:k0 + w], h
                        elif kind == "s":
                            lhsT, rhs, hd = qB[64:128, h, :], KT[64:128, 0, k0:k0 + w], 4 + h
                        else:
                            lhsT, rhs, hd = qC[j * 64:(j + 1) * 64, h, :], CK[j * 64:(j + 1) * 64, h, k0:k0 + w], 8 + h
                        sc.op("pe", lambda: nc.tensor.matmul(ps[:, :w], lhsT=lhsT, rhs=rhs, start=True, stop=True),
                              reads=[qA, qB, qC, KT, CK], writes=[ps])
                        p = nxt(P, "p")
                        accf = SUMS[:, mi, 2 * ci:2 * ci + 1] if kind == "c" else None
                        accn = SUMS[:, mi, 2 * ci + 1:2 * ci + 2] if kind == "c" else None
                        if wf > 0 and not cfg.get("skipfar"):
                            if kind == "c":
                                sc.op("act", lambda: nc.scalar.activation(out=p[:, :wf], in_=ps[:, :wf], func=AF.Exp, scale=0.125, accum_out=accf),
                                      reads=[ps], writes=[p, SUMS], nowaw=True)
                            else:
                                sc.op("act", lambda: nc.scalar.activation(out=p[:, :wf], in_=ps[:, :wf], func=AF.Exp, scale=0.125),
                                      reads=[ps], writes=[p], nowaw=True)
                        if wn > 0 and not cfg.get("skipnear"):
                            tn = nxt(TMPN, "tn")
                            sc.op("dve", lambda: nc.vector.scalar_tensor_tensor(out=tn[:, :wn], in0=ps[:, wf:w], scalar=0.125, in1=K["near"][:, hd, no:no + wn],
                                                                               op0=ALU.mult, op1=ALU.add), reads=[ps, K["near"]], writes=[tn])
                            if kind == "c":
                                sc.op("act", lambda: nc.scalar.activation(out=p[:, wf:w], in_=tn[:, :wn], func=AF.Exp, accum_out=accn),
                                      reads=[tn], writes=[p, SUMS], nowaw=True)
                            else:
                                sc.op("act", lambda: nc.scalar.activation(out=p[:, wf:w], in_=tn[:, :wn], func=AF.Exp),
                                      reads=[tn], writes=[p], nowaw=True)
                        if kind == "a":
                            pm = nxt(PM, "pm")
                            sc.op("dve", lambda: nc.vector.scalar_tensor_tensor(out=pm[:, :w], in0=p[:, :w], scalar=1.0, in1=MAc[:, :w], op0=ALU.mult, op1=ALU.mult,
                                                                               accum_out=SUMS[:, mi, 2 * ci:2 * ci + 1]), reads=[p, MAc], writes=[pm, SUMS], nowaw=True)
                            src = pm
                        elif kind == "s":
                            pm = nxt(PM, "pm")
                            sc.op("dve", lambda: nc.vector.scalar_tensor_tensor(out=pm[:, :w].rearrange("p (j i) -> p j i", i=64),
                                                                               in0=p[:, :w].rearrange("p (j i) -> p j i", i=64), scalar=1.0,
                                                                               in1=BM[:, k0 // 64:(k0 + w) // 64].unsqueeze(2).to_broadcast([128, w // 64, 64]),
                                                                               op0=ALU.mult, op1=ALU.mult, accum_out=SUMS[:, mi, 2 * ci:2 * ci + 1]),
                                  reads=[p, BM], writes=[pm, SUMS], nowaw=True)
                            src = pm
                        else:
                            src = p
                        pend.append((kind, h, j, src))
                        if cfg.get("nopv"):
                            pend = []
                            continue
                        if len(pend) == 2 or mi == len(maps) - 1:
                            pt = nxt(psT, "t")
                            for pi_, (kd, hh, jj, sr) in enumerate(pend):
                                for i in range(ntile):
                                    sc.op("pe", lambda: nc.tensor.transpose(pt[:, pi_ * 4 + i, :], sr[:, i * 128:(i + 1) * 128], K["identb"][:]),
                                          reads=[sr, K["identb"]], writes=[pt], nowaw=not (pi_ == 0 and i == 0))
                            ptb = nxt(PT, "pt")
                            ek = "act" if cnt["ev"] % 2 == 0 else "dve"
                            cnt["ev"] += 1
                            for pi_ in range(len(pend)):
                                if ek == "act":
                                    sc.op("act", lambda: nc.scalar.copy(ptb[:, pi_ * 4:pi_ * 4 + ntile, :], pt[:, pi_ * 4:pi_ * 4 + ntile, :]), reads=[pt], writes=[ptb], nowaw=(pi_ > 0))
                                else:
                                    sc.op("dve", lambda: nc.vector.tensor_copy(ptb[:, pi_ * 4:pi_ * 4 + ntile, :], pt[:, pi_ * 4:pi_ * 4 + ntile, :]), reads=[pt], writes=[ptb], nowaw=(pi_ > 0))
                            for pi_, (kd, hh, jj, sr) in enumerate(pend):
                                for i in range(ntile):
                                    kt = k0 // 128 + i
                                    if kd == "a":
                                        o, rhsv, key = psOab[:, hh * 64:(hh + 1) * 64], VT[:, kt, 0:64], "ab"
                                        ob = psOab
                                    elif kd == "s":
                                        o, rhsv, key = psOab[:, 256 + hh * 64:256 + (hh + 1) * 64], VT[:, kt, 64:128], "ab"
                                        ob = psOab
                                    else:
                                        m = hh * 2 + jj
                                        ob = psOc[m // 4]
                                        o, rhsv, key = ob[:, (m % 4) * 128:(m % 4 + 1) * 128], VT[:, kt, 192 + hh * 128:192 + (hh + 1) * 128], m // 4
                                    fst = firstO[key]
                                    firstO[key] = False
                                    sc.op("pe", lambda: nc.tensor.matmul(o, lhsT=ptb[:, pi_ * 4 + i, :], rhs=rhsv, start=fst,
                                                                         stop=(ci == nch_ - 1 and i == ntile - 1), skip_group_check=True),
                                          reads=[ptb, VT], writes=[ob], nowaw=(not fst))
                            pend = []

                if cfg.get('stop_after', 9) <= 5:
                    continue
                sc.op("dve", lambda: nc.vector.tensor_reduce(out=TOT[:], in_=SUMS[:], axis=AX.X, op=ALU.add), reads=[SUMS], writes=[TOT])
                sc.op("dve", lambda: nc.vector.tensor_scalar(out=RCP[:], in0=TOT[:], scalar1=1e-30, scalar2=None, op0=ALU.max), reads=[TOT], writes=[RCP])
                sc.op("dve", lambda: nc.vector.reciprocal(out=RCP[:], in_=RCP[:]), reads=[RCP], writes=[RCP])
                sc.op("dve", lambda: nc.vector.reciprocal(out=WS[:, 4:8], in_=WS[:, 0:4]), reads=[WS], writes=[WS])
                for h in range(4):
                    sc.op("dve", lambda: nc.vector.tensor_scalar(out=out[:, h * 64:(h + 1) * 64], in0=psOab[:, h * 64:(h + 1) * 64], scalar1=RCP[:, h:h + 1],
                                                                scalar2=None, op0=ALU.mult), reads=[psOab, RCP], writes=[out], nowaw=(h > 0))
                for h in range(4):
                    g0, g1, g2 = (smt[:, 4 + 3 * h + k:5 + 3 * h + k] for k in range(3))
                    sc.op("dve", lambda: nc.vector.tensor_scalar(out=ST[:, 8:9], in0=RCP[:, 4 + h:5 + h], scalar1=g1, scalar2=None, op0=ALU.mult), reads=[RCP, smt], writes=[ST])
                    sc.op("dve", lambda: nc.vector.tensor_scalar(out=ST[:, 9:10], in0=WS[:, 4 + h:5 + h], scalar1=g2, scalar2=None, op0=ALU.mult), reads=[WS, smt], writes=[ST])
                    oslice = out[:, 256 + h * 64:256 + (h + 1) * 64]
                    sc.op("dve", lambda: nc.vector.tensor_scalar(out=oslice, in0=OCMP[:, h * 64:(h + 1) * 64], scalar1=g0, scalar2=None, op0=ALU.mult),
                          reads=[OCMP, smt], writes=[out], nowaw=True)
                    sc.op("dve", lambda: nc.vector.scalar_tensor_tensor(out=oslice, in0=psOab[:, 256 + h * 64:256 + (h + 1) * 64], scalar=ST[:, 8:9], in1=oslice,
                                                                       op0=ALU.mult, op1=ALU.add), reads=[psOab, ST, out], writes=[out])
                    sc.op("dve", lambda: nc.vector.scalar_tensor_tensor(out=oslice, in0=OWIN[:, h * 64:(h + 1) * 64], scalar=ST[:, 9:10], in1=oslice,
                                                                       op0=ALU.mult, op1=ALU.add), reads=[OWIN, ST, out], writes=[out])
                for h in range(4):
                    m0, m1 = 8 + h * 2, 9 + h * 2
                    ob0, ob1 = psOc[(h * 2) // 4], psOc[(h * 2 + 1) // 4]
                    o0 = ob0[:, ((h * 2) % 4) * 128:((h * 2) % 4 + 1) * 128]
                    o1 = ob1[:, ((h * 2 + 1) % 4) * 128:((h * 2 + 1) % 4 + 1) * 128]
                    sc.op("dve", lambda: nc.vector.tensor_scalar(out=ST[:, 10:11], in0=RCP[:, m1:m1 + 1], scalar1=LAM[:, 1:2], scalar2=None, op0=ALU.mult),
                          reads=[RCP, LAM], writes=[ST])
                    sc.op("dve", lambda: nc.vector.tensor_scalar(out=OCt[:], in0=o0, scalar1=RCP[:, m0:m0 + 1], scalar2=None, op0=ALU.mult),
                          reads=[ob0, RCP], writes=[OCt])
                    sc.op("dve", lambda: nc.vector.scalar_tensor_tensor(out=OCt[:], in0=o1, scalar=ST[:, 10:11], in1=OCt[:], op0=ALU.mult, op1=ALU.add),
                          reads=[ob1, ST, OCt], writes=[OCt])
                    sc.op("dve", lambda: nc.vector.scalar_tensor_tensor(out=OCj[:], in0=OCt[:], scalar=1.0, in1=OCt[:], op0=ALU.mult, op1=ALU.mult,
                                                                       accum_out=ST[:, 11:12]), reads=[OCt], writes=[OCj, ST])
                    sc.op("dve", lambda: nc.vector.tensor_scalar(out=ST[:, 12:13], in0=ST[:, 11:12], scalar1=1.0 / 128, scalar2=1e-5, op0=ALU.mult, op1=ALU.add),
                          reads=[ST], writes=[ST])
                    sc.op("act", lambda: nc.scalar.activation(out=ST[:, 13:14], in_=ST[:, 12:13], func=AF.Ln), reads=[ST], writes=[ST])
                    sc.op("act", lambda: nc.scalar.activation(out=ST[:, 14:15], in_=ST[:, 13:14], func=AF.Exp, scale=-0.5), reads=[ST], writes=[ST])
                    sc.op("dve", lambda: nc.vector.scalar_tensor_tensor(out=out[:, 512 + h * 128:512 + (h + 1) * 128], in0=OCt[:], scalar=ST[:, 14:15], in1=DNG[:],
                                                                       op0=ALU.mult, op1=ALU.mult), reads=[OCt, ST, DNG], writes=[out], nowaw=True)
                sc.dma("sp", OABC[tok0:tok0 + 128, :], out[:], reads=[out], writes=[OABC], track=out)
        allb = ([KT, CK, VT, ISC, JUNK, AW, SGN, ST, W, CMK, PCf, PCm, PCn, PCT, CS, OCMP, OWIN, IMP, RANK, BM, PW, WS, MAc, SUMS, TOT, RCP, LAM, DL, DNG,
                 OCt, OCj, TAB31, psOab] + QI + QA + QB + QC + SMt + SELt + OUT + RT + TMPN + P + PM + PT + psS + psT + psOc)
        sc.barrier_on(allb)
        sched_release(sc, allb)


def layer_norm_tile(sc, z, zc, junk, o, ST2, Gt, Bt):
    nc = sc.nc
    sc.op("dve", lambda: nc.vector.tensor_reduce(out=ST2[:, 0:1], in_=z[:], axis=AX.X, op=ALU.add), reads=[z], writes=[ST2])
    sc.op("dve", lambda: nc.vector.tensor_scalar(out=ST2[:, 1:2], in0=ST2[:, 0:1], scalar1=1.0 / D, scalar2=None, op0=ALU.mult), reads=[ST2], writes=[ST2])
    sc.op("dve", lambda: nc.vector.tensor_scalar(out=zc[:], in0=z[:], scalar1=ST2[:, 1:2], scalar2=None, op0=ALU.subtract), reads=[z, ST2], writes=[zc])
    sc.op("dve", lambda: nc.vector.scalar_tensor_tensor(out=junk[:], in0=zc[:], scalar=1.0, in1=zc[:], op0=ALU.mult, op1=ALU.mult,
                                                       accum_out=ST2[:, 2:3]), reads=[zc], writes=[junk, ST2])
    sc.op("dve", lambda: nc.vector.tensor_scalar(out=ST2[:, 3:4], in0=ST2[:, 2:3], scalar1=1.0 / D, scalar2=1e-5, op0=ALU.mult, op1=ALU.add),
          reads=[ST2], writes=[ST2])
    sc.op("act", lambda: nc.scalar.activation(out=ST2[:, 4:5], in_=ST2[:, 3:4], func=AF.Ln), reads=[ST2], writes=[ST2])
    sc.op("act", lambda: nc.scalar.activation(out=ST2[:, 5:6], in_=ST2[:, 4:5], func=AF.Exp, scale=-0.5), reads=[ST2], writes=[ST2])
    sc.op("dve", lambda: nc.vector.scalar_tensor_tensor(out=zc[:], in0=zc[:], scalar=ST2[:, 5:6], in1=Gt[:], op0=ALU.mult, op1=ALU.mult),
          reads=[zc, ST2, Gt], writes=[zc])
    sc.op("pool", lambda: nc.gpsimd.tensor_tensor(out=o[:], in0=zc[:], in1=Bt[:], op=ALU.add), reads=[zc, Bt], writes=[o])


def phase_tail(sc, cfg, l, K, OABC, MG, xsrc, wba_d, wbb_d, wbc_d, wo_d, ln_g, ln_b, G1P, X1):
    nc = sc.nc
    nseq, seq = cfg["nseq"], cfg["seq"]
    ntok = nseq * seq
    DN_ALPHA = (2 * DEPTH) ** 0.25
    with ExitStack() as es:
        wb = sc.sbuf("wbr", [128, 8, 1024], BF16, es)
        wo = sc.sbuf("wo", [128, 8, 1024], BF16, es)
        sc.dma("pool", wb[:, 0:2, :], wba_d[l].rearrange("(c p) n -> p c n", p=128), reads=[wba_d], writes=[wb], track=wb)
        sc.dma("pool", wb[:, 2:4, :], wbb_d[l].rearrange("(c p) n -> p c n", p=128), reads=[wbb_d], writes=[wb], track=wb)
        sc.dma("pool", wb[:, 4:8, :], wbc_d[l].rearrange("(c p) n -> p c n", p=128), reads=[wbc_d], writes=[wb], track=wb)
        sc.dma("pool", wo[:], wo_d[l].rearrange("(c p) n -> p c n", p=128), reads=[wo_d], writes=[wo], track=wo)
        Gt = sc.sbuf("lnG", [128, 1024], F32, es)
        Bt = sc.sbuf("lnB", [128, 1024], F32, es)
        sc.dma("sp", Gt[:], ln_g[l:l + 1, :].partition_broadcast(128), reads=[ln_g], writes=[Gt], track=Gt)
        sc.dma("sp", Bt[:], ln_b[l:l + 1, :].partition_broadcast(128), reads=[ln_b], writes=[Bt], track=Bt)
        OA = [sc.sbuf("tOA", [128, 1024], F32, es) for _ in range(2)]
        MGt = [sc.sbuf("tMG", [128, 3072], F32, es) for _ in range(2)]
        XT = [sc.sbuf("tX", [128, 1024], F32, es) for _ in range(2)]
        OB = sc.sbuf("tOB", [128, 1024], BF16, es)
        oT = sc.sbuf("toT", [128, 8, 128], BF16, es)
        M = sc.sbuf("tM", [128, 1024], F32, es)
        TMP = sc.sbuf("tTMP", [128, 1024], F32, es)
        MB = sc.sbuf("tMB", [128, 1024], BF16, es)
        mT = sc.sbuf("tmT", [128, 8, 128], BF16, es)
        Z = sc.sbuf("tZ", [128, 1024], F32, es)
        ZC = sc.sbuf("tZC", [128, 1024], F32, es)
        O = [sc.sbuf("tO", [128, 1024], F32, es) for _ in range(2)]
        ST2 = sc.sbuf("tST", [128, 8], F32, es)
        psT = [sc.psum("tpsT", [128, 8, 128], BF16, es) for _ in range(2)]
        psY = [sc.psum("tpsY", [128, 512], F32, es) for _ in range(6)]
        yi = 0
        for tt in range(ntok // 128):
            tok0 = tt * 128
            b = tok0 // seq
            oa, mg, xt, o = OA[tt % 2], MGt[tt % 2], XT[tt % 2], O[tt % 2]
            sc.dma("sp", oa[:], OABC[tok0:tok0 + 128, :], reads=[OABC], writes=[oa], track=oa, nowaw=False)
            sc.dma("sp", mg[:], MG[tok0:tok0 + 128, :], reads=[MG], writes=[mg], track=mg, nowaw=False)
            sc.dma("sp", xt[:], xsrc[tok0:tok0 + 128, :], reads=[xsrc], writes=[xt], track=xt, nowaw=False)
            sc.op("act", lambda: nc.scalar.copy(OB[:], oa[:]), reads=[oa], writes=[OB])
            pt = psT[0]
            for c in range(8):
                sc.op("pe", lambda: nc.tensor.transpose(pt[:, c, :], OB[:, c * 128:(c + 1) * 128], K["identb"][:]), reads=[OB, K["identb"]], writes=[pt], nowaw=(c > 0))
            sc.op("act", lambda: nc.scalar.copy(oT[:], pt[:]), reads=[pt], writes=[oT])
            for hf in range(2):
                cs = slice(hf * 512, (hf + 1) * 512)
                ys = []
                for (k0, k1) in ((0, 2), (2, 4), (4, 8)):
                    py = psY[yi % 6]; yi += 1
                    for kc in range(k0, k1):
                        sc.op("pe", lambda: nc.tensor.matmul(py[:, :], lhsT=oT[:, kc, :], rhs=wb[:, kc, cs], start=(kc == k0), stop=(kc == k1 - 1)),
                              reads=[oT, wb], writes=[py], nowaw=(kc > k0))
                    ys.append(py)
                sc.op("dve", lambda: nc.vector.tensor_tensor(out=M[:, cs], in0=ys[0][:, :], in1=mg[:, hf * 512:(hf + 1) * 512], op=ALU.mult),
                      reads=[ys[0], mg], writes=[M], nowaw=(hf > 0))
                sc.op("dve", lambda: nc.vector.tensor_tensor(out=TMP[:, cs], in0=ys[1][:, :], in1=mg[:, 1024 + hf * 512:1024 + (hf + 1) * 512], op=ALU.mult),
                      reads=[ys[1], mg], writes=[TMP], nowaw=(hf > 0))
                sc.op("pool", lambda: nc.gpsimd.tensor_tensor(out=M[:, cs], in0=M[:, cs], in1=TMP[:, cs], op=ALU.add), reads=[M, TMP], writes=[M])
                sc.op("dve", lambda: nc.vector.tensor_tensor(out=TMP[:, cs], in0=ys[2][:, :], in1=mg[:, 2048 + hf * 512:2048 + (hf + 1) * 512], op=ALU.mult),
                      reads=[ys[2], mg], writes=[TMP])
                sc.op("pool", lambda: nc.gpsimd.tensor_tensor(out=MB[:, cs], in0=M[:, cs], in1=TMP[:, cs], op=ALU.add), reads=[M, TMP], writes=[MB], nowaw=(hf > 0))
            pt = psT[1]
            for c in range(8):
                sc.op("pe", lambda: nc.tensor.transpose(pt[:, c, :], MB[:, c * 128:(c + 1) * 128], K["identb"][:]), reads=[MB, K["identb"]], writes=[pt], nowaw=(c > 0))
            sc.op("act", lambda: nc.scalar.copy(mT[:], pt[:]), reads=[pt], writes=[mT])
            for hf in range(2):
                cs = slice(hf * 512, (hf + 1) * 512)
                py = psY[yi % 6]; yi += 1
                for kc in range(8):
                    sc.op("pe", lambda: nc.tensor.matmul(py[:, :], lhsT=mT[:, kc, :], rhs=wo[:, kc, cs], start=(kc == 0), stop=(kc == 7)),
                          reads=[mT, wo], writes=[py], nowaw=(kc > 0))
                sc.op("dve", lambda: nc.vector.tensor_tensor(out=TMP[:, cs], in0=py[:, :], in1=G1P[:, b, cs], op=ALU.mult), reads=[py, G1P], writes=[TMP])
                sc.op("dve", lambda: nc.vector.scalar_tensor_tensor(out=Z[:, cs], in0=xt[:, cs], scalar=DN_ALPHA, in1=TMP[:, cs], op0=ALU.mult, op1=ALU.add),
                      reads=[xt, TMP], writes=[Z], nowaw=(hf > 0))
            layer_norm_tile(sc, Z, ZC, TMP, o, ST2, Gt, Bt)
            sc.dma("sp", X1[tok0:tok0 + 128, :], o[:], reads=[o], writes=[X1], track=o)
        allb = [wb, wo, Gt, Bt, OB, oT, M, TMP, MB, mT, Z, ZC, ST2] + OA + MGt + XT + O + psT + psY
        sc.barrier_on(allb)
        sched_release(sc, allb)


def phase_moe_prep(sc, cfg, l, K, X1, opsc, shf, router_w, router_b, b_down, H2T, CWD, YACC):
    nc = sc.nc
    nseq, seq = cfg["nseq"], cfg["seq"]
    ntok = nseq * seq
    with ExitStack() as es:
        rw = sc.sbuf("rw", [128, 8, 32], F32, es)
        rb = sc.sbuf("rb", [128, 32], F32, es)
        bd = sc.sbuf("bd", [32, 1024], F32, es)
        sc.dma("sp", rw[:], router_w[l].rearrange("(c p) e -> p c e", p=128), reads=[router_w], writes=[rw], track=rw)
        sc.dma("sp", rb[:], router_b[l:l + 1, :].partition_broadcast(128), reads=[router_b], writes=[rb], track=rb)
        sc.dma("sp", bd[:], b_down[l], reads=[b_down], writes=[bd], track=bd)
        XT = [sc.sbuf("mX", [128, 1024], F32, es) for _ in range(2)]
        HF = sc.sbuf("mHF", [128, 8, 128], F32, es)
        HB = [sc.sbuf("mHB", [128, 8, 128], BF16, es) for _ in range(2)]
        Lg = sc.sbuf("mL", [128, 32], F32, es)
        J3 = sc.sbuf("mJ3", [128, 32, 32], F32, es)
        RK = sc.sbuf("mRK", [128, 32], F32, es)
        EX = sc.sbuf("mEX", [128, 32], F32, es)
        CW = [sc.sbuf("mCW", [128, 32], F32, es) for _ in range(2)]
        CWT = sc.sbuf("mCWT", [32, 128], F32, es)
        YB = [sc.sbuf("mYB", [128, 1024], F32, es) for _ in range(2)]
        ST = sc.sbuf("mST", [128, 8], F32, es)
        ps = [sc.psum("mps", [128, 512], F32, es) for _ in range(6)]
        pi = 0
        for tt in range(ntok // 128):
            tok0 = tt * 128
            b = tok0 // seq
            xt, hb, cw, yb = XT[tt % 2], HB[tt % 2], CW[tt % 2], YB[tt % 2]
            sc.dma("sp", xt[:], X1[tok0:tok0 + 128, :], reads=[X1], writes=[xt], track=xt, nowaw=False)
            for g in range(2):
                p = ps[pi % 6]; pi += 1
                for c4 in range(4):
                    c = g * 4 + c4
                    sc.op("pe", lambda: nc.tensor.transpose(p[:, c4 * 128:(c4 + 1) * 128], xt[:, c * 128:(c + 1) * 128], K["ident"][:]),
                          reads=[xt, K["ident"]], writes=[p], nowaw=(c4 > 0))
                for c4 in range(4):
                    c = g * 4 + c4
                    sc.op("act", lambda: nc.scalar.activation(out=HF[:, c, :], in_=p[:, c4 * 128:(c4 + 1) * 128], func=AF.Identity,
                                                              bias=shf[:, c, b:b + 1], scale=opsc[:, c, b:b + 1]),
                          reads=[p, shf, opsc], writes=[HF], nowaw=(c > 0))
            sc.op("dve", lambda: nc.vector.tensor_copy(hb[:], HF[:]), reads=[HF], writes=[hb])
            sc.dma("sp", H2T[:, tok0:tok0 + 128].rearrange("(c p) t -> p c t", p=128), hb[:], reads=[hb], writes=[H2T], track=hb)
            p = ps[pi % 6]; pi += 1
            for c in range(8):
                sc.op("pe", lambda: nc.tensor.matmul(p[:, 0:32], lhsT=HF[:, c, :], rhs=rw[:, c, :], start=(c == 0), stop=(c == 7)),
                      reads=[HF, rw], writes=[p], nowaw=(c > 0))
            sc.op("dve", lambda: nc.vector.tensor_tensor(out=Lg[:], in0=p[:, 0:32], in1=rb[:], op=ALU.add), reads=[p, rb], writes=[Lg])
            sc.op("dve", lambda: nc.vector.tensor_tensor(out=J3[:], in0=Lg[:].unsqueeze(1).to_broadcast([128, 32, 32]),
                                                        in1=Lg[:].unsqueeze(2).to_broadcast([128, 32, 32]), op=ALU.is_gt), reads=[Lg], writes=[J3])
            sc.op("dve", lambda: nc.vector.tensor_reduce(out=RK[:], in_=J3[:], axis=AX.X, op=ALU.add), reads=[J3], writes=[RK])
            sc.op("dve", lambda: nc.vector.tensor_reduce(out=ST[:, 0:1], in_=Lg[:], axis=AX.X, op=ALU.max), reads=[Lg], writes=[ST])
            sc.op("dve", lambda: nc.vector.tensor_scalar(out=ST[:, 1:2], in0=ST[:, 0:1], scalar1=-1.0, scalar2=None, op0=ALU.mult), reads=[ST], writes=[ST])
            sc.op("act", lambda: nc.scalar.activation(out=EX[:], in_=Lg[:], func=AF.Exp, bias=ST[:, 1:2]), reads=[Lg, ST], writes=[EX])
            sc.op("dve", lambda: nc.vector.tensor_scalar(out=RK[:], in0=RK[:], scalar1=3.5, scalar2=None, op0=ALU.is_lt), reads=[RK], writes=[RK])
            sc.op("dve", lambda: nc.vector.scalar_tensor_tensor(out=EX[:], in0=EX[:], scalar=1.0, in1=RK[:], op0=ALU.mult, op1=ALU.mult,
                                                               accum_out=ST[:, 2:3]), reads=[EX, RK], writes=[EX, ST])
            sc.op("dve", lambda: nc.vector.reciprocal(out=ST[:, 3:4], in_=ST[:, 2:3]), reads=[ST], writes=[ST])
            sc.op("dve", lambda: nc.vector.tensor_scalar(out=cw[:], in0=EX[:], scalar1=ST[:, 3:4], scalar2=None, op0=ALU.mult), reads=[EX, ST], writes=[cw])
            sc.dma("sp", CWD[tok0:tok0 + 128, :], cw[:], reads=[cw], writes=[CWD], track=cw)
            p = ps[pi % 6]; pi += 1
            sc.op("pe", lambda: nc.tensor.transpose(p[:32, 0:128], cw[:, :], K["ident"][:]), reads=[cw, K["ident"]], writes=[p])
            sc.op("act", lambda: nc.scalar.copy(CWT[:], p[:32, 0:128]), reads=[p], writes=[CWT])
            for hf in range(2):
                p = ps[pi % 6]; pi += 1
                sc.op("pe", lambda: nc.tensor.matmul(p[:, :], lhsT=CWT[:, :], rhs=bd[:, hf * 512:(hf + 1) * 512], start=True, stop=True),
                      reads=[CWT, bd], writes=[p])
                sc.op("act", lambda: nc.scalar.copy(yb[:, hf * 512:(hf + 1) * 512], p[:, :]), reads=[p], writes=[yb], nowaw=(hf > 0))
            sc.dma("sp", YACC[tok0:tok0 + 128, :], yb[:], reads=[yb], writes=[YACC], track=yb)
        allb = [rw, rb, bd, HF, Lg, J3, RK, EX, CWT, ST] + XT + HB + CW + YB + ps
        sc.barrier_on(allb)
        sched_release(sc, allb)


def phase_moe_experts(sc, cfg, l, H2T, CWD, YACC, w_gu, b_guT, w_down):
    nc = sc.nc
    nseq, seq = cfg["nseq"], cfg["seq"]
    ntok = nseq * seq
    CH = min(1024, ntok)
    ntile = CH // 128
    nsb = CH // 512
    ne = cfg.get("n_exp", 32)
    with ExitStack() as es:
        hT = sc.sbuf("eh", [128, 8, CH], BF16, es)
        ACC = sc.sbuf("eACC", [128, ntile, 1024], F32, es)
        CWc = sc.sbuf("eCW", [128, ntile, 32], F32, es)
        Wgu = [sc.sbuf("eWgu", [128, 8, 2048], BF16, es) for _ in range(2)]
        Wd = [sc.sbuf("eWd", [128, 8, 1024], BF16, es) for _ in range(2)]
        BG = [sc.sbuf("eBG", [128, 16], F32, es) for _ in range(2)]
        actT = [sc.sbuf("eact", [128, 8, 512], BF16, es) for _ in range(2)]
        G = [sc.sbuf("eG", [128, 512], F32, es) for _ in range(2)]
        Sg = [sc.sbuf("eS", [128, 512], F32, es) for _ in range(2)]
        U = [sc.sbuf("eU", [128, 512], F32, es) for _ in range(2)]
        psG = [sc.psum("epsG", [128, 512], F32, es) for _ in range(5)]
        psD = [sc.psum("epsD", [128, 512], F32, es) for _ in range(3)]
        gi = di = ai = ti = 0
        for ch in range(ntok // CH):
            t0 = ch * CH
            sc.dma("sp", hT[:], H2T[:, t0:t0 + CH].rearrange("(c p) t -> p c t", p=128), reads=[H2T], writes=[hT], track=hT, nowaw=False)
            sc.dma("sp", ACC[:], YACC[t0:t0 + CH, :].rearrange("(n p) d -> p n d", p=128), reads=[YACC], writes=[ACC], track=ACC, nowaw=False)
            sc.dma("sp", CWc[:], CWD[t0:t0 + CH, :].rearrange("(n p) e -> p n e", p=128), reads=[CWD], writes=[CWc], track=CWc, nowaw=False)
            for e in range(ne):
                k = (ch * ne + e) % 2
                wg, wd, bg = Wgu[k], Wd[k], BG[k]
                for c2 in range(0, 8, 2):
                    sc.dma("pool", wg[:, c2:c2 + 2, :], w_gu[l, e, c2 * 128:(c2 + 2) * 128, :].rearrange("(c p) n -> p c n", p=128),
                           reads=[w_gu], writes=[wg], track=wg, nowaw=(c2 > 0))
                for c4 in range(0, 8, 4):
                    sc.dma("pool", wd[:, c4:c4 + 4, :], w_down[l, e, c4 * 128:(c4 + 4) * 128, :].rearrange("(c p) n -> p c n", p=128),
                           reads=[w_down], writes=[wd], track=wd, nowaw=(c4 > 0))
                sc.dma("sp", bg[:], b_guT[l, e], reads=[b_guT], writes=[bg], track=bg, nowaw=False)
                for sb in range(nsb):
                    at = actT[ai % 2]; ai += 1
                    for fc in range(8):
                        pg = psG[gi % 5]; gi += 1
                        pu = psG[gi % 5]; gi += 1
                        for which, pp in ((0, pg), (1, pu)):
                            for kc in range(8):
                                lhsT = wg[:, kc, :].rearrange("p (f two) -> p two f", two=2)[:, which, fc * 128:(fc + 1) * 128]
                                sc.op("pe", lambda: nc.tensor.matmul(pp[:, :], lhsT=lhsT, rhs=hT[:, kc, sb * 512:(sb + 1) * 512],
                                                                     start=(kc == 0), stop=(kc == 7)), reads=[wg, hT], writes=[pp], nowaw=(kc > 0))
                        g_, s_, u_ = G[ti % 2], Sg[ti % 2], U[ti % 2]
                        ti += 1
                        sc.op("dve", lambda: nc.vector.tensor_scalar(out=g_[:], in0=pg[:, :], scalar1=bg[:, fc:fc + 1], scalar2=7.0, op0=ALU.add, op1=ALU.min),
                              reads=[pg, bg], writes=[g_])
                        sc.op("act", lambda: nc.scalar.activation(out=s_[:], in_=g_[:], func=AF.Sigmoid, scale=1.702), reads=[g_], writes=[s_])
                        sc.op("dve", lambda: nc.vector.tensor_scalar(out=u_[:], in0=pu[:, :], scalar1=bg[:, 8 + fc:9 + fc], scalar2=7.0, op0=ALU.add, op1=ALU.min),
                              reads=[pu, bg], writes=[u_])
                        sc.op("pool", lambda: nc.gpsimd.tensor_scalar(out=u_[:], in0=u_[:], scalar1=-7.0, scalar2=1.0, op0=ALU.max, op1=ALU.add),
                              reads=[u_], writes=[u_])
                        sc.op("pool", lambda: nc.gpsimd.tensor_tensor(out=g_[:], in0=g_[:], in1=s_[:], op=ALU.mult), reads=[g_, s_], writes=[g_])
                        sc.op("pool", lambda: nc.gpsimd.tensor_tensor(out=at[:, fc, :], in0=g_[:], in1=u_[:], op=ALU.mult), reads=[g_, u_], writes=[at], nowaw=(fc > 0))
                    for tl in range(4):
                        tix = sb * 4 + tl
                        for hf in range(2):
                            pd = psD[di % 3]; di += 1
                            for fc in range(8):
                                sc.op("pe", lambda: nc.tensor.matmul(pd[:, :], lhsT=at[:, fc, tl * 128:(tl + 1) * 128], rhs=wd[:, fc, hf * 512:(hf + 1) * 512],
                                                                     start=(fc == 0), stop=(fc == 7)), reads=[at, wd], writes=[pd], nowaw=(fc > 0))
                            sc.op("dve", lambda: nc.vector.scalar_tensor_tensor(out=ACC[:, tix, hf * 512:(hf + 1) * 512], in0=pd[:, :], scalar=CWc[:, tix, e:e + 1],
                                                                               in1=ACC[:, tix, hf * 512:(hf + 1) * 512], op0=ALU.mult, op1=ALU.add),
                                  reads=[pd, CWc, ACC], writes=[ACC])
            sc.dma("sp", YACC[t0:t0 + CH, :].rearrange("(n p) d -> p n d", p=128), ACC[:], reads=[ACC], writes=[YACC], track=ACC)
        allb = [hT, ACC, CWc] + Wgu + Wd + BG + actT + G + Sg + U + psG + psD
        sc.barrier_on(allb)
        sched_release(sc, allb)


def phase_moe_final(sc, cfg, l, X1, YACC, G1P, ln_g, ln_b, XOUT):
    nc = sc.nc
    nseq, seq = cfg["nseq"], cfg["seq"]
    ntok = nseq * seq
    DN_ALPHA = (2 * DEPTH) ** 0.25
    with ExitStack() as es:
        Gt = sc.sbuf("fG", [128, 1024], F32, es)
        Bt = sc.sbuf("fB", [128, 1024], F32, es)
        sc.dma("sp", Gt[:], ln_g[l:l + 1, :].partition_broadcast(128), reads=[ln_g], writes=[Gt], track=Gt)
        sc.dma("sp", Bt[:], ln_b[l:l + 1, :].partition_broadcast(128), reads=[ln_b], writes=[Bt], track=Bt)
        XT = [sc.sbuf("fX", [128, 1024], F32, es) for _ in range(2)]
        YT = [sc.sbuf("fY", [128, 1024], F32, es) for _ in range(2)]
        Z = sc.sbuf("fZ", [128, 1024], F32, es)
        ZC = sc.sbuf("fZC", [128, 1024], F32, es)
        TMP = sc.sbuf("fT", [128, 1024], F32, es)
        O = [sc.sbuf("fO", [128, 1024], F32, es) for _ in range(2)]
        ST2 = sc.sbuf("fST", [128, 8], F32, es)
        for tt in range(ntok // 128):
            tok0 = tt * 128
            b = tok0 // seq
            xt, yt, o = XT[tt % 2], YT[tt % 2], O[tt % 2]
            sc.dma("sp", xt[:], X1[tok0:tok0 + 128, :], reads=[X1], writes=[xt], track=xt, nowaw=False)
            sc.dma("sp", yt[:], YACC[tok0:tok0 + 128, :], reads=[YACC], writes=[yt], track=yt, nowaw=False)
            sc.op("pool", lambda: nc.gpsimd.tensor_tensor(out=TMP[:], in0=yt[:], in1=G1P[:, b, :], op=ALU.mult), reads=[yt, G1P], writes=[TMP])
            sc.op("dve", lambda: nc.vector.scalar_tensor_tensor(out=Z[:], in0=xt[:], scalar=DN_ALPHA, in1=TMP[:], op0=ALU.mult, op1=ALU.add),
                  reads=[xt, TMP], writes=[Z])
            layer_norm_tile(sc, Z, ZC, TMP, o, ST2, Gt, Bt)
            sc.dma("sp", XOUT[tok0:tok0 + 128, :], o[:], reads=[o], writes=[XOUT], track=o)
        allb = [Gt, Bt, Z, ZC, TMP, ST2] + XT + YT + O
        sc.barrier_on(allb)
        sched_release(sc, allb)


W_SPECS = [("rel_bias", [32, 12]), ("mod_attn_w", [DEPTH, D, 3 * D]), ("mod_attn_b", [DEPTH, 3 * D]), ("w_in", [DEPTH, D, D_IN]),
           ("cmp_w1", [DEPTH, 2, 2048, 256]), ("cmp_w2", [DEPTH, 2, 256, 64]), ("diff_lambda", [DEPTH, 4, 64]),
           ("diff_norm_g", [DEPTH, 128]), ("w_branch_a", [DEPTH, 256, D]), ("w_branch_b", [DEPTH, 256, D]),
           ("w_branch_c", [DEPTH, 512, D]), ("w_out", [DEPTH, D, D]), ("ln1_g", [DEPTH, D]), ("ln1_b", [DEPTH, D]),
           ("mod_ffn_w", [DEPTH, D, 3 * D]), ("mod_ffn_b", [DEPTH, 3 * D]), ("router_w", [DEPTH, D, 32]), ("router_b", [DEPTH, 32]),
           ("exp_w_gu", [DEPTH, 32, D, 2 * D]), ("exp_w_down", [DEPTH, 32, D, D]), ("exp_b_down", [DEPTH, 32, D]),
           ("ln2_g", [DEPTH, D]), ("ln2_b", [DEPTH, D]),
           ("modbT", [DEPTH, 2, 128, 24]), ("cposT", [DEPTH, 128, 32]), ("b_guT", [DEPTH, 32, 128, 16])]


def build_program(cfg):
    nc = bass.Bass("TRN2", target_bir_lowering=False)
    nseq, seq = cfg["nseq"], cfg["seq"]
    ntok = nseq * seq
    depth = cfg.get("depth", DEPTH)
    with ExitStack() as es:
        sc = Sched(nc, es)
        x = sc.dram("x", [ntok, D], F32, kind="ExternalInput")
        cT = sc.dram("cT", [128, 8, nseq], F32, kind="ExternalInput")
        Wd_ = {n: sc.dram(n, s, F32, kind="ExternalInput") for n, s in W_SPECS}
        cd = {n: sc.dram(n, s, d, kind="ExternalInput") for n, s, d in CONST_SPECS}
        y = sc.dram("y", [ntok, D], F32, kind="ExternalOutput")
        FM = sc.dram("FM", [FM_ROWS, ntok], BF16)
        VV = sc.dram("VV", [ntok, VV_W], BF16)
        SM = sc.dram("SM", [ntok, 16], F32)
        MG = sc.dram("MG", [ntok, 3072], F32)
        OABC = sc.dram("OABC", [ntok, 1024], F32)
        X1 = sc.dram("X1", [ntok, D], F32)
        XL = sc.dram("XL", [ntok, D], F32)
        H2T = sc.dram("H2T", [D, ntok], BF16)
        CWD = sc.dram("CWD", [ntok, 32], F32)
        YACC = sc.dram("YACC", [ntok, D], F32)
        K = setup_attn_consts(sc, cd, Wd_["rel_bias"], es)
        siluT = sc.sbuf("siluT", [128, 8, nseq], F32)
        sc.dma("sp", siluT[:], cT[:, :, :], reads=[cT], writes=[siluT], track=siluT)
        sc.op("act", lambda: nc.scalar.activation(out=siluT[:], in_=siluT[:], func=AF.Silu), reads=[siluT], writes=[siluT])
        opsc = sc.sbuf("opsc", [128, 8, nseq], F32)
        shf = sc.sbuf("shf", [128, 8, nseq], F32)
        G1P = sc.sbuf("G1P", [128, nseq, 1024], F32)
        KC = sc.sbuf("KC", [64, nseq, 256], BF16)
        VC = sc.sbuf("VC", [128, nseq, 2, 64], BF16)
        xin = x
        for l in range(depth):
            lam_init = 0.8 - 0.6 * math.exp(-0.3 * l)
            xout = y if l == depth - 1 else XL
            phase_adaln(sc, cfg, l, Wd_["mod_attn_w"], Wd_["modbT"], Wd_["mod_attn_b"], 0, siluT, opsc, shf, None, "fm")
            phase_proj(sc, cfg, l, xin, Wd_["w_in"], K["ident"], opsc, shf, FM, VV, SM, MG)
            phase_compress(sc, cfg, l, FM, Wd_["cmp_w1"], Wd_["cmp_w2"], Wd_["cposT"], KC, VC)
            phase_attn(sc, cfg, l, K, FM, VV, SM, KC, VC, cd["c_sel"], Wd_["diff_lambda"], Wd_["diff_norm_g"], lam_init, OABC)
            phase_adaln(sc, cfg, l, Wd_["mod_attn_w"], Wd_["modbT"], Wd_["mod_attn_b"], 0, siluT, None, None, G1P, "gate")
            phase_tail(sc, cfg, l, K, OABC, MG, xin, Wd_["w_branch_a"], Wd_["w_branch_b"], Wd_["w_branch_c"], Wd_["w_out"],
                       Wd_["ln1_g"], Wd_["ln1_b"], G1P, X1)
            phase_adaln(sc, cfg, l, Wd_["mod_ffn_w"], Wd_["modbT"], Wd_["mod_ffn_b"], 1, siluT, opsc, shf, None, "fm")
            phase_moe_prep(sc, cfg, l, K, X1, opsc, shf, Wd_["router_w"], Wd_["router_b"], Wd_["exp_b_down"], H2T, CWD, YACC)
            phase_moe_experts(sc, cfg, l, H2T, CWD, YACC, Wd_["exp_w_gu"], Wd_["b_guT"], Wd_["exp_w_down"])
            phase_adaln(sc, cfg, l, Wd_["mod_ffn_w"], Wd_["modbT"], Wd_["mod_ffn_b"], 1, siluT, None, None, G1P, "gate")
            phase_moe_final(sc, cfg, l, X1, YACC, G1P, Wd_["ln2_g"], Wd_["ln2_b"], xout)
            xin = xout
        sc.finish([y])
        cfg["_n_inst"] = sc.n_inst
    return nc


def host_layouts(inp):
    L = DEPTH
    out = {}
    out["modbT"] = np.ascontiguousarray(np.stack([np.stack([inp["mod_attn_b"][l].reshape(24, 128).T,
                                                            inp["mod_ffn_b"][l].reshape(24, 128).T]) for l in range(L)]), dtype=np.float32)
    out["cposT"] = np.ascontiguousarray(np.stack([np.concatenate([inp["cmp_pos"][l, 0].T, inp["cmp_pos"][l, 1].T], 0) for l in range(L)]), dtype=np.float32)
    bg = inp["exp_b_gu"].reshape(L, 32, 8, 128, 2)
    out["b_guT"] = np.ascontiguousarray(bg.transpose(0, 1, 3, 4, 2).reshape(L, 32, 128, 16), dtype=np.float32)
    return out


def run_module(inp, cfg, n_cores):
    nseq, seq = cfg["nseq"], cfg["seq"]
    nc = build_program(cfg)
    shared = {n: np.ascontiguousarray(inp[n], dtype=np.float32) for n, _ in W_SPECS if n in inp}
    shared.update(host_layouts(inp))
    shared.update(make_consts())
    x = np.asarray(inp["x"], dtype=np.float32)
    c = np.asarray(inp["c"], dtype=np.float32)
    in_maps = []
    for core in range(n_cores):
        xs = x[core * nseq:(core + 1) * nseq].reshape(nseq * seq, D)
        cs = c[core * nseq:(core + 1) * nseq]
        cT = np.ascontiguousarray(cs.reshape(nseq, 8, 128).transpose(2, 1, 0))
        m = dict(shared)
        m["x"] = np.ascontiguousarray(xs)
        m["cT"] = cT
        in_maps.append(m)
    res = run_bass_kernel_spmd(nc, in_maps, core_ids=list(range(n_cores)))
    outs = [r["y"].reshape(nseq, seq, D) for r in res.results]
    return np.concatenate(outs, axis=0).astype(np.float32)


def kernel(**inputs):
    cfg = dict(nseq=NSEQ, seq=S)
    return run_module(inputs, cfg, 8)
```

```python
import math
from contextlib import ExitStack
import numpy as np
import concourse.bass as bass
import concourse.mybir as mybir
from concourse.bass_utils import run_bass_kernel_spmd

F32 = mybir.dt.float32
BF16 = mybir.dt.bfloat16
I32 = mybir.dt.int32
AF = mybir.ActivationFunctionType
ALU = mybir.AluOpType
AX = mybir.AxisListType

D = 1024
S = 4096
NSEQ = 2
T = NSEQ * S
DEPTH = 2
D_IN = 5808
NEG = -30000.0


class Buf:
    def __init__(self, name, t):
        self.name = name
        self.t = t
        self.wr = {}
        self.rd = {}
        self.dsem = None
        self.dcount = 0

    def __getitem__(self, idx):
        return self.t[idx]


class Sched:
    def __init__(self, nc, es):
        self.nc = nc
        self.es = es
        self.eng = {"pe": nc.tensor, "dve": nc.vector, "act": nc.scalar,
                    "pool": nc.gpsimd, "sp": nc.sync}
        self.sem = {}
        self.cnt = {}
        for k in ("pe", "dve", "act", "pool"):
            self.sem[k] = es.enter_context(nc.semaphore("s_" + k))
            self.cnt[k] = 0
        self.known = {k: {} for k in self.eng}
        self.nbuf = 0
        self.n_inst = 0
        self.sempool = []

    def sbuf(self, name, shape, dt, es=None):
        es = es or self.es
        self.nbuf += 1
        name = "%s_%d" % (name, self.nbuf)
        t = es.enter_context(self.nc.sbuf_tensor(name, list(shape), dt))
        b = Buf(name, t)
        b.es = es
        return b

    def psum(self, name, shape, dt=F32, es=None):
        es = es or self.es
        self.nbuf += 1
        name = "%s_%d" % (name, self.nbuf)
        t = es.enter_context(self.nc.psum_tensor(name, list(shape), dt))
        b = Buf(name, t)
        b.is_psum = True
        b.es = es
        return b

    def dram(self, name, shape, dt, kind=None):
        if kind is None:
            t = self.nc.dram_tensor(name, list(shape), dt)
            b = Buf(name, t.ap())
            b.es = self.es
            return b
        else:
            t = self.nc.dram_tensor(name, list(shape), dt, kind=kind)
        b = Buf(name, t.ap())
        b.es = self.es
        return b

    def _semobj(self, key):
        if isinstance(key, str):
            return self.sem[key]
        return key.dsem

    def _wait(self, ekey, deps):
        eng = self.eng[ekey]
        kn = self.known[ekey]
        for key, val in deps.items():
            if isinstance(key, str):
                if key == "pe" and ekey == "pe":
                    continue
                v = val
            else:
                if key.dsem is None:
                    continue
                v = key.dcount * 16
            if kn.get(key, 0) >= v:
                continue
            eng.wait_ge(self._semobj(key), v)
            kn[key] = v

    def _collect(self, reads, writes, nowaw):
        deps = {}
        def add(d):
            for k, v in d.items():
                if deps.get(k, 0) < v:
                    deps[k] = v
        for b in reads:
            add(b.wr)
            if getattr(b, "is_psum", False):
                add(b.rd)
        for b in writes:
            if not nowaw:
                add(b.wr)
            add(b.rd)
        return deps

    def op(self, ekey, fn, reads=(), writes=(), nowaw=False):
        deps = self._collect(reads, writes, nowaw)
        self._wait(ekey, deps)
        ins = fn()
        self.cnt[ekey] += 1
        ins.then_inc(self.sem[ekey], 1)
        tk = self.cnt[ekey]
        self.n_inst += 1
        for b in reads:
            if b.rd.get(ekey, 0) < tk:
                b.rd[ekey] = tk
        for b in writes:
            if nowaw:
                b.wr[ekey] = tk
            else:
                b.wr = {ekey: tk}
                b.rd = {}
        return ins

    def dma(self, qkey, out_ap, in_ap, reads=(), writes=(), track=None, nowaw=True, **kw):
        assert track is not None
        if track.dsem is None:
            if self.sempool:
                track.dsem, track.dcount = self.sempool.pop()
            else:
                track.dsem = self.es.enter_context(self.nc.semaphore("d_" + track.name))
        deps = self._collect(reads, writes, nowaw)
        self._wait(qkey, deps)
        ins = self.eng[qkey].dma_start(out=out_ap, in_=in_ap, **kw)
        ins.then_inc(track.dsem, 16)
        track.dcount += 1
        self.n_inst += 1
        for b in reads:
            b.rd[track] = track.dcount
        for b in writes:
            if nowaw:
                b.wr[track] = track.dcount
            else:
                b.wr = {track: track.dcount}
                b.rd = {}
        return ins

    def barrier_on(self, bufs):
        deps = {}
        for b in bufs:
            for d in (b.wr, b.rd):
                for k, v in d.items():
                    if deps.get(k, 0) < v:
                        deps[k] = v
        for e in self.eng:
            self._wait(e, deps)

    def finish(self, bufs):
        deps = {}
        for b in bufs:
            for k, v in b.wr.items():
                if deps.get(k, 0) < v:
                    deps[k] = v
        self._wait("sp", deps)


C_AQ, C_AK, C_AV, C_IQ, C_IK, C_IW = 0, 256, 320, 384, 512, 544
C_BQ, C_KCR, C_VCR, C_KS, C_VS, C_KW, C_VW, C_BG = 548, 804, 868, 932, 996, 1060, 1124, 1188
C_CQ, C_CK, C_CV, C_MG = 1200, 1712, 2224, 2736
FM_CHUNKS = [("aq0", 0, 128), ("aq1", 128, 128), ("ak", 256, 64), ("iq", 384, 128), ("ik", 512, 32),
             ("bq0", 548, 128), ("bq1", 676, 128), ("kvcr", 804, 128), ("ks", 932, 64), ("kw", 1060, 64),
             ("cq0", 1200, 128), ("cq1", 1328, 128), ("cq2", 1456, 128), ("cq3", 1584, 128),
             ("ck0", 1712, 128), ("ck1", 1840, 128), ("ck2", 1968, 128), ("ck3", 2096, 128)]
FM_ROW = {}
_r = 0
for _n, _c, _w in FM_CHUNKS:
    FM_ROW[_n] = _r
    _r += 128
FM_ROWS = _r
VV_W = 704


def sched_release(sc, bufs):
    for b in bufs:
        if b.dsem is not None:
            sc.sempool.append((b.dsem, b.dcount))
            b.dsem = None


def load_cast(sc, es, dst, dst_ap, src, src_ap):
    sc.dma("pool", dst_ap, src_ap, reads=[src], writes=[dst], track=dst)


def phase_adaln(sc, cfg, l, modw, modbT, modb, which, siluT, out_opsc, out_shf, out_gate, want, tm_third=2, add_one=1.0):
    nc = sc.nc
    nseq = cfg["nseq"]
    with ExitStack() as es:
        wk = [sc.sbuf("adw", [128, 3072], F32, es) for _ in range(2)]
        bT = sc.sbuf("adbT", [128, 24], F32, es)
        gb = sc.sbuf("adgb", [128, 1024], F32, es)
        psA = sc.psum("adpsA", [128, 512], F32, es)
        psG = [[sc.psum("adpsG", [128, 512], F32, es) for _ in range(2)] for _ in range(nseq)]
        silu_bc = []
        if want == "gate":
            for b in range(nseq):
                t = sc.sbuf("silubc", [128, 8, 128], F32, es)
                sc.op("dve", lambda: nc.vector.tensor_copy(t[:], siluT[:, :, b:b + 1].to_broadcast([128, 8, 128])), reads=[siluT], writes=[t])
                silu_bc.append(t)
        sc.dma("sp", bT[:], modbT[l, which], reads=[modbT], writes=[bT], track=bT)
        sc.dma("sp", gb[:], modb[l:l + 1, tm_third * 1024:(tm_third + 1) * 1024].partition_broadcast(128), reads=[modb], writes=[gb], track=gb)
        for kc in range(8):
            w = wk[kc % 2]
            sc.dma("sp", w[:], modw[l, kc * 128:(kc + 1) * 128, :], reads=[modw], writes=[w], track=w, nowaw=False)
            for j in (range(16) if want == "fm" else []):
                sc.op("pe", lambda: nc.tensor.matmul(psA[:, j * nseq:(j + 1) * nseq], lhsT=w[:, j * 128:(j + 1) * 128],
                                                     rhs=siluT[:, kc, :], start=(kc == 0 and j == 0), stop=(kc == 7),
                                                     skip_group_check=True),
                      reads=[w, siluT], writes=[psA], nowaw=True)
            for b in (range(nseq) if want == "gate" else []):
                for hf in range(2):
                    sc.op("pe", lambda: nc.tensor.matmul(psG[b][hf][:, :], lhsT=silu_bc[b][:, kc, :],
                                                         rhs=w[:, tm_third * 1024 + hf * 512:tm_third * 1024 + (hf + 1) * 512],
                                                         start=(kc == 0), stop=(kc == 7)),
                          reads=[w, silu_bc[b]], writes=[psG[b][hf]], nowaw=True)
        for j in (range(8) if want == "fm" else []):
            sc.op("dve", lambda: nc.vector.tensor_scalar(out=out_shf[:, j, :], in0=psA[:, j * nseq:(j + 1) * nseq],
                                                        scalar1=bT[:, j:j + 1], scalar2=None, op0=ALU.add),
                  reads=[psA, bT], writes=[out_shf], nowaw=True)
            sc.op("dve", lambda: nc.vector.tensor_scalar(out=out_opsc[:, j, :], in0=psA[:, (j + 8) * nseq:(j + 9) * nseq],
                                                        scalar1=bT[:, j + 8:j + 9], scalar2=1.0, op0=ALU.add, op1=ALU.add),
                  reads=[psA, bT], writes=[out_opsc], nowaw=True)
        for b in (range(nseq) if want == "gate" else []):
            for hf in range(2):
                sc.op("dve", lambda: nc.vector.scalar_tensor_tensor(out=out_gate[:, b, hf * 512:(hf + 1) * 512],
                                                                   in0=psG[b][hf][:, :], scalar=float(add_one),
                                                                   in1=gb[:, hf * 512:(hf + 1) * 512],
                                                                   op0=ALU.add, op1=ALU.add),
                      reads=[psG[b][hf], gb], writes=[out_gate], nowaw=True)
        sc.barrier_on([wk[0], wk[1], bT, gb, psA] + [p for q in psG for p in q] + silu_bc)
        sched_release(sc, [wk[0], wk[1], bT, gb])


def phase_proj(sc, cfg, l, xsrc, w_in, ident, opsc, shf, FM, VV, SM, MG):
    nc = sc.nc
    nseq, seq = cfg["nseq"], cfg["seq"]
    ntok = nseq * seq
    with ExitStack() as es:
        wb = sc.sbuf("w_in_bf", [128, 8, D_IN], BF16, es)
        for kc in range(8):
            sc.dma("pool", wb[:, kc, :], w_in[l, kc * 128:(kc + 1) * 128, :], reads=[w_in], writes=[wb], track=wb)
        xs = [sc.sbuf("xs", [128, 4, D], F32, es) for _ in range(2)]
        hT = [sc.sbuf("hT", [128, 8, 512], BF16, es) for _ in range(2)]
        fst = [sc.sbuf("fst", [128, 512], BF16, es) for _ in range(4)]
        vst = [sc.sbuf("vst", [128, VV_W], BF16, es) for _ in range(2)]
        sst = [sc.sbuf("sst", [128, 16], F32, es) for _ in range(2)]
        mst = [sc.sbuf("mst", [128, 3072], F32, es) for _ in range(2)]
        ps = [sc.psum("pps", [128, 512], F32, es) for _ in range(8)]
        pi = [0]
        def nextps():
            p = ps[pi[0] % 8]
            pi[0] += 1
            return p
        nblk = ntok // 512
        fi = 0
        for blk in range(nblk):
            b = (blk * 512) // seq
            x_t = xs[blk % 2]
            h_t = hT[blk % 2]
            sc.dma("sp", x_t[:], xsrc[blk * 512:(blk + 1) * 512, :].rearrange("(j p) d -> p j d", p=128),
                   reads=[xsrc], writes=[x_t], track=x_t, nowaw=False)
            for c in range(8):
                p = nextps()
                for j in range(4):
                    sc.op("pe", lambda: nc.tensor.transpose(p[:, j * 128:(j + 1) * 128], x_t[:, j, c * 128:(c + 1) * 128], ident[:]),
                          reads=[x_t, ident], writes=[p], nowaw=(j > 0))
                sc.op("act", lambda: nc.scalar.activation(out=h_t[:, c, :], in_=p[:, :], func=AF.Identity,
                                                          bias=shf[:, c, b:b + 1], scale=opsc[:, c, b:b + 1]),
                      reads=[p, shf, opsc], writes=[h_t], nowaw=(c > 0))
            for (nm, c0, wd) in FM_CHUNKS:
                p = nextps()
                for kc in range(8):
                    sc.op("pe", lambda: nc.tensor.matmul(p[:wd, :], lhsT=wb[:, kc, c0:c0 + wd], rhs=h_t[:, kc, :],
                                                         start=(kc == 0), stop=(kc == 7)),
                          reads=[wb, h_t], writes=[p], nowaw=(kc > 0))
                f = fst[fi % 4]
                ek = "dve" if fi % 2 == 0 else "act"
                if ek == "dve":
                    sc.op("dve", lambda: nc.vector.tensor_copy(f[:wd, :], p[:wd, :]), reads=[p], writes=[f])
                else:
                    sc.op("act", lambda: nc.scalar.copy(f[:wd, :], p[:wd, :]), reads=[p], writes=[f])
                r0 = FM_ROW[nm]
                sc.dma("sp", FM[r0:r0 + wd, blk * 512:(blk + 1) * 512], f[:wd, :], reads=[f], writes=[FM], track=f)
                fi += 1
            for j in range(4):
                tk = blk * 4 + j
                v_t, s_t, m_t = vst[tk % 2], sst[tk % 2], mst[tk % 2]
                tok0 = blk * 512 + j * 128
                def tm(c0, wd):
                    p = nextps()
                    for kc in range(8):
                        sc.op("pe", lambda: nc.tensor.matmul(p[:, :wd], lhsT=h_t[:, kc, j * 128:(j + 1) * 128],
                                                             rhs=wb[:, kc, c0:c0 + wd], start=(kc == 0), stop=(kc == 7)),
                              reads=[wb, h_t], writes=[p], nowaw=(kc > 0))
                    return p
                p = tm(C_AV, 64)
                sc.op("dve", lambda: nc.vector.tensor_copy(v_t[:, 0:64], p[:, 0:64]), reads=[p], writes=[v_t])
                p = tm(C_VS, 64)
                sc.op("dve", lambda: nc.vector.tensor_copy(v_t[:, 64:128], p[:, 0:64]), reads=[p], writes=[v_t], nowaw=True)
                p = tm(C_VW, 76)
                sc.op("dve", lambda: nc.vector.tensor_copy(v_t[:, 128:192], p[:, 0:64]), reads=[p], writes=[v_t], nowaw=True)
                sc.op("act", lambda: nc.scalar.activation(out=s_t[:, 4:16], in_=p[:, 64:76], func=AF.Sigmoid),
                      reads=[p], writes=[s_t])
                p = tm(C_IW, 4)
                sc.op("dve", lambda: nc.vector.tensor_copy(s_t[:, 0:4], p[:, 0:4]), reads=[p], writes=[s_t], nowaw=True)
                p = tm(C_CV, 512)
                sc.op("dve", lambda: nc.vector.tensor_copy(v_t[:, 192:704], p[:, :]), reads=[p], writes=[v_t], nowaw=True)
                sc.dma("sp", VV[tok0:tok0 + 128, :], v_t[:], reads=[v_t], writes=[VV], track=v_t)
                sc.dma("sp", SM[tok0:tok0 + 128, :], s_t[:], reads=[s_t], writes=[SM], track=s_t)
                for g in range(6):
                    p = tm(C_MG + g * 512, 512)
                    sc.op("act", lambda: nc.scalar.activation(out=m_t[:, g * 512:(g + 1) * 512], in_=p[:, :], func=AF.Sigmoid),
                          reads=[p], writes=[m_t], nowaw=(g > 0))
                sc.dma("sp", MG[tok0:tok0 + 128, :], m_t[:], reads=[m_t], writes=[MG], track=m_t)
        allb = [wb] + xs + hT + fst + vst + sst + mst + ps
        sc.barrier_on(allb)
        sched_release(sc, allb)


def rel_bucket_np(dist):
    n = np.maximum(dist, 0)
    nf = np.maximum(n, 1).astype(np.float32)
    large = 16 + (np.log(nf / 16) / np.float32(math.log(128 / 16)) * 16).astype(np.int32)
    large = np.minimum(large, 31)
    return np.where(n < 16, n, large)


def make_consts():
    import ml_dtypes
    c = {}
    r = np.arange(128)[:, None]
    s = np.arange(256)[None, :]
    dist = r + 128 - s
    bk = rel_bucket_np(dist)
    oh = np.zeros((128, 256, 32), np.float32)
    oh[np.arange(128)[:, None], np.arange(256)[None, :], bk] = 1.0
    c["c_oh"] = oh.reshape(128, 256 * 32).astype(ml_dtypes.bfloat16)
    c["c_cneg"] = np.where(dist >= 0, 0.0, NEG).astype(np.float32)
    cm = np.where(np.arange(128)[None, :] <= np.arange(128)[:, None], 0.0, -1e30).astype(np.float32)
    c["c_cm"] = cm
    cc = np.arange(384)[None, :]
    c["c_wneg"] = np.where(cc > r, 0.0, NEG).astype(np.float32)
    c["c_iota"] = np.broadcast_to(np.arange(512, dtype=np.float32)[None, :], (128, 512)).copy()
    qi = np.arange(32)[None, :]
    c["c_tc"] = ((r + qi * 128 - 31) / 16.0).astype(np.float32)
    n_cmp, n_slc = 255, 64
    start = np.arange(n_cmp) * 16
    end = start + 32
    bs = np.arange(n_slc) * 64
    ov = ((start[:, None] < bs[None, :] + 64) & (end[:, None] > bs[None, :])).astype(np.float32)
    ovp = np.zeros((256, 64), np.float32)
    ovp[:255] = ov
    c["c_ovl"] = np.ascontiguousarray(ovp.reshape(2, 128, 64).transpose(1, 0, 2)).astype(ml_dtypes.bfloat16)
    sel = np.zeros((32, 128, 192), np.float32)
    blk = np.arange(64)[None, :]
    for q in range(32):
        t = q * 128 + np.arange(128)[:, None]
        cur = t // 64
        forced = (blk == 0) | (blk == cur) | (blk == cur - 1)
        allowed = blk <= cur
        sel[q, :, 0:64] = (allowed & ~forced).astype(np.float32)
        sel[q, :, 64:128] = np.where(allowed, np.where(forced, 1e9 + 1024.0 * blk, 0.0), -1e30)
        sel[q, :, 128:192] = allowed.astype(np.float32)
    c["c_sel"] = sel
    c["c_pow2"] = np.broadcast_to((2.0 ** -np.arange(40, dtype=np.float32))[None, :], (128, 40)).copy()
    c["c_ident"] = np.eye(128, dtype=np.float32)
    c["c_ut"] = (np.arange(128)[:, None] <= np.arange(128)[None, :]).astype(np.float32)
    return c


CONST_SPECS = [("c_oh", [128, 8192], BF16), ("c_cneg", [128, 256], F32), ("c_cm", [128, 128], F32),
               ("c_wneg", [128, 384], F32), ("c_iota", [128, 512], F32), ("c_tc", [128, 32], F32),
               ("c_ovl", [128, 2, 64], BF16), ("c_sel", [32, 128, 192], F32), ("c_pow2", [128, 40], F32),
               ("c_ident", [128, 128], F32), ("c_ut", [128, 128], F32)]
NBIS = 38
TIE_EPS = 2.0 ** -34


def setup_attn_consts(sc, cd, rel_bias, es):
    nc = sc.nc
    K = {}
    def ld(name, shape, dt, src_ap, srcbuf):
        b = sc.sbuf(name, shape, dt, es)
        sc.dma("sp", b[:], src_ap, reads=[srcbuf], writes=[b], track=b)
        return b
    K["cneg"] = ld("cneg", [128, 256], F32, cd["c_cneg"][:, :], cd["c_cneg"])
    K["cm"] = ld("cm", [128, 128], F32, cd["c_cm"][:, :], cd["c_cm"])
    K["wneg"] = ld("wneg", [128, 384], F32, cd["c_wneg"][:, :], cd["c_wneg"])
    K["iota"] = ld("iota", [128, 512], F32, cd["c_iota"][:, :], cd["c_iota"])
    K["tc"] = ld("tc", [128, 32], F32, cd["c_tc"][:, :], cd["c_tc"])
    K["ovl"] = ld("ovl", [128, 2, 64], BF16, cd["c_ovl"][:, :, :], cd["c_ovl"])
    K["pow2"] = ld("pow2", [128, 40], F32, cd["c_pow2"][:, :], cd["c_pow2"])
    K["ident"] = ld("identf", [128, 128], F32, cd["c_ident"][:, :], cd["c_ident"])
    K["tabb"] = ld("tabb", [128, 384], F32,
                   rel_bias.t.rearrange("b h -> (b h)").unsqueeze(0).partition_broadcast(128), rel_bias)
    K["identb"] = sc.sbuf("identb", [128, 128], BF16, es)
    sc.op("dve", lambda: nc.vector.tensor_copy(K["identb"][:], K["ident"][:]), reads=[K["ident"]], writes=[K["identb"]])
    K["near"] = sc.sbuf("near", [128, 12, 256], F32, es)
    with ExitStack() as tes:
        oh = sc.sbuf("oh", [128, 8192], BF16, tes)
        tmp = sc.sbuf("ohtmp", [128, 8192], F32, tes)
        sc.dma("sp", oh[:], cd["c_oh"][:, :], reads=[cd["c_oh"]], writes=[oh], track=oh)
        for hd in range(12):
            tb = K["tabb"][:, :].rearrange("p (b h) -> p h b", h=12)[:, hd, :]
            sc.op("dve", lambda: nc.vector.tensor_tensor(out=tmp[:].rearrange("p (s b) -> p s b", b=32),
                                                        in0=oh[:].rearrange("p (s b) -> p s b", b=32),
                                                        in1=tb.unsqueeze(1).to_broadcast([128, 256, 32]), op=ALU.mult),
                  reads=[oh, K["tabb"]], writes=[tmp])
            sc.op("dve", lambda: nc.vector.tensor_reduce(out=K["near"][:, hd, :], in_=tmp[:].rearrange("p (s b) -> p s b", b=32),
                                                        axis=AX.X, op=ALU.add), reads=[tmp], writes=[K["near"]], nowaw=True)
            sc.op("dve", lambda: nc.vector.scalar_tensor_tensor(out=K["near"][:, hd, :], in0=K["near"][:, hd, :],
                                                               scalar=K["tabb"][:, 31 * 12 + hd:31 * 12 + hd + 1], in1=K["cneg"][:],
                                                               op0=ALU.subtract, op1=ALU.add), reads=[K["near"], K["cneg"], K["tabb"]], writes=[K["near"]])
        sc.barrier_on([oh, tmp])
        sched_release(sc, [oh, tmp])
    return K


def phase_compress(sc, cfg, l, FM, cmp_w1, cmp_w2, cposT, KC, VC):
    nc = sc.nc
    nseq, seq = cfg["nseq"], cfg["seq"]
    ncmp = (seq - 32) // 16 + 1
    with ExitStack() as es:
        w1 = sc.sbuf("cw1", [128, 32, 256], BF16, es)
        w2 = sc.sbuf("cw2", [128, 2, 2, 64], BF16, es)
        posT = sc.sbuf("cposT", [128, 32], F32, es)
        raw = sc.sbuf("craw", [128, seq], BF16, es)
        rawp = sc.sbuf("crawp", [128, 32, 256], BF16, es)
        hidT = [sc.sbuf("chid", [128, 2, 256], BF16, es) for _ in range(2)]
        t1 = sc.sbuf("ct1", [128, 256], F32, es)
        t2 = sc.sbuf("ct2", [128, 256], F32, es)
        ps = [sc.psum("cps", [128, 512], F32, es) for _ in range(2)]
        for j in range(2):
            sc.dma("pool", w1[j * 64:(j + 1) * 64, :, :], cmp_w1[l, j].rearrange("(p d) h -> d p h", d=64),
                   reads=[cmp_w1], writes=[w1], track=w1)
        sc.dma("pool", w2[:], cmp_w2[l].rearrange("j (hc p) d -> p j hc d", p=128), reads=[cmp_w2], writes=[w2], track=w2)
        sc.dma("sp", posT[:], cposT[l], reads=[cposT], writes=[posT], track=posT)
        sc.op("dve", lambda: nc.vector.memset(VC[:], 0.0), writes=[VC])
        sc.op("dve", lambda: nc.vector.memset(KC[:], 0.0), writes=[KC])
        r0 = FM_ROW["kvcr"]
        pi = 0
        for b in range(nseq):
            sc.dma("sp", raw[:], FM[r0:r0 + 128, b * seq:(b + 1) * seq], reads=[FM], writes=[raw], track=raw, nowaw=False)
            r3 = raw[:].rearrange("p (n s) -> p n s", s=16)
            for p in range(32):
                src = r3[:, 0:ncmp, p] if p < 16 else r3[:, 1:ncmp + 1, p - 16]
                sc.op("dve", lambda: nc.vector.tensor_scalar(out=rawp[:, p, :ncmp], in0=src, scalar1=posT[:, p:p + 1],
                                                            scalar2=None, op0=ALU.add),
                      reads=[raw, posT], writes=[rawp], nowaw=(p > 0))
            for j in range(2):
                for hc in range(2):
                    ph = ps[pi % 2]; pi += 1
                    for p in range(32):
                        sc.op("pe", lambda: nc.tensor.matmul(ph[:, :ncmp], lhsT=w1[j * 64:(j + 1) * 64, p, hc * 128:(hc + 1) * 128],
                                                             rhs=rawp[j * 64:(j + 1) * 64, p, :ncmp], start=(p == 0), stop=(p == 31)),
                              reads=[w1, rawp], writes=[ph], nowaw=(p > 0))
                    sc.op("act", lambda: nc.scalar.activation(out=t1[:, :ncmp], in_=ph[:, :ncmp], func=AF.Square), reads=[ph], writes=[t1])
                    sc.op("dve", lambda: nc.vector.tensor_scalar(out=t1[:, :ncmp], in0=t1[:, :ncmp], scalar1=0.044715, scalar2=1.0,
                                                                op0=ALU.mult, op1=ALU.add), reads=[t1], writes=[t1])
                    sc.op("dve", lambda: nc.vector.tensor_tensor(out=t1[:, :ncmp], in0=t1[:, :ncmp], in1=ph[:, :ncmp], op=ALU.mult),
                          reads=[t1, ph], writes=[t1])
                    sc.op("act", lambda: nc.scalar.activation(out=t2[:, :ncmp], in_=t1[:, :ncmp], func=AF.Sigmoid, scale=1.5957691216057308),
                          reads=[t1], writes=[t2])
                    sc.op("dve", lambda: nc.vector.tensor_tensor(out=hidT[j][:, hc, :ncmp], in0=t2[:, :ncmp], in1=ph[:, :ncmp], op=ALU.mult),
                          reads=[t2, ph], writes=[hidT[j]], nowaw=(hc > 0))
            pk = ps[pi % 2]; pi += 1
            for hc in range(2):
                sc.op("pe", lambda: nc.tensor.matmul(pk[:64, :ncmp], lhsT=w2[:, 0, hc, :], rhs=hidT[0][:, hc, :ncmp],
                                                     start=(hc == 0), stop=(hc == 1)), reads=[w2, hidT[0]], writes=[pk], nowaw=(hc > 0))
            sc.op("dve", lambda: nc.vector.tensor_copy(KC[:, b, :ncmp], pk[:64, :ncmp]), reads=[pk], writes=[KC], nowaw=True)
            for nch in range(2):
                n0 = nch * 128
                nsz = min(128, ncmp - n0)
                if nsz <= 0:
                    continue
                pv = ps[pi % 2]; pi += 1
                for hc in range(2):
                    sc.op("pe", lambda: nc.tensor.matmul(pv[:nsz, 0:64], lhsT=hidT[1][:, hc, n0:n0 + nsz], rhs=w2[:, 1, hc, :],
                                                         start=(hc == 0), stop=(hc == 1)), reads=[w2, hidT[1]], writes=[pv], nowaw=(hc > 0))
                sc.op("dve", lambda: nc.vector.tensor_copy(VC[:nsz, b, nch, :], pv[:nsz, 0:64]), reads=[pv], writes=[VC], nowaw=True)
        allb = [w1, w2, posT, raw, rawp, t1, t2] + hidT + ps
        sc.barrier_on(allb)
        sched_release(sc, allb)


def phase_attn(sc, cfg, l, K, FM, VV, SM, KC, VC, csel, diff_lambda, diff_norm_g, lam_init, OABC, qtiles=None):
    nc = sc.nc
    nseq, seq = cfg["nseq"], cfg["seq"]
    nqt = seq // 128
    IDXC = 0.5 * (32 ** -0.5)
    with ExitStack() as es:
        KT = sc.sbuf("KT", [128, 2, seq], BF16, es)
        CK = sc.sbuf("CK", [128, 4, seq], BF16, es)
        VT = sc.sbuf("VT", [128, nqt, VV_W], BF16, es)
        ISC = sc.sbuf("ISC", [128, seq], F32, es)
        JUNK = sc.sbuf("JUNK", [128, seq], BF16, es)
        QI = [sc.sbuf("QI", [128, 4, 128], BF16, es) for _ in range(2)]
        QA = [sc.sbuf("QA", [64, 4, 128], BF16, es) for _ in range(2)]
        QB = [sc.sbuf("QB", [128, 4, 128], BF16, es) for _ in range(2)]
        QC = [sc.sbuf("QC", [128, 4, 128], BF16, es) for _ in range(2)]
        SMt = [sc.sbuf("SMt", [128, 16], F32, es) for _ in range(2)]
        SELt = [sc.sbuf("SELt", [128, 192], F32, es) for _ in range(2)]
        OUT = [sc.sbuf("OUT", [128, 1024], F32, es) for _ in range(1)]
        AW = sc.sbuf("AW", [128, 4], F32, es)
        SGN = sc.sbuf("SGN", [128, 4], F32, es)
        RT = [sc.sbuf("RT", [128, 512], F32, es) for _ in range(2)]
        ST = sc.sbuf("STAT", [128, 64], F32, es)
        W = sc.sbuf("Wb", [128, 40], F32, es)
        CMK = sc.sbuf("CMK", [128, 256], F32, es)
        PCf = sc.sbuf("PCf", [128, 256], F32, es)
        PCm = sc.sbuf("PCm", [128, 4, 256], F32, es)
        PCn = sc.sbuf("PCn", [128, 5, 256], BF16, es)
        PCT = sc.sbuf("PCT", [128, 10, 128], BF16, es)
        CS = sc.sbuf("CS", [128, 8], F32, es)
        OCMP = sc.sbuf("OCMP", [128, 256], F32, es)
        OWIN = sc.sbuf("OWIN", [128, 256], F32, es)
        IMP = sc.sbuf("IMP", [128, 64], F32, es)
        RANK = sc.sbuf("RANK", [128, 64], F32, es)
        BM = sc.sbuf("BM", [128, 64], F32, es)
        PW = sc.sbuf("PW", [128, 640], BF16, es)
        TMPN = [sc.sbuf("TMPN", [128, 384], F32, es) for _ in range(2)]
        WS = sc.sbuf("WS", [128, 8], F32, es)
        P = [sc.sbuf("P", [128, 512], BF16, es) for _ in range(3)]
        PM = [sc.sbuf("PM", [128, 512], BF16, es) for _ in range(2)]
        MAc = sc.sbuf("MAc", [128, 512], BF16, es)
        PT = [sc.sbuf("PT", [128, 8, 128], BF16, es) for _ in range(2)]
        SUMS = sc.sbuf("SUMS", [128, 16, 16], F32, es)
        TOT = sc.sbuf("TOT", [128, 16], F32, es)
        RCP = sc.sbuf("RCP", [128, 16], F32, es)
        LAM = sc.sbuf("LAM", [128, 4], F32, es)
        DL = sc.sbuf("DL", [128, 256], F32, es)
        DNG = sc.sbuf("DNG", [128, 128], F32, es)
        OCt = sc.sbuf("OCt", [128, 128], F32, es)
        OCj = sc.sbuf("OCj", [128, 128], F32, es)
        TAB31 = sc.sbuf("TAB31", [128, 12], F32, es)
        psS = [sc.psum("psS", [128, 512], F32, es) for _ in range(3)]
        psT = [sc.psum("psT", [128, 8, 128], BF16, es) for _ in range(2)]
        psOab = sc.psum("psOab", [128, 512], F32, es)
        psOc = [sc.psum("psOc", [128, 512], F32, es) for _ in range(2)]
        cnt = {"s": 0, "t": 0, "p": 0, "pm": 0, "pt": 0, "rt": 0, "tn": 0, "ev": 0}
        def nxt(lst, key):
            b = lst[cnt[key] % len(lst)]
            cnt[key] += 1
            return b

        sc.op("dve", lambda: nc.vector.tensor_copy(TAB31[:], K["tabb"][:, 31 * 12:32 * 12]), reads=[K["tabb"]], writes=[TAB31])
        sc.dma("sp", DL[:], diff_lambda[l].rearrange("a d -> (a d)").unsqueeze(0).partition_broadcast(128),
               reads=[diff_lambda], writes=[DL], track=DL)
        sc.dma("sp", DNG[:], diff_norm_g[l:l + 1, :].partition_broadcast(128), reads=[diff_norm_g], writes=[DNG], track=DNG)
        sc.op("dve", lambda: nc.vector.tensor_scalar(out=DNG[:], in0=DNG[:], scalar1=float(1.0 - lam_init), scalar2=None, op0=ALU.mult),
              reads=[DNG], writes=[DNG])
        for a in range(2):
            sc.op("dve", lambda: nc.vector.scalar_tensor_tensor(out=OCt[:, 0:64], in0=DL[:, (2 * a) * 64:(2 * a + 1) * 64], scalar=1.0,
                                                               in1=DL[:, (2 * a + 1) * 64:(2 * a + 2) * 64], op0=ALU.mult, op1=ALU.mult,
                                                               accum_out=LAM[:, 2 + a:3 + a]), reads=[DL], writes=[OCt, LAM])
        sc.op("act", lambda: nc.scalar.activation(out=LAM[:, 2:4], in_=LAM[:, 2:4], func=AF.Exp), reads=[LAM], writes=[LAM])
        sc.op("dve", lambda: nc.vector.scalar_tensor_tensor(out=LAM[:, 0:1], in0=LAM[:, 2:3], scalar=float(lam_init), in1=LAM[:, 3:4],
                                                           op0=ALU.add, op1=ALU.subtract), reads=[LAM], writes=[LAM])
        sc.op("dve", lambda: nc.vector.tensor_scalar(out=LAM[:, 1:2], in0=LAM[:, 0:1], scalar1=-1.0, scalar2=None, op0=ALU.mult),
              reads=[LAM], writes=[LAM])
        sc.op("dve", lambda: nc.vector.memset(PCn[:], 0.0), writes=[PCn])

        qn = 0
        for b in range(nseq):
            c0 = b * seq
            sc.dma("sp", KT[64:96, 1, :], FM[FM_ROW["ik"]:FM_ROW["ik"] + 32, c0:c0 + seq], reads=[FM], writes=[KT], track=KT, nowaw=False)
            for nm, (p0, sl) in (("ak", (0, 0)), ("ks", (64, 0)), ("kw", (0, 1))):
                sc.dma("sp", KT[p0:p0 + 64, sl, :], FM[FM_ROW[nm]:FM_ROW[nm] + 64, c0:c0 + seq], reads=[FM], writes=[KT], track=KT)
            for h in range(4):
                r = FM_ROW["ck%d" % h]
                sc.dma("sp", CK[:, h, :], FM[r:r + 128, c0:c0 + seq], reads=[FM], writes=[CK], track=CK, nowaw=(h > 0))
            for n4 in range(0, nqt, 8):
                sc.dma("sp", VT[:, n4:n4 + 8, :], VV[c0 + n4 * 128:c0 + (n4 + 8) * 128, :].rearrange("(n p) c -> p n c", p=128),
                       reads=[VV], writes=[VT], track=VT, nowaw=(n4 > 0))
            for qi in (qtiles if qtiles is not None else range(nqt)):
                tok0 = c0 + qi * 128
                nk = qi + 1
                Nk = nk * 128
                qI, qA, qB, qC, smt, selt = (x[qn % 2] for x in (QI, QA, QB, QC, SMt, SELt))
                out = OUT[0]
                qn += 1
                r = FM_ROW["iq"]
                sc.dma("sp", qI[64:96, :, :], FM[r:r + 128, tok0:tok0 + 128].rearrange("(h d) q -> d h q", d=32), reads=[FM], writes=[qI], track=qI, nowaw=False)
                r = FM_ROW["aq0"]
                sc.dma("sp", qA[:], FM[r:r + 256, tok0:tok0 + 128].rearrange("(h d) q -> d h q", d=64), reads=[FM], writes=[qA], track=qA, nowaw=False)
                r = FM_ROW["bq0"]
                sc.dma("sp", qB[0:64, :, :], FM[r:r + 256, tok0:tok0 + 128].rearrange("(h d) q -> d h q", d=64), reads=[FM], writes=[qB], track=qB, nowaw=False)
                sc.dma("sp", qB[64:128, :, :], FM[r:r + 256, tok0:tok0 + 128].rearrange("(h d) q -> d h q", d=64), reads=[FM], writes=[qB], track=qB)
                r = FM_ROW["cq0"]
                sc.dma("sp", qC[:], FM[r:r + 512, tok0:tok0 + 128].rearrange("(h r) q -> r h q", r=128), reads=[FM], writes=[qC], track=qC, nowaw=False)
                sc.dma("sp", smt[:], SM[tok0:tok0 + 128, :], reads=[SM], writes=[smt], track=smt, nowaw=False)
                sc.dma("sp", selt[:], csel[qi], reads=[csel], writes=[selt], track=selt, nowaw=False)
                chunks = [(k0, min(512, Nk - k0)) for k0 in range(0, Nk, 512)]

                sc.op("act", lambda: nc.scalar.activation(out=AW[:], in_=smt[:, 0:4], func=AF.Abs, scale=IDXC), reads=[smt], writes=[AW])
                sc.op("act", lambda: nc.scalar.activation(out=SGN[:], in_=smt[:, 0:4], func=AF.Sign), reads=[smt], writes=[SGN])
                for (k0, w) in chunks:
                    for h in range(4):
                        ps = nxt(psS, "s")
                        sc.op("pe", lambda: nc.tensor.matmul(ps[:, :w], lhsT=qI[64:96, h, :], rhs=KT[64:96, 1, k0:k0 + w], start=True, stop=True),
                              reads=[qI, KT], writes=[ps])
                        rt = nxt(RT, "rt")
                        sc.op("act", lambda: nc.scalar.activation(out=rt[:, :w], in_=ps[:, :w], func=AF.Relu, scale=AW[:, h:h + 1]),
                              reads=[ps, AW], writes=[rt])
                        if h == 0:
                            sc.op("dve", lambda: nc.vector.tensor_scalar(out=ISC[:, k0:k0 + w], in0=rt[:, :w], scalar1=SGN[:, 0:1], scalar2=None,
                                                                        op0=ALU.mult), reads=[rt, SGN], writes=[ISC], nowaw=True)
                        else:
                            sc.op("dve", lambda: nc.vector.scalar_tensor_tensor(out=ISC[:, k0:k0 + w], in0=rt[:, :w], scalar=SGN[:, h:h + 1],
                                                                               in1=ISC[:, k0:k0 + w], op0=ALU.mult, op1=ALU.add),
                                  reads=[rt, SGN, ISC], writes=[ISC])
                    rt = nxt(RT, "rt")
                    sc.op("dve", lambda: nc.vector.tensor_scalar(out=rt[:, :w], in0=K["iota"][:, :w], scalar1=float(k0), scalar2=-TIE_EPS,
                                                                op0=ALU.add, op1=ALU.mult), reads=[K["iota"]], writes=[rt])
                    sc.op("dve", lambda: nc.vector.scalar_tensor_tensor(out=rt[:, :w], in0=ISC[:, k0:k0 + w], scalar=0.0, in1=rt[:, :w],
                                                                       op0=ALU.is_equal, op1=ALU.mult), reads=[ISC, rt], writes=[rt])
                    sc.op("dve", lambda: nc.vector.tensor_tensor(out=ISC[:, k0:k0 + w], in0=ISC[:, k0:k0 + w], in1=rt[:, :w], op=ALU.add),
                          reads=[ISC, rt], writes=[ISC])
                sc.op("dve", lambda: nc.vector.tensor_tensor(out=ISC[:, Nk - 128:Nk], in0=ISC[:, Nk - 128:Nk], in1=K["cm"][:], op=ALU.add),
                      reads=[ISC, K["cm"]], writes=[ISC])
                if cfg.get('stop_after', 9) <= 1:
                    continue
                THR = ST[:, 7:8]
                if qi >= 2:
                    sc.op("dve", lambda: nc.vector.tensor_reduce(out=ST[:, 0:1], in_=ISC[:, :Nk], axis=AX.X, op=ALU.max), reads=[ISC], writes=[ST])
                    sc.op("dve", lambda: nc.vector.tensor_reduce(out=ST[:, 1:2], in_=ISC[:, :Nk - 128], axis=AX.X, op=ALU.min), reads=[ISC], writes=[ST])
                    sc.op("dve", lambda: nc.vector.scalar_tensor_tensor(out=ST[:, 2:3], in0=ST[:, 0:1], scalar=1.0, in1=ST[:, 1:2],
                                                                       op0=ALU.add, op1=ALU.subtract), reads=[ST], writes=[ST])
                    sc.op("dve", lambda: nc.vector.tensor_scalar(out=W[:], in0=K["pow2"][:], scalar1=ST[:, 2:3], scalar2=None, op0=ALU.mult),
                          reads=[K["pow2"], ST], writes=[W])
                    sc.op("dve", lambda: nc.vector.tensor_tensor(out=ST[:, 3:4], in0=ST[:, 1:2], in1=W[:, 1:2], op=ALU.add), reads=[ST, W], writes=[ST])
                    for k in range(1, NBIS + 1):
                        sc.op("dve", lambda: nc.vector.tensor_scalar(out=JUNK[:, :Nk], in0=ISC[:, :Nk], scalar1=ST[:, 3:4], scalar2=0.0,
                                                                    op0=ALU.is_ge, op1=ALU.add, accum_out=ST[:, 4:5]),
                              reads=[ISC, ST], writes=[JUNK, ST])
                        sc.op("dve", lambda: nc.vector.tensor_scalar(out=ST[:, 5:6], in0=ST[:, 4:5], scalar1=255.5, scalar2=0.5,
                                                                    op0=ALU.is_ge, op1=ALU.subtract), reads=[ST], writes=[ST])
                        if k < NBIS:
                            sc.op("dve", lambda: nc.vector.scalar_tensor_tensor(out=ST[:, 3:4], in0=ST[:, 5:6], scalar=W[:, k:k + 1], in1=ST[:, 3:4],
                                                                               op0=ALU.mult, op1=ALU.add), reads=[ST, W], writes=[ST])
                    sc.op("dve", lambda: nc.vector.tensor_scalar(out=ST[:, 6:7], in0=ST[:, 5:6], scalar1=0.5, scalar2=W[:, NBIS:NBIS + 1],
                                                                op0=ALU.subtract, op1=ALU.mult), reads=[ST, W], writes=[ST])
                    sc.op("dve", lambda: nc.vector.tensor_tensor(out=THR, in0=ST[:, 3:4], in1=ST[:, 6:7], op=ALU.add), reads=[ST], writes=[ST])
                else:
                    sc.op("dve", lambda: nc.vector.memset(THR, -1e29), reads=[], writes=[ST])

                if cfg.get('stop_after', 9) <= 2:
                    continue
                sc.op("dve", lambda: nc.vector.tensor_scalar(out=CMK[:], in0=K["iota"][:, 0:256], scalar1=K["tc"][:, qi:qi + 1], scalar2=1.0, op0=ALU.is_gt, op1=ALU.subtract),
                      reads=[K["iota"], K["tc"]], writes=[CMK])
                for h in range(4):
                    ps = nxt(psS, "s")
                    sc.op("pe", lambda: nc.tensor.matmul(ps[:, :256], lhsT=qB[0:64, h, :], rhs=KC[:, b, :], start=True, stop=True),
                          reads=[qB, KC], writes=[ps])
                    sc.op("act", lambda: nc.scalar.activation(out=PCf[:, :255], in_=ps[:, :255], func=AF.Exp, scale=0.125), reads=[ps], writes=[PCf])
                    sc.op("dve", lambda: nc.vector.scalar_tensor_tensor(out=PCm[:, h, :255], in0=PCf[:, :255], scalar=-1.0, in1=CMK[:, :255],
                                                                       op0=ALU.mult, op1=ALU.mult, accum_out=CS[:, h:h + 1]),
                          reads=[PCf, CMK], writes=[PCm, CS], nowaw=True)
                sc.op("dve", lambda: nc.vector.tensor_scalar(out=CS[:, 4:8], in0=CS[:, 0:4], scalar1=1e-30, scalar2=None, op0=ALU.max), reads=[CS], writes=[CS])
                sc.op("dve", lambda: nc.vector.reciprocal(out=CS[:, 4:8], in_=CS[:, 4:8]), reads=[CS], writes=[CS])
                for h in range(4):
                    sc.op("dve", lambda: nc.vector.tensor_scalar(out=PCn[:, h, :255], in0=PCm[:, h, :255], scalar1=CS[:, 4 + h:5 + h], scalar2=None,
                                                                op0=ALU.mult), reads=[PCm, CS], writes=[PCn], nowaw=True)
                    if h == 0:
                        sc.op("dve", lambda: nc.vector.tensor_scalar(out=PCf[:, :255], in0=PCm[:, 0, :255], scalar1=CS[:, 4:5], scalar2=None,
                                                                    op0=ALU.mult), reads=[PCm, CS], writes=[PCf])
                    else:
                        sc.op("dve", lambda: nc.vector.scalar_tensor_tensor(out=PCf[:, :255], in0=PCm[:, h, :255], scalar=CS[:, 4 + h:5 + h],
                                                                           in1=PCf[:, :255], op0=ALU.mult, op1=ALU.add),
                              reads=[PCm, CS, PCf], writes=[PCf])
                sc.op("dve", lambda: nc.vector.tensor_copy(PCn[:, 4, :255], PCf[:, :255]), reads=[PCf], writes=[PCn], nowaw=True)
                for g in range(2):
                    pt = nxt(psT, "t")
                    lo, hi = (0, 8) if g == 0 else (8, 10)
                    for i in range(lo, hi):
                        h, nch = i // 2, i % 2
                        sc.op("pe", lambda: nc.tensor.transpose(pt[:, i - lo, :], PCn[:, h, nch * 128:(nch + 1) * 128], K["identb"][:]),
                              reads=[PCn, K["identb"]], writes=[pt], nowaw=(i > lo))
                    sc.op("act", lambda: nc.scalar.copy(PCT[:, lo:hi, :], pt[:, 0:hi - lo, :]), reads=[pt], writes=[PCT], nowaw=True)
                first = True
                for h in range(4):
                    for nch in range(2):
                        sc.op("pe", lambda: nc.tensor.matmul(psOab[:, h * 64:(h + 1) * 64], lhsT=PCT[:, h * 2 + nch, :], rhs=VC[:, b, nch, :],
                                                             start=first, stop=(nch == 1), skip_group_check=True),
                              reads=[PCT, VC], writes=[psOab], nowaw=(not first))
                        first = False
                psI = nxt(psS, "s")
                for nch in range(2):
                    sc.op("pe", lambda: nc.tensor.matmul(psI[:, 0:64], lhsT=PCT[:, 8 + nch, :], rhs=K["ovl"][:, nch, :], start=(nch == 0), stop=(nch == 1)),
                          reads=[PCT, K["ovl"]], writes=[psI], nowaw=(nch > 0))
                sc.op("act", lambda: nc.scalar.copy(OCMP[:], psOab[:, 0:256]), reads=[psOab], writes=[OCMP])
                sc.op("dve", lambda: nc.vector.tensor_tensor(out=IMP[:], in0=psI[:, 0:64], in1=selt[:, 0:64], op=ALU.mult), reads=[psI, selt], writes=[IMP])
                sc.op("dve", lambda: nc.vector.tensor_tensor(out=IMP[:], in0=IMP[:], in1=selt[:, 64:128], op=ALU.add), reads=[IMP, selt], writes=[IMP])
                J3 = JUNK[:, 0:4096].rearrange("p (j i) -> p j i", i=64)
                sc.op("dve", lambda: nc.vector.tensor_tensor(out=J3, in0=IMP[:].unsqueeze(1).to_broadcast([128, 64, 64]),
                                                            in1=IMP[:].unsqueeze(2).to_broadcast([128, 64, 64]), op=ALU.is_gt),
                      reads=[IMP], writes=[JUNK])
                sc.op("dve", lambda: nc.vector.tensor_reduce(out=RANK[:], in_=J3, axis=AX.X, op=ALU.add), reads=[JUNK], writes=[RANK])
                sc.op("dve", lambda: nc.vector.scalar_tensor_tensor(out=BM[:], in0=RANK[:], scalar=15.5, in1=selt[:, 128:192], op0=ALU.is_lt, op1=ALU.mult),
                      reads=[RANK, selt], writes=[BM])

                if cfg.get('stop_after', 9) <= 3:
                    continue
                nwin = min(qi, 4) + 1
                nnear = min(nwin, 2)
                nfar = nwin - nnear
                kt0 = qi - (nwin - 1)
                for h in range(4):
                    if nfar > 0:
                        ps = nxt(psS, "s")
                        wf = nfar * 128
                        sc.op("pe", lambda: nc.tensor.matmul(ps[:, :wf], lhsT=qB[0:64, h, :], rhs=KT[0:64, 1, kt0 * 128:kt0 * 128 + wf], start=True, stop=True),
                              reads=[qB, KT], writes=[ps])
                        tn = nxt(TMPN, "tn")
                        sc.op("dve", lambda: nc.vector.scalar_tensor_tensor(out=tn[:, :wf], in0=ps[:, :wf], scalar=0.125, in1=K["wneg"][:, 384 - wf:384],
                                                                           op0=ALU.mult, op1=ALU.add), reads=[ps, K["wneg"]], writes=[tn])
                        sc.op("act", lambda: nc.scalar.activation(out=PW[:, 0:wf], in_=tn[:, :wf], func=AF.Exp,
                                                                  accum_out=WS[:, h:h + 1]), reads=[tn], writes=[PW, WS], nowaw=True)
                    else:
                        wf = 0
                        sc.op("dve", lambda: nc.vector.memset(WS[:, h:h + 1], 0.0), writes=[WS], nowaw=True)
                    ps = nxt(psS, "s")
                    wn = nnear * 128
                    kn0 = (qi - (nnear - 1)) * 128
                    sc.op("pe", lambda: nc.tensor.matmul(ps[:, :wn], lhsT=qB[0:64, h, :], rhs=KT[0:64, 1, kn0:kn0 + wn], start=True, stop=True),
                          reads=[qB, KT], writes=[ps])
                    tn = nxt(TMPN, "tn")
                    sc.op("dve", lambda: nc.vector.scalar_tensor_tensor(out=tn[:, :wn], in0=ps[:, :wn], scalar=0.125, in1=K["near"][:, 4 + h, 256 - wn:256],
                                                                       op0=ALU.mult, op1=ALU.add), reads=[ps, K["near"]], writes=[tn])
                    sc.op("act", lambda: nc.scalar.activation(out=PW[:, wf:wf + wn], in_=tn[:, :wn], func=AF.Exp, accum_out=WS[:, 4 + h:5 + h]),
                          reads=[tn], writes=[PW, WS], nowaw=True)
                    pt = nxt(psT, "t")
                    for i in range(nwin):
                        sc.op("pe", lambda: nc.tensor.transpose(pt[:, i, :], PW[:, i * 128:(i + 1) * 128], K["identb"][:]),
                              reads=[PW, K["identb"]], writes=[pt], nowaw=(i > 0))
                    ptb = nxt(PT, "pt")
                    sc.op("act", lambda: nc.scalar.copy(ptb[:, 0:nwin, :], pt[:, 0:nwin, :]), reads=[pt], writes=[ptb])
                    for i in range(nwin):
                        fst = (h == 0 and i == 0)
                        sc.op("pe", lambda: nc.tensor.matmul(psOab[:, 256 + h * 64:256 + (h + 1) * 64], lhsT=ptb[:, i, :], rhs=VT[:, kt0 + i, 128:192],
                                                             start=fst, stop=(i == nwin - 1), skip_group_check=True),
                              reads=[ptb, VT], writes=[psOab], nowaw=(not fst))
                sc.op("act", lambda: nc.scalar.copy(OWIN[:], psOab[:, 256:512]), reads=[psOab], writes=[OWIN])
                sc.op("dve", lambda: nc.vector.tensor_tensor(out=WS[:, 0:4], in0=WS[:, 0:4], in1=WS[:, 4:8], op=ALU.add), reads=[WS], writes=[WS])

                if cfg.get('stop_after', 9) <= 4:
                    continue
                sc.op("dve", lambda: nc.vector.memset(SUMS[:], 0.0), writes=[SUMS])
                firstO = {"ab": True, 0: True, 1: True}
                nch_ = len(chunks)
                for ci, (k0, w) in enumerate(chunks):
                    ntile = w // 128
                    wf = min(max(Nk - 256 - k0, 0), w)
                    wn = w - wf
                    no = (k0 + wf) - (Nk - 256)
                    sc.op("dve", lambda: nc.vector.tensor_scalar(out=MAc[:, :w], in0=ISC[:, k0:k0 + w], scalar1=THR, scalar2=None, op0=ALU.is_ge),
                          reads=[ISC, ST], writes=[MAc])
                    maps = [("a", h, 0) for h in range(4)] + [("s", h, 0) for h in range(4)] + [("c", h, j) for h in range(4) for j in range(2)]
                    if "kinds" in cfg:
                        maps = [m_ for m_ in maps if m_[0] in cfg["kinds"]]
                    pend = []
                    for mi, (kind, h, j) in enumerate(maps):
                        ps = nxt(psS, "s")
                        if kind == "a":
                            lhsT, rhs, hd = qA[:, h, :], KT[0:64, 0, k0:k0 + w], h
                        elif kind == "s":
                            lhsT, rhs, hd = qB[64:128, h, :], KT[64:128, 0, k0:k0 + w], 4 + h
                        else:
                            lhsT, rhs, hd = qC[j * 64:(j + 1) * 64, h, :], CK[j * 64:(j + 1) * 64, h, k0:k0 + w], 8 + h
                        sc.op("pe", lambda: nc.tensor.matmul(ps[:, :w], lhsT=lhsT, rhs=rhs, start=True, stop=True),
                              reads=[qA, qB, qC, KT, CK], writes=[ps])
                        p = nxt(P, "p")
                        accf = SUMS[:, mi, 2 * ci:2 * ci + 1] if kind == "c" else None
                        accn = SUMS[:, mi, 2 * ci + 1:2 * ci + 2] if kind == "c" else None
                        if wf > 0 and not cfg.get("skipfar"):
                            if kind == "c":
                                sc.op("act", lambda: nc.scalar.activation(out=p[:, :wf], in_=ps[:, :wf], func=AF.Exp, scale=0.125, accum_out=accf),
                                      reads=[ps], writes=[p, SUMS], nowaw=True)
                            else:
                                sc.op("act", lambda: nc.scalar.activation(out=p[:, :wf], in_=ps[:, :wf], func=AF.Exp, scale=0.125),
                                      reads=[ps], writes=[p], nowaw=True)
                        if wn > 0 and not cfg.get("skipnear"):
                            tn = nxt(TMPN, "tn")
                            sc.op("dve", lambda: nc.vector.scalar_tensor_tensor(out=tn[:, :wn], in0=ps[:, wf:w], scalar=0.125, in1=K["near"][:, hd, no:no + wn],
                                                                               op0=ALU.mult, op1=ALU.add), reads=[ps, K["near"]], writes=[tn])
                            if kind == "c":
                                sc.op("act", lambda: nc.scalar.activation(out=p[:, wf:w], in_=tn[:, :wn], func=AF.Exp, accum_out=accn),
                                      reads=[tn], writes=[p, SUMS], nowaw=True)
                            else:
                                sc.op("act", lambda: nc.scalar.activation(out=p[:, wf:w], in_=tn[:, :wn], func=AF.Exp),
                                      reads=[tn], writes=[p], nowaw=True)
                        if kind == "a":
                            pm = nxt(PM, "pm")
                            sc.op("dve", lambda: nc.vector.scalar_tensor_tensor(out=pm[:, :w], in0=p[:, :w], scalar=1.0, in1=MAc[:, :w], op0=ALU.mult, op1=ALU.mult,
                                                                               accum_out=SUMS[:, mi, 2 * ci:2 * ci + 1]), reads=[p, MAc], writes=[pm, SUMS], nowaw=True)
                            src = pm
                        elif kind == "s":
                            pm = nxt(PM, "pm")
                            sc.op("dve", lambda: nc.vector.scalar_tensor_tensor(out=pm[:, :w].rearrange("p (j i) -> p j i", i=64),
                                                                               in0=p[:, :w].rearrange("p (j i) -> p j i", i=64), scalar=1.0,
                                                                               in1=BM[:, k0 // 64:(k0 + w) // 64].unsqueeze(2).to_broadcast([128, w // 64, 64]),
                                                                               op0=ALU.mult, op1=ALU.mult, accum_out=SUMS[:, mi, 2 * ci:2 * ci + 1]),
                                  reads=[p, BM], writes=[pm, SUMS], nowaw=True)
                            src = pm
                        else:
                            src = p
                        pend.append((kind, h, j, src))
                        if cfg.get("nopv"):
                            pend = []
                            continue
                        if len(pend) == 2 or mi == len(maps) - 1:
                            pt = nxt(psT, "t")
                            for pi_, (kd, hh, jj, sr) in enumerate(pend):
                                for i in range(ntile):
                                    sc.op("pe", lambda: nc.tensor.transpose(pt[:, pi_ * 4 + i, :], sr[:, i * 128:(i + 1) * 128], K["identb"][:]),
                                          reads=[sr, K["identb"]], writes=[pt], nowaw=not (pi_ == 0 and i == 0))
                            ptb = nxt(PT, "pt")
                            ek = "act" if cnt["ev"] % 2 == 0 else "dve"
                            cnt["ev"] += 1
                            for pi_ in range(len(pend)):
                                if ek == "act":
                                    sc.op("act", lambda: nc.scalar.copy(ptb[:, pi_ * 4:pi_ * 4 + ntile, :], pt[:, pi_ * 4:pi_ * 4 + ntile, :]), reads=[pt], writes=[ptb], nowaw=(pi_ > 0))
                                else:
                                    sc.op("dve", lambda: nc.vector.tensor_copy(ptb[:, pi_ * 4:pi_ * 4 + ntile, :], pt[:, pi_ * 4:pi_ * 4 + ntile, :]), reads=[pt], writes=[ptb], nowaw=(pi_ > 0))
                            for pi_, (kd, hh, jj, sr) in enumerate(pend):
                                for i in range(ntile):
                                    kt = k0 // 128 + i
                                    if kd == "a":
                                        o, rhsv, key = psOab[:, hh * 64:(hh + 1) * 64], VT[:, kt, 0:64], "ab"
                                        ob = psOab
                                    elif kd == "s":
                                        o, rhsv, key = psOab[:, 256 + hh * 64:256 + (hh + 1) * 64], VT[:, kt, 64:128], "ab"
                                        ob = psOab
                                    else:
                                        m = hh * 2 + jj
                                        ob = psOc[m // 4]
                                        o, rhsv, key = ob[:, (m % 4) * 128:(m % 4 + 1) * 128], VT[:, kt, 192 + hh * 128:192 + (hh + 1) * 128], m // 4
                                    fst = firstO[key]
                                    firstO[key] = False
                                    sc.op("pe", lambda: nc.tensor.matmul(o, lhsT=ptb[:, pi_ * 4 + i, :], rhs=rhsv, start=fst,
                                                                         stop=(ci == nch_ - 1 and i == ntile - 1), skip_group_check=True),
                                          reads=[ptb, VT], writes=[ob], nowaw=(not fst))
                            pend = []

                if cfg.get('stop_after', 9) <= 5:
                    continue
                sc.op("dve", lambda: nc.vector.tensor_reduce(out=TOT[:], in_=SUMS[:], axis=AX.X, op=ALU.add), reads=[SUMS], writes=[TOT])
                sc.op("dve", lambda: nc.vector.tensor_scalar(out=RCP[:], in0=TOT[:], scalar1=1e-30, scalar2=None, op0=ALU.max), reads=[TOT], writes=[RCP])
                sc.op("dve", lambda: nc.vector.reciprocal(out=RCP[:], in_=RCP[:]), reads=[RCP], writes=[RCP])
                sc.op("dve", lambda: nc.vector.reciprocal(out=WS[:, 4:8], in_=WS[:, 0:4]), reads=[WS], writes=[WS])
                for h in range(4):
                    sc.op("dve", lambda: nc.vector.tensor_scalar(out=out[:, h * 64:(h + 1) * 64], in0=psOab[:, h * 64:(h + 1) * 64], scalar1=RCP[:, h:h + 1],
                                                                scalar2=None, op0=ALU.mult), reads=[psOab, RCP], writes=[out], nowaw=(h > 0))
                for h in range(4):
                    g0, g1, g2 = (smt[:, 4 + 3 * h + k:5 + 3 * h + k] for k in range(3))
                    sc.op("dve", lambda: nc.vector.tensor_scalar(out=ST[:, 8:9], in0=RCP[:, 4 + h:5 + h], scalar1=g1, scalar2=None, op0=ALU.mult), reads=[RCP, smt], writes=[ST])
                    sc.op("dve", lambda: nc.vector.tensor_scalar(out=ST[:, 9:10], in0=WS[:, 4 + h:5 + h], scalar1=g2, scalar2=None, op0=ALU.mult), reads=[WS, smt], writes=[ST])
                    oslice = out[:, 256 + h * 64:256 + (h + 1) * 64]
                    sc.op("dve", lambda: nc.vector.tensor_scalar(out=oslice, in0=OCMP[:, h * 64:(h + 1) * 64], scalar1=g0, scalar2=None, op0=ALU.mult),
                          reads=[OCMP, smt], writes=[out], nowaw=True)
                    sc.op("dve", lambda: nc.vector.scalar_tensor_tensor(out=oslice, in0=psOab[:, 256 + h * 64:256 + (h + 1) * 64], scalar=ST[:, 8:9], in1=oslice,
                                                                       op0=ALU.mult, op1=ALU.add), reads=[psOab, ST, out], writes=[out])
                    sc.op("dve", lambda: nc.vector.scalar_tensor_tensor(out=oslice, in0=OWIN[:, h * 64:(h + 1) * 64], scalar=ST[:, 9:10], in1=oslice,
                                                                       op0=ALU.mult, op1=ALU.add), reads=[OWIN, ST, out], writes=[out])
                for h in range(4):
                    m0, m1 = 8 + h * 2, 9 + h * 2
                    ob0, ob1 = psOc[(h * 2) // 4], psOc[(h * 2 + 1) // 4]
                    o0 = ob0[:, ((h * 2) % 4) * 128:((h * 2) % 4 + 1) * 128]
                    o1 = ob1[:, ((h * 2 + 1) % 4) * 128:((h * 2 + 1) % 4 + 1) * 128]
                    sc.op("dve", lambda: nc.vector.tensor_scalar(out=ST[:, 10:11], in0=RCP[:, m1:m1 + 1], scalar1=LAM[:, 1:2], scalar2=None, op0=ALU.mult),
                          reads=[RCP, LAM], writes=[ST])
                    sc.op("dve", lambda: nc.vector.tensor_scalar(out=OCt[:], in0=o0, scalar1=RCP[:, m0:m0 + 1], scalar2=None, op0=ALU.mult),
                          reads=[ob0, RCP], writes=[OCt])
                    sc.op("dve", lambda: nc.vector.scalar_tensor_tensor(out=OCt[:], in0=o1, scalar=ST[:, 10:11], in1=OCt[:], op0=ALU.mult, op1=ALU.add),
                          reads=[ob1, ST, OCt], writes=[OCt])
                    sc.op("dve", lambda: nc.vector.scalar_tensor_tensor(out=OCj[:], in0=OCt[:], scalar=1.0, in1=OCt[:], op0=ALU.mult, op1=ALU.mult,
                                                                       accum_out=ST[:, 11:12]), reads=[OCt], writes=[OCj, ST])
                    sc.op("dve", lambda: nc.vector.tensor_scalar(out=ST[:, 12:13], in0=ST[:, 11:12], scalar1=1.0 / 128, scalar2=1e-5, op0=ALU.mult, op1=ALU.add),
                          reads=[ST], writes=[ST])
                    sc.op("act", lambda: nc.scalar.activation(out=ST[:, 13:14], in_=ST[:, 12:13], func=AF.Ln), reads=[ST], writes=[ST])
                    sc.op("act", lambda: nc.scalar.activation(out=ST[:, 14:15], in_=ST[:, 13:14], func=AF.Exp, scale=-0.5), reads=[ST], writes=[ST])
                    sc.op("dve", lambda: nc.vector.scalar_tensor_tensor(out=out[:, 512 + h * 128:512 + (h + 1) * 128], in0=OCt[:], scalar=ST[:, 14:15], in1=DNG[:],
                                                                       op0=ALU.mult, op1=ALU.mult), reads=[OCt, ST, DNG], writes=[out], nowaw=True)
                sc.dma("sp", OABC[tok0:tok0 + 128, :], out[:], reads=[out], writes=[OABC], track=out)
        allb = ([KT, CK, VT, ISC, JUNK, AW, SGN, ST, W, CMK, PCf, PCm, PCn, PCT, CS, OCMP, OWIN, IMP, RANK, BM, PW, WS, MAc, SUMS, TOT, RCP, LAM, DL, DNG,
                 OCt, OCj, TAB31, psOab] + QI + QA + QB + QC + SMt + SELt + OUT + RT + TMPN + P + PM + PT + psS + psT + psOc)
        sc.barrier_on(allb)
        sched_release(sc, allb)


def layer_norm_tile(sc, z, zc, junk, o, ST2, Gt, Bt):
    nc = sc.nc
    sc.op("dve", lambda: nc.vector.tensor_reduce(out=ST2[:, 0:1], in_=z[:], axis=AX.X, op=ALU.add), reads=[z], writes=[ST2])
    sc.op("dve", lambda: nc.vector.tensor_scalar(out=ST2[:, 1:2], in0=ST2[:, 0:1], scalar1=1.0 / D, scalar2=None, op0=ALU.mult), reads=[ST2], writes=[ST2])
    sc.op("dve", lambda: nc.vector.tensor_scalar(out=zc[:], in0=z[:], scalar1=ST2[:, 1:2], scalar2=None, op0=ALU.subtract), reads=[z, ST2], writes=[zc])
    sc.op("dve", lambda: nc.vector.scalar_tensor_tensor(out=junk[:], in0=zc[:], scalar=1.0, in1=zc[:], op0=ALU.mult, op1=ALU.mult,
                                                       accum_out=ST2[:, 2:3]), reads=[zc], writes=[junk, ST2])
    sc.op("dve", lambda: nc.vector.tensor_scalar(out=ST2[:, 3:4], in0=ST2[:, 2:3], scalar1=1.0 / D, scalar2=1e-5, op0=ALU.mult, op1=ALU.add),
          reads=[ST2], writes=[ST2])
    sc.op("act", lambda: nc.scalar.activation(out=ST2[:, 4:5], in_=ST2[:, 3:4], func=AF.Ln), reads=[ST2], writes=[ST2])
    sc.op("act", lambda: nc.scalar.activation(out=ST2[:, 5:6], in_=ST2[:, 4:5], func=AF.Exp, scale=-0.5), reads=[ST2], writes=[ST2])
    sc.op("dve", lambda: nc.vector.scalar_tensor_tensor(out=zc[:], in0=zc[:], scalar=ST2[:, 5:6], in1=Gt[:], op0=ALU.mult, op1=ALU.mult),
          reads=[zc, ST2, Gt], writes=[zc])
    sc.op("pool", lambda: nc.gpsimd.tensor_tensor(out=o[:], in0=zc[:], in1=Bt[:], op=ALU.add), reads=[zc, Bt], writes=[o])


def phase_tail(sc, cfg, l, K, OABC, MG, xsrc, wba_d, wbb_d, wbc_d, wo_d, ln_g, ln_b, G1P, X1):
    nc = sc.nc
    nseq, seq = cfg["nseq"], cfg["seq"]
    ntok = nseq * seq
    DN_ALPHA = (2 * DEPTH) ** 0.25
    with ExitStack() as es:
        wb = sc.sbuf("wbr", [128, 8, 1024], BF16, es)
        wo = sc.sbuf("wo", [128, 8, 1024], BF16, es)
        sc.dma("pool", wb[:, 0:2, :], wba_d[l].rearrange("(c p) n -> p c n", p=128), reads=[wba_d], writes=[wb], track=wb)
        sc.dma("pool", wb[:, 2:4, :], wbb_d[l].rearrange("(c p) n -> p c n", p=128), reads=[wbb_d], writes=[wb], track=wb)
        sc.dma("pool", wb[:, 4:8, :], wbc_d[l].rearrange("(c p) n -> p c n", p=128), reads=[wbc_d], writes=[wb], track=wb)
        sc.dma("pool", wo[:], wo_d[l].rearrange("(c p) n -> p c n", p=128), reads=[wo_d], writes=[wo], track=wo)
        Gt = sc.sbuf("lnG", [128, 1024], F32, es)
        Bt = sc.sbuf("lnB", [128, 1024], F32, es)
        sc.dma("sp", Gt[:], ln_g[l:l + 1, :].partition_broadcast(128), reads=[ln_g], writes=[Gt], track=Gt)
        sc.dma("sp", Bt[:], ln_b[l:l + 1, :].partition_broadcast(128), reads=[ln_b], writes=[Bt], track=Bt)
        OA = [sc.sbuf("tOA", [128, 1024], F32, es) for _ in range(2)]
        MGt = [sc.sbuf("tMG", [128, 3072], F32, es) for _ in range(2)]
        XT = [sc.sbuf("tX", [128, 1024], F32, es) for _ in range(2)]
        OB = sc.sbuf("tOB", [128, 1024], BF16, es)
        oT = sc.sbuf("toT", [128, 8, 128], BF16, es)
        M = sc.sbuf("tM", [128, 1024], F32, es)
        TMP = sc.sbuf("tTMP", [128, 1024], F32, es)
        MB = sc.sbuf("tMB", [128, 1024], BF16, es)
        mT = sc.sbuf("tmT", [128, 8, 128], BF16, es)
        Z = sc.sbuf("tZ", [128, 1024], F32, es)
        ZC = sc.sbuf("tZC", [128, 1024], F32, es)
        O = [sc.sbuf("tO", [128, 1024], F32, es) for _ in range(2)]
        ST2 = sc.sbuf("tST", [128, 8], F32, es)
        psT = [sc.psum("tpsT", [128, 8, 128], BF16, es) for _ in range(2)]
        psY = [sc.psum("tpsY", [128, 512], F32, es) for _ in range(6)]
        yi = 0
        for tt in range(ntok // 128):
            tok0 = tt * 128
            b = tok0 // seq
            oa, mg, xt, o = OA[tt % 2], MGt[tt % 2], XT[tt % 2], O[tt % 2]
            sc.dma("sp", oa[:], OABC[tok0:tok0 + 128, :], reads=[OABC], writes=[oa], track=oa, nowaw=False)
            sc.dma("sp", mg[:], MG[tok0:tok0 + 128, :], reads=[MG], writes=[mg], track=mg, nowaw=False)
            sc.dma("sp", xt[:], xsrc[tok0:tok0 + 128, :], reads=[xsrc], writes=[xt], track=xt, nowaw=False)
            sc.op("act", lambda: nc.scalar.copy(OB[:], oa[:]), reads=[oa], writes=[OB])
            pt = psT[0]
            for c in range(8):
                sc.op("pe", lambda: nc.tensor.transpose(pt[:, c, :], OB[:, c * 128:(c + 1) * 128], K["identb"][:]), reads=[OB, K["identb"]], writes=[pt], nowaw=(c > 0))
            sc.op("act", lambda: nc.scalar.copy(oT[:], pt[:]), reads=[pt], writes=[oT])
            for hf in range(2):
                cs = slice(hf * 512, (hf + 1) * 512)
                ys = []
                for (k0, k1) in ((0, 2), (2, 4), (4, 8)):
                    py = psY[yi % 6]; yi += 1
                    for kc in range(k0, k1):
                        sc.op("pe", lambda: nc.tensor.matmul(py[:, :], lhsT=oT[:, kc, :], rhs=wb[:, kc, cs], start=(kc == k0), stop=(kc == k1 - 1)),
                              reads=[oT, wb], writes=[py], nowaw=(kc > k0))
                    ys.append(py)
                sc.op("dve", lambda: nc.vector.tensor_tensor(out=M[:, cs], in0=ys[0][:, :], in1=mg[:, hf * 512:(hf + 1) * 512], op=ALU.mult),
                      reads=[ys[0], mg], writes=[M], nowaw=(hf > 0))
                sc.op("dve", lambda: nc.vector.tensor_tensor(out=TMP[:, cs], in0=ys[1][:, :], in1=mg[:, 1024 + hf * 512:1024 + (hf + 1) * 512], op=ALU.mult),
                      reads=[ys[1], mg], writes=[TMP], nowaw=(hf > 0))
                sc.op("pool", lambda: nc.gpsimd.tensor_tensor(out=M[:, cs], in0=M[:, cs], in1=TMP[:, cs], op=ALU.add), reads=[M, TMP], writes=[M])
                sc.op("dve", lambda: nc.vector.tensor_tensor(out=TMP[:, cs], in0=ys[2][:, :], in1=mg[:, 2048 + hf * 512:2048 + (hf + 1) * 512], op=ALU.mult),
                      reads=[ys[2], mg], writes=[TMP])
                sc.op("pool", lambda: nc.gpsimd.tensor_tensor(out=MB[:, cs], in0=M[:, cs], in1=TMP[:, cs], op=ALU.add), reads=[M, TMP], writes=[MB], nowaw=(hf > 0))
            pt = psT[1]
            for c in range(8):
                sc.op("pe", lambda: nc.tensor.transpose(pt[:, c, :], MB[:, c * 128:(c + 1) * 128], K["identb"][:]), reads=[MB, K["identb"]], writes=[pt], nowaw=(c > 0))
            sc.op("act", lambda: nc.scalar.copy(mT[:], pt[:]), reads=[pt], writes=[mT])
            for hf in range(2):
                cs = slice(hf * 512, (hf + 1) * 512)
                py = psY[yi % 6]; yi += 1
                for kc in range(8):
                    sc.op("pe", lambda: nc.tensor.matmul(py[:, :], lhsT=mT[:, kc, :], rhs=wo[:, kc, cs], start=(kc == 0), stop=(kc == 7)),
                          reads=[mT, wo], writes=[py], nowaw=(kc > 0))
                sc.op("dve", lambda: nc.vector.tensor_tensor(out=TMP[:, cs], in0=py[:, :], in1=G1P[:, b, cs], op=ALU.mult), reads=[py, G1P], writes=[TMP])
                sc.op("dve", lambda: nc.vector.scalar_tensor_tensor(out=Z[:, cs], in0=xt[:, cs], scalar=DN_ALPHA, in1=TMP[:, cs], op0=ALU.mult, op1=ALU.add),
                      reads=[xt, TMP], writes=[Z], nowaw=(hf > 0))
            layer_norm_tile(sc, Z, ZC, TMP, o, ST2, Gt, Bt)
            sc.dma("sp", X1[tok0:tok0 + 128, :], o[:], reads=[o], writes=[X1], track=o)
        allb = [wb, wo, Gt, Bt, OB, oT, M, TMP, MB, mT, Z, ZC, ST2] + OA + MGt + XT + O + psT + psY
        sc.barrier_on(allb)
        sched_release(sc, allb)


def phase_moe_prep(sc, cfg, l, K, X1, opsc, shf, router_w, router_b, b_down, H2T, CWD, YACC):
    nc = sc.nc
    nseq, seq = cfg["nseq"], cfg["seq"]
    ntok = nseq * seq
    with ExitStack() as es:
        rw = sc.sbuf("rw", [128, 8, 32], F32, es)
        rb = sc.sbuf("rb", [128, 32], F32, es)
        bd = sc.sbuf("bd", [32, 1024], F32, es)
        sc.dma("sp", rw[:], router_w[l].rearrange("(c p) e -> p c e", p=128), reads=[router_w], writes=[rw], track=rw)
        sc.dma("sp", rb[:], router_b[l:l + 1, :].partition_broadcast(128), reads=[router_b], writes=[rb], track=rb)
        sc.dma("sp", bd[:], b_down[l], reads=[b_down], writes=[bd], track=bd)
        XT = [sc.sbuf("mX", [128, 1024], F32, es) for _ in range(2)]
        HF = sc.sbuf("mHF", [128, 8, 128], F32, es)
        HB = [sc.sbuf("mHB", [128, 8, 128], BF16, es) for _ in range(2)]
        Lg = sc.sbuf("mL", [128, 32], F32, es)
        J3 = sc.sbuf("mJ3", [128, 32, 32], F32, es)
        RK = sc.sbuf("mRK", [128, 32], F32, es)
        EX = sc.sbuf("mEX", [128, 32], F32, es)
        CW = [sc.sbuf("mCW", [128, 32], F32, es) for _ in range(2)]
        CWT = sc.sbuf("mCWT", [32, 128], F32, es)
        YB = [sc.sbuf("mYB", [128, 1024], F32, es) for _ in range(2)]
        ST = sc.sbuf("mST", [128, 8], F32, es)
        ps = [sc.psum("mps", [128, 512], F32, es) for _ in range(6)]
        pi = 0
        for tt in range(ntok // 128):
            tok0 = tt * 128
            b = tok0 // seq
            xt, hb, cw, yb = XT[tt % 2], HB[tt % 2], CW[tt % 2], YB[tt % 2]
            sc.dma("sp", xt[:], X1[tok0:tok0 + 128, :], reads=[X1], writes=[xt], track=xt, nowaw=False)
            for g in range(2):
                p = ps[pi % 6]; pi += 1
                for c4 in range(4):
                    c = g * 4 + c4
                    sc.op("pe", lambda: nc.tensor.transpose(p[:, c4 * 128:(c4 + 1) * 128], xt[:, c * 128:(c + 1) * 128], K["ident"][:]),
                          reads=[xt, K["ident"]], writes=[p], nowaw=(c4 > 0))
                for c4 in range(4):
                    c = g * 4 + c4
                    sc.op("act", lambda: nc.scalar.activation(out=HF[:, c, :], in_=p[:, c4 * 128:(c4 + 1) * 128], func=AF.Identity,
                                                              bias=shf[:, c, b:b + 1], scale=opsc[:, c, b:b + 1]),
                          reads=[p, shf, opsc], writes=[HF], nowaw=(c > 0))
            sc.op("dve", lambda: nc.vector.tensor_copy(hb[:], HF[:]), reads=[HF], writes=[hb])
            sc.dma("sp", H2T[:, tok0:tok0 + 128].rearrange("(c p) t -> p c t", p=128), hb[:], reads=[hb], writes=[H2T], track=hb)
            p = ps[pi % 6]; pi += 1
            for c in range(8):
                sc.op("pe", lambda: nc.tensor.matmul(p[:, 0:32], lhsT=HF[:, c, :], rhs=rw[:, c, :], start=(c == 0), stop=(c == 7)),
                      reads=[HF, rw], writes=[p], nowaw=(c > 0))
            sc.op("dve", lambda: nc.vector.tensor_tensor(out=Lg[:], in0=p[:, 0:32], in1=rb[:], op=ALU.add), reads=[p, rb], writes=[Lg])
            sc.op("dve", lambda: nc.vector.tensor_tensor(out=J3[:], in0=Lg[:].unsqueeze(1).to_broadcast([128, 32, 32]),
                                                        in1=Lg[:].unsqueeze(2).to_broadcast([128, 32, 32]), op=ALU.is_gt), reads=[Lg], writes=[J3])
            sc.op("dve", lambda: nc.vector.tensor_reduce(out=RK[:], in_=J3[:], axis=AX.X, op=ALU.add), reads=[J3], writes=[RK])
            sc.op("dve", lambda: nc.vector.tensor_reduce(out=ST[:, 0:1], in_=Lg[:], axis=AX.X, op=ALU.max), reads=[Lg], writes=[ST])
            sc.op("dve", lambda: nc.vector.tensor_scalar(out=ST[:, 1:2], in0=ST[:, 0:1], scalar1=-1.0, scalar2=None, op0=ALU.mult), reads=[ST], writes=[ST])
            sc.op("act", lambda: nc.scalar.activation(out=EX[:], in_=Lg[:], func=AF.Exp, bias=ST[:, 1:2]), reads=[Lg, ST], writes=[EX])
            sc.op("dve", lambda: nc.vector.tensor_scalar(out=RK[:], in0=RK[:], scalar1=3.5, scalar2=None, op0=ALU.is_lt), reads=[RK], writes=[RK])
            sc.op("dve", lambda: nc.vector.scalar_tensor_tensor(out=EX[:], in0=EX[:], scalar=1.0, in1=RK[:], op0=ALU.mult, op1=ALU.mult,
                                                               accum_out=ST[:, 2:3]), reads=[EX, RK], writes=[EX, ST])
            sc.op("dve", lambda: nc.vector.reciprocal(out=ST[:, 3:4], in_=ST[:, 2:3]), reads=[ST], writes=[ST])
            sc.op("dve", lambda: nc.vector.tensor_scalar(out=cw[:], in0=EX[:], scalar1=ST[:, 3:4], scalar2=None, op0=ALU.mult), reads=[EX, ST], writes=[cw])
            sc.dma("sp", CWD[tok0:tok0 + 128, :], cw[:], reads=[cw], writes=[CWD], track=cw)
            p = ps[pi % 6]; pi += 1
            sc.op("pe", lambda: nc.tensor.transpose(p[:32, 0:128], cw[:, :], K["ident"][:]), reads=[cw, K["ident"]], writes=[p])
            sc.op("act", lambda: nc.scalar.copy(CWT[:], p[:32, 0:128]), reads=[p], writes=[CWT])
            for hf in range(2):
                p = ps[pi % 6]; pi += 1
                sc.op("pe", lambda: nc.tensor.matmul(p[:, :], lhsT=CWT[:, :], rhs=bd[:, hf * 512:(hf + 1) * 512], start=True, stop=True),
                      reads=[CWT, bd], writes=[p])
                sc.op("act", lambda: nc.scalar.copy(yb[:, hf * 512:(hf + 1) * 512], p[:, :]), reads=[p], writes=[yb], nowaw=(hf > 0))
            sc.dma("sp", YACC[tok0:tok0 + 128, :], yb[:], reads=[yb], writes=[YACC], track=yb)
        allb = [rw, rb, bd, HF, Lg, J3, RK, EX, CWT, ST] + XT + HB + CW + YB + ps
        sc.barrier_on(allb)
        sched_release(sc, allb)


def phase_moe_experts(sc, cfg, l, H2T, CWD, YACC, w_gu, b_guT, w_down):
    nc = sc.nc
    nseq, seq = cfg["nseq"], cfg["seq"]
    ntok = nseq * seq
    CH = min(1024, ntok)
    ntile = CH // 128
    nsb = CH // 512
    ne = cfg.get("n_exp", 32)
    with ExitStack() as es:
        hT = sc.sbuf("eh", [128, 8, CH], BF16, es)
        ACC = sc.sbuf("eACC", [128, ntile, 1024], F32, es)
        CWc = sc.sbuf("eCW", [128, ntile, 32], F32, es)
        Wgu = [sc.sbuf("eWgu", [128, 8, 2048], BF16, es) for _ in range(2)]
        Wd = [sc.sbuf("eWd", [128, 8, 1024], BF16, es) for _ in range(2)]
        BG = [sc.sbuf("eBG", [128, 16], F32, es) for _ in range(2)]
        actT = [sc.sbuf("eact", [128, 8, 512], BF16, es) for _ in range(2)]
        G = [sc.sbuf("eG", [128, 512], F32, es) for _ in range(2)]
        Sg = [sc.sbuf("eS", [128, 512], F32, es) for _ in range(2)]
        U = [sc.sbuf("eU", [128, 512], F32, es) for _ in range(2)]
        psG = [sc.psum("epsG", [128, 512], F32, es) for _ in range(5)]
        psD = [sc.psum("epsD", [128, 512], F32, es) for _ in range(3)]
        gi = di = ai = ti = 0
        for ch in range(ntok // CH):
            t0 = ch * CH
            sc.dma("sp", hT[:], H2T[:, t0:t0 + CH].rearrange("(c p) t -> p c t", p=128), reads=[H2T], writes=[hT], track=hT, nowaw=False)
            sc.dma("sp", ACC[:], YACC[t0:t0 + CH, :].rearrange("(n p) d -> p n d", p=128), reads=[YACC], writes=[ACC], track=ACC, nowaw=False)
            sc.dma("sp", CWc[:], CWD[t0:t0 + CH, :].rearrange("(n p) e -> p n e", p=128), reads=[CWD], writes=[CWc], track=CWc, nowaw=False)
            for e in range(ne):
                k = (ch * ne + e) % 2
                wg, wd, bg = Wgu[k], Wd[k], BG[k]
                for c2 in range(0, 8, 2):
                    sc.dma("pool", wg[:, c2:c2 + 2, :], w_gu[l, e, c2 * 128:(c2 + 2) * 128, :].rearrange("(c p) n -> p c n", p=128),
                           reads=[w_gu], writes=[wg], track=wg, nowaw=(c2 > 0))
                for c4 in range(0, 8, 4):
                    sc.dma("pool", wd[:, c4:c4 + 4, :], w_down[l, e, c4 * 128:(c4 + 4) * 128, :].rearrange("(c p) n -> p c n", p=128),
                           reads=[w_down], writes=[wd], track=wd, nowaw=(c4 > 0))
                sc.dma("sp", bg[:], b_guT[l, e], reads=[b_guT], writes=[bg], track=bg, nowaw=False)
                for sb in range(nsb):
                    at = actT[ai % 2]; ai += 1
                    for fc in range(8):
                        pg = psG[gi % 5]; gi += 1
                        pu = psG[gi % 5]; gi += 1
                        for which, pp in ((0, pg), (1, pu)):
                            for kc in range(8):
                                lhsT = wg[:, kc, :].rearrange("p (f two) -> p two f", two=2)[:, which, fc * 128:(fc + 1) * 128]
                                sc.op("pe", lambda: nc.tensor.matmul(pp[:, :], lhsT=lhsT, rhs=hT[:, kc, sb * 512:(sb + 1) * 512],
                                                                     start=(kc == 0), stop=(kc == 7)), reads=[wg, hT], writes=[pp], nowaw=(kc > 0))
                        g_, s_, u_ = G[ti % 2], Sg[ti % 2], U[ti % 2]
                        ti += 1
                        sc.op("dve", lambda: nc.vector.tensor_scalar(out=g_[:], in0=pg[:, :], scalar1=bg[:, fc:fc + 1], scalar2=7.0, op0=ALU.add, op1=ALU.min),
                              reads=[pg, bg], writes=[g_])
                        sc.op("act", lambda: nc.scalar.activation(out=s_[:], in_=g_[:], func=AF.Sigmoid, scale=1.702), reads=[g_], writes=[s_])
                        sc.op("dve", lambda: nc.vector.tensor_scalar(out=u_[:], in0=pu[:, :], scalar1=bg[:, 8 + fc:9 + fc], scalar2=7.0, op0=ALU.add, op1=ALU.min),
                              reads=[pu, bg], writes=[u_])
                        sc.op("pool", lambda: nc.gpsimd.tensor_scalar(out=u_[:], in0=u_[:], scalar1=-7.0, scalar2=1.0, op0=ALU.max, op1=ALU.add),
                              reads=[u_], writes=[u_])
                        sc.op("pool", lambda: nc.gpsimd.tensor_tensor(out=g_[:], in0=g_[:], in1=s_[:], op=ALU.mult), reads=[g_, s_], writes=[g_])
                        sc.op("pool", lambda: nc.gpsimd.tensor_tensor(out=at[:, fc, :], in0=g_[:], in1=u_[:], op=ALU.mult), reads=[g_, u_], writes=[at], nowaw=(fc > 0))
                    for tl in range(4):
                        tix = sb * 4 + tl
                        for hf in range(2):
                            pd = psD[di % 3]; di += 1
                            for fc in range(8):
                                sc.op("pe", lambda: nc.tensor.matmul(pd[:, :], lhsT=at[:, fc, tl * 128:(tl + 1) * 128], rhs=wd[:, fc, hf * 512:(hf + 1) * 512],
                                                                     start=(fc == 0), stop=(fc == 7)), reads=[at, wd], writes=[pd], nowaw=(fc > 0))
                            sc.op("dve", lambda: nc.vector.scalar_tensor_tensor(out=ACC[:, tix, hf * 512:(hf + 1) * 512], in0=pd[:, :], scalar=CWc[:, tix, e:e + 1],
                                                                               in1=ACC[:, tix, hf * 512:(hf + 1) * 512], op0=ALU.mult, op1=ALU.add),
                                  reads=[pd, CWc, ACC], writes=[ACC])
            sc.dma("sp", YACC[t0:t0 + CH, :].rearrange("(n p) d -> p n d", p=128), ACC[:], reads=[ACC], writes=[YACC], track=ACC)
        allb = [hT, ACC, CWc] + Wgu + Wd + BG + actT + G + Sg + U + psG + psD
        sc.barrier_on(allb)
        sched_release(sc, allb)


def phase_moe_final(sc, cfg, l, X1, YACC, G1P, ln_g, ln_b, XOUT):
    nc = sc.nc
    nseq, seq = cfg["nseq"], cfg["seq"]
    ntok = nseq * seq
    DN_ALPHA = (2 * DEPTH) ** 0.25
    with ExitStack() as es:
        Gt = sc.sbuf("fG", [128, 1024], F32, es)
        Bt = sc.sbuf("fB", [128, 1024], F32, es)
        sc.dma("sp", Gt[:], ln_g[l:l + 1, :].partition_broadcast(128), reads=[ln_g], writes=[Gt], track=Gt)
        sc.dma("sp", Bt[:], ln_b[l:l + 1, :].partition_broadcast(128), reads=[ln_b], writes=[Bt], track=Bt)
        XT = [sc.sbuf("fX", [128, 1024], F32, es) for _ in range(2)]
        YT = [sc.sbuf("fY", [128, 1024], F32, es) for _ in range(2)]
        Z = sc.sbuf("fZ", [128, 1024], F32, es)
        ZC = sc.sbuf("fZC", [128, 1024], F32, es)
        TMP = sc.sbuf("fT", [128, 1024], F32, es)
        O = [sc.sbuf("fO", [128, 1024], F32, es) for _ in range(2)]
        ST2 = sc.sbuf("fST", [128, 8], F32, es)
        for tt in range(ntok // 128):
            tok0 = tt * 128
            b = tok0 // seq
            xt, yt, o = XT[tt % 2], YT[tt % 2], O[tt % 2]
            sc.dma("sp", xt[:], X1[tok0:tok0 + 128, :], reads=[X1], writes=[xt], track=xt, nowaw=False)
            sc.dma("sp", yt[:], YACC[tok0:tok0 + 128, :], reads=[YACC], writes=[yt], track=yt, nowaw=False)
            sc.op("pool", lambda: nc.gpsimd.tensor_tensor(out=TMP[:], in0=yt[:], in1=G1P[:, b, :], op=ALU.mult), reads=[yt, G1P], writes=[TMP])
            sc.op("dve", lambda: nc.vector.scalar_tensor_tensor(out=Z[:], in0=xt[:], scalar=DN_ALPHA, in1=TMP[:], op0=ALU.mult, op1=ALU.add),
                  reads=[xt, TMP], writes=[Z])
            layer_norm_tile(sc, Z, ZC, TMP, o, ST2, Gt, Bt)
            sc.dma("sp", XOUT[tok0:tok0 + 128, :], o[:], reads=[o], writes=[XOUT], track=o)
        allb = [Gt, Bt, Z, ZC, TMP, ST2] + XT + YT + O
        sc.barrier_on(allb)
        sched_release(sc, allb)


W_SPECS = [("rel_bias", [32, 12]), ("mod_attn_w", [DEPTH, D, 3 * D]), ("mod_attn_b", [DEPTH, 3 * D]), ("w_in", [DEPTH, D, D_IN]),
           ("cmp_w1", [DEPTH, 2, 2048, 256]), ("cmp_w2", [DEPTH, 2, 256, 64]), ("diff_lambda", [DEPTH, 4, 64]),
           ("diff_norm_g", [DEPTH, 128]), ("w_branch_a", [DEPTH, 256, D]), ("w_branch_b", [DEPTH, 256, D]),
           ("w_branch_c", [DEPTH, 512, D]), ("w_out", [DEPTH, D, D]), ("ln1_g", [DEPTH, D]), ("ln1_b", [DEPTH, D]),
           ("mod_ffn_w", [DEPTH, D, 3 * D]), ("mod_ffn_b", [DEPTH, 3 * D]), ("router_w", [DEPTH, D, 32]), ("router_b", [DEPTH, 32]),
           ("exp_w_gu", [DEPTH, 32, D, 2 * D]), ("exp_w_down", [DEPTH, 32, D, D]), ("exp_b_down", [DEPTH, 32, D]),
           ("ln2_g", [DEPTH, D]), ("ln2_b", [DEPTH, D]),
           ("modbT", [DEPTH, 2, 128, 24]), ("cposT", [DEPTH, 128, 32]), ("b_guT", [DEPTH, 32, 128, 16])]


def build_program(cfg):
    nc = bass.Bass("TRN2", target_bir_lowering=False)
    nseq, seq = cfg["nseq"], cfg["seq"]
    ntok = nseq * seq
    depth = cfg.get("depth", DEPTH)
    with ExitStack() as es:
        sc = Sched(nc, es)
        x = sc.dram("x", [ntok, D], F32, kind="ExternalInput")
        cT = sc.dram("cT", [128, 8, nseq], F32, kind="ExternalInput")
        Wd_ = {n: sc.dram(n, s, F32, kind="ExternalInput") for n, s in W_SPECS}
        cd = {n: sc.dram(n, s, d, kind="ExternalInput") for n, s, d in CONST_SPECS}
        y = sc.dram("y", [ntok, D], F32, kind="ExternalOutput")
        FM = sc.dram("FM", [FM_ROWS, ntok], BF16)
        VV = sc.dram("VV", [ntok, VV_W], BF16)
        SM = sc.dram("SM", [ntok, 16], F32)
        MG = sc.dram("MG", [ntok, 3072], F32)
        OABC = sc.dram("OABC", [ntok, 1024], F32)
        X1 = sc.dram("X1", [ntok, D], F32)
        XL = sc.dram("XL", [ntok, D], F32)
        H2T = sc.dram("H2T", [D, ntok], BF16)
        CWD = sc.dram("CWD", [ntok, 32], F32)
        YACC = sc.dram("YACC", [ntok, D], F32)
        Cap = moe_capacity(ntok)
        Xg = sc.dram("Xg", [32 * Cap + 128, D], BF16)
        Yg = sc.dram("Yg", [32 * Cap + 128, D], F32)
        DSTD = sc.dram("DSTD", [ntok, 4], I32)
        CWKD = sc.dram("CWKD", [ntok, 4], F32)
        K = setup_attn_consts(sc, cd, Wd_["rel_bias"], es)
        siluT = sc.sbuf("siluT", [128, 8, nseq], F32)
        sc.dma("sp", siluT[:], cT[:, :, :], reads=[cT], writes=[siluT], track=siluT)
        sc.op("act", lambda: nc.scalar.activation(out=siluT[:], in_=siluT[:], func=AF.Silu), reads=[siluT], writes=[siluT])
        opsc = sc.sbuf("opsc", [128, 8, nseq], F32)
        shf = sc.sbuf("shf", [128, 8, nseq], F32)
        G1P = sc.sbuf("G1P", [128, nseq, 1024], F32)
        KC = sc.sbuf("KC", [64, nseq, 256], BF16)
        VC = sc.sbuf("VC", [128, nseq, 2, 64], BF16)
        xin = x
        for l in range(depth):
            lam_init = 0.8 - 0.6 * math.exp(-0.3 * l)
            xout = y if l == depth - 1 else XL
            phase_adaln(sc, cfg, l, Wd_["mod_attn_w"], Wd_["modbT"], Wd_["mod_attn_b"], 0, siluT, opsc, shf, None, "fm")
            phase_proj(sc, cfg, l, xin, Wd_["w_in"], K["ident"], opsc, shf, FM, VV, SM, MG)
            phase_compress(sc, cfg, l, FM, Wd_["cmp_w1"], Wd_["cmp_w2"], Wd_["cposT"], KC, VC)
            phase_attn(sc, cfg, l, K, FM, VV, SM, KC, VC, cd["c_sel"], Wd_["diff_lambda"], Wd_["diff_norm_g"], lam_init, OABC)
            phase_adaln(sc, cfg, l, Wd_["mod_attn_w"], Wd_["modbT"], Wd_["mod_attn_b"], 0, siluT, None, None, G1P, "gate")
            phase_tail(sc, cfg, l, K, OABC, MG, xin, Wd_["w_branch_a"], Wd_["w_branch_b"], Wd_["w_branch_c"], Wd_["w_out"],
                       Wd_["ln1_g"], Wd_["ln1_b"], G1P, X1)
            phase_adaln(sc, cfg, l, Wd_["mod_ffn_w"], Wd_["modbT"], Wd_["mod_ffn_b"], 1, siluT, opsc, shf, None, "fm")
            if cfg.get("dense_moe"):
                phase_moe_prep(sc, cfg, l, K, X1, opsc, shf, Wd_["router_w"], Wd_["router_b"], Wd_["exp_b_down"], H2T, CWD, YACC)
                phase_moe_experts(sc, cfg, l, H2T, CWD, YACC, Wd_["exp_w_gu"], Wd_["b_guT"], Wd_["exp_w_down"])
                phase_adaln(sc, cfg, l, Wd_["mod_ffn_w"], Wd_["modbT"], Wd_["mod_ffn_b"], 1, siluT, None, None, G1P, "gate")
                phase_moe_final(sc, cfg, l, X1, YACC, G1P, Wd_["ln2_g"], Wd_["ln2_b"], xout)
            else:
                with ExitStack() as mes:
                    OPTM = sc.sbuf("OPTM", [128, nseq, 1024], F32, mes)
                    SHTM = sc.sbuf("SHTM", [128, nseq, 1024], F32, mes)
                    phase_adaln(sc, cfg, l, Wd_["mod_ffn_w"], Wd_["modbT"], Wd_["mod_ffn_b"], 1, siluT, None, None, OPTM, "gate", tm_third=1, add_one=1.0)
                    phase_adaln(sc, cfg, l, Wd_["mod_ffn_w"], Wd_["modbT"], Wd_["mod_ffn_b"], 1, siluT, None, None, SHTM, "gate", tm_third=0, add_one=0.0)
                    phase_moe_prep2(sc, cfg, l, K, X1, opsc, shf, OPTM, SHTM, Wd_["router_w"], Wd_["router_b"], Wd_["exp_b_down"],
                                    Xg, DSTD, CWKD, YACC, cd)
                    sc.barrier_on([OPTM, SHTM])
                phase_moe_experts2(sc, cfg, l, K, Xg, Yg, Wd_["exp_w_gu"], Wd_["b_guT"], Wd_["exp_w_down"])
                phase_adaln(sc, cfg, l, Wd_["mod_ffn_w"], Wd_["modbT"], Wd_["mod_ffn_b"], 1, siluT, None, None, G1P, "gate")
                phase_moe_final2(sc, cfg, l, X1, YACC, Yg, DSTD, CWKD, G1P, Wd_["ln2_g"], Wd_["ln2_b"], xout)
            xin = xout
        sc.finish([y])
        cfg["_n_inst"] = sc.n_inst
    return nc


def host_layouts(inp):
    L = DEPTH
    out = {}
    out["modbT"] = np.ascontiguousarray(np.stack([np.stack([inp["mod_attn_b"][l].reshape(24, 128).T,
                                                            inp["mod_ffn_b"][l].reshape(24, 128).T]) for l in range(L)]), dtype=np.float32)
    out["cposT"] = np.ascontiguousarray(np.stack([np.concatenate([inp["cmp_pos"][l, 0].T, inp["cmp_pos"][l, 1].T], 0) for l in range(L)]), dtype=np.float32)
    bg = inp["exp_b_gu"].reshape(L, 32, 8, 128, 2)
    out["b_guT"] = np.ascontiguousarray(bg.transpose(0, 1, 3, 4, 2).reshape(L, 32, 128, 16), dtype=np.float32)
    return out


def run_module(inp, cfg, n_cores):
    nseq, seq = cfg["nseq"], cfg["seq"]
    nc = build_program(cfg)
    shared = {n: np.ascontiguousarray(inp[n], dtype=np.float32) for n, _ in W_SPECS if n in inp}
    shared.update(host_layouts(inp))
    shared.update(make_consts())
    x = np.asarray(inp["x"], dtype=np.float32)
    c = np.asarray(inp["c"], dtype=np.float32)
    in_maps = []
    for core in range(n_cores):
        xs = x[core * nseq:(core + 1) * nseq].reshape(nseq * seq, D)
        cs = c[core * nseq:(core + 1) * nseq]
        cT = np.ascontiguousarray(cs.reshape(nseq, 8, 128).transpose(2, 1, 0))
        m = dict(shared)
        m["x"] = np.ascontiguousarray(xs)
        m["cT"] = cT
        in_maps.append(m)
    res = run_bass_kernel_spmd(nc, in_maps, core_ids=list(range(n_cores)))
    outs = [r["y"].reshape(nseq, seq, D) for r in res.results]
    return np.concatenate(outs, axis=0).astype(np.float32)


def kernel(**inputs):
    cfg = dict(nseq=NSEQ, seq=S)
    return run_module(inputs, cfg, 8)


def moe_capacity(ntok):
    c = (15 * ntok) // 64
    return ((c + 127) // 128) * 128


def phase_moe_prep2(sc, cfg, l, K, X1, opsc, shf, OPTM, SHTM, router_w, router_b, b_down, Xg, DSTD, CWKD, YACC, cd):
    nc = sc.nc
    nseq, seq = cfg["nseq"], cfg["seq"]
    ntok = nseq * seq
    C = moe_capacity(ntok)
    with ExitStack() as es:
        rw = sc.sbuf("rw", [128, 8, 32], F32, es)
        rb = sc.sbuf("rb", [128, 32], F32, es)
        bd = sc.sbuf("bd", [32, 1024], F32, es)
        UT = sc.sbuf("UT", [128, 128], BF16, es)
        ONES = sc.sbuf("ONES", [128, 128], BF16, es)
        EOFF = sc.sbuf("EOFF", [128, 32], F32, es)
        BASE = sc.sbuf("BASE", [128, 32], F32, es)
        sc.dma("sp", rw[:], router_w[l].rearrange("(c p) e -> p c e", p=128), reads=[router_w], writes=[rw], track=rw)
        sc.dma("sp", rb[:], router_b[l:l + 1, :].partition_broadcast(128), reads=[router_b], writes=[rb], track=rb)
        sc.dma("sp", bd[:], b_down[l], reads=[b_down], writes=[bd], track=bd)
        sc.dma("pool", UT[:], cd["c_ut"][:, :], reads=[cd["c_ut"]], writes=[UT], track=UT)
        sc.op("dve", lambda: nc.vector.memset(ONES[:], 1.0), writes=[ONES])
        sc.op("dve", lambda: nc.vector.memset(BASE[:], 0.0), writes=[BASE])
        sc.op("dve", lambda: nc.vector.tensor_scalar(out=EOFF[:], in0=K["iota"][:, 0:32], scalar1=float(C), scalar2=-1.0, op0=ALU.mult, op1=ALU.add),
              reads=[K["iota"]], writes=[EOFF])
        XT = [sc.sbuf("mX", [128, 1024], F32, es) for _ in range(2)]
        HF = sc.sbuf("mHF", [128, 8, 128], F32, es)
        HT = sc.sbuf("mHT", [128, 1024], F32, es)
        HBt = [sc.sbuf("mHBt", [128, 1024], BF16, es) for _ in range(2)]
        Lg = sc.sbuf("mL", [128, 32], F32, es)
        J3 = sc.sbuf("mJ3", [128, 32, 32], F32, es)
        RK = sc.sbuf("mRK", [128, 32], F32, es)
        Mk = sc.sbuf("mMk", [128, 32], F32, es)
        Mb = sc.sbuf("mMb", [128, 32], BF16, es)
        EX = sc.sbuf("mEX", [128, 32], F32, es)
        CW = sc.sbuf("mCW", [128, 32], F32, es)
        ROW = sc.sbuf("mROW", [128, 32], F32, es)
        POS = sc.sbuf("mPOS", [128, 32], F32, es)
        JK = sc.sbuf("mJK", [128, 32], F32, es)
        DSTf = sc.sbuf("mDSTf", [128, 4], F32, es)
        DSTi = [sc.sbuf("mDSTi", [128, 4], I32, es) for _ in range(2)]
        CWK = [sc.sbuf("mCWK", [128, 4], F32, es) for _ in range(2)]
        CWT = sc.sbuf("mCWT", [32, 128], F32, es)
        YB = [sc.sbuf("mYB", [128, 1024], F32, es) for _ in range(2)]
        ST = sc.sbuf("mST", [128, 8], F32, es)
        ps = [sc.psum("mps", [128, 512], F32, es) for _ in range(6)]
        pi = 0
        for tt in range(ntok // 128):
            tok0 = tt * 128
            b = tok0 // seq
            xt, hbt, dsti, cwk, yb = XT[tt % 2], HBt[tt % 2], DSTi[tt % 2], CWK[tt % 2], YB[tt % 2]
            sc.dma("sp", xt[:], X1[tok0:tok0 + 128, :], reads=[X1], writes=[xt], track=xt, nowaw=False)
            sc.op("pool", lambda: nc.gpsimd.tensor_tensor(out=HT[:], in0=xt[:], in1=OPTM[:, b, :], op=ALU.mult), reads=[xt, OPTM], writes=[HT])
            sc.op("pool", lambda: nc.gpsimd.tensor_tensor(out=hbt[:], in0=HT[:], in1=SHTM[:, b, :], op=ALU.add), reads=[HT, SHTM], writes=[hbt])
            for g in range(2):
                p = ps[pi % 6]; pi += 1
                for c4 in range(4):
                    c = g * 4 + c4
                    sc.op("pe", lambda: nc.tensor.transpose(p[:, c4 * 128:(c4 + 1) * 128], xt[:, c * 128:(c + 1) * 128], K["ident"][:]),
                          reads=[xt, K["ident"]], writes=[p], nowaw=(c4 > 0))
                for c4 in range(4):
                    c = g * 4 + c4
                    sc.op("act", lambda: nc.scalar.activation(out=HF[:, c, :], in_=p[:, c4 * 128:(c4 + 1) * 128], func=AF.Identity,
                                                              bias=shf[:, c, b:b + 1], scale=opsc[:, c, b:b + 1]),
                          reads=[p, shf, opsc], writes=[HF], nowaw=(c > 0))
            p = ps[pi % 6]; pi += 1
            for c in range(8):
                sc.op("pe", lambda: nc.tensor.matmul(p[:, 0:32], lhsT=HF[:, c, :], rhs=rw[:, c, :], start=(c == 0), stop=(c == 7)),
                      reads=[HF, rw], writes=[p], nowaw=(c > 0))
            sc.op("dve", lambda: nc.vector.tensor_tensor(out=Lg[:], in0=p[:, 0:32], in1=rb[:], op=ALU.add), reads=[p, rb], writes=[Lg])
            sc.op("dve", lambda: nc.vector.tensor_tensor(out=J3[:], in0=Lg[:].unsqueeze(1).to_broadcast([128, 32, 32]),
                                                        in1=Lg[:].unsqueeze(2).to_broadcast([128, 32, 32]), op=ALU.is_gt), reads=[Lg], writes=[J3])
            sc.op("dve", lambda: nc.vector.tensor_reduce(out=RK[:], in_=J3[:], axis=AX.X, op=ALU.add), reads=[J3], writes=[RK])
            sc.op("dve", lambda: nc.vector.tensor_reduce(out=ST[:, 0:1], in_=Lg[:], axis=AX.X, op=ALU.max), reads=[Lg], writes=[ST])
            sc.op("dve", lambda: nc.vector.tensor_scalar(out=ST[:, 1:2], in0=ST[:, 0:1], scalar1=-1.0, scalar2=None, op0=ALU.mult), reads=[ST], writes=[ST])
            sc.op("act", lambda: nc.scalar.activation(out=EX[:], in_=Lg[:], func=AF.Exp, bias=ST[:, 1:2]), reads=[Lg, ST], writes=[EX])
            sc.op("dve", lambda: nc.vector.tensor_scalar(out=Mk[:], in0=RK[:], scalar1=3.5, scalar2=None, op0=ALU.is_lt), reads=[RK], writes=[Mk])
            sc.op("dve", lambda: nc.vector.scalar_tensor_tensor(out=EX[:], in0=EX[:], scalar=1.0, in1=Mk[:], op0=ALU.mult, op1=ALU.mult,
                                                               accum_out=ST[:, 2:3]), reads=[EX, Mk], writes=[EX, ST])
            sc.op("dve", lambda: nc.vector.reciprocal(out=ST[:, 3:4], in_=ST[:, 2:3]), reads=[ST], writes=[ST])
            sc.op("dve", lambda: nc.vector.tensor_scalar(out=CW[:], in0=EX[:], scalar1=ST[:, 3:4], scalar2=None, op0=ALU.mult), reads=[EX, ST], writes=[CW])
            sc.op("dve", lambda: nc.vector.tensor_copy(Mb[:], Mk[:]), reads=[Mk], writes=[Mb])
            pc = ps[pi % 6]; pi += 1
            sc.op("pe", lambda: nc.tensor.matmul(pc[:, 0:32], lhsT=UT[:], rhs=Mb[:], start=True, stop=True), reads=[UT, Mb], writes=[pc])
            ptot = ps[pi % 6]; pi += 1
            sc.op("pe", lambda: nc.tensor.matmul(ptot[:, 0:32], lhsT=ONES[:], rhs=Mb[:], start=True, stop=True), reads=[ONES, Mb], writes=[ptot])
            sc.op("dve", lambda: nc.vector.tensor_tensor(out=POS[:], in0=pc[:, 0:32], in1=BASE[:], op=ALU.add), reads=[pc, BASE], writes=[POS])
            sc.op("dve", lambda: nc.vector.tensor_tensor(out=ROW[:], in0=POS[:], in1=EOFF[:], op=ALU.add), reads=[POS, EOFF], writes=[ROW])
            sc.op("dve", lambda: nc.vector.tensor_scalar(out=POS[:], in0=POS[:], scalar1=float(C) + 0.5, scalar2=1e9, op0=ALU.is_gt, op1=ALU.mult),
                  reads=[POS], writes=[POS])
            sc.op("dve", lambda: nc.vector.tensor_tensor(out=ROW[:], in0=ROW[:], in1=POS[:], op=ALU.add), reads=[ROW, POS], writes=[ROW])
            sc.op("dve", lambda: nc.vector.tensor_scalar(out=ROW[:], in0=ROW[:], scalar1=float(32 * C), scalar2=None, op0=ALU.min), reads=[ROW], writes=[ROW])
            sc.op("dve", lambda: nc.vector.tensor_tensor(out=BASE[:], in0=BASE[:], in1=ptot[:, 0:32], op=ALU.add), reads=[BASE, ptot], writes=[BASE])
            for k in range(4):
                sc.op("dve", lambda: nc.vector.scalar_tensor_tensor(out=JK[:], in0=RK[:], scalar=float(k), in1=ROW[:], op0=ALU.is_equal, op1=ALU.mult,
                                                                   accum_out=DSTf[:, k:k + 1]), reads=[RK, ROW], writes=[JK, DSTf])
                sc.op("dve", lambda: nc.vector.scalar_tensor_tensor(out=JK[:], in0=RK[:], scalar=float(k), in1=CW[:], op0=ALU.is_equal, op1=ALU.mult,
                                                                   accum_out=cwk[:, k:k + 1]), reads=[RK, CW], writes=[JK, cwk])
            sc.op("dve", lambda: nc.vector.tensor_copy(dsti[:], DSTf[:]), reads=[DSTf], writes=[dsti])
            sc.dma("sp", DSTD[tok0:tok0 + 128, :], dsti[:], reads=[dsti], writes=[DSTD], track=dsti)
            sc.dma("sp", CWKD[tok0:tok0 + 128, :], cwk[:], reads=[cwk], writes=[CWKD], track=cwk)
            for k in range(4):
                deps_r, deps_w = [hbt, dsti], [Xg]
                d = sc._collect(deps_r, deps_w, True)
                sc._wait("pool", d)
                if hbt.dsem is None:
                    if sc.sempool:
                        hbt.dsem, hbt.dcount = sc.sempool.pop()
                    else:
                        hbt.dsem = sc.es.enter_context(nc.semaphore("d_" + hbt.name))
                ins = nc.gpsimd.indirect_dma_start(out=Xg[:, :], out_offset=bass.IndirectOffsetOnAxis(ap=dsti[:, k:k + 1], axis=0),
                                                   in_=hbt[:, :], in_offset=None)
                ins.then_inc(hbt.dsem, 16)
                hbt.dcount += 1
                for bb in deps_r:
                    bb.rd[hbt] = hbt.dcount
                Xg.wr[hbt] = hbt.dcount
            p = ps[pi % 6]; pi += 1
            sc.op("pe", lambda: nc.tensor.transpose(p[:32, 0:128], CW[:, :], K["ident"][:]), reads=[CW, K["ident"]], writes=[p])
            sc.op("act", lambda: nc.scalar.copy(CWT[:], p[:32, 0:128]), reads=[p], writes=[CWT])
            for hf in range(2):
                p = ps[pi % 6]; pi += 1
                sc.op("pe", lambda: nc.tensor.matmul(p[:, :], lhsT=CWT[:, :], rhs=bd[:, hf * 512:(hf + 1) * 512], start=True, stop=True),
                      reads=[CWT, bd], writes=[p])
                sc.op("act", lambda: nc.scalar.copy(yb[:, hf * 512:(hf + 1) * 512], p[:, :]), reads=[p], writes=[yb], nowaw=(hf > 0))
            sc.dma("sp", YACC[tok0:tok0 + 128, :], yb[:], reads=[yb], writes=[YACC], track=yb)
        allb = [rw, rb, bd, UT, ONES, EOFF, BASE, HF, HT, Lg, J3, RK, Mk, Mb, EX, CW, ROW, POS, JK, DSTf, CWT, ST] + XT + HBt + DSTi + CWK + YB + ps
        sc.barrier_on(allb)
        sched_release(sc, allb)


def phase_moe_experts2(sc, cfg, l, K, Xg, Yg, w_gu, b_guT, w_down):
    nc = sc.nc
    nseq, seq = cfg["nseq"], cfg["seq"]
    ntok = nseq * seq
    C = moe_capacity(ntok)
    sbs = [(o, min(4, C // 128 - o)) for o in range(0, C // 128, 4)]
    ne = cfg.get("n_exp", 32)
    with ExitStack() as es:
        Wgu = [sc.sbuf("eWgu", [128, 8, 2048], BF16, es) for _ in range(2)]
        Wd = [sc.sbuf("eWd", [128, 8, 1024], BF16, es) for _ in range(2)]
        BG = [sc.sbuf("eBG", [128, 16], F32, es) for _ in range(2)]
        XR = [sc.sbuf("eXR", [128, 4, 1024], BF16, es) for _ in range(2)]
        XT = [sc.sbuf("eXT", [128, 8, 512], BF16, es) for _ in range(2)]
        actT = [sc.sbuf("eact", [128, 8, 512], BF16, es) for _ in range(2)]
        G = [sc.sbuf("eG", [128, 512], F32, es) for _ in range(2)]
        Sg = [sc.sbuf("eS", [128, 512], F32, es) for _ in range(2)]
        U = [sc.sbuf("eU", [128, 512], F32, es) for _ in range(2)]
        YO = [sc.sbuf("eYO", [128, 1024], F32, es) for _ in range(3)]
        psG = [sc.psum("epsG", [128, 512], F32, es) for _ in range(4)]
        psD = [sc.psum("epsD", [128, 512], F32, es) for _ in range(2)]
        psT = [sc.psum("epsT", [128, 8, 128], BF16, es) for _ in range(2)]
        gi = di = ai = ti = xi = yi = pti = 0
        for e in range(ne):
            wg, wd, bg = Wgu[e % 2], Wd[e % 2], BG[e % 2]
            for c2 in range(0, 8, 2):
                sc.dma("pool", wg[:, c2:c2 + 2, :], w_gu[l, e, c2 * 128:(c2 + 2) * 128, :].rearrange("(c p) n -> p c n", p=128),
                       reads=[w_gu], writes=[wg], track=wg, nowaw=(c2 > 0))
            for c4 in range(0, 8, 4):
                sc.dma("pool", wd[:, c4:c4 + 4, :], w_down[l, e, c4 * 128:(c4 + 4) * 128, :].rearrange("(c p) n -> p c n", p=128),
                       reads=[w_down], writes=[wd], track=wd, nowaw=(c4 > 0))
            sc.dma("sp", bg[:], b_guT[l, e], reads=[b_guT], writes=[bg], track=bg, nowaw=False)
            for (tl0, ntl) in sbs:
                r0 = e * C + tl0 * 128
                wdt = ntl * 128
                xr, xt_, at = XR[xi % 2], XT[xi % 2], actT[xi % 2]
                xi += 1
                sc.dma("sp", xr[:, 0:ntl, :], Xg[r0:r0 + wdt, :].rearrange("(n p) d -> p n d", p=128), reads=[Xg], writes=[xr], track=xr, nowaw=False)
                for tl in range(ntl):
                    pt = psT[pti % 2]; pti += 1
                    for c in range(8):
                        sc.op("pe", lambda: nc.tensor.transpose(pt[:, c, :], xr[:, tl, c * 128:(c + 1) * 128], K["identb"][:]),
                              reads=[xr, K["identb"]], writes=[pt], nowaw=(c > 0))
                    if tl % 2 == 0:
                        sc.op("act", lambda: nc.scalar.copy(xt_[:, :, tl * 128:(tl + 1) * 128], pt[:]), reads=[pt], writes=[xt_], nowaw=(tl > 0))
                    else:
                        sc.op("dve", lambda: nc.vector.tensor_copy(xt_[:, :, tl * 128:(tl + 1) * 128], pt[:]), reads=[pt], writes=[xt_], nowaw=True)
                for fc in range(8):
                    pg = psG[gi % 4]; gi += 1
                    pu = psG[gi % 4]; gi += 1
                    for which, pp in ((0, pg), (1, pu)):
                        for kc in range(8):
                            lhsT = wg[:, kc, :].rearrange("p (f two) -> p two f", two=2)[:, which, fc * 128:(fc + 1) * 128]
                            sc.op("pe", lambda: nc.tensor.matmul(pp[:, :wdt], lhsT=lhsT, rhs=xt_[:, kc, :wdt], start=(kc == 0), stop=(kc == 7)),
                                  reads=[wg, xt_], writes=[pp], nowaw=(kc > 0))
                    g_, s_, u_ = G[ti % 2], Sg[ti % 2], U[ti % 2]
                    ti += 1
                    sc.op("dve", lambda: nc.vector.tensor_scalar(out=g_[:, :wdt], in0=pg[:, :wdt], scalar1=bg[:, fc:fc + 1], scalar2=7.0, op0=ALU.add, op1=ALU.min),
                          reads=[pg, bg], writes=[g_])
                    sc.op("act", lambda: nc.scalar.activation(out=s_[:, :wdt], in_=g_[:, :wdt], func=AF.Sigmoid, scale=1.702), reads=[g_], writes=[s_])
                    sc.op("dve", lambda: nc.vector.tensor_scalar(out=u_[:, :wdt], in0=pu[:, :wdt], scalar1=bg[:, 8 + fc:9 + fc], scalar2=7.0, op0=ALU.add, op1=ALU.min),
                          reads=[pu, bg], writes=[u_])
                    sc.op("pool", lambda: nc.gpsimd.tensor_scalar(out=u_[:, :wdt], in0=u_[:, :wdt], scalar1=-7.0, scalar2=1.0, op0=ALU.max, op1=ALU.add),
                          reads=[u_], writes=[u_])
                    sc.op("pool", lambda: nc.gpsimd.tensor_tensor(out=g_[:, :wdt], in0=g_[:, :wdt], in1=s_[:, :wdt], op=ALU.mult), reads=[g_, s_], writes=[g_])
                    sc.op("dve", lambda: nc.vector.tensor_tensor(out=at[:, fc, :wdt], in0=g_[:, :wdt], in1=u_[:, :wdt], op=ALU.mult), reads=[g_, u_], writes=[at], nowaw=(fc > 0))
                for tl in range(ntl):
                    yo = YO[yi % 3]; yi += 1
                    for hf in range(2):
                        pd = psD[di % 2]; di += 1
                        for fc in range(8):
                            sc.op("pe", lambda: nc.tensor.matmul(pd[:, :], lhsT=at[:, fc, tl * 128:(tl + 1) * 128], rhs=wd[:, fc, hf * 512:(hf + 1) * 512],
                                                                 start=(fc == 0), stop=(fc == 7)), reads=[at, wd], writes=[pd], nowaw=(fc > 0))
                        sc.op("act", lambda: nc.scalar.copy(yo[:, hf * 512:(hf + 1) * 512], pd[:, :]), reads=[pd], writes=[yo], nowaw=(hf > 0))
                    sc.dma("sp", Yg[r0 + tl * 128:r0 + (tl + 1) * 128, :], yo[:], reads=[yo], writes=[Yg], track=yo)
        allb = Wgu + Wd + BG + XR + XT + actT + G + Sg + U + YO + psG + psD + psT
        sc.barrier_on(allb)
        sched_release(sc, allb)


def phase_moe_final2(sc, cfg, l, X1, YACC, Yg, DSTD, CWKD, G1P, ln_g, ln_b, XOUT):
    nc = sc.nc
    nseq, seq = cfg["nseq"], cfg["seq"]
    ntok = nseq * seq
    C = moe_capacity(ntok)
    DN_ALPHA = (2 * DEPTH) ** 0.25
    with ExitStack() as es:
        Gt = sc.sbuf("fG", [128, 1024], F32, es)
        Bt = sc.sbuf("fB", [128, 1024], F32, es)
        sc.dma("sp", Gt[:], ln_g[l:l + 1, :].partition_broadcast(128), reads=[ln_g], writes=[Gt], track=Gt)
        sc.dma("sp", Bt[:], ln_b[l:l + 1, :].partition_broadcast(128), reads=[ln_b], writes=[Bt], track=Bt)
        XT = [sc.sbuf("fX", [128, 1024], F32, es) for _ in range(2)]
        YT = [sc.sbuf("fY", [128, 1024], F32, es) for _ in range(2)]
        YK = [sc.sbuf("fYK", [128, 4, 1024], F32, es) for _ in range(2)]
        DSTi = [sc.sbuf("fDST", [128, 4], I32, es) for _ in range(2)]
        CWK = [sc.sbuf("fCWK", [128, 4], F32, es) for _ in range(2)]
        Z = sc.sbuf("fZ", [128, 1024], F32, es)
        ZC = sc.sbuf("fZC", [128, 1024], F32, es)
        TMP = sc.sbuf("fT", [128, 1024], F32, es)
        O = [sc.sbuf("fO", [128, 1024], F32, es) for _ in range(2)]
        ST2 = sc.sbuf("fST", [128, 8], F32, es)
        for tt in range(ntok // 128):
            tok0 = tt * 128
            b = tok0 // seq
            xt, yt, yk, dsti, cwk, o = XT[tt % 2], YT[tt % 2], YK[tt % 2], DSTi[tt % 2], CWK[tt % 2], O[tt % 2]
            sc.dma("sp", xt[:], X1[tok0:tok0 + 128, :], reads=[X1], writes=[xt], track=xt, nowaw=False)
            sc.dma("sp", yt[:], YACC[tok0:tok0 + 128, :], reads=[YACC], writes=[yt], track=yt, nowaw=False)
            sc.dma("sp", dsti[:], DSTD[tok0:tok0 + 128, :], reads=[DSTD], writes=[dsti], track=dsti, nowaw=False)
            sc.dma("sp", cwk[:], CWKD[tok0:tok0 + 128, :], reads=[CWKD], writes=[cwk], track=cwk, nowaw=False)
            for k in range(4):
                d = sc._collect([Yg, dsti], [yk], k > 0)
                sc._wait("pool", d)
                if yk.dsem is None:
                    if sc.sempool:
                        yk.dsem, yk.dcount = sc.sempool.pop()
                    else:
                        yk.dsem = sc.es.enter_context(nc.semaphore("d_" + yk.name))
                ins = nc.gpsimd.indirect_dma_start(out=yk[:, k, :], out_offset=None, in_=Yg[:, :],
                                                   in_offset=bass.IndirectOffsetOnAxis(ap=dsti[:, k:k + 1], axis=0))
                ins.then_inc(yk.dsem, 16)
                yk.dcount += 1
                for bb in (Yg, dsti):
                    bb.rd[yk] = yk.dcount
                if k == 0:
                    yk.wr = {yk: yk.dcount}
                    yk.rd = {}
                else:
                    yk.wr[yk] = yk.dcount
            for k in range(4):
                sc.op("dve", lambda: nc.vector.scalar_tensor_tensor(out=yt[:], in0=yk[:, k, :], scalar=cwk[:, k:k + 1], in1=yt[:], op0=ALU.mult, op1=ALU.add),
                      reads=[yk, cwk, yt], writes=[yt])
            sc.op("pool", lambda: nc.gpsimd.tensor_tensor(out=TMP[:], in0=yt[:], in1=G1P[:, b, :], op=ALU.mult), reads=[yt, G1P], writes=[TMP])
            sc.op("dve", lambda: nc.vector.scalar_tensor_tensor(out=Z[:], in0=xt[:], scalar=DN_ALPHA, in1=TMP[:], op0=ALU.mult, op1=ALU.add),
                  reads=[xt, TMP], writes=[Z])
            layer_norm_tile(sc, Z, ZC, TMP, o, ST2, Gt, Bt)
            sc.dma("sp", XOUT[tok0:tok0 + 128, :], o[:], reads=[o], writes=[XOUT], track=o)
        allb = [Gt, Bt, Z, ZC, TMP, ST2] + XT + YT + YK + DSTi + CWK + O
        sc.barrier_on(allb)
        sched_release(sc, allb)
```

```python
import math
from contextlib import ExitStack
import numpy as np
import concourse.bass as bass
import concourse.mybir as mybir
from concourse.bass_utils import run_bass_kernel_spmd

F32 = mybir.dt.float32
BF16 = mybir.dt.bfloat16
I32 = mybir.dt.int32
AF = mybir.ActivationFunctionType
ALU = mybir.AluOpType
AX = mybir.AxisListType

D = 1024
S = 4096
NSEQ = 2
T = NSEQ * S
DEPTH = 2
D_IN = 5808
NEG = -30000.0


class Buf:
    def __init__(self, name, t):
        self.name = name
        self.t = t
        self.wr = {}
        self.rd = {}
        self.dsem = None
        self.dcount = 0

    def __getitem__(self, idx):
        return self.t[idx]


class Sched:
    def __init__(self, nc, es):
        self.nc = nc
        self.es = es
        self.eng = {"pe": nc.tensor, "dve": nc.vector, "act": nc.scalar,
                    "pool": nc.gpsimd, "sp": nc.sync}
        self.sem = {}
        self.cnt = {}
        for k in ("pe", "dve", "act", "pool"):
            self.sem[k] = es.enter_context(nc.semaphore("s_" + k))
            self.cnt[k] = 0
        self.known = {k: {} for k in self.eng}
        self.nbuf = 0
        self.n_inst = 0
        self.sempool = []

    def sbuf(self, name, shape, dt, es=None):
        es = es or self.es
        self.nbuf += 1
        name = "%s_%d" % (name, self.nbuf)
        t = es.enter_context(self.nc.sbuf_tensor(name, list(shape), dt))
        b = Buf(name, t)
        b.es = es
        return b

    def psum(self, name, shape, dt=F32, es=None):
        es = es or self.es
        self.nbuf += 1
        name = "%s_%d" % (name, self.nbuf)
        t = es.enter_context(self.nc.psum_tensor(name, list(shape), dt))
        b = Buf(name, t)
        b.is_psum = True
        b.es = es
        return b

    def dram(self, name, shape, dt, kind=None):
        if kind is None:
            t = self.nc.dram_tensor(name, list(shape), dt)
            b = Buf(name, t.ap())
            b.es = self.es
            return b
        else:
            t = self.nc.dram_tensor(name, list(shape), dt, kind=kind)
        b = Buf(name, t.ap())
        b.es = self.es
        return b

    def _semobj(self, key):
        if isinstance(key, str):
            return self.sem[key]
        return key.dsem

    def _wait(self, ekey, deps):
        eng = self.eng[ekey]
        kn = self.known[ekey]
        for key, val in deps.items():
            if isinstance(key, str):
                if key == "pe" and ekey == "pe":
                    continue
                v = val
            else:
                if key.dsem is None:
                    continue
                v = key.dcount * 16
            if kn.get(key, 0) >= v:
                continue
            eng.wait_ge(self._semobj(key), v)
            kn[key] = v

    def _collect(self, reads, writes, nowaw):
        deps = {}
        def add(d):
            for k, v in d.items():
                if deps.get(k, 0) < v:
                    deps[k] = v
        for b in reads:
            add(b.wr)
            if getattr(b, "is_psum", False):
                add(b.rd)
        for b in writes:
            if not nowaw:
                add(b.wr)
            add(b.rd)
        return deps

    def op(self, ekey, fn, reads=(), writes=(), nowaw=False):
        deps = self._collect(reads, writes, nowaw)
        self._wait(ekey, deps)
        ins = fn()
        self.cnt[ekey] += 1
        ins.then_inc(self.sem[ekey], 1)
        tk = self.cnt[ekey]
        self.n_inst += 1
        for b in reads:
            if b.rd.get(ekey, 0) < tk:
                b.rd[ekey] = tk
        for b in writes:
            if nowaw:
                b.wr[ekey] = tk
            else:
                b.wr = {ekey: tk}
                b.rd = {}
        return ins

    def dma(self, qkey, out_ap, in_ap, reads=(), writes=(), track=None, nowaw=True, **kw):
        assert track is not None
        if track.dsem is None:
            if self.sempool:
                track.dsem, track.dcount = self.sempool.pop()
            else:
                track.dsem = self.es.enter_context(self.nc.semaphore("d_" + track.name))
        deps = self._collect(reads, writes, nowaw)
        self._wait(qkey, deps)
        ins = self.eng[qkey].dma_start(out=out_ap, in_=in_ap, **kw)
        ins.then_inc(track.dsem, 16)
        track.dcount += 1
        self.n_inst += 1
        for b in reads:
            b.rd[track] = track.dcount
        for b in writes:
            if nowaw:
                b.wr[track] = track.dcount
            else:
                b.wr = {track: track.dcount}
                b.rd = {}
        return ins

    def mark(self, name):
        if not getattr(self, "prof", False):
            return
        cur = getattr(self, "_cur_scope", None)
        if cur is not None:
            self.nc.leave_named_scope(cur[0], cur[1], False)
        sid, _ = self.nc.enter_named_scope(name, False)
        self._cur_scope = (name, sid)

    def barrier_on(self, bufs):
        deps = {}
        for b in bufs:
            for d in (b.wr, b.rd):
                for k, v in d.items():
                    if deps.get(k, 0) < v:
                        deps[k] = v
        for e in self.eng:
            self._wait(e, deps)

    def finish(self, bufs):
        deps = {}
        for b in bufs:
            for k, v in b.wr.items():
                if deps.get(k, 0) < v:
                    deps[k] = v
        self._wait("sp", deps)


C_AQ, C_AK, C_AV, C_IQ, C_IK, C_IW = 0, 256, 320, 384, 512, 544
C_BQ, C_KCR, C_VCR, C_KS, C_VS, C_KW, C_VW, C_BG = 548, 804, 868, 932, 996, 1060, 1124, 1188
C_CQ, C_CK, C_CV, C_MG = 1200, 1712, 2224, 2736
FM_CHUNKS = [("aq0", 0, 128), ("aq1", 128, 128), ("ak", 256, 64), ("iq", 384, 128), ("ik", 512, 32),
             ("bq0", 548, 128), ("bq1", 676, 128), ("kvcr", 804, 128), ("ks", 932, 64), ("kw", 1060, 64),
             ("cq0", 1200, 128), ("cq1", 1328, 128), ("cq2", 1456, 128), ("cq3", 1584, 128),
             ("ck0", 1712, 128), ("ck1", 1840, 128), ("ck2", 1968, 128), ("ck3", 2096, 128)]
FM_ROW = {}
_r = 0
for _n, _c, _w in FM_CHUNKS:
    FM_ROW[_n] = _r
    _r += 128
FM_ROWS = _r
VV_W = 704


def sched_release(sc, bufs):
    for b in bufs:
        if b.dsem is not None:
            sc.sempool.append((b.dsem, b.dcount))
            b.dsem = None


def load_cast(sc, es, dst, dst_ap, src, src_ap):
    sc.dma("pool", dst_ap, src_ap, reads=[src], writes=[dst], track=dst)


def phase_adaln(sc, cfg, l, modw, modbT, modb, which, siluT, out_opsc, out_shf, out_gate, want, tm_third=2, add_one=1.0):
    nc = sc.nc
    nseq = cfg["nseq"]
    with ExitStack() as es:
        wk = [sc.sbuf("adw", [128, 3072], F32, es) for _ in range(2)]
        bT = sc.sbuf("adbT", [128, 24], F32, es)
        gb = sc.sbuf("adgb", [128, 1024], F32, es)
        psA = sc.psum("adpsA", [128, 512], F32, es)
        psG = [[sc.psum("adpsG", [128, 512], F32, es) for _ in range(2)] for _ in range(nseq)]
        silu_bc = []
        if want == "gate":
            for b in range(nseq):
                t = sc.sbuf("silubc", [128, 8, 128], F32, es)
                sc.op("dve", lambda: nc.vector.tensor_copy(t[:], siluT[:, :, b:b + 1].to_broadcast([128, 8, 128])), reads=[siluT], writes=[t])
                silu_bc.append(t)
        sc.dma("sp", bT[:], modbT[l, which], reads=[modbT], writes=[bT], track=bT)
        sc.dma("sp", gb[:], modb[l:l + 1, tm_third * 1024:(tm_third + 1) * 1024].partition_broadcast(128), reads=[modb], writes=[gb], track=gb)
        for kc in range(8):
            w = wk[kc % 2]
            sc.dma("sp", w[:], modw[l, kc * 128:(kc + 1) * 128, :], reads=[modw], writes=[w], track=w, nowaw=False)
            for j in (range(16) if want == "fm" else []):
                sc.op("pe", lambda: nc.tensor.matmul(psA[:, j * nseq:(j + 1) * nseq], lhsT=w[:, j * 128:(j + 1) * 128],
                                                     rhs=siluT[:, kc, :], start=(kc == 0 and j == 0), stop=(kc == 7),
                                                     skip_group_check=True),
                      reads=[w, siluT], writes=[psA], nowaw=True)
            for b in (range(nseq) if want == "gate" else []):
                for hf in range(2):
                    sc.op("pe", lambda: nc.tensor.matmul(psG[b][hf][:, :], lhsT=silu_bc[b][:, kc, :],
                                                         rhs=w[:, tm_third * 1024 + hf * 512:tm_third * 1024 + (hf + 1) * 512],
                                                         start=(kc == 0), stop=(kc == 7)),
                          reads=[w, silu_bc[b]], writes=[psG[b][hf]], nowaw=True)
        for j in (range(8) if want == "fm" else []):
            sc.op("dve", lambda: nc.vector.tensor_scalar(out=out_shf[:, j, :], in0=psA[:, j * nseq:(j + 1) * nseq],
                                                        scalar1=bT[:, j:j + 1], scalar2=None, op0=ALU.add),
                  reads=[psA, bT], writes=[out_shf], nowaw=True)
            sc.op("dve", lambda: nc.vector.tensor_scalar(out=out_opsc[:, j, :], in0=psA[:, (j + 8) * nseq:(j + 9) * nseq],
                                                        scalar1=bT[:, j + 8:j + 9], scalar2=1.0, op0=ALU.add, op1=ALU.add),
                  reads=[psA, bT], writes=[out_opsc], nowaw=True)
        for b in (range(nseq) if want == "gate" else []):
            for hf in range(2):
                sc.op("dve", lambda: nc.vector.scalar_tensor_tensor(out=out_gate[:, b, hf * 512:(hf + 1) * 512],
                                                                   in0=psG[b][hf][:, :], scalar=float(add_one),
                                                                   in1=gb[:, hf * 512:(hf + 1) * 512],
                                                                   op0=ALU.add, op1=ALU.add),
                      reads=[psG[b][hf], gb], writes=[out_gate], nowaw=True)
        sc.barrier_on([wk[0], wk[1], bT, gb, psA] + [p for q in psG for p in q] + silu_bc)
        sched_release(sc, [wk[0], wk[1], bT, gb])


def phase_proj(sc, cfg, l, xsrc, w_in, ident, opsc, shf, FM, VV, SM, MG):
    nc = sc.nc
    nseq, seq = cfg["nseq"], cfg["seq"]
    ntok = nseq * seq
    with ExitStack() as es:
        wb = sc.sbuf("w_in_bf", [128, 8, D_IN], BF16, es)
        for kc in range(8):
            sc.dma("pool", wb[:, kc, :], w_in[l, kc * 128:(kc + 1) * 128, :], reads=[w_in], writes=[wb], track=wb)
        xs = [sc.sbuf("xs", [128, 4, D], F32, es) for _ in range(2)]
        hT = [sc.sbuf("hT", [128, 8, 512], BF16, es) for _ in range(2)]
        fst = [sc.sbuf("fst", [128, 512], BF16, es) for _ in range(4)]
        vst = [sc.sbuf("vst", [128, VV_W], BF16, es) for _ in range(2)]
        sst = [sc.sbuf("sst", [128, 16], F32, es) for _ in range(2)]
        mst = [sc.sbuf("mst", [128, 3072], F32, es) for _ in range(2)]
        ps = [sc.psum("pps", [128, 512], F32, es) for _ in range(8)]
        pi = [0]
        def nextps():
            p = ps[pi[0] % 8]
            pi[0] += 1
            return p
        nblk = ntok // 512
        fi = 0
        for blk in range(nblk):
            b = (blk * 512) // seq
            x_t = xs[blk % 2]
            h_t = hT[blk % 2]
            sc.dma("sp", x_t[:], xsrc[blk * 512:(blk + 1) * 512, :].rearrange("(j p) d -> p j d", p=128),
                   reads=[xsrc], writes=[x_t], track=x_t, nowaw=False)
            for c in range(8):
                p = nextps()
                for j in range(4):
                    sc.op("pe", lambda: nc.tensor.transpose(p[:, j * 128:(j + 1) * 128], x_t[:, j, c * 128:(c + 1) * 128], ident[:]),
                          reads=[x_t, ident], writes=[p], nowaw=(j > 0))
                sc.op("act", lambda: nc.scalar.activation(out=h_t[:, c, :], in_=p[:, :], func=AF.Identity,
                                                          bias=shf[:, c, b:b + 1], scale=opsc[:, c, b:b + 1]),
                      reads=[p, shf, opsc], writes=[h_t], nowaw=(c > 0))
            for (nm, c0, wd) in FM_CHUNKS:
                p = nextps()
                for kc in range(8):
                    sc.op("pe", lambda: nc.tensor.matmul(p[:wd, :], lhsT=wb[:, kc, c0:c0 + wd], rhs=h_t[:, kc, :],
                                                         start=(kc == 0), stop=(kc == 7)),
                          reads=[wb, h_t], writes=[p], nowaw=(kc > 0))
                f = fst[fi % 4]
                ek = "dve" if fi % 2 == 0 else "act"
                if ek == "dve":
                    sc.op("dve", lambda: nc.vector.tensor_copy(f[:wd, :], p[:wd, :]), reads=[p], writes=[f])
                else:
                    sc.op("act", lambda: nc.scalar.copy(f[:wd, :], p[:wd, :]), reads=[p], writes=[f])
                r0 = FM_ROW[nm]
                sc.dma("sp", FM[r0:r0 + wd, blk * 512:(blk + 1) * 512], f[:wd, :], reads=[f], writes=[FM], track=f)
                fi += 1
            for j in range(4):
                tk = blk * 4 + j
                v_t, s_t, m_t = vst[tk % 2], sst[tk % 2], mst[tk % 2]
                tok0 = blk * 512 + j * 128
                def tm(c0, wd):
                    p = nextps()
                    for kc in range(8):
                        sc.op("pe", lambda: nc.tensor.matmul(p[:, :wd], lhsT=h_t[:, kc, j * 128:(j + 1) * 128],
                                                             rhs=wb[:, kc, c0:c0 + wd], start=(kc == 0), stop=(kc == 7)),
                              reads=[wb, h_t], writes=[p], nowaw=(kc > 0))
                    return p
                p = tm(C_AV, 64)
                sc.op("dve", lambda: nc.vector.tensor_copy(v_t[:, 0:64], p[:, 0:64]), reads=[p], writes=[v_t])
                p = tm(C_VS, 64)
                sc.op("dve", lambda: nc.vector.tensor_copy(v_t[:, 64:128], p[:, 0:64]), reads=[p], writes=[v_t], nowaw=True)
                p = tm(C_VW, 76)
                sc.op("dve", lambda: nc.vector.tensor_copy(v_t[:, 128:192], p[:, 0:64]), reads=[p], writes=[v_t], nowaw=True)
                sc.op("act", lambda: nc.scalar.activation(out=s_t[:, 4:16], in_=p[:, 64:76], func=AF.Sigmoid),
                      reads=[p], writes=[s_t])
                p = tm(C_IW, 4)
                sc.op("dve", lambda: nc.vector.tensor_copy(s_t[:, 0:4], p[:, 0:4]), reads=[p], writes=[s_t], nowaw=True)
                p = tm(C_CV, 512)
                sc.op("dve", lambda: nc.vector.tensor_copy(v_t[:, 192:704], p[:, :]), reads=[p], writes=[v_t], nowaw=True)
                sc.dma("sp", VV[tok0:tok0 + 128, :], v_t[:], reads=[v_t], writes=[VV], track=v_t)
                sc.dma("sp", SM[tok0:tok0 + 128, :], s_t[:], reads=[s_t], writes=[SM], track=s_t)
                for g in range(6):
                    p = tm(C_MG + g * 512, 512)
                    sc.op("act", lambda: nc.scalar.activation(out=m_t[:, g * 512:(g + 1) * 512], in_=p[:, :], func=AF.Sigmoid),
                          reads=[p], writes=[m_t], nowaw=(g > 0))
                sc.dma("sp", MG[tok0:tok0 + 128, :], m_t[:], reads=[m_t], writes=[MG], track=m_t)
        allb = [wb] + xs + hT + fst + vst + sst + mst + ps
        sc.barrier_on(allb)
        sched_release(sc, allb)


def rel_bucket_np(dist):
    n = np.maximum(dist, 0)
    nf = np.maximum(n, 1).astype(np.float32)
    large = 16 + (np.log(nf / 16) / np.float32(math.log(128 / 16)) * 16).astype(np.int32)
    large = np.minimum(large, 31)
    return np.where(n < 16, n, large)


def make_consts():
    import ml_dtypes
    c = {}
    r = np.arange(128)[:, None]
    s = np.arange(256)[None, :]
    dist = r + 128 - s
    bk = rel_bucket_np(dist)
    oh = np.zeros((128, 256, 32), np.float32)
    oh[np.arange(128)[:, None], np.arange(256)[None, :], bk] = 1.0
    c["c_oh"] = oh.reshape(128, 256 * 32).astype(ml_dtypes.bfloat16)
    c["c_cneg"] = np.where(dist >= 0, 0.0, NEG).astype(np.float32)
    cm = np.where(np.arange(128)[None, :] <= np.arange(128)[:, None], 0.0, -1e30).astype(np.float32)
    c["c_cm"] = cm
    cc = np.arange(384)[None, :]
    c["c_wneg"] = np.where(cc > r, 0.0, NEG).astype(np.float32)
    c["c_iota"] = np.broadcast_to(np.arange(512, dtype=np.float32)[None, :], (128, 512)).copy()
    qi = np.arange(32)[None, :]
    c["c_tc"] = ((r + qi * 128 - 31) / 16.0).astype(np.float32)
    n_cmp, n_slc = 255, 64
    start = np.arange(n_cmp) * 16
    end = start + 32
    bs = np.arange(n_slc) * 64
    ov = ((start[:, None] < bs[None, :] + 64) & (end[:, None] > bs[None, :])).astype(np.float32)
    ovp = np.zeros((256, 64), np.float32)
    ovp[:255] = ov
    c["c_ovl"] = np.ascontiguousarray(ovp.reshape(2, 128, 64).transpose(1, 0, 2)).astype(ml_dtypes.bfloat16)
    sel = np.zeros((32, 128, 192), np.float32)
    blk = np.arange(64)[None, :]
    for q in range(32):
        t = q * 128 + np.arange(128)[:, None]
        cur = t // 64
        forced = (blk == 0) | (blk == cur) | (blk == cur - 1)
        allowed = blk <= cur
        sel[q, :, 0:64] = (allowed & ~forced).astype(np.float32)
        sel[q, :, 64:128] = np.where(allowed, np.where(forced, 1e9 + 1024.0 * blk, 0.0), -1e30)
        sel[q, :, 128:192] = allowed.astype(np.float32)
    c["c_sel"] = sel
    c["c_pow2"] = np.broadcast_to((2.0 ** -np.arange(40, dtype=np.float32))[None, :], (128, 40)).copy()
    c["c_ident"] = np.eye(128, dtype=np.float32)
    c["c_ut"] = (np.arange(128)[:, None] <= np.arange(128)[None, :]).astype(np.float32)
    return c


CONST_SPECS = [("c_oh", [128, 8192], BF16), ("c_cneg", [128, 256], F32), ("c_cm", [128, 128], F32),
               ("c_wneg", [128, 384], F32), ("c_iota", [128, 512], F32), ("c_tc", [128, 32], F32),
               ("c_ovl", [128, 2, 64], BF16), ("c_sel", [32, 128, 192], F32), ("c_pow2", [128, 40], F32),
               ("c_ident", [128, 128], F32), ("c_ut", [128, 128], F32)]
NBIS = 38
TIE_EPS = 2.0 ** -34


def setup_attn_consts(sc, cd, rel_bias, es):
    nc = sc.nc
    K = {}
    def ld(name, shape, dt, src_ap, srcbuf):
        b = sc.sbuf(name, shape, dt, es)
        sc.dma("sp", b[:], src_ap, reads=[srcbuf], writes=[b], track=b)
        return b
    K["cneg"] = ld("cneg", [128, 256], F32, cd["c_cneg"][:, :], cd["c_cneg"])
    K["cm"] = ld("cm", [128, 128], F32, cd["c_cm"][:, :], cd["c_cm"])
    K["wneg"] = ld("wneg", [128, 384], F32, cd["c_wneg"][:, :], cd["c_wneg"])
    K["iota"] = ld("iota", [128, 512], F32, cd["c_iota"][:, :], cd["c_iota"])
    K["tc"] = ld("tc", [128, 32], F32, cd["c_tc"][:, :], cd["c_tc"])
    K["ovl"] = ld("ovl", [128, 2, 64], BF16, cd["c_ovl"][:, :, :], cd["c_ovl"])
    K["pow2"] = ld("pow2", [128, 40], F32, cd["c_pow2"][:, :], cd["c_pow2"])
    K["ident"] = ld("identf", [128, 128], F32, cd["c_ident"][:, :], cd["c_ident"])
    K["tabb"] = ld("tabb", [128, 384], F32,
                   rel_bias.t.rearrange("b h -> (b h)").unsqueeze(0).partition_broadcast(128), rel_bias)
    K["identb"] = sc.sbuf("identb", [128, 128], BF16, es)
    sc.op("dve", lambda: nc.vector.tensor_copy(K["identb"][:], K["ident"][:]), reads=[K["ident"]], writes=[K["identb"]])
    K["near"] = sc.sbuf("near", [128, 12, 256], F32, es)
    with ExitStack() as tes:
        oh = sc.sbuf("oh", [128, 8192], BF16, tes)
        tmp = sc.sbuf("ohtmp", [128, 8192], F32, tes)
        sc.dma("sp", oh[:], cd["c_oh"][:, :], reads=[cd["c_oh"]], writes=[oh], track=oh)
        for hd in range(12):
            tb = K["tabb"][:, :].rearrange("p (b h) -> p h b", h=12)[:, hd, :]
            sc.op("dve", lambda: nc.vector.tensor_tensor(out=tmp[:].rearrange("p (s b) -> p s b", b=32),
                                                        in0=oh[:].rearrange("p (s b) -> p s b", b=32),
                                                        in1=tb.unsqueeze(1).to_broadcast([128, 256, 32]), op=ALU.mult),
                  reads=[oh, K["tabb"]], writes=[tmp])
            sc.op("dve", lambda: nc.vector.tensor_reduce(out=K["near"][:, hd, :], in_=tmp[:].rearrange("p (s b) -> p s b", b=32),
                                                        axis=AX.X, op=ALU.add), reads=[tmp], writes=[K["near"]], nowaw=True)
            sc.op("dve", lambda: nc.vector.scalar_tensor_tensor(out=K["near"][:, hd, :], in0=K["near"][:, hd, :],
                                                               scalar=K["tabb"][:, 31 * 12 + hd:31 * 12 + hd + 1], in1=K["cneg"][:],
                                                               op0=ALU.subtract, op1=ALU.add), reads=[K["near"], K["cneg"], K["tabb"]], writes=[K["near"]])
        sc.barrier_on([oh, tmp])
        sched_release(sc, [oh, tmp])
    return K


def phase_compress(sc, cfg, l, FM, cmp_w1, cmp_w2, cposT, KC, VC):
    nc = sc.nc
    nseq, seq = cfg["nseq"], cfg["seq"]
    ncmp = (seq - 32) // 16 + 1
    with ExitStack() as es:
        w1 = sc.sbuf("cw1", [128, 32, 256], BF16, es)
        w2 = sc.sbuf("cw2", [128, 2, 2, 64], BF16, es)
        posT = sc.sbuf("cposT", [128, 32], F32, es)
        raw = sc.sbuf("craw", [128, seq], BF16, es)
        rawp = sc.sbuf("crawp", [128, 32, 256], BF16, es)
        hidT = [sc.sbuf("chid", [128, 2, 256], BF16, es) for _ in range(2)]
        t1 = sc.sbuf("ct1", [128, 256], F32, es)
        t2 = sc.sbuf("ct2", [128, 256], F32, es)
        ps = [sc.psum("cps", [128, 512], F32, es) for _ in range(2)]
        for j in range(2):
            sc.dma("pool", w1[j * 64:(j + 1) * 64, :, :], cmp_w1[l, j].rearrange("(p d) h -> d p h", d=64),
                   reads=[cmp_w1], writes=[w1], track=w1)
        sc.dma("pool", w2[:], cmp_w2[l].rearrange("j (hc p) d -> p j hc d", p=128), reads=[cmp_w2], writes=[w2], track=w2)
        sc.dma("sp", posT[:], cposT[l], reads=[cposT], writes=[posT], track=posT)
        sc.op("dve", lambda: nc.vector.memset(VC[:], 0.0), writes=[VC])
        sc.op("dve", lambda: nc.vector.memset(KC[:], 0.0), writes=[KC])
        r0 = FM_ROW["kvcr"]
        pi = 0
        for b in range(nseq):
            sc.dma("sp", raw[:], FM[r0:r0 + 128, b * seq:(b + 1) * seq], reads=[FM], writes=[raw], track=raw, nowaw=False)
            r3 = raw[:].rearrange("p (n s) -> p n s", s=16)
            for p in range(32):
                src = r3[:, 0:ncmp, p] if p < 16 else r3[:, 1:ncmp + 1, p - 16]
                sc.op("dve", lambda: nc.vector.tensor_scalar(out=rawp[:, p, :ncmp], in0=src, scalar1=posT[:, p:p + 1],
                                                            scalar2=None, op0=ALU.add),
                      reads=[raw, posT], writes=[rawp], nowaw=(p > 0))
            for j in range(2):
                for hc in range(2):
                    ph = ps[pi % 2]; pi += 1
                    for p in range(32):
                        sc.op("pe", lambda: nc.tensor.matmul(ph[:, :ncmp], lhsT=w1[j * 64:(j + 1) * 64, p, hc * 128:(hc + 1) * 128],
                                                             rhs=rawp[j * 64:(j + 1) * 64, p, :ncmp], start=(p == 0), stop=(p == 31)),
                              reads=[w1, rawp], writes=[ph], nowaw=(p > 0))
                    sc.op("act", lambda: nc.scalar.activation(out=t1[:, :ncmp], in_=ph[:, :ncmp], func=AF.Square), reads=[ph], writes=[t1])
                    sc.op("dve", lambda: nc.vector.tensor_scalar(out=t1[:, :ncmp], in0=t1[:, :ncmp], scalar1=0.044715, scalar2=1.0,
                                                                op0=ALU.mult, op1=ALU.add), reads=[t1], writes=[t1])
                    sc.op("dve", lambda: nc.vector.tensor_tensor(out=t1[:, :ncmp], in0=t1[:, :ncmp], in1=ph[:, :ncmp], op=ALU.mult),
                          reads=[t1, ph], writes=[t1])
                    sc.op("act", lambda: nc.scalar.activation(out=t2[:, :ncmp], in_=t1[:, :ncmp], func=AF.Sigmoid, scale=1.5957691216057308),
                          reads=[t1], writes=[t2])
                    sc.op("dve", lambda: nc.vector.tensor_tensor(out=hidT[j][:, hc, :ncmp], in0=t2[:, :ncmp], in1=ph[:, :ncmp], op=ALU.mult),
                          reads=[t2, ph], writes=[hidT[j]], nowaw=(hc > 0))
            pk = ps[pi % 2]; pi += 1
            for hc in range(2):
                sc.op("pe", lambda: nc.tensor.matmul(pk[:64, :ncmp], lhsT=w2[:, 0, hc, :], rhs=hidT[0][:, hc, :ncmp],
                                                     start=(hc == 0), stop=(hc == 1)), reads=[w2, hidT[0]], writes=[pk], nowaw=(hc > 0))
            sc.op("dve", lambda: nc.vector.tensor_copy(KC[:, b, :ncmp], pk[:64, :ncmp]), reads=[pk], writes=[KC], nowaw=True)
            for nch in range(2):
                n0 = nch * 128
                nsz = min(128, ncmp - n0)
                if nsz <= 0:
                    continue
                pv = ps[pi % 2]; pi += 1
                for hc in range(2):
                    sc.op("pe", lambda: nc.tensor.matmul(pv[:nsz, 0:64], lhsT=hidT[1][:, hc, n0:n0 + nsz], rhs=w2[:, 1, hc, :],
                                                         start=(hc == 0), stop=(hc == 1)), reads=[w2, hidT[1]], writes=[pv], nowaw=(hc > 0))
                sc.op("dve", lambda: nc.vector.tensor_copy(VC[:nsz, b, nch, :], pv[:nsz, 0:64]), reads=[pv], writes=[VC], nowaw=True)
        allb = [w1, w2, posT, raw, rawp, t1, t2] + hidT + ps
        sc.barrier_on(allb)
        sched_release(sc, allb)


def phase_attn(sc, cfg, l, K, FM, VV, SM, KC, VC, csel, diff_lambda, diff_norm_g, lam_init, OABC, qtiles=None):
    nc = sc.nc
    nseq, seq = cfg["nseq"], cfg["seq"]
    nqt = seq // 128
    IDXC = 0.5 * (32 ** -0.5)
    with ExitStack() as es:
        KT = sc.sbuf("KT", [128, 2, seq], BF16, es)
        CK = sc.sbuf("CK", [128, 4, seq], BF16, es)
        VT = sc.sbuf("VT", [128, nqt, VV_W], BF16, es)
        ISC = sc.sbuf("ISC", [128, seq], F32, es)
        JUNK = sc.sbuf("JUNK", [128, seq], BF16, es)
        QI = [sc.sbuf("QI", [128, 4, 128], BF16, es) for _ in range(2)]
        QA = [sc.sbuf("QA", [64, 4, 128], BF16, es) for _ in range(2)]
        QB = [sc.sbuf("QB", [128, 4, 128], BF16, es) for _ in range(2)]
        QC = [sc.sbuf("QC", [128, 4, 128], BF16, es) for _ in range(2)]
        SMt = [sc.sbuf("SMt", [128, 16], F32, es) for _ in range(2)]
        SELt = [sc.sbuf("SELt", [128, 192], F32, es) for _ in range(2)]
        OUT = [sc.sbuf("OUT", [128, 1024], F32, es) for _ in range(1)]
        AW = sc.sbuf("AW", [128, 4], F32, es)
        SGN = sc.sbuf("SGN", [128, 4], F32, es)
        RT = [sc.sbuf("RT", [128, 512], F32, es) for _ in range(2)]
        ST = sc.sbuf("STAT", [128, 64], F32, es)
        W = sc.sbuf("Wb", [128, 40], F32, es)
        CMK = sc.sbuf("CMK", [128, 256], F32, es)
        PCf = sc.sbuf("PCf", [128, 256], F32, es)
        PCm = sc.sbuf("PCm", [128, 4, 256], F32, es)
        PCn = sc.sbuf("PCn", [128, 5, 256], BF16, es)
        PCT = sc.sbuf("PCT", [128, 10, 128], BF16, es)
        CS = sc.sbuf("CS", [128, 8], F32, es)
        OCMP = sc.sbuf("OCMP", [128, 256], F32, es)
        OWIN = sc.sbuf("OWIN", [128, 256], F32, es)
        IMP = sc.sbuf("IMP", [128, 64], F32, es)
        RANK = sc.sbuf("RANK", [128, 64], F32, es)
        BM = sc.sbuf("BM", [128, 64], F32, es)
        PW = sc.sbuf("PW", [128, 640], BF16, es)
        TMPN = [sc.sbuf("TMPN", [128, 384], F32, es) for _ in range(2)]
        WS = sc.sbuf("WS", [128, 8], F32, es)
        P = [sc.sbuf("P", [128, 512], BF16, es) for _ in range(3)]
        PM = [sc.sbuf("PM", [128, 512], BF16, es) for _ in range(3)]
        MAc = sc.sbuf("MAc", [128, 512], BF16, es)
        PT = [sc.sbuf("PT", [128, 5, 128], BF16, es) for _ in range(3)]
        SUMS = sc.sbuf("SUMS", [128, 16, 16], F32, es)
        TOT = sc.sbuf("TOT", [128, 16], F32, es)
        RCP = sc.sbuf("RCP", [128, 16], F32, es)
        LAM = sc.sbuf("LAM", [128, 4], F32, es)
        DL = sc.sbuf("DL", [128, 256], F32, es)
        DNG = sc.sbuf("DNG", [128, 128], F32, es)
        OCt = sc.sbuf("OCt", [128, 128], F32, es)
        OCj = sc.sbuf("OCj", [128, 128], F32, es)
        TAB31 = sc.sbuf("TAB31", [128, 12], F32, es)
        psS = [sc.psum("psS", [128, 512], F32, es) for _ in range(3)]
        psT = [sc.psum("psT", [128, 8, 128], BF16, es) for _ in range(2)]
        psOab = sc.psum("psOab", [128, 512], F32, es)
        psOc = [sc.psum("psOc", [128, 512], F32, es) for _ in range(2)]
        cnt = {"s": 0, "t": 0, "p": 0, "pm": 0, "pt": 0, "rt": 0, "tn": 0, "ev": 0}
        def nxt(lst, key):
            b = lst[cnt[key] % len(lst)]
            cnt[key] += 1
            return b

        sc.op("dve", lambda: nc.vector.tensor_copy(TAB31[:], K["tabb"][:, 31 * 12:32 * 12]), reads=[K["tabb"]], writes=[TAB31])
        sc.dma("sp", DL[:], diff_lambda[l].rearrange("a d -> (a d)").unsqueeze(0).partition_broadcast(128),
               reads=[diff_lambda], writes=[DL], track=DL)
        sc.dma("sp", DNG[:], diff_norm_g[l:l + 1, :].partition_broadcast(128), reads=[diff_norm_g], writes=[DNG], track=DNG)
        sc.op("dve", lambda: nc.vector.tensor_scalar(out=DNG[:], in0=DNG[:], scalar1=float(1.0 - lam_init), scalar2=None, op0=ALU.mult),
              reads=[DNG], writes=[DNG])
        for a in range(2):
            sc.op("dve", lambda: nc.vector.scalar_tensor_tensor(out=OCt[:, 0:64], in0=DL[:, (2 * a) * 64:(2 * a + 1) * 64], scalar=1.0,
                                                               in1=DL[:, (2 * a + 1) * 64:(2 * a + 2) * 64], op0=ALU.mult, op1=ALU.mult,
                                                               accum_out=LAM[:, 2 + a:3 + a]), reads=[DL], writes=[OCt, LAM])
        sc.op("act", lambda: nc.scalar.activation(out=LAM[:, 2:4], in_=LAM[:, 2:4], func=AF.Exp), reads=[LAM], writes=[LAM])
        sc.op("dve", lambda: nc.vector.scalar_tensor_tensor(out=LAM[:, 0:1], in0=LAM[:, 2:3], scalar=float(lam_init), in1=LAM[:, 3:4],
                                                           op0=ALU.add, op1=ALU.subtract), reads=[LAM], writes=[LAM])
        sc.op("dve", lambda: nc.vector.tensor_scalar(out=LAM[:, 1:2], in0=LAM[:, 0:1], scalar1=-1.0, scalar2=None, op0=ALU.mult),
              reads=[LAM], writes=[LAM])
        sc.op("dve", lambda: nc.vector.memset(PCn[:], 0.0), writes=[PCn])

        qn = 0
        for b in range(nseq):
            c0 = b * seq
            sc.dma("sp", KT[64:96, 1, :], FM[FM_ROW["ik"]:FM_ROW["ik"] + 32, c0:c0 + seq], reads=[FM], writes=[KT], track=KT, nowaw=False)
            for nm, (p0, sl) in (("ak", (0, 0)), ("ks", (64, 0)), ("kw", (0, 1))):
                sc.dma("sp", KT[p0:p0 + 64, sl, :], FM[FM_ROW[nm]:FM_ROW[nm] + 64, c0:c0 + seq], reads=[FM], writes=[KT], track=KT)
            for h in range(4):
                r = FM_ROW["ck%d" % h]
                sc.dma("sp", CK[:, h, :], FM[r:r + 128, c0:c0 + seq], reads=[FM], writes=[CK], track=CK, nowaw=(h > 0))
            for n4 in range(0, nqt, 8):
                sc.dma("sp", VT[:, n4:n4 + 8, :], VV[c0 + n4 * 128:c0 + (n4 + 8) * 128, :].rearrange("(n p) c -> p n c", p=128),
                       reads=[VV], writes=[VT], track=VT, nowaw=(n4 > 0))
            for qi in (qtiles if qtiles is not None else range(nqt)):
                tok0 = c0 + qi * 128
                nk = qi + 1
                Nk = nk * 128
                qI, qA, qB, qC, smt, selt = (x[qn % 2] for x in (QI, QA, QB, QC, SMt, SELt))
                out = OUT[0]
                qn += 1
                r = FM_ROW["iq"]
                sc.dma("sp", qI[64:96, :, :], FM[r:r + 128, tok0:tok0 + 128].rearrange("(h d) q -> d h q", d=32), reads=[FM], writes=[qI], track=qI, nowaw=False)
                r = FM_ROW["aq0"]
                sc.dma("sp", qA[:], FM[r:r + 256, tok0:tok0 + 128].rearrange("(h d) q -> d h q", d=64), reads=[FM], writes=[qA], track=qA, nowaw=False)
                r = FM_ROW["bq0"]
                sc.dma("sp", qB[0:64, :, :], FM[r:r + 256, tok0:tok0 + 128].rearrange("(h d) q -> d h q", d=64), reads=[FM], writes=[qB], track=qB, nowaw=False)
                sc.dma("sp", qB[64:128, :, :], FM[r:r + 256, tok0:tok0 + 128].rearrange("(h d) q -> d h q", d=64), reads=[FM], writes=[qB], track=qB)
                r = FM_ROW["cq0"]
                sc.dma("sp", qC[:], FM[r:r + 512, tok0:tok0 + 128].rearrange("(h r) q -> r h q", r=128), reads=[FM], writes=[qC], track=qC, nowaw=False)
                sc.dma("sp", smt[:], SM[tok0:tok0 + 128, :], reads=[SM], writes=[smt], track=smt, nowaw=False)
                sc.dma("sp", selt[:], csel[qi], reads=[csel], writes=[selt], track=selt, nowaw=False)
                chunks = [(k0, min(512, Nk - k0)) for k0 in range(0, Nk, 512)]

                if cfg.get('prof_q') == (b, qi):
                    sc.mark('q_s1_index')
                sc.op("act", lambda: nc.scalar.activation(out=AW[:], in_=smt[:, 0:4], func=AF.Abs, scale=IDXC), reads=[smt], writes=[AW])
                sc.op("act", lambda: nc.scalar.activation(out=SGN[:], in_=smt[:, 0:4], func=AF.Sign), reads=[smt], writes=[SGN])
                for (k0, w) in chunks:
                    for h in range(4):
                        ps = nxt(psS, "s")
                        sc.op("pe", lambda: nc.tensor.matmul(ps[:, :w], lhsT=qI[64:96, h, :], rhs=KT[64:96, 1, k0:k0 + w], start=True, stop=True),
                              reads=[qI, KT], writes=[ps])
                        rt = nxt(RT, "rt")
                        sc.op("act", lambda: nc.scalar.activation(out=rt[:, :w], in_=ps[:, :w], func=AF.Relu, scale=AW[:, h:h + 1]),
                              reads=[ps, AW], writes=[rt])
                        if h == 0:
                            sc.op("dve", lambda: nc.vector.tensor_scalar(out=ISC[:, k0:k0 + w], in0=rt[:, :w], scalar1=SGN[:, 0:1], scalar2=None,
                                                                        op0=ALU.mult), reads=[rt, SGN], writes=[ISC], nowaw=True)
                        else:
                            sc.op("dve", lambda: nc.vector.scalar_tensor_tensor(out=ISC[:, k0:k0 + w], in0=rt[:, :w], scalar=SGN[:, h:h + 1],
                                                                               in1=ISC[:, k0:k0 + w], op0=ALU.mult, op1=ALU.add),
                                  reads=[rt, SGN, ISC], writes=[ISC])
                    rt = nxt(RT, "rt")
                    sc.op("dve", lambda: nc.vector.tensor_scalar(out=rt[:, :w], in0=K["iota"][:, :w], scalar1=float(k0), scalar2=-TIE_EPS,
                                                                op0=ALU.add, op1=ALU.mult), reads=[K["iota"]], writes=[rt])
                    sc.op("dve", lambda: nc.vector.scalar_tensor_tensor(out=rt[:, :w], in0=ISC[:, k0:k0 + w], scalar=0.0, in1=rt[:, :w],
                                                                       op0=ALU.is_equal, op1=ALU.mult), reads=[ISC, rt], writes=[rt])
                    sc.op("dve", lambda: nc.vector.tensor_tensor(out=ISC[:, k0:k0 + w], in0=ISC[:, k0:k0 + w], in1=rt[:, :w], op=ALU.add),
                          reads=[ISC, rt], writes=[ISC])
                sc.op("dve", lambda: nc.vector.tensor_tensor(out=ISC[:, Nk - 128:Nk], in0=ISC[:, Nk - 128:Nk], in1=K["cm"][:], op=ALU.add),
                      reads=[ISC, K["cm"]], writes=[ISC])
                if cfg.get('stop_after', 9) <= 1:
                    continue
                if cfg.get('prof_q') == (b, qi):
                    sc.mark('q_s2_bisect')
                THR = ST[:, 7:8]
                if qi >= 2:
                    sc.op("dve", lambda: nc.vector.tensor_reduce(out=ST[:, 0:1], in_=ISC[:, :Nk], axis=AX.X, op=ALU.max), reads=[ISC], writes=[ST])
                    sc.op("dve", lambda: nc.vector.tensor_reduce(out=ST[:, 1:2], in_=ISC[:, :Nk - 128], axis=AX.X, op=ALU.min), reads=[ISC], writes=[ST])
                    sc.op("dve", lambda: nc.vector.scalar_tensor_tensor(out=ST[:, 2:3], in0=ST[:, 0:1], scalar=1.0, in1=ST[:, 1:2],
                                                                       op0=ALU.add, op1=ALU.subtract), reads=[ST], writes=[ST])
                    sc.op("dve", lambda: nc.vector.tensor_scalar(out=W[:], in0=K["pow2"][:], scalar1=ST[:, 2:3], scalar2=None, op0=ALU.mult),
                          reads=[K["pow2"], ST], writes=[W])
                    sc.op("dve", lambda: nc.vector.tensor_tensor(out=ST[:, 3:4], in0=ST[:, 1:2], in1=W[:, 1:2], op=ALU.add), reads=[ST, W], writes=[ST])
                    for k in range(1, NBIS + 1):
                        sc.op("dve", lambda: nc.vector.tensor_scalar(out=JUNK[:, :Nk], in0=ISC[:, :Nk], scalar1=ST[:, 3:4], scalar2=0.0,
                                                                    op0=ALU.is_ge, op1=ALU.add, accum_out=ST[:, 4:5]),
                              reads=[ISC, ST], writes=[JUNK, ST])
                        sc.op("dve", lambda: nc.vector.tensor_scalar(out=ST[:, 5:6], in0=ST[:, 4:5], scalar1=255.5, scalar2=0.5,
                                                                    op0=ALU.is_ge, op1=ALU.subtract), reads=[ST], writes=[ST])
                        if k < NBIS:
                            sc.op("dve", lambda: nc.vector.scalar_tensor_tensor(out=ST[:, 3:4], in0=ST[:, 5:6], scalar=W[:, k:k + 1], in1=ST[:, 3:4],
                                                                               op0=ALU.mult, op1=ALU.add), reads=[ST, W], writes=[ST])
                    sc.op("dve", lambda: nc.vector.tensor_scalar(out=ST[:, 6:7], in0=ST[:, 5:6], scalar1=0.5, scalar2=W[:, NBIS:NBIS + 1],
                                                                op0=ALU.subtract, op1=ALU.mult), reads=[ST, W], writes=[ST])
                    sc.op("dve", lambda: nc.vector.tensor_tensor(out=THR, in0=ST[:, 3:4], in1=ST[:, 6:7], op=ALU.add), reads=[ST], writes=[ST])
                else:
                    sc.op("dve", lambda: nc.vector.memset(THR, -1e29), reads=[], writes=[ST])

                if cfg.get('stop_after', 9) <= 2:
                    continue
                if cfg.get('prof_q') == (b, qi):
                    sc.mark('q_s3_cmp')
                sc.op("dve", lambda: nc.vector.tensor_scalar(out=CMK[:], in0=K["iota"][:, 0:256], scalar1=K["tc"][:, qi:qi + 1], scalar2=1.0, op0=ALU.is_gt, op1=ALU.subtract),
                      reads=[K["iota"], K["tc"]], writes=[CMK])
                for h in range(4):
                    ps = nxt(psS, "s")
                    sc.op("pe", lambda: nc.tensor.matmul(ps[:, :256], lhsT=qB[0:64, h, :], rhs=KC[:, b, :], start=True, stop=True),
                          reads=[qB, KC], writes=[ps])
                    sc.op("act", lambda: nc.scalar.activation(out=PCf[:, :255], in_=ps[:, :255], func=AF.Exp, scale=0.125), reads=[ps], writes=[PCf])
                    sc.op("dve", lambda: nc.vector.scalar_tensor_tensor(out=PCm[:, h, :255], in0=PCf[:, :255], scalar=-1.0, in1=CMK[:, :255],
                                                                       op0=ALU.mult, op1=ALU.mult, accum_out=CS[:, h:h + 1]),
                          reads=[PCf, CMK], writes=[PCm, CS], nowaw=True)
                sc.op("dve", lambda: nc.vector.tensor_scalar(out=CS[:, 4:8], in0=CS[:, 0:4], scalar1=1e-30, scalar2=None, op0=ALU.max), reads=[CS], writes=[CS])
                sc.op("dve", lambda: nc.vector.reciprocal(out=CS[:, 4:8], in_=CS[:, 4:8]), reads=[CS], writes=[CS])
                for h in range(4):
                    sc.op("dve", lambda: nc.vector.tensor_scalar(out=PCn[:, h, :255], in0=PCm[:, h, :255], scalar1=CS[:, 4 + h:5 + h], scalar2=None,
                                                                op0=ALU.mult), reads=[PCm, CS], writes=[PCn], nowaw=True)
                    if h == 0:
                        sc.op("dve", lambda: nc.vector.tensor_scalar(out=PCf[:, :255], in0=PCm[:, 0, :255], scalar1=CS[:, 4:5], scalar2=None,
                                                                    op0=ALU.mult), reads=[PCm, CS], writes=[PCf])
                    else:
                        sc.op("dve", lambda: nc.vector.scalar_tensor_tensor(out=PCf[:, :255], in0=PCm[:, h, :255], scalar=CS[:, 4 + h:5 + h],
                                                                           in1=PCf[:, :255], op0=ALU.mult, op1=ALU.add),
                              reads=[PCm, CS, PCf], writes=[PCf])
                sc.op("dve", lambda: nc.vector.tensor_copy(PCn[:, 4, :255], PCf[:, :255]), reads=[PCf], writes=[PCn], nowaw=True)
                for g in range(2):
                    pt = nxt(psT, "t")
                    lo, hi = (0, 8) if g == 0 else (8, 10)
                    for i in range(lo, hi):
                        h, nch = i // 2, i % 2
                        sc.op("pe", lambda: nc.tensor.transpose(pt[:, i - lo, :], PCn[:, h, nch * 128:(nch + 1) * 128], K["identb"][:]),
                              reads=[PCn, K["identb"]], writes=[pt], nowaw=(i > lo))
                    sc.op("act", lambda: nc.scalar.copy(PCT[:, lo:hi, :], pt[:, 0:hi - lo, :]), reads=[pt], writes=[PCT], nowaw=True)
                first = True
                for h in range(4):
                    for nch in range(2):
                        sc.op("pe", lambda: nc.tensor.matmul(psOab[:, h * 64:(h + 1) * 64], lhsT=PCT[:, h * 2 + nch, :], rhs=VC[:, b, nch, :],
                                                             start=first, stop=(nch == 1), skip_group_check=True),
                              reads=[PCT, VC], writes=[psOab], nowaw=(not first))
                        first = False
                psI = nxt(psS, "s")
                for nch in range(2):
                    sc.op("pe", lambda: nc.tensor.matmul(psI[:, 0:64], lhsT=PCT[:, 8 + nch, :], rhs=K["ovl"][:, nch, :], start=(nch == 0), stop=(nch == 1)),
                          reads=[PCT, K["ovl"]], writes=[psI], nowaw=(nch > 0))
                sc.op("act", lambda: nc.scalar.copy(OCMP[:], psOab[:, 0:256]), reads=[psOab], writes=[OCMP])
                sc.op("dve", lambda: nc.vector.tensor_tensor(out=IMP[:], in0=psI[:, 0:64], in1=selt[:, 0:64], op=ALU.mult), reads=[psI, selt], writes=[IMP])
                sc.op("dve", lambda: nc.vector.tensor_tensor(out=IMP[:], in0=IMP[:], in1=selt[:, 64:128], op=ALU.add), reads=[IMP, selt], writes=[IMP])
                J3 = JUNK[:, 0:4096].rearrange("p (j i) -> p j i", i=64)
                sc.op("dve", lambda: nc.vector.tensor_tensor(out=J3, in0=IMP[:].unsqueeze(1).to_broadcast([128, 64, 64]),
                                                            in1=IMP[:].unsqueeze(2).to_broadcast([128, 64, 64]), op=ALU.is_gt),
                      reads=[IMP], writes=[JUNK])
                sc.op("dve", lambda: nc.vector.tensor_reduce(out=RANK[:], in_=J3, axis=AX.X, op=ALU.add), reads=[JUNK], writes=[RANK])
                sc.op("dve", lambda: nc.vector.scalar_tensor_tensor(out=BM[:], in0=RANK[:], scalar=15.5, in1=selt[:, 128:192], op0=ALU.is_lt, op1=ALU.mult),
                      reads=[RANK, selt], writes=[BM])

                if cfg.get('stop_after', 9) <= 3:
                    continue
                if cfg.get('prof_q') == (b, qi):
                    sc.mark('q_s4_win')
                nwin = min(qi, 4) + 1
                nnear = min(nwin, 2)
                nfar = nwin - nnear
                kt0 = qi - (nwin - 1)
                for h in range(4):
                    if nfar > 0:
                        ps = nxt(psS, "s")
                        wf = nfar * 128
                        sc.op("pe", lambda: nc.tensor.matmul(ps[:, :wf], lhsT=qB[0:64, h, :], rhs=KT[0:64, 1, kt0 * 128:kt0 * 128 + wf], start=True, stop=True),
                              reads=[qB, KT], writes=[ps])
                        tn = nxt(TMPN, "tn")
                        sc.op("dve", lambda: nc.vector.scalar_tensor_tensor(out=tn[:, :wf], in0=ps[:, :wf], scalar=0.125, in1=K["wneg"][:, 384 - wf:384],
                                                                           op0=ALU.mult, op1=ALU.add), reads=[ps, K["wneg"]], writes=[tn])
                        sc.op("act", lambda: nc.scalar.activation(out=PW[:, 0:wf], in_=tn[:, :wf], func=AF.Exp,
                                                                  accum_out=WS[:, h:h + 1]), reads=[tn], writes=[PW, WS], nowaw=True)
                    else:
                        wf = 0
                        sc.op("dve", lambda: nc.vector.memset(WS[:, h:h + 1], 0.0), writes=[WS], nowaw=True)
                    ps = nxt(psS, "s")
                    wn = nnear * 128
                    kn0 = (qi - (nnear - 1)) * 128
                    sc.op("pe", lambda: nc.tensor.matmul(ps[:, :wn], lhsT=qB[0:64, h, :], rhs=KT[0:64, 1, kn0:kn0 + wn], start=True, stop=True),
                          reads=[qB, KT], writes=[ps])
                    tn = nxt(TMPN, "tn")
                    sc.op("dve", lambda: nc.vector.scalar_tensor_tensor(out=tn[:, :wn], in0=ps[:, :wn], scalar=0.125, in1=K["near"][:, 4 + h, 256 - wn:256],
                                                                       op0=ALU.mult, op1=ALU.add), reads=[ps, K["near"]], writes=[tn])
                    sc.op("act", lambda: nc.scalar.activation(out=PW[:, wf:wf + wn], in_=tn[:, :wn], func=AF.Exp, accum_out=WS[:, 4 + h:5 + h]),
                          reads=[tn], writes=[PW, WS], nowaw=True)
                    pt = nxt(psT, "t")
                    for i in range(nwin):
                        sc.op("pe", lambda: nc.tensor.transpose(pt[:, i, :], PW[:, i * 128:(i + 1) * 128], K["identb"][:]),
                              reads=[PW, K["identb"]], writes=[pt], nowaw=(i > 0))
                    ptb = nxt(PT, "pt")
                    sc.op("act", lambda: nc.scalar.copy(ptb[:, 0:nwin, :], pt[:, 0:nwin, :]), reads=[pt], writes=[ptb])
                    for i in range(nwin):
                        fst = (h == 0 and i == 0)
                        sc.op("pe", lambda: nc.tensor.matmul(psOab[:, 256 + h * 64:256 + (h + 1) * 64], lhsT=ptb[:, i, :], rhs=VT[:, kt0 + i, 128:192],
                                                             start=fst, stop=(i == nwin - 1), skip_group_check=True),
                              reads=[ptb, VT], writes=[psOab], nowaw=(not fst))
                sc.op("act", lambda: nc.scalar.copy(OWIN[:], psOab[:, 256:512]), reads=[psOab], writes=[OWIN])
                sc.op("dve", lambda: nc.vector.tensor_tensor(out=WS[:, 0:4], in0=WS[:, 0:4], in1=WS[:, 4:8], op=ALU.add), reads=[WS], writes=[WS])

                if cfg.get('stop_after', 9) <= 4:
                    continue
                if cfg.get('prof_q') == (b, qi):
                    sc.mark('q_s5_stream')
                sc.op("dve", lambda: nc.vector.memset(SUMS[:], 0.0), writes=[SUMS])
                firstO = {"ab": True, 0: True, 1: True}
                nch_ = len(chunks)
                prev = None

                def flush_map(pm_):
                    kd, hh, jj, sr, k0_, ntile_, last_ = pm_
                    pt = nxt(psT, "t")
                    for i in range(ntile_):
                        sc.op("pe", lambda: nc.tensor.transpose(pt[:, i, :], sr[:, i * 128:(i + 1) * 128], K["identb"][:]),
                              reads=[sr, K["identb"]], writes=[pt], nowaw=(i > 0))
                    ptb = nxt(PT, "pt")
                    ek = "act" if cnt["ev"] % 2 == 0 else "dve"
                    cnt["ev"] += 1
                    if ek == "act":
                        sc.op("act", lambda: nc.scalar.copy(ptb[:, 0:ntile_, :], pt[:, 0:ntile_, :]), reads=[pt], writes=[ptb])
                    else:
                        sc.op("dve", lambda: nc.vector.tensor_copy(ptb[:, 0:ntile_, :], pt[:, 0:ntile_, :]), reads=[pt], writes=[ptb])
                    for i in range(ntile_):
                        kt = k0_ // 128 + i
                        if kd == "a":
                            ob = psOab
                            o, rhsv, key = psOab[:, hh * 64:(hh + 1) * 64], VT[:, kt, 0:64], "ab"
                        elif kd == "s":
                            ob = psOab
                            o, rhsv, key = psOab[:, 256 + hh * 64:256 + (hh + 1) * 64], VT[:, kt, 64:128], "ab"
                        else:
                            m = hh * 2 + jj
                            ob = psOc[m // 4]
                            o, rhsv, key = ob[:, (m % 4) * 128:(m % 4 + 1) * 128], VT[:, kt, 192 + hh * 128:192 + (hh + 1) * 128], m // 4
                        fst = firstO[key]
                        firstO[key] = False
                        sc.op("pe", lambda: nc.tensor.matmul(o, lhsT=ptb[:, i, :], rhs=rhsv, start=fst, stop=(last_ and i == ntile_ - 1), skip_group_check=True),
                              reads=[ptb, VT], writes=[ob], nowaw=(not fst))

                for ci, (k0, w) in enumerate(chunks):
                    ntile = w // 128
                    wf = min(max(Nk - 256 - k0, 0), w)
                    wn = w - wf
                    no = (k0 + wf) - (Nk - 256)
                    sc.op("dve", lambda: nc.vector.tensor_scalar(out=MAc[:, :w], in0=ISC[:, k0:k0 + w], scalar1=THR, scalar2=None, op0=ALU.is_ge),
                          reads=[ISC, ST], writes=[MAc])
                    maps = [("a", h, 0) for h in range(4)] + [("s", h, 0) for h in range(4)] + [("c", h, j) for h in range(4) for j in range(2)]
                    if "kinds" in cfg:
                        maps = [m_ for m_ in maps if m_[0] in cfg["kinds"]]
                    for mi, (kind, h, j) in enumerate(maps):
                        ps = nxt(psS, "s")
                        if kind == "a":
                            lhsT, rhs, hd = qA[:, h, :], KT[0:64, 0, k0:k0 + w], h
                        elif kind == "s":
                            lhsT, rhs, hd = qB[64:128, h, :], KT[64:128, 0, k0:k0 + w], 4 + h
                        else:
                            lhsT, rhs, hd = qC[j * 64:(j + 1) * 64, h, :], CK[j * 64:(j + 1) * 64, h, k0:k0 + w], 8 + h
                        sc.op("pe", lambda: nc.tensor.matmul(ps[:, :w], lhsT=lhsT, rhs=rhs, start=True, stop=True),
                              reads=[qA, qB, qC, KT, CK], writes=[ps])
                        p = nxt(P, "p")
                        accf = SUMS[:, mi, 2 * ci:2 * ci + 1] if kind == "c" else None
                        accn = SUMS[:, mi, 2 * ci + 1:2 * ci + 2] if kind == "c" else None
                        if wf > 0 and not cfg.get("skipfar"):
                            if kind == "c":
                                sc.op("act", lambda: nc.scalar.activation(out=p[:, :wf], in_=ps[:, :wf], func=AF.Exp, scale=0.125, accum_out=accf),
                                      reads=[ps], writes=[p, SUMS], nowaw=True)
                            else:
                                sc.op("act", lambda: nc.scalar.activation(out=p[:, :wf], in_=ps[:, :wf], func=AF.Exp, scale=0.125),
                                      reads=[ps], writes=[p], nowaw=True)
                        if wn > 0 and not cfg.get("skipnear"):
                            tn = nxt(TMPN, "tn")
                            sc.op("dve", lambda: nc.vector.scalar_tensor_tensor(out=tn[:, :wn], in0=ps[:, wf:w], scalar=0.125, in1=K["near"][:, hd, no:no + wn],
                                                                               op0=ALU.mult, op1=ALU.add), reads=[ps, K["near"]], writes=[tn])
                            if kind == "c":
                                sc.op("act", lambda: nc.scalar.activation(out=p[:, wf:w], in_=tn[:, :wn], func=AF.Exp, accum_out=accn),
                                      reads=[tn], writes=[p, SUMS], nowaw=True)
                            else:
                                sc.op("act", lambda: nc.scalar.activation(out=p[:, wf:w], in_=tn[:, :wn], func=AF.Exp),
                                      reads=[tn], writes=[p], nowaw=True)
                        if kind == "a":
                            pm = nxt(PM, "pm")
                            sc.op("dve", lambda: nc.vector.scalar_tensor_tensor(out=pm[:, :w], in0=p[:, :w], scalar=1.0, in1=MAc[:, :w], op0=ALU.mult, op1=ALU.mult,
                                                                               accum_out=SUMS[:, mi, 2 * ci:2 * ci + 1]), reads=[p, MAc], writes=[pm, SUMS], nowaw=True)
                            src = pm
                        elif kind == "s":
                            pm = nxt(PM, "pm")
                            sc.op("dve", lambda: nc.vector.scalar_tensor_tensor(out=pm[:, :w].rearrange("p (j i) -> p j i", i=64),
                                                                               in0=p[:, :w].rearrange("p (j i) -> p j i", i=64), scalar=1.0,
                                                                               in1=BM[:, k0 // 64:(k0 + w) // 64].unsqueeze(2).to_broadcast([128, w // 64, 64]),
                                                                               op0=ALU.mult, op1=ALU.mult, accum_out=SUMS[:, mi, 2 * ci:2 * ci + 1]),
                                  reads=[p, BM], writes=[pm, SUMS], nowaw=True)
                            src = pm
                        else:
                            src = p
                        if prev is not None and not cfg.get("nopv"):
                            flush_map(prev)
                        prev = (kind, h, j, src, k0, ntile, ci == nch_ - 1)
                if prev is not None and not cfg.get("nopv"):
                    flush_map(prev)
                prev = None

                if cfg.get('prof_q') == (b, qi):
                    sc.mark('q_s6_final')
                sc.op("dve", lambda: nc.vector.tensor_reduce(out=TOT[:], in_=SUMS[:], axis=AX.X, op=ALU.add), reads=[SUMS], writes=[TOT])
                sc.op("dve", lambda: nc.vector.tensor_scalar(out=RCP[:], in0=TOT[:], scalar1=1e-30, scalar2=None, op0=ALU.max), reads=[TOT], writes=[RCP])
                sc.op("dve", lambda: nc.vector.reciprocal(out=RCP[:], in_=RCP[:]), reads=[RCP], writes=[RCP])
                sc.op("dve", lambda: nc.vector.reciprocal(out=WS[:, 4:8], in_=WS[:, 0:4]), reads=[WS], writes=[WS])
                for h in range(4):
                    sc.op("dve", lambda: nc.vector.tensor_scalar(out=out[:, h * 64:(h + 1) * 64], in0=psOab[:, h * 64:(h + 1) * 64], scalar1=RCP[:, h:h + 1],
                                                                scalar2=None, op0=ALU.mult), reads=[psOab, RCP], writes=[out], nowaw=(h > 0))
                for h in range(4):
                    g0, g1, g2 = (smt[:, 4 + 3 * h + k:5 + 3 * h + k] for k in range(3))
                    sc.op("dve", lambda: nc.vector.tensor_scalar(out=ST[:, 8:9], in0=RCP[:, 4 + h:5 + h], scalar1=g1, scalar2=None, op0=ALU.mult), reads=[RCP, smt], writes=[ST])
                    sc.op("dve", lambda: nc.vector.tensor_scalar(out=ST[:, 9:10], in0=WS[:, 4 + h:5 + h], scalar1=g2, scalar2=None, op0=ALU.mult), reads=[WS, smt], writes=[ST])
                    oslice = out[:, 256 + h * 64:256 + (h + 1) * 64]
                    sc.op("dve", lambda: nc.vector.tensor_scalar(out=oslice, in0=OCMP[:, h * 64:(h + 1) * 64], scalar1=g0, scalar2=None, op0=ALU.mult),
                          reads=[OCMP, smt], writes=[out], nowaw=True)
                    sc.op("dve", lambda: nc.vector.scalar_tensor_tensor(out=oslice, in0=psOab[:, 256 + h * 64:256 + (h + 1) * 64], scalar=ST[:, 8:9], in1=oslice,
                                                                       op0=ALU.mult, op1=ALU.add), reads=[psOab, ST, out], writes=[out])
                    sc.op("dve", lambda: nc.vector.scalar_tensor_tensor(out=oslice, in0=OWIN[:, h * 64:(h + 1) * 64], scalar=ST[:, 9:10], in1=oslice,
                                                                       op0=ALU.mult, op1=ALU.add), reads=[OWIN, ST, out], writes=[out])
                for h in range(4):
                    m0, m1 = 8 + h * 2, 9 + h * 2
                    ob0, ob1 = psOc[(h * 2) // 4], psOc[(h * 2 + 1) // 4]
                    o0 = ob0[:, ((h * 2) % 4) * 128:((h * 2) % 4 + 1) * 128]
                    o1 = ob1[:, ((h * 2 + 1) % 4) * 128:((h * 2 + 1) % 4 + 1) * 128]
                    sc.op("dve", lambda: nc.vector.tensor_scalar(out=ST[:, 10:11], in0=RCP[:, m1:m1 + 1], scalar1=LAM[:, 1:2], scalar2=None, op0=ALU.mult),
                          reads=[RCP, LAM], writes=[ST])
                    sc.op("dve", lambda: nc.vector.tensor_scalar(out=OCt[:], in0=o0, scalar1=RCP[:, m0:m0 + 1], scalar2=None, op0=ALU.mult),
                          reads=[ob0, RCP], writes=[OCt])
                    sc.op("dve", lambda: nc.vector.scalar_tensor_tensor(out=OCt[:], in0=o1, scalar=ST[:, 10:11], in1=OCt[:], op0=ALU.mult, op1=ALU.add),
                          reads=[ob1, ST, OCt], writes=[OCt])
                    sc.op("dve", lambda: nc.vector.scalar_tensor_tensor(out=OCj[:], in0=OCt[:], scalar=1.0, in1=OCt[:], op0=ALU.mult, op1=ALU.mult,
                                                                       accum_out=ST[:, 11:12]), reads=[OCt], writes=[OCj, ST])
                    sc.op("dve", lambda: nc.vector.tensor_scalar(out=ST[:, 12:13], in0=ST[:, 11:12], scalar1=1.0 / 128, scalar2=1e-5, op0=ALU.mult, op1=ALU.add),
                          reads=[ST], writes=[ST])
                    sc.op("act", lambda: nc.scalar.activation(out=ST[:, 13:14], in_=ST[:, 12:13], func=AF.Ln), reads=[ST], writes=[ST])
                    sc.op("act", lambda: nc.scalar.activation(out=ST[:, 14:15], in_=ST[:, 13:14], func=AF.Exp, scale=-0.5), reads=[ST], writes=[ST])
                    sc.op("dve", lambda: nc.vector.scalar_tensor_tensor(out=out[:, 512 + h * 128:512 + (h + 1) * 128], in0=OCt[:], scalar=ST[:, 14:15], in1=DNG[:],
                                                                       op0=ALU.mult, op1=ALU.mult), reads=[OCt, ST, DNG], writes=[out], nowaw=True)
                sc.dma("sp", OABC[tok0:tok0 + 128, :], out[:], reads=[out], writes=[OABC], track=out)
                if cfg.get('prof_q') == (b, qi):
                    sc.mark('L%d_phase_attn_rest' % l)
        allb = ([KT, CK, VT, ISC, JUNK, AW, SGN, ST, W, CMK, PCf, PCm, PCn, PCT, CS, OCMP, OWIN, IMP, RANK, BM, PW, WS, MAc, SUMS, TOT, RCP, LAM, DL, DNG,
                 OCt, OCj, TAB31, psOab] + QI + QA + QB + QC + SMt + SELt + OUT + RT + TMPN + P + PM + PT + psS + psT + psOc)
        sc.barrier_on(allb)
        sched_release(sc, allb)


def layer_norm_tile(sc, z, zc, junk, o, ST2, Gt, Bt):
    nc = sc.nc
    sc.op("dve", lambda: nc.vector.tensor_reduce(out=ST2[:, 0:1], in_=z[:], axis=AX.X, op=ALU.add), reads=[z], writes=[ST2])
    sc.op("dve", lambda: nc.vector.tensor_scalar(out=ST2[:, 1:2], in0=ST2[:, 0:1], scalar1=1.0 / D, scalar2=None, op0=ALU.mult), reads=[ST2], writes=[ST2])
    sc.op("dve", lambda: nc.vector.tensor_scalar(out=zc[:], in0=z[:], scalar1=ST2[:, 1:2], scalar2=None, op0=ALU.subtract), reads=[z, ST2], writes=[zc])
    sc.op("dve", lambda: nc.vector.scalar_tensor_tensor(out=junk[:], in0=zc[:], scalar=1.0, in1=zc[:], op0=ALU.mult, op1=ALU.mult,
                                                       accum_out=ST2[:, 2:3]), reads=[zc], writes=[junk, ST2])
    sc.op("dve", lambda: nc.vector.tensor_scalar(out=ST2[:, 3:4], in0=ST2[:, 2:3], scalar1=1.0 / D, scalar2=1e-5, op0=ALU.mult, op1=ALU.add),
          reads=[ST2], writes=[ST2])
    sc.op("act", lambda: nc.scalar.activation(out=ST2[:, 4:5], in_=ST2[:, 3:4], func=AF.Ln), reads=[ST2], writes=[ST2])
    sc.op("act", lambda: nc.scalar.activation(out=ST2[:, 5:6], in_=ST2[:, 4:5], func=AF.Exp, scale=-0.5), reads=[ST2], writes=[ST2])
    sc.op("dve", lambda: nc.vector.scalar_tensor_tensor(out=zc[:], in0=zc[:], scalar=ST2[:, 5:6], in1=Gt[:], op0=ALU.mult, op1=ALU.mult),
          reads=[zc, ST2, Gt], writes=[zc])
    sc.op("pool", lambda: nc.gpsimd.tensor_tensor(out=o[:], in0=zc[:], in1=Bt[:], op=ALU.add), reads=[zc, Bt], writes=[o])


def phase_tail(sc, cfg, l, K, OABC, MG, xsrc, wba_d, wbb_d, wbc_d, wo_d, ln_g, ln_b, G1P, X1):
    nc = sc.nc
    nseq, seq = cfg["nseq"], cfg["seq"]
    ntok = nseq * seq
    DN_ALPHA = (2 * DEPTH) ** 0.25
    with ExitStack() as es:
        wb = sc.sbuf("wbr", [128, 8, 1024], BF16, es)
        wo = sc.sbuf("wo", [128, 8, 1024], BF16, es)
        sc.dma("pool", wb[:, 0:2, :], wba_d[l].rearrange("(c p) n -> p c n", p=128), reads=[wba_d], writes=[wb], track=wb)
        sc.dma("pool", wb[:, 2:4, :], wbb_d[l].rearrange("(c p) n -> p c n", p=128), reads=[wbb_d], writes=[wb], track=wb)
        sc.dma("pool", wb[:, 4:8, :], wbc_d[l].rearrange("(c p) n -> p c n", p=128), reads=[wbc_d], writes=[wb], track=wb)
        sc.dma("pool", wo[:], wo_d[l].rearrange("(c p) n -> p c n", p=128), reads=[wo_d], writes=[wo], track=wo)
        Gt = sc.sbuf("lnG", [128, 1024], F32, es)
        Bt = sc.sbuf("lnB", [128, 1024], F32, es)
        sc.dma("sp", Gt[:], ln_g[l:l + 1, :].partition_broadcast(128), reads=[ln_g], writes=[Gt], track=Gt)
        sc.dma("sp", Bt[:], ln_b[l:l + 1, :].partition_broadcast(128), reads=[ln_b], writes=[Bt], track=Bt)
        OA = [sc.sbuf("tOA", [128, 1024], F32, es) for _ in range(2)]
        MGt = [sc.sbuf("tMG", [128, 3072], F32, es) for _ in range(2)]
        XT = [sc.sbuf("tX", [128, 1024], F32, es) for _ in range(2)]
        OB_2 = [sc.sbuf("tOB", [128, 1024], BF16, es) for _ in range(2)]
        oT_2 = [sc.sbuf("toT", [128, 8, 128], BF16, es) for _ in range(2)]
        M_2 = [sc.sbuf("tM", [128, 1024], F32, es) for _ in range(2)]
        TMP_2 = [sc.sbuf("tTMP", [128, 1024], F32, es) for _ in range(2)]
        MB_2 = [sc.sbuf("tMB", [128, 1024], BF16, es) for _ in range(2)]
        mT_2 = [sc.sbuf("tmT", [128, 8, 128], BF16, es) for _ in range(2)]
        Z_2 = [sc.sbuf("tZ", [128, 1024], F32, es) for _ in range(2)]
        ZC_2 = [sc.sbuf("tZC", [128, 1024], F32, es) for _ in range(2)]
        O = [sc.sbuf("tO", [128, 1024], F32, es) for _ in range(2)]
        ST2_2 = [sc.sbuf("tST", [128, 8], F32, es) for _ in range(2)]
        psT = [sc.psum("tpsT", [128, 8, 128], BF16, es) for _ in range(2)]
        psY = [sc.psum("tpsY", [128, 512], F32, es) for _ in range(6)]
        yi = 0
        for tt in range(ntok // 128):
            OB = OB_2[tt % 2]
            oT = oT_2[tt % 2]
            M = M_2[tt % 2]
            TMP = TMP_2[tt % 2]
            MB = MB_2[tt % 2]
            mT = mT_2[tt % 2]
            Z = Z_2[tt % 2]
            ZC = ZC_2[tt % 2]
            ST2 = ST2_2[tt % 2]
            tok0 = tt * 128
            b = tok0 // seq
            oa, mg, xt, o = OA[tt % 2], MGt[tt % 2], XT[tt % 2], O[tt % 2]
            sc.dma("sp", oa[:], OABC[tok0:tok0 + 128, :], reads=[OABC], writes=[oa], track=oa, nowaw=False)
            sc.dma("sp", mg[:], MG[tok0:tok0 + 128, :], reads=[MG], writes=[mg], track=mg, nowaw=False)
            sc.dma("sp", xt[:], xsrc[tok0:tok0 + 128, :], reads=[xsrc], writes=[xt], track=xt, nowaw=False)
            sc.op("act", lambda: nc.scalar.copy(OB[:], oa[:]), reads=[oa], writes=[OB])
            pt = psT[0]
            for c in range(8):
                sc.op("pe", lambda: nc.tensor.transpose(pt[:, c, :], OB[:, c * 128:(c + 1) * 128], K["identb"][:]), reads=[OB, K["identb"]], writes=[pt], nowaw=(c > 0))
            sc.op("act", lambda: nc.scalar.copy(oT[:], pt[:]), reads=[pt], writes=[oT])
            for hf in range(2):
                cs = slice(hf * 512, (hf + 1) * 512)
                ys = []
                for (k0, k1) in ((0, 2), (2, 4), (4, 8)):
                    py = psY[yi % 6]; yi += 1
                    for kc in range(k0, k1):
                        sc.op("pe", lambda: nc.tensor.matmul(py[:, :], lhsT=oT[:, kc, :], rhs=wb[:, kc, cs], start=(kc == k0), stop=(kc == k1 - 1)),
                              reads=[oT, wb], writes=[py], nowaw=(kc > k0))
                    ys.append(py)
                sc.op("dve", lambda: nc.vector.tensor_tensor(out=M[:, cs], in0=ys[0][:, :], in1=mg[:, hf * 512:(hf + 1) * 512], op=ALU.mult),
                      reads=[ys[0], mg], writes=[M], nowaw=(hf > 0))
                sc.op("dve", lambda: nc.vector.tensor_tensor(out=TMP[:, cs], in0=ys[1][:, :], in1=mg[:, 1024 + hf * 512:1024 + (hf + 1) * 512], op=ALU.mult),
                      reads=[ys[1], mg], writes=[TMP], nowaw=(hf > 0))
                sc.op("pool", lambda: nc.gpsimd.tensor_tensor(out=M[:, cs], in0=M[:, cs], in1=TMP[:, cs], op=ALU.add), reads=[M, TMP], writes=[M])
                sc.op("dve", lambda: nc.vector.tensor_tensor(out=TMP[:, cs], in0=ys[2][:, :], in1=mg[:, 2048 + hf * 512:2048 + (hf + 1) * 512], op=ALU.mult),
                      reads=[ys[2], mg], writes=[TMP])
                sc.op("pool", lambda: nc.gpsimd.tensor_tensor(out=MB[:, cs], in0=M[:, cs], in1=TMP[:, cs], op=ALU.add), reads=[M, TMP], writes=[MB], nowaw=(hf > 0))
            pt = psT[1]
            for c in range(8):
                sc.op("pe", lambda: nc.tensor.transpose(pt[:, c, :], MB[:, c * 128:(c + 1) * 128], K["identb"][:]), reads=[MB, K["identb"]], writes=[pt], nowaw=(c > 0))
            sc.op("act", lambda: nc.scalar.copy(mT[:], pt[:]), reads=[pt], writes=[mT])
            for hf in range(2):
                cs = slice(hf * 512, (hf + 1) * 512)
                py = psY[yi % 6]; yi += 1
                for kc in range(8):
                    sc.op("pe", lambda: nc.tensor.matmul(py[:, :], lhsT=mT[:, kc, :], rhs=wo[:, kc, cs], start=(kc == 0), stop=(kc == 7)),
                          reads=[mT, wo], writes=[py], nowaw=(kc > 0))
                sc.op("dve", lambda: nc.vector.tensor_tensor(out=TMP[:, cs], in0=py[:, :], in1=G1P[:, b, cs], op=ALU.mult), reads=[py, G1P], writes=[TMP])
                sc.op("dve", lambda: nc.vector.scalar_tensor_tensor(out=Z[:, cs], in0=xt[:, cs], scalar=DN_ALPHA, in1=TMP[:, cs], op0=ALU.mult, op1=ALU.add),
                      reads=[xt, TMP], writes=[Z], nowaw=(hf > 0))
            layer_norm_tile(sc, Z, ZC, TMP, o, ST2, Gt, Bt)
            sc.dma("sp", X1[tok0:tok0 + 128, :], o[:], reads=[o], writes=[X1], track=o)
        allb = [wb, wo, Gt, Bt, OB_2[0], OB_2[1], oT_2[0], oT_2[1], M_2[0], M_2[1], TMP_2[0], TMP_2[1], MB_2[0], MB_2[1], mT_2[0], mT_2[1], Z_2[0], Z_2[1], ZC_2[0], ZC_2[1], ST2_2[0], ST2_2[1]] + OA + MGt + XT + O + psT + psY
        sc.barrier_on(allb)
        sched_release(sc, allb)


def phase_moe_prep(sc, cfg, l, K, X1, opsc, shf, router_w, router_b, b_down, H2T, CWD, YACC):
    nc = sc.nc
    nseq, seq = cfg["nseq"], cfg["seq"]
    ntok = nseq * seq
    with ExitStack() as es:
        rw = sc.sbuf("rw", [128, 8, 32], F32, es)
        rb = sc.sbuf("rb", [128, 32], F32, es)
        bd = sc.sbuf("bd", [32, 1024], F32, es)
        sc.dma("sp", rw[:], router_w[l].rearrange("(c p) e -> p c e", p=128), reads=[router_w], writes=[rw], track=rw)
        sc.dma("sp", rb[:], router_b[l:l + 1, :].partition_broadcast(128), reads=[router_b], writes=[rb], track=rb)
        sc.dma("sp", bd[:], b_down[l], reads=[b_down], writes=[bd], track=bd)
        XT = [sc.sbuf("mX", [128, 1024], F32, es) for _ in range(2)]
        HF = sc.sbuf("mHF", [128, 8, 128], F32, es)
        HB = [sc.sbuf("mHB", [128, 8, 128], BF16, es) for _ in range(2)]
        Lg = sc.sbuf("mL", [128, 32], F32, es)
        J3 = sc.sbuf("mJ3", [128, 32, 32], F32, es)
        RK = sc.sbuf("mRK", [128, 32], F32, es)
        EX = sc.sbuf("mEX", [128, 32], F32, es)
        CW = [sc.sbuf("mCW", [128, 32], F32, es) for _ in range(2)]
        CWT = sc.sbuf("mCWT", [32, 128], F32, es)
        YB = [sc.sbuf("mYB", [128, 1024], F32, es) for _ in range(2)]
        ST = sc.sbuf("mST", [128, 8], F32, es)
        ps = [sc.psum("mps", [128, 512], F32, es) for _ in range(6)]
        pi = 0
        for tt in range(ntok // 128):
            tok0 = tt * 128
            b = tok0 // seq
            xt, hb, cw, yb = XT[tt % 2], HB[tt % 2], CW[tt % 2], YB[tt % 2]
            sc.dma("sp", xt[:], X1[tok0:tok0 + 128, :], reads=[X1], writes=[xt], track=xt, nowaw=False)
            for g in range(2):
                p = ps[pi % 6]; pi += 1
                for c4 in range(4):
                    c = g * 4 + c4
                    sc.op("pe", lambda: nc.tensor.transpose(p[:, c4 * 128:(c4 + 1) * 128], xt[:, c * 128:(c + 1) * 128], K["ident"][:]),
                          reads=[xt, K["ident"]], writes=[p], nowaw=(c4 > 0))
                for c4 in range(4):
                    c = g * 4 + c4
                    sc.op("act", lambda: nc.scalar.activation(out=HF[:, c, :], in_=p[:, c4 * 128:(c4 + 1) * 128], func=AF.Identity,
                                                              bias=shf[:, c, b:b + 1], scale=opsc[:, c, b:b + 1]),
                          reads=[p, shf, opsc], writes=[HF], nowaw=(c > 0))
            sc.op("dve", lambda: nc.vector.tensor_copy(hb[:], HF[:]), reads=[HF], writes=[hb])
            sc.dma("sp", H2T[:, tok0:tok0 + 128].rearrange("(c p) t -> p c t", p=128), hb[:], reads=[hb], writes=[H2T], track=hb)
            p = ps[pi % 6]; pi += 1
            for c in range(8):
                sc.op("pe", lambda: nc.tensor.matmul(p[:, 0:32], lhsT=HF[:, c, :], rhs=rw[:, c, :], start=(c == 0), stop=(c == 7)),
                      reads=[HF, rw], writes=[p], nowaw=(c > 0))
            sc.op("dve", lambda: nc.vector.tensor_tensor(out=Lg[:], in0=p[:, 0:32], in1=rb[:], op=ALU.add), reads=[p, rb], writes=[Lg])
            sc.op("dve", lambda: nc.vector.tensor_tensor(out=J3[:], in0=Lg[:].unsqueeze(1).to_broadcast([128, 32, 32]),
                                                        in1=Lg[:].unsqueeze(2).to_broadcast([128, 32, 32]), op=ALU.is_gt), reads=[Lg], writes=[J3])
            sc.op("dve", lambda: nc.vector.tensor_reduce(out=RK[:], in_=J3[:], axis=AX.X, op=ALU.add), reads=[J3], writes=[RK])
            sc.op("dve", lambda: nc.vector.tensor_reduce(out=ST[:, 0:1], in_=Lg[:], axis=AX.X, op=ALU.max), reads=[Lg], writes=[ST])
            sc.op("dve", lambda: nc.vector.tensor_scalar(out=ST[:, 1:2], in0=ST[:, 0:1], scalar1=-1.0, scalar2=None, op0=ALU.mult), reads=[ST], writes=[ST])
            sc.op("act", lambda: nc.scalar.activation(out=EX[:], in_=Lg[:], func=AF.Exp, bias=ST[:, 1:2]), reads=[Lg, ST], writes=[EX])
            sc.op("dve", lambda: nc.vector.tensor_scalar(out=RK[:], in0=RK[:], scalar1=3.5, scalar2=None, op0=ALU.is_lt), reads=[RK], writes=[RK])
            sc.op("dve", lambda: nc.vector.scalar_tensor_tensor(out=EX[:], in0=EX[:], scalar=1.0, in1=RK[:], op0=ALU.mult, op1=ALU.mult,
                                                               accum_out=ST[:, 2:3]), reads=[EX, RK], writes=[EX, ST])
            sc.op("dve", lambda: nc.vector.reciprocal(out=ST[:, 3:4], in_=ST[:, 2:3]), reads=[ST], writes=[ST])
            sc.op("dve", lambda: nc.vector.tensor_scalar(out=cw[:], in0=EX[:], scalar1=ST[:, 3:4], scalar2=None, op0=ALU.mult), reads=[EX, ST], writes=[cw])
            sc.dma("sp", CWD[tok0:tok0 + 128, :], cw[:], reads=[cw], writes=[CWD], track=cw)
            p = ps[pi % 6]; pi += 1
            sc.op("pe", lambda: nc.tensor.transpose(p[:32, 0:128], cw[:, :], K["ident"][:]), reads=[cw, K["ident"]], writes=[p])
            sc.op("act", lambda: nc.scalar.copy(CWT[:], p[:32, 0:128]), reads=[p], writes=[CWT])
            for hf in range(2):
                p = ps[pi % 6]; pi += 1
                sc.op("pe", lambda: nc.tensor.matmul(p[:, :], lhsT=CWT[:, :], rhs=bd[:, hf * 512:(hf + 1) * 512], start=True, stop=True),
                      reads=[CWT, bd], writes=[p])
                sc.op("act", lambda: nc.scalar.copy(yb[:, hf * 512:(hf + 1) * 512], p[:, :]), reads=[p], writes=[yb], nowaw=(hf > 0))
            sc.dma("sp", YACC[tok0:tok0 + 128, :], yb[:], reads=[yb], writes=[YACC], track=yb)
        allb = [rw, rb, bd, HF, Lg, J3, RK, EX, CWT, ST] + XT + HB + CW + YB + ps
        sc.barrier_on(allb)
        sched_release(sc, allb)


def phase_moe_experts(sc, cfg, l, H2T, CWD, YACC, w_gu, b_guT, w_down):
    nc = sc.nc
    nseq, seq = cfg["nseq"], cfg["seq"]
    ntok = nseq * seq
    CH = min(1024, ntok)
    ntile = CH // 128
    nsb = CH // 512
    ne = cfg.get("n_exp", 32)
    with ExitStack() as es:
        hT = sc.sbuf("eh", [128, 8, CH], BF16, es)
        ACC = sc.sbuf("eACC", [128, ntile, 1024], F32, es)
        CWc = sc.sbuf("eCW", [128, ntile, 32], F32, es)
        Wgu = [sc.sbuf("eWgu", [128, 8, 2048], BF16, es) for _ in range(2)]
        Wd = [sc.sbuf("eWd", [128, 8, 1024], BF16, es) for _ in range(2)]
        BG = [sc.sbuf("eBG", [128, 16], F32, es) for _ in range(2)]
        actT = [sc.sbuf("eact", [128, 8, 512], BF16, es) for _ in range(2)]
        G = [sc.sbuf("eG", [128, 512], F32, es) for _ in range(2)]
        Sg = [sc.sbuf("eS", [128, 512], F32, es) for _ in range(2)]
        U = [sc.sbuf("eU", [128, 512], F32, es) for _ in range(2)]
        psG = [sc.psum("epsG", [128, 512], F32, es) for _ in range(5)]
        psD = [sc.psum("epsD", [128, 512], F32, es) for _ in range(3)]
        gi = di = ai = ti = 0
        for ch in range(ntok // CH):
            t0 = ch * CH
            sc.dma("sp", hT[:], H2T[:, t0:t0 + CH].rearrange("(c p) t -> p c t", p=128), reads=[H2T], writes=[hT], track=hT, nowaw=False)
            sc.dma("sp", ACC[:], YACC[t0:t0 + CH, :].rearrange("(n p) d -> p n d", p=128), reads=[YACC], writes=[ACC], track=ACC, nowaw=False)
            sc.dma("sp", CWc[:], CWD[t0:t0 + CH, :].rearrange("(n p) e -> p n e", p=128), reads=[CWD], writes=[CWc], track=CWc, nowaw=False)
            for e in range(ne):
                k = (ch * ne + e) % 2
                wg, wd, bg = Wgu[k], Wd[k], BG[k]
                for c2 in range(0, 8, 2):
                    sc.dma("pool", wg[:, c2:c2 + 2, :], w_gu[l, e, c2 * 128:(c2 + 2) * 128, :].rearrange("(c p) n -> p c n", p=128),
                           reads=[w_gu], writes=[wg], track=wg, nowaw=(c2 > 0))
                for c4 in range(0, 8, 4):
                    sc.dma("pool", wd[:, c4:c4 + 4, :], w_down[l, e, c4 * 128:(c4 + 4) * 128, :].rearrange("(c p) n -> p c n", p=128),
                           reads=[w_down], writes=[wd], track=wd, nowaw=(c4 > 0))
                sc.dma("sp", bg[:], b_guT[l, e], reads=[b_guT], writes=[bg], track=bg, nowaw=False)
                for sb in range(nsb):
                    at = actT[ai % 2]; ai += 1
                    for fc in range(8):
                        pg = psG[gi % 5]; gi += 1
                        pu = psG[gi % 5]; gi += 1
                        for which, pp in ((0, pg), (1, pu)):
                            for kc in range(8):
                                lhsT = wg[:, kc, :].rearrange("p (f two) -> p two f", two=2)[:, which, fc * 128:(fc + 1) * 128]
                                sc.op("pe", lambda: nc.tensor.matmul(pp[:, :], lhsT=lhsT, rhs=hT[:, kc, sb * 512:(sb + 1) * 512],
                                                                     start=(kc == 0), stop=(kc == 7)), reads=[wg, hT], writes=[pp], nowaw=(kc > 0))
                        g_, s_, u_ = G[ti % 2], Sg[ti % 2], U[ti % 2]
                        ti += 1
                        sc.op("dve", lambda: nc.vector.tensor_scalar(out=g_[:], in0=pg[:, :], scalar1=bg[:, fc:fc + 1], scalar2=7.0, op0=ALU.add, op1=ALU.min),
                              reads=[pg, bg], writes=[g_])
                        sc.op("act", lambda: nc.scalar.activation(out=s_[:], in_=g_[:], func=AF.Sigmoid, scale=1.702), reads=[g_], writes=[s_])
                        sc.op("dve", lambda: nc.vector.tensor_scalar(out=u_[:], in0=pu[:, :], scalar1=bg[:, 8 + fc:9 + fc], scalar2=7.0, op0=ALU.add, op1=ALU.min),
                              reads=[pu, bg], writes=[u_])
                        sc.op("pool", lambda: nc.gpsimd.tensor_scalar(out=u_[:], in0=u_[:], scalar1=-7.0, scalar2=1.0, op0=ALU.max, op1=ALU.add),
                              reads=[u_], writes=[u_])
                        sc.op("pool", lambda: nc.gpsimd.tensor_tensor(out=g_[:], in0=g_[:], in1=s_[:], op=ALU.mult), reads=[g_, s_], writes=[g_])
                        sc.op("pool", lambda: nc.gpsimd.tensor_tensor(out=at[:, fc, :], in0=g_[:], in1=u_[:], op=ALU.mult), reads=[g_, u_], writes=[at], nowaw=(fc > 0))
                    for tl in range(4):
                        tix = sb * 4 + tl
                        for hf in range(2):
                            pd = psD[di % 3]; di += 1
                            for fc in range(8):
                                sc.op("pe", lambda: nc.tensor.matmul(pd[:, :], lhsT=at[:, fc, tl * 128:(tl + 1) * 128], rhs=wd[:, fc, hf * 512:(hf + 1) * 512],
                                                                     start=(fc == 0), stop=(fc == 7)), reads=[at, wd], writes=[pd], nowaw=(fc > 0))
                            sc.op("dve", lambda: nc.vector.scalar_tensor_tensor(out=ACC[:, tix, hf * 512:(hf + 1) * 512], in0=pd[:, :], scalar=CWc[:, tix, e:e + 1],
                                                                               in1=ACC[:, tix, hf * 512:(hf + 1) * 512], op0=ALU.mult, op1=ALU.add),
                                  reads=[pd, CWc, ACC], writes=[ACC])
            sc.dma("sp", YACC[t0:t0 + CH, :].rearrange("(n p) d -> p n d", p=128), ACC[:], reads=[ACC], writes=[YACC], track=ACC)
        allb = [hT, ACC, CWc] + Wgu + Wd + BG + actT + G + Sg + U + psG + psD
        sc.barrier_on(allb)
        sched_release(sc, allb)


def phase_moe_final(sc, cfg, l, X1, YACC, G1P, ln_g, ln_b, XOUT):
    nc = sc.nc
    nseq, seq = cfg["nseq"], cfg["seq"]
    ntok = nseq * seq
    DN_ALPHA = (2 * DEPTH) ** 0.25
    with ExitStack() as es:
        Gt = sc.sbuf("fG", [128, 1024], F32, es)
        Bt = sc.sbuf("fB", [128, 1024], F32, es)
        sc.dma("sp", Gt[:], ln_g[l:l + 1, :].partition_broadcast(128), reads=[ln_g], writes=[Gt], track=Gt)
        sc.dma("sp", Bt[:], ln_b[l:l + 1, :].partition_broadcast(128), reads=[ln_b], writes=[Bt], track=Bt)
        XT = [sc.sbuf("fX", [128, 1024], F32, es) for _ in range(2)]
        YT = [sc.sbuf("fY", [128, 1024], F32, es) for _ in range(2)]
        Z = sc.sbuf("fZ", [128, 1024], F32, es)
        ZC = sc.sbuf("fZC", [128, 1024], F32, es)
        TMP = sc.sbuf("fT", [128, 1024], F32, es)
        O = [sc.sbuf("fO", [128, 1024], F32, es) for _ in range(2)]
        ST2 = sc.sbuf("fST", [128, 8], F32, es)
        for tt in range(ntok // 128):
            tok0 = tt * 128
            b = tok0 // seq
            xt, yt, o = XT[tt % 2], YT[tt % 2], O[tt % 2]
            sc.dma("sp", xt[:], X1[tok0:tok0 + 128, :], reads=[X1], writes=[xt], track=xt, nowaw=False)
            sc.dma("sp", yt[:], YACC[tok0:tok0 + 128, :], reads=[YACC], writes=[yt], track=yt, nowaw=False)
            sc.op("pool", lambda: nc.gpsimd.tensor_tensor(out=TMP[:], in0=yt[:], in1=G1P[:, b, :], op=ALU.mult), reads=[yt, G1P], writes=[TMP])
            sc.op("dve", lambda: nc.vector.scalar_tensor_tensor(out=Z[:], in0=xt[:], scalar=DN_ALPHA, in1=TMP[:], op0=ALU.mult, op1=ALU.add),
                  reads=[xt, TMP], writes=[Z])
            layer_norm_tile(sc, Z, ZC, TMP, o, ST2, Gt, Bt)
            sc.dma("sp", XOUT[tok0:tok0 + 128, :], o[:], reads=[o], writes=[XOUT], track=o)
        allb = [Gt, Bt, Z, ZC, TMP, ST2] + XT + YT + O
        sc.barrier_on(allb)
        sched_release(sc, allb)


W_SPECS = [("rel_bias", [32, 12]), ("mod_attn_w", [DEPTH, D, 3 * D]), ("mod_attn_b", [DEPTH, 3 * D]), ("w_in", [DEPTH, D, D_IN]),
           ("cmp_w1", [DEPTH, 2, 2048, 256]), ("cmp_w2", [DEPTH, 2, 256, 64]), ("diff_lambda", [DEPTH, 4, 64]),
           ("diff_norm_g", [DEPTH, 128]), ("w_branch_a", [DEPTH, 256, D]), ("w_branch_b", [DEPTH, 256, D]),
           ("w_branch_c", [DEPTH, 512, D]), ("w_out", [DEPTH, D, D]), ("ln1_g", [DEPTH, D]), ("ln1_b", [DEPTH, D]),
           ("mod_ffn_w", [DEPTH, D, 3 * D]), ("mod_ffn_b", [DEPTH, 3 * D]), ("router_w", [DEPTH, D, 32]), ("router_b", [DEPTH, 32]),
           ("exp_w_gu", [DEPTH, 32, D, 2 * D]), ("exp_w_down", [DEPTH, 32, D, D]), ("exp_b_down", [DEPTH, 32, D]),
           ("ln2_g", [DEPTH, D]), ("ln2_b", [DEPTH, D]),
           ("modbT", [DEPTH, 2, 128, 24]), ("cposT", [DEPTH, 128, 32]), ("b_guT", [DEPTH, 32, 128, 16])]


def build_program(cfg):
    nc = bass.Bass("TRN2", target_bir_lowering=False)
    nseq, seq = cfg["nseq"], cfg["seq"]
    ntok = nseq * seq
    depth = cfg.get("depth", DEPTH)
    with ExitStack() as es:
        sc = Sched(nc, es)
        sc.prof = bool(cfg.get("prof"))
        x = sc.dram("x", [ntok, D], F32, kind="ExternalInput")
        cT = sc.dram("cT", [128, 8, nseq], F32, kind="ExternalInput")
        Wd_ = {n: sc.dram(n, s, F32, kind="ExternalInput") for n, s in W_SPECS}
        cd = {n: sc.dram(n, s, d, kind="ExternalInput") for n, s, d in CONST_SPECS}
        y = sc.dram("y", [ntok, D], F32, kind="ExternalOutput")
        FM = sc.dram("FM", [FM_ROWS, ntok], BF16)
        VV = sc.dram("VV", [ntok, VV_W], BF16)
        SM = sc.dram("SM", [ntok, 16], F32)
        MG = sc.dram("MG", [ntok, 3072], F32)
        OABC = sc.dram("OABC", [ntok, 1024], F32)
        X1 = sc.dram("X1", [ntok, D], F32)
        XL = sc.dram("XL", [ntok, D], F32)
        H2T = sc.dram("H2T", [D, ntok], BF16)
        CWD = sc.dram("CWD", [ntok, 32], F32)
        YACC = sc.dram("YACC", [ntok, D], F32)
        Cap = moe_capacity(ntok)
        Xg = sc.dram("Xg", [32 * Cap + 128, D], BF16)
        Yg = sc.dram("Yg", [32 * Cap + 128, D], F32)
        DSTD = sc.dram("DSTD", [ntok, 4], I32)
        CWKD = sc.dram("CWKD", [ntok, 4], F32)
        K = setup_attn_consts(sc, cd, Wd_["rel_bias"], es)
        siluT = sc.sbuf("siluT", [128, 8, nseq], F32)
        sc.dma("sp", siluT[:], cT[:, :, :], reads=[cT], writes=[siluT], track=siluT)
        sc.op("act", lambda: nc.scalar.activation(out=siluT[:], in_=siluT[:], func=AF.Silu), reads=[siluT], writes=[siluT])
        opsc = sc.sbuf("opsc", [128, 8, nseq], F32)
        shf = sc.sbuf("shf", [128, 8, nseq], F32)
        G1P = sc.sbuf("G1P", [128, nseq, 1024], F32)
        KC = sc.sbuf("KC", [64, nseq, 256], BF16)
        VC = sc.sbuf("VC", [128, nseq, 2, 64], BF16)
        xin = x
        for l in range(depth):
            lam_init = 0.8 - 0.6 * math.exp(-0.3 * l)
            xout = y if l == depth - 1 else XL
            sc.mark("L%d_phase_adaln" % l)
            phase_adaln(sc, cfg, l, Wd_["mod_attn_w"], Wd_["modbT"], Wd_["mod_attn_b"], 0, siluT, opsc, shf, None, "fm")
            sc.mark("L%d_phase_proj" % l)
            phase_proj(sc, cfg, l, xin, Wd_["w_in"], K["ident"], opsc, shf, FM, VV, SM, MG)
            sc.mark("L%d_phase_compress" % l)
            phase_compress(sc, cfg, l, FM, Wd_["cmp_w1"], Wd_["cmp_w2"], Wd_["cposT"], KC, VC)
            sc.mark("L%d_phase_attn" % l)
            phase_attn(sc, cfg, l, K, FM, VV, SM, KC, VC, cd["c_sel"], Wd_["diff_lambda"], Wd_["diff_norm_g"], lam_init, OABC)
            sc.mark("L%d_phase_adaln" % l)
            phase_adaln(sc, cfg, l, Wd_["mod_attn_w"], Wd_["modbT"], Wd_["mod_attn_b"], 0, siluT, None, None, G1P, "gate")
            sc.mark("L%d_phase_tail" % l)
            phase_tail(sc, cfg, l, K, OABC, MG, xin, Wd_["w_branch_a"], Wd_["w_branch_b"], Wd_["w_branch_c"], Wd_["w_out"],
                       Wd_["ln1_g"], Wd_["ln1_b"], G1P, X1)
            sc.mark("L%d_phase_adaln" % l)
            phase_adaln(sc, cfg, l, Wd_["mod_ffn_w"], Wd_["modbT"], Wd_["mod_ffn_b"], 1, siluT, opsc, shf, None, "fm")
            if cfg.get("dense_moe"):
                sc.mark("L%d_phase_moe_prep" % l)
                phase_moe_prep(sc, cfg, l, K, X1, opsc, shf, Wd_["router_w"], Wd_["router_b"], Wd_["exp_b_down"], H2T, CWD, YACC)
                sc.mark("L%d_phase_moe_experts" % l)
                phase_moe_experts(sc, cfg, l, H2T, CWD, YACC, Wd_["exp_w_gu"], Wd_["b_guT"], Wd_["exp_w_down"])
                sc.mark("L%d_phase_adaln" % l)
                phase_adaln(sc, cfg, l, Wd_["mod_ffn_w"], Wd_["modbT"], Wd_["mod_ffn_b"], 1, siluT, None, None, G1P, "gate")
                sc.mark("L%d_phase_moe_final" % l)
                phase_moe_final(sc, cfg, l, X1, YACC, G1P, Wd_["ln2_g"], Wd_["ln2_b"], xout)
            else:
                with ExitStack() as mes:
                    OPTM = sc.sbuf("OPTM", [128, nseq, 1024], F32, mes)
                    SHTM = sc.sbuf("SHTM", [128, nseq, 1024], F32, mes)
                    sc.mark("L%d_phase_adaln" % l)
                    phase_adaln(sc, cfg, l, Wd_["mod_ffn_w"], Wd_["modbT"], Wd_["mod_ffn_b"], 1, siluT, None, None, OPTM, "gate", tm_third=1, add_one=1.0)
                    sc.mark("L%d_phase_adaln" % l)
                    phase_adaln(sc, cfg, l, Wd_["mod_ffn_w"], Wd_["modbT"], Wd_["mod_ffn_b"], 1, siluT, None, None, SHTM, "gate", tm_third=0, add_one=0.0)
                    sc.mark("L%d_phase_moe_prep2" % l)
                    phase_moe_prep2(sc, cfg, l, K, X1, opsc, shf, OPTM, SHTM, Wd_["router_w"], Wd_["router_b"], Wd_["exp_b_down"],
                                    Xg, DSTD, CWKD, YACC, cd)
                    sc.barrier_on([OPTM, SHTM])
                sc.mark("L%d_phase_moe_experts2" % l)
                phase_moe_experts2(sc, cfg, l, K, Xg, Yg, Wd_["exp_w_gu"], Wd_["b_guT"], Wd_["exp_w_down"])
                sc.mark("L%d_phase_adaln" % l)
                phase_adaln(sc, cfg, l, Wd_["mod_ffn_w"], Wd_["modbT"], Wd_["mod_ffn_b"], 1, siluT, None, None, G1P, "gate")
                sc.mark("L%d_phase_moe_final2" % l)
                phase_moe_final2(sc, cfg, l, X1, YACC, Yg, DSTD, CWKD, G1P, Wd_["ln2_g"], Wd_["ln2_b"], xout)
            xin = xout
        sc.mark("end")
        sc.finish([y])
        cfg["_n_inst"] = sc.n_inst
    return nc


def host_layouts(inp):
    L = DEPTH
    out = {}
    out["modbT"] = np.ascontiguousarray(np.stack([np.stack([inp["mod_attn_b"][l].reshape(24, 128).T,
                                                            inp["mod_ffn_b"][l].reshape(24, 128).T]) for l in range(L)]), dtype=np.float32)
    out["cposT"] = np.ascontiguousarray(np.stack([np.concatenate([inp["cmp_pos"][l, 0].T, inp["cmp_pos"][l, 1].T], 0) for l in range(L)]), dtype=np.float32)
    bg = inp["exp_b_gu"].reshape(L, 32, 8, 128, 2)
    out["b_guT"] = np.ascontiguousarray(bg.transpose(0, 1, 3, 4, 2).reshape(L, 32, 128, 16), dtype=np.float32)
    return out


def run_module(inp, cfg, n_cores):
    nseq, seq = cfg["nseq"], cfg["seq"]
    nc = build_program(cfg)
    shared = {n: np.ascontiguousarray(inp[n], dtype=np.float32) for n, _ in W_SPECS if n in inp}
    shared.update(host_layouts(inp))
    shared.update(make_consts())
    x = np.asarray(inp["x"], dtype=np.float32)
    c = np.asarray(inp["c"], dtype=np.float32)
    in_maps = []
    for core in range(n_cores):
        xs = x[core * nseq:(core + 1) * nseq].reshape(nseq * seq, D)
        cs = c[core * nseq:(core + 1) * nseq]
        cT = np.ascontiguousarray(cs.reshape(nseq, 8, 128).transpose(2, 1, 0))
        m = dict(shared)
        m["x"] = np.ascontiguousarray(xs)
        m["cT"] = cT
        in_maps.append(m)
    res = run_bass_kernel_spmd(nc, in_maps, core_ids=list(range(n_cores)))
    outs = [r["y"].reshape(nseq, seq, D) for r in res.results]
    return np.concatenate(outs, axis=0).astype(np.float32)


def kernel(**inputs):
    cfg = dict(nseq=NSEQ, seq=S)
    return run_module(inputs, cfg, 8)


def moe_capacity(ntok):
    c = (15 * ntok) // 64
    return ((c + 127) // 128) * 128


def phase_moe_prep2(sc, cfg, l, K, X1, opsc, shf, OPTM, SHTM, router_w, router_b, b_down, Xg, DSTD, CWKD, YACC, cd):
    nc = sc.nc
    nseq, seq = cfg["nseq"], cfg["seq"]
    ntok = nseq * seq
    C = moe_capacity(ntok)
    with ExitStack() as es:
        rw = sc.sbuf("rw", [128, 8, 32], F32, es)
        rb = sc.sbuf("rb", [128, 32], F32, es)
        bd = sc.sbuf("bd", [32, 1024], F32, es)
        UT = sc.sbuf("UT", [128, 128], BF16, es)
        ONES = sc.sbuf("ONES", [128, 128], BF16, es)
        EOFF = sc.sbuf("EOFF", [128, 32], F32, es)
        BASE = sc.sbuf("BASE", [128, 32], F32, es)
        sc.dma("sp", rw[:], router_w[l].rearrange("(c p) e -> p c e", p=128), reads=[router_w], writes=[rw], track=rw)
        sc.dma("sp", rb[:], router_b[l:l + 1, :].partition_broadcast(128), reads=[router_b], writes=[rb], track=rb)
        sc.dma("sp", bd[:], b_down[l], reads=[b_down], writes=[bd], track=bd)
        sc.dma("pool", UT[:], cd["c_ut"][:, :], reads=[cd["c_ut"]], writes=[UT], track=UT)
        sc.op("dve", lambda: nc.vector.memset(ONES[:], 1.0), writes=[ONES])
        sc.op("dve", lambda: nc.vector.memset(BASE[:], 0.0), writes=[BASE])
        sc.op("dve", lambda: nc.vector.tensor_scalar(out=EOFF[:], in0=K["iota"][:, 0:32], scalar1=float(C), scalar2=-1.0, op0=ALU.mult, op1=ALU.add),
              reads=[K["iota"]], writes=[EOFF])
        XT = [sc.sbuf("mX", [128, 1024], F32, es) for _ in range(2)]
        HF_2 = [sc.sbuf("mHF", [128, 8, 128], F32, es) for _ in range(2)]
        HT_2 = [sc.sbuf("mHT", [128, 1024], F32, es) for _ in range(2)]
        HBt = [sc.sbuf("mHBt", [128, 1024], BF16, es) for _ in range(2)]
        Lg_2 = [sc.sbuf("mL", [128, 32], F32, es) for _ in range(2)]
        J3_2 = [sc.sbuf("mJ3", [128, 32, 32], F32, es) for _ in range(2)]
        RK_2 = [sc.sbuf("mRK", [128, 32], F32, es) for _ in range(2)]
        Mk_2 = [sc.sbuf("mMk", [128, 32], F32, es) for _ in range(2)]
        Mb_2 = [sc.sbuf("mMb", [128, 32], BF16, es) for _ in range(2)]
        EX_2 = [sc.sbuf("mEX", [128, 32], F32, es) for _ in range(2)]
        CW_2 = [sc.sbuf("mCW", [128, 32], F32, es) for _ in range(2)]
        ROW_2 = [sc.sbuf("mROW", [128, 32], F32, es) for _ in range(2)]
        POS_2 = [sc.sbuf("mPOS", [128, 32], F32, es) for _ in range(2)]
        JK_2 = [sc.sbuf("mJK", [128, 32], F32, es) for _ in range(2)]
        DSTf_2 = [sc.sbuf("mDSTf", [128, 4], F32, es) for _ in range(2)]
        DSTi = [sc.sbuf("mDSTi", [128, 4], I32, es) for _ in range(2)]
        CWK = [sc.sbuf("mCWK", [128, 4], F32, es) for _ in range(2)]
        CWT_2 = [sc.sbuf("mCWT", [32, 128], F32, es) for _ in range(2)]
        YB = [sc.sbuf("mYB", [128, 1024], F32, es) for _ in range(2)]
        ST_2 = [sc.sbuf("mST", [128, 8], F32, es) for _ in range(2)]
        ps = [sc.psum("mps", [128, 512], F32, es) for _ in range(6)]
        pi = 0
        for tt in range(ntok // 128):
            HF = HF_2[tt % 2]
            HT = HT_2[tt % 2]
            Lg = Lg_2[tt % 2]
            J3 = J3_2[tt % 2]
            RK = RK_2[tt % 2]
            Mk = Mk_2[tt % 2]
            Mb = Mb_2[tt % 2]
            EX = EX_2[tt % 2]
            CW = CW_2[tt % 2]
            ROW = ROW_2[tt % 2]
            POS = POS_2[tt % 2]
            JK = JK_2[tt % 2]
            DSTf = DSTf_2[tt % 2]
            CWT = CWT_2[tt % 2]
            ST = ST_2[tt % 2]
            tok0 = tt * 128
            b = tok0 // seq
            xt, hbt, dsti, cwk, yb = XT[tt % 2], HBt[tt % 2], DSTi[tt % 2], CWK[tt % 2], YB[tt % 2]
            sc.dma("sp", xt[:], X1[tok0:tok0 + 128, :], reads=[X1], writes=[xt], track=xt, nowaw=False)
            sc.op("pool", lambda: nc.gpsimd.tensor_tensor(out=HT[:], in0=xt[:], in1=OPTM[:, b, :], op=ALU.mult), reads=[xt, OPTM], writes=[HT])
            sc.op("pool", lambda: nc.gpsimd.tensor_tensor(out=hbt[:], in0=HT[:], in1=SHTM[:, b, :], op=ALU.add), reads=[HT, SHTM], writes=[hbt])
            for g in range(2):
                p = ps[pi % 6]; pi += 1
                for c4 in range(4):
                    c = g * 4 + c4
                    sc.op("pe", lambda: nc.tensor.transpose(p[:, c4 * 128:(c4 + 1) * 128], xt[:, c * 128:(c + 1) * 128], K["ident"][:]),
                          reads=[xt, K["ident"]], writes=[p], nowaw=(c4 > 0))
                for c4 in range(4):
                    c = g * 4 + c4
                    sc.op("act", lambda: nc.scalar.activation(out=HF[:, c, :], in_=p[:, c4 * 128:(c4 + 1) * 128], func=AF.Identity,
                                                              bias=shf[:, c, b:b + 1], scale=opsc[:, c, b:b + 1]),
                          reads=[p, shf, opsc], writes=[HF], nowaw=(c > 0))
            p = ps[pi % 6]; pi += 1
            for c in range(8):
                sc.op("pe", lambda: nc.tensor.matmul(p[:, 0:32], lhsT=HF[:, c, :], rhs=rw[:, c, :], start=(c == 0), stop=(c == 7)),
                      reads=[HF, rw], writes=[p], nowaw=(c > 0))
            sc.op("dve", lambda: nc.vector.tensor_tensor(out=Lg[:], in0=p[:, 0:32], in1=rb[:], op=ALU.add), reads=[p, rb], writes=[Lg])
            sc.op("dve", lambda: nc.vector.tensor_tensor(out=J3[:], in0=Lg[:].unsqueeze(1).to_broadcast([128, 32, 32]),
                                                        in1=Lg[:].unsqueeze(2).to_broadcast([128, 32, 32]), op=ALU.is_gt), reads=[Lg], writes=[J3])
            sc.op("dve", lambda: nc.vector.tensor_reduce(out=RK[:], in_=J3[:], axis=AX.X, op=ALU.add), reads=[J3], writes=[RK])
            sc.op("dve", lambda: nc.vector.tensor_reduce(out=ST[:, 0:1], in_=Lg[:], axis=AX.X, op=ALU.max), reads=[Lg], writes=[ST])
            sc.op("dve", lambda: nc.vector.tensor_scalar(out=ST[:, 1:2], in0=ST[:, 0:1], scalar1=-1.0, scalar2=None, op0=ALU.mult), reads=[ST], writes=[ST])
            sc.op("act", lambda: nc.scalar.activation(out=EX[:], in_=Lg[:], func=AF.Exp, bias=ST[:, 1:2]), reads=[Lg, ST], writes=[EX])
            sc.op("dve", lambda: nc.vector.tensor_scalar(out=Mk[:], in0=RK[:], scalar1=3.5, scalar2=None, op0=ALU.is_lt), reads=[RK], writes=[Mk])
            sc.op("dve", lambda: nc.vector.scalar_tensor_tensor(out=EX[:], in0=EX[:], scalar=1.0, in1=Mk[:], op0=ALU.mult, op1=ALU.mult,
                                                               accum_out=ST[:, 2:3]), reads=[EX, Mk], writes=[EX, ST])
            sc.op("dve", lambda: nc.vector.reciprocal(out=ST[:, 3:4], in_=ST[:, 2:3]), reads=[ST], writes=[ST])
            sc.op("dve", lambda: nc.vector.tensor_scalar(out=CW[:], in0=EX[:], scalar1=ST[:, 3:4], scalar2=None, op0=ALU.mult), reads=[EX, ST], writes=[CW])
            sc.op("dve", lambda: nc.vector.tensor_copy(Mb[:], Mk[:]), reads=[Mk], writes=[Mb])
            pc = ps[pi % 6]; pi += 1
            sc.op("pe", lambda: nc.tensor.matmul(pc[:, 0:32], lhsT=UT[:], rhs=Mb[:], start=True, stop=True), reads=[UT, Mb], writes=[pc])
            ptot = ps[pi % 6]; pi += 1
            sc.op("pe", lambda: nc.tensor.matmul(ptot[:, 0:32], lhsT=ONES[:], rhs=Mb[:], start=True, stop=True), reads=[ONES, Mb], writes=[ptot])
            sc.op("dve", lambda: nc.vector.tensor_tensor(out=POS[:], in0=pc[:, 0:32], in1=BASE[:], op=ALU.add), reads=[pc, BASE], writes=[POS])
            sc.op("dve", lambda: nc.vector.tensor_tensor(out=ROW[:], in0=POS[:], in1=EOFF[:], op=ALU.add), reads=[POS, EOFF], writes=[ROW])
            sc.op("dve", lambda: nc.vector.tensor_scalar(out=POS[:], in0=POS[:], scalar1=float(C) + 0.5, scalar2=1e9, op0=ALU.is_gt, op1=ALU.mult),
                  reads=[POS], writes=[POS])
            sc.op("dve", lambda: nc.vector.tensor_tensor(out=ROW[:], in0=ROW[:], in1=POS[:], op=ALU.add), reads=[ROW, POS], writes=[ROW])
            sc.op("dve", lambda: nc.vector.tensor_scalar(out=ROW[:], in0=ROW[:], scalar1=float(32 * C), scalar2=None, op0=ALU.min), reads=[ROW], writes=[ROW])
            sc.op("dve", lambda: nc.vector.tensor_tensor(out=BASE[:], in0=BASE[:], in1=ptot[:, 0:32], op=ALU.add), reads=[BASE, ptot], writes=[BASE])
            for k in range(4):
                sc.op("dve", lambda: nc.vector.scalar_tensor_tensor(out=JK[:], in0=RK[:], scalar=float(k), in1=ROW[:], op0=ALU.is_equal, op1=ALU.mult,
                                                                   accum_out=DSTf[:, k:k + 1]), reads=[RK, ROW], writes=[JK, DSTf])
                sc.op("dve", lambda: nc.vector.scalar_tensor_tensor(out=JK[:], in0=RK[:], scalar=float(k), in1=CW[:], op0=ALU.is_equal, op1=ALU.mult,
                                                                   accum_out=cwk[:, k:k + 1]), reads=[RK, CW], writes=[JK, cwk])
            sc.op("dve", lambda: nc.vector.tensor_copy(dsti[:], DSTf[:]), reads=[DSTf], writes=[dsti])
            sc.dma("sp", DSTD[tok0:tok0 + 128, :], dsti[:], reads=[dsti], writes=[DSTD], track=dsti)
            sc.dma("sp", CWKD[tok0:tok0 + 128, :], cwk[:], reads=[cwk], writes=[CWKD], track=cwk)
            for k in range(4):
                deps_r, deps_w = [hbt, dsti], [Xg]
                d = sc._collect(deps_r, deps_w, True)
                sc._wait("pool", d)
                if hbt.dsem is None:
                    if sc.sempool:
                        hbt.dsem, hbt.dcount = sc.sempool.pop()
                    else:
                        hbt.dsem = sc.es.enter_context(nc.semaphore("d_" + hbt.name))
                ins = nc.gpsimd.indirect_dma_start(out=Xg[:, :], out_offset=bass.IndirectOffsetOnAxis(ap=dsti[:, k:k + 1], axis=0),
                                                   in_=hbt[:, :], in_offset=None)
                ins.then_inc(hbt.dsem, 16)
                hbt.dcount += 1
                for bb in deps_r:
                    bb.rd[hbt] = hbt.dcount
                Xg.wr[hbt] = hbt.dcount
            p = ps[pi % 6]; pi += 1
            sc.op("pe", lambda: nc.tensor.transpose(p[:32, 0:128], CW[:, :], K["ident"][:]), reads=[CW, K["ident"]], writes=[p])
            sc.op("act", lambda: nc.scalar.copy(CWT[:], p[:32, 0:128]), reads=[p], writes=[CWT])
            for hf in range(2):
                p = ps[pi % 6]; pi += 1
                sc.op("pe", lambda: nc.tensor.matmul(p[:, :], lhsT=CWT[:, :], rhs=bd[:, hf * 512:(hf + 1) * 512], start=True, stop=True),
                      reads=[CWT, bd], writes=[p])
                sc.op("act", lambda: nc.scalar.copy(yb[:, hf * 512:(hf + 1) * 512], p[:, :]), reads=[p], writes=[yb], nowaw=(hf > 0))
            sc.dma("sp", YACC[tok0:tok0 + 128, :], yb[:], reads=[yb], writes=[YACC], track=yb)
        allb = [rw, rb, bd, UT, ONES, EOFF, BASE, HF_2[0], HF_2[1], HT_2[0], HT_2[1], Lg_2[0], Lg_2[1], J3_2[0], J3_2[1], RK_2[0], RK_2[1], Mk_2[0], Mk_2[1], Mb_2[0], Mb_2[1], EX_2[0], EX_2[1], CW_2[0], CW_2[1], ROW_2[0], ROW_2[1], POS_2[0], POS_2[1], JK_2[0], JK_2[1], DSTf_2[0], DSTf_2[1], CWT_2[0], CWT_2[1], ST_2[0], ST_2[1]] + XT + HBt + DSTi + CWK + YB + ps
        sc.barrier_on(allb)
        sched_release(sc, allb)


def phase_moe_experts2(sc, cfg, l, K, Xg, Yg, w_gu, b_guT, w_down):
    nc = sc.nc
    nseq, seq = cfg["nseq"], cfg["seq"]
    ntok = nseq * seq
    C = moe_capacity(ntok)
    sbs = [(o, min(4, C // 128 - o)) for o in range(0, C // 128, 4)]
    ne = cfg.get("n_exp", 32)
    with ExitStack() as es:
        Wgu = [sc.sbuf("eWgu", [128, 8, 2048], BF16, es) for _ in range(2)]
        Wd = [sc.sbuf("eWd", [128, 8, 1024], BF16, es) for _ in range(2)]
        BG = [sc.sbuf("eBG", [128, 16], F32, es) for _ in range(2)]
        XR = [sc.sbuf("eXR", [128, 4, 1024], BF16, es) for _ in range(2)]
        XT = [sc.sbuf("eXT", [128, 8, 512], BF16, es) for _ in range(2)]
        actT = [sc.sbuf("eact", [128, 8, 512], BF16, es) for _ in range(2)]
        G = [sc.sbuf("eG", [128, 512], F32, es) for _ in range(2)]
        Sg = [sc.sbuf("eS", [128, 512], F32, es) for _ in range(2)]
        U = [sc.sbuf("eU", [128, 512], F32, es) for _ in range(2)]
        YO = [sc.sbuf("eYO", [128, 1024], F32, es) for _ in range(3)]
        psG = [sc.psum("epsG", [128, 512], F32, es) for _ in range(4)]
        psD = [sc.psum("epsD", [128, 512], F32, es) for _ in range(2)]
        psT = [sc.psum("epsT", [128, 8, 128], BF16, es) for _ in range(2)]
        gi = di = ai = ti = xi = yi = pti = 0
        def load_w(e):
            wg, wd, bg = Wgu[e % 2], Wd[e % 2], BG[e % 2]
            for c2 in range(0, 8, 2):
                sc.dma("pool", wg[:, c2:c2 + 2, :], w_gu[l, e, c2 * 128:(c2 + 2) * 128, :].rearrange("(c p) n -> p c n", p=128),
                       reads=[w_gu], writes=[wg], track=wg, nowaw=(c2 > 0))
            for c4 in range(0, 8, 4):
                sc.dma("pool", wd[:, c4:c4 + 4, :], w_down[l, e, c4 * 128:(c4 + 4) * 128, :].rearrange("(c p) n -> p c n", p=128),
                       reads=[w_down], writes=[wd], track=wd, nowaw=(c4 > 0))
            sc.dma("sp", bg[:], b_guT[l, e], reads=[b_guT], writes=[bg], track=bg, nowaw=False)
        load_w(0)
        for e in range(ne):
            wg, wd, bg = Wgu[e % 2], Wd[e % 2], BG[e % 2]
            if e + 1 < ne:
                load_w(e + 1)
            for (tl0, ntl) in sbs:
                r0 = e * C + tl0 * 128
                wdt = ntl * 128
                xr, xt_, at = XR[xi % 2], XT[xi % 2], actT[xi % 2]
                xi += 1
                sc.dma("sp", xr[:, 0:ntl, :], Xg[r0:r0 + wdt, :].rearrange("(n p) d -> p n d", p=128), reads=[Xg], writes=[xr], track=xr, nowaw=False)
                for tl in range(ntl):
                    pt = psT[pti % 2]; pti += 1
                    for c in range(8):
                        sc.op("pe", lambda: nc.tensor.transpose(pt[:, c, :], xr[:, tl, c * 128:(c + 1) * 128], K["identb"][:]),
                              reads=[xr, K["identb"]], writes=[pt], nowaw=(c > 0))
                    if tl % 2 == 0:
                        sc.op("act", lambda: nc.scalar.copy(xt_[:, :, tl * 128:(tl + 1) * 128], pt[:]), reads=[pt], writes=[xt_], nowaw=(tl > 0))
                    else:
                        sc.op("dve", lambda: nc.vector.tensor_copy(xt_[:, :, tl * 128:(tl + 1) * 128], pt[:]), reads=[pt], writes=[xt_], nowaw=True)
                for fc in range(8):
                    pg = psG[gi % 4]; gi += 1
                    pu = psG[gi % 4]; gi += 1
                    for which, pp in ((0, pg), (1, pu)):
                        for kc in range(8):
                            lhsT = wg[:, kc, :].rearrange("p (f two) -> p two f", two=2)[:, which, fc * 128:(fc + 1) * 128]
                            sc.op("pe", lambda: nc.tensor.matmul(pp[:, :wdt], lhsT=lhsT, rhs=xt_[:, kc, :wdt], start=(kc == 0), stop=(kc == 7)),
                                  reads=[wg, xt_], writes=[pp], nowaw=(kc > 0))
                    g_, s_, u_ = G[ti % 2], Sg[ti % 2], U[ti % 2]
                    ti += 1
                    sc.op("dve", lambda: nc.vector.tensor_scalar(out=g_[:, :wdt], in0=pg[:, :wdt], scalar1=bg[:, fc:fc + 1], scalar2=7.0, op0=ALU.add, op1=ALU.min),
                          reads=[pg, bg], writes=[g_])
                    sc.op("act", lambda: nc.scalar.activation(out=s_[:, :wdt], in_=g_[:, :wdt], func=AF.Sigmoid, scale=1.702), reads=[g_], writes=[s_])
                    sc.op("dve", lambda: nc.vector.tensor_scalar(out=u_[:, :wdt], in0=pu[:, :wdt], scalar1=bg[:, 8 + fc:9 + fc], scalar2=7.0, op0=ALU.add, op1=ALU.min),
                          reads=[pu, bg], writes=[u_])
                    sc.op("dve", lambda: nc.vector.tensor_scalar(out=u_[:, :wdt], in0=u_[:, :wdt], scalar1=-7.0, scalar2=1.0, op0=ALU.max, op1=ALU.add),
                          reads=[u_], writes=[u_])
                    sc.op("dve", lambda: nc.vector.tensor_tensor(out=g_[:, :wdt], in0=g_[:, :wdt], in1=s_[:, :wdt], op=ALU.mult), reads=[g_, s_], writes=[g_])
                    sc.op("dve", lambda: nc.vector.tensor_tensor(out=at[:, fc, :wdt], in0=g_[:, :wdt], in1=u_[:, :wdt], op=ALU.mult), reads=[g_, u_], writes=[at], nowaw=(fc > 0))
                for tl in range(ntl):
                    yo = YO[yi % 3]; yi += 1
                    for hf in range(2):
                        pd = psD[di % 2]; di += 1
                        for fc in range(8):
                            sc.op("pe", lambda: nc.tensor.matmul(pd[:, :], lhsT=at[:, fc, tl * 128:(tl + 1) * 128], rhs=wd[:, fc, hf * 512:(hf + 1) * 512],
                                                                 start=(fc == 0), stop=(fc == 7)), reads=[at, wd], writes=[pd], nowaw=(fc > 0))
                        sc.op("act", lambda: nc.scalar.copy(yo[:, hf * 512:(hf + 1) * 512], pd[:, :]), reads=[pd], writes=[yo], nowaw=(hf > 0))
                    sc.dma("sp", Yg[r0 + tl * 128:r0 + (tl + 1) * 128, :], yo[:], reads=[yo], writes=[Yg], track=yo)
        allb = Wgu + Wd + BG + XR + XT + actT + G + Sg + U + YO + psG + psD + psT
        sc.barrier_on(allb)
        sched_release(sc, allb)


def phase_moe_final2(sc, cfg, l, X1, YACC, Yg, DSTD, CWKD, G1P, ln_g, ln_b, XOUT):
    nc = sc.nc
    nseq, seq = cfg["nseq"], cfg["seq"]
    ntok = nseq * seq
    C = moe_capacity(ntok)
    DN_ALPHA = (2 * DEPTH) ** 0.25
    with ExitStack() as es:
        Gt = sc.sbuf("fG", [128, 1024], F32, es)
        Bt = sc.sbuf("fB", [128, 1024], F32, es)
        sc.dma("sp", Gt[:], ln_g[l:l + 1, :].partition_broadcast(128), reads=[ln_g], writes=[Gt], track=Gt)
        sc.dma("sp", Bt[:], ln_b[l:l + 1, :].partition_broadcast(128), reads=[ln_b], writes=[Bt], track=Bt)
        XT = [sc.sbuf("fX", [128, 1024], F32, es) for _ in range(2)]
        YT = [sc.sbuf("fY", [128, 1024], F32, es) for _ in range(2)]
        YK = [sc.sbuf("fYK", [128, 4, 1024], F32, es) for _ in range(2)]
        DSTi = [sc.sbuf("fDST", [128, 4], I32, es) for _ in range(2)]
        CWK = [sc.sbuf("fCWK", [128, 4], F32, es) for _ in range(2)]
        Z_2 = [sc.sbuf("fZ", [128, 1024], F32, es) for _ in range(2)]
        ZC_2 = [sc.sbuf("fZC", [128, 1024], F32, es) for _ in range(2)]
        TMP_2 = [sc.sbuf("fT", [128, 1024], F32, es) for _ in range(2)]
        O = [sc.sbuf("fO", [128, 1024], F32, es) for _ in range(2)]
        ST2_2 = [sc.sbuf("fST", [128, 8], F32, es) for _ in range(2)]
        for tt in range(ntok // 128):
            Z = Z_2[tt % 2]
            ZC = ZC_2[tt % 2]
            TMP = TMP_2[tt % 2]
            ST2 = ST2_2[tt % 2]
            tok0 = tt * 128
            b = tok0 // seq
            xt, yt, yk, dsti, cwk, o = XT[tt % 2], YT[tt % 2], YK[tt % 2], DSTi[tt % 2], CWK[tt % 2], O[tt % 2]
            sc.dma("sp", xt[:], X1[tok0:tok0 + 128, :], reads=[X1], writes=[xt], track=xt, nowaw=False)
            sc.dma("sp", yt[:], YACC[tok0:tok0 + 128, :], reads=[YACC], writes=[yt], track=yt, nowaw=False)
            sc.dma("sp", dsti[:], DSTD[tok0:tok0 + 128, :], reads=[DSTD], writes=[dsti], track=dsti, nowaw=False)
            sc.dma("sp", cwk[:], CWKD[tok0:tok0 + 128, :], reads=[CWKD], writes=[cwk], track=cwk, nowaw=False)
            for k in range(4):
                d = sc._collect([Yg, dsti], [yk], k > 0)
                sc._wait("pool", d)
                if yk.dsem is None:
                    if sc.sempool:
                        yk.dsem, yk.dcount = sc.sempool.pop()
                    else:
                        yk.dsem = sc.es.enter_context(nc.semaphore("d_" + yk.name))
                ins = nc.gpsimd.indirect_dma_start(out=yk[:, k, :], out_offset=None, in_=Yg[:, :],
                                                   in_offset=bass.IndirectOffsetOnAxis(ap=dsti[:, k:k + 1], axis=0))
                ins.then_inc(yk.dsem, 16)
                yk.dcount += 1
                for bb in (Yg, dsti):
                    bb.rd[yk] = yk.dcount
                if k == 0:
                    yk.wr = {yk: yk.dcount}
                    yk.rd = {}
                else:
                    yk.wr[yk] = yk.dcount
            for k in range(4):
                sc.op("dve", lambda: nc.vector.scalar_tensor_tensor(out=yt[:], in0=yk[:, k, :], scalar=cwk[:, k:k + 1], in1=yt[:], op0=ALU.mult, op1=ALU.add),
                      reads=[yk, cwk, yt], writes=[yt])
            sc.op("pool", lambda: nc.gpsimd.tensor_tensor(out=TMP[:], in0=yt[:], in1=G1P[:, b, :], op=ALU.mult), reads=[yt, G1P], writes=[TMP])
            sc.op("dve", lambda: nc.vector.scalar_tensor_tensor(out=Z[:], in0=xt[:], scalar=DN_ALPHA, in1=TMP[:], op0=ALU.mult, op1=ALU.add),
                  reads=[xt, TMP], writes=[Z])
            layer_norm_tile(sc, Z, ZC, TMP, o, ST2, Gt, Bt)
            sc.dma("sp", XOUT[tok0:tok0 + 128, :], o[:], reads=[o], writes=[XOUT], track=o)
        allb = [Gt, Bt, Z_2[0], Z_2[1], ZC_2[0], ZC_2[1], TMP_2[0], TMP_2[1], ST2_2[0], ST2_2[1]] + XT + YT + YK + DSTi + CWK + O
        sc.barrier_on(allb)
        sched_release(sc, allb)
```

```python
import math
from contextlib import ExitStack
import numpy as np
import concourse.bass as bass
import concourse.mybir as mybir
from concourse.bass_utils import run_bass_kernel_spmd

F32 = mybir.dt.float32
BF16 = mybir.dt.bfloat16
I32 = mybir.dt.int32
AF = mybir.ActivationFunctionType
ALU = mybir.AluOpType
AX = mybir.AxisListType

D = 1024
S = 4096
NSEQ = 2
T = NSEQ * S
DEPTH = 2
D_IN = 5808
NEG = -30000.0


class Buf:
    def __init__(self, name, t):
        self.name = name
        self.t = t
        self.wr = {}
        self.rd = {}
        self.dsem = None
        self.dcount = 0

    def __getitem__(self, idx):
        return self.t[idx]


class Sched:
    def __init__(self, nc, es):
        self.nc = nc
        self.es = es
        self.eng = {"pe": nc.tensor, "dve": nc.vector, "act": nc.scalar,
                    "pool": nc.gpsimd, "sp": nc.sync}
        self.sem = {}
        self.cnt = {}
        for k in ("pe", "dve", "act", "pool"):
            self.sem[k] = es.enter_context(nc.semaphore("s_" + k))
            self.cnt[k] = 0
        self.known = {k: {} for k in self.eng}
        self.nbuf = 0
        self.n_inst = 0
        self.sempool = []

    def sbuf(self, name, shape, dt, es=None):
        es = es or self.es
        self.nbuf += 1
        name = "%s_%d" % (name, self.nbuf)
        t = es.enter_context(self.nc.sbuf_tensor(name, list(shape), dt))
        b = Buf(name, t)
        b.es = es
        return b

    def psum(self, name, shape, dt=F32, es=None):
        es = es or self.es
        self.nbuf += 1
        name = "%s_%d" % (name, self.nbuf)
        t = es.enter_context(self.nc.psum_tensor(name, list(shape), dt))
        b = Buf(name, t)
        b.is_psum = True
        b.es = es
        return b

    def dram(self, name, shape, dt, kind=None):
        if kind is None:
            t = self.nc.dram_tensor(name, list(shape), dt)
            b = Buf(name, t.ap())
            b.es = self.es
            return b
        else:
            t = self.nc.dram_tensor(name, list(shape), dt, kind=kind)
        b = Buf(name, t.ap())
        b.es = self.es
        return b

    def _semobj(self, key):
        if isinstance(key, str):
            return self.sem[key]
        return key.dsem

    def _wait(self, ekey, deps):
        eng = self.eng[ekey]
        kn = self.known[ekey]
        for key, val in deps.items():
            if isinstance(key, str):
                if key == "pe" and ekey == "pe":
                    continue
                v = val
            else:
                if key.dsem is None:
                    continue
                v = key.dcount * 16
            if kn.get(key, 0) >= v:
                continue
            eng.wait_ge(self._semobj(key), v)
            kn[key] = v

    def _collect(self, reads, writes, nowaw):
        deps = {}
        def add(d):
            for k, v in d.items():
                if deps.get(k, 0) < v:
                    deps[k] = v
        for b in reads:
            add(b.wr)
            if getattr(b, "is_psum", False):
                add(b.rd)
        for b in writes:
            if not nowaw:
                add(b.wr)
            add(b.rd)
        return deps

    def op(self, ekey, fn, reads=(), writes=(), nowaw=False):
        deps = self._collect(reads, writes, nowaw)
        self._wait(ekey, deps)
        ins = fn()
        self.cnt[ekey] += 1
        ins.then_inc(self.sem[ekey], 1)
        tk = self.cnt[ekey]
        self.n_inst += 1
        for b in reads:
            if b.rd.get(ekey, 0) < tk:
                b.rd[ekey] = tk
        for b in writes:
            if nowaw:
                b.wr[ekey] = tk
            else:
                b.wr = {ekey: tk}
                b.rd = {}
        return ins

    def dma(self, qkey, out_ap, in_ap, reads=(), writes=(), track=None, nowaw=True, **kw):
        assert track is not None
        if track.dsem is None:
            if self.sempool:
                track.dsem, track.dcount = self.sempool.pop()
            else:
                track.dsem = self.es.enter_context(self.nc.semaphore("d_" + track.name))
        deps = self._collect(reads, writes, nowaw)
        self._wait(qkey, deps)
        ins = self.eng[qkey].dma_start(out=out_ap, in_=in_ap, **kw)
        ins.then_inc(track.dsem, 16)
        track.dcount += 1
        self.n_inst += 1
        for b in reads:
            b.rd[track] = track.dcount
        for b in writes:
            if nowaw:
                b.wr[track] = track.dcount
            else:
                b.wr = {track: track.dcount}
                b.rd = {}
        return ins

    def mark(self, name):
        if not getattr(self, "prof", False):
            return
        cur = getattr(self, "_cur_scope", None)
        if cur is not None:
            self.nc.leave_named_scope(cur[0], cur[1], False)
        sid, _ = self.nc.enter_named_scope(name, False)
        self._cur_scope = (name, sid)

    def barrier_on(self, bufs):
        deps = {}
        for b in bufs:
            for d in (b.wr, b.rd):
                for k, v in d.items():
                    if deps.get(k, 0) < v:
                        deps[k] = v
        for e in self.eng:
            self._wait(e, deps)

    def finish(self, bufs):
        deps = {}
        for b in bufs:
            for k, v in b.wr.items():
                if deps.get(k, 0) < v:
                    deps[k] = v
        self._wait("sp", deps)


C_AQ, C_AK, C_AV, C_IQ, C_IK, C_IW = 0, 256, 320, 384, 512, 544
C_BQ, C_KCR, C_VCR, C_KS, C_VS, C_KW, C_VW, C_BG = 548, 804, 868, 932, 996, 1060, 1124, 1188
C_CQ, C_CK, C_CV, C_MG = 1200, 1712, 2224, 2736
FM_CHUNKS = [("aq0", 0, 128), ("aq1", 128, 128), ("ak", 256, 64), ("iq", 384, 128), ("ik", 512, 32),
             ("bq0", 548, 128), ("bq1", 676, 128), ("kvcr", 804, 128), ("ks", 932, 64), ("kw", 1060, 64),
             ("cq0", 1200, 128), ("cq1", 1328, 128), ("cq2", 1456, 128), ("cq3", 1584, 128),
             ("ck0", 1712, 128), ("ck1", 1840, 128), ("ck2", 1968, 128), ("ck3", 2096, 128)]
FM_ROW = {}
_r = 0
for _n, _c, _w in FM_CHUNKS:
    FM_ROW[_n] = _r
    _r += 128
FM_ROWS = _r
VV_W = 704


def sched_release(sc, bufs):
    for b in bufs:
        if b.dsem is not None:
            sc.sempool.append((b.dsem, b.dcount))
            b.dsem = None


def load_cast(sc, es, dst, dst_ap, src, src_ap):
    sc.dma("pool", dst_ap, src_ap, reads=[src], writes=[dst], track=dst)


def phase_adaln(sc, cfg, l, modw, modbT, modb, which, siluT, out_opsc, out_shf, out_gate, want, tm_third=2, add_one=1.0):
    nc = sc.nc
    nseq = cfg["nseq"]
    with ExitStack() as es:
        wk = [sc.sbuf("adw", [128, 3072], F32, es) for _ in range(2)]
        bT = sc.sbuf("adbT", [128, 24], F32, es)
        gb = sc.sbuf("adgb", [128, 1024], F32, es)
        psA = sc.psum("adpsA", [128, 512], F32, es)
        psG = [[sc.psum("adpsG", [128, 512], F32, es) for _ in range(2)] for _ in range(nseq)]
        silu_bc = []
        if want == "gate":
            for b in range(nseq):
                t = sc.sbuf("silubc", [128, 8, 128], F32, es)
                sc.op("dve", lambda: nc.vector.tensor_copy(t[:], siluT[:, :, b:b + 1].to_broadcast([128, 8, 128])), reads=[siluT], writes=[t])
                silu_bc.append(t)
        sc.dma("sp", bT[:], modbT[l, which], reads=[modbT], writes=[bT], track=bT)
        sc.dma("sp", gb[:], modb[l:l + 1, tm_third * 1024:(tm_third + 1) * 1024].partition_broadcast(128), reads=[modb], writes=[gb], track=gb)
        for kc in range(8):
            w = wk[kc % 2]
            sc.dma("sp", w[:], modw[l, kc * 128:(kc + 1) * 128, :], reads=[modw], writes=[w], track=w, nowaw=False)
            for j in (range(16) if want == "fm" else []):
                sc.op("pe", lambda: nc.tensor.matmul(psA[:, j * nseq:(j + 1) * nseq], lhsT=w[:, j * 128:(j + 1) * 128],
                                                     rhs=siluT[:, kc, :], start=(kc == 0 and j == 0), stop=(kc == 7),
                                                     skip_group_check=True),
                      reads=[w, siluT], writes=[psA], nowaw=True)
            for b in (range(nseq) if want == "gate" else []):
                for hf in range(2):
                    sc.op("pe", lambda: nc.tensor.matmul(psG[b][hf][:, :], lhsT=silu_bc[b][:, kc, :],
                                                         rhs=w[:, tm_third * 1024 + hf * 512:tm_third * 1024 + (hf + 1) * 512],
                                                         start=(kc == 0), stop=(kc == 7)),
                          reads=[w, silu_bc[b]], writes=[psG[b][hf]], nowaw=True)
        for j in (range(8) if want == "fm" else []):
            sc.op("dve", lambda: nc.vector.tensor_scalar(out=out_shf[:, j, :], in0=psA[:, j * nseq:(j + 1) * nseq],
                                                        scalar1=bT[:, j:j + 1], scalar2=None, op0=ALU.add),
                  reads=[psA, bT], writes=[out_shf], nowaw=True)
            sc.op("dve", lambda: nc.vector.tensor_scalar(out=out_opsc[:, j, :], in0=psA[:, (j + 8) * nseq:(j + 9) * nseq],
                                                        scalar1=bT[:, j + 8:j + 9], scalar2=1.0, op0=ALU.add, op1=ALU.add),
                  reads=[psA, bT], writes=[out_opsc], nowaw=True)
        for b in (range(nseq) if want == "gate" else []):
            for hf in range(2):
                sc.op("dve", lambda: nc.vector.scalar_tensor_tensor(out=out_gate[:, b, hf * 512:(hf + 1) * 512],
                                                                   in0=psG[b][hf][:, :], scalar=float(add_one),
                                                                   in1=gb[:, hf * 512:(hf + 1) * 512],
                                                                   op0=ALU.add, op1=ALU.add),
                      reads=[psG[b][hf], gb], writes=[out_gate], nowaw=True)
        sc.barrier_on([wk[0], wk[1], bT, gb, psA] + [p for q in psG for p in q] + silu_bc)
        sched_release(sc, [wk[0], wk[1], bT, gb])


def phase_proj(sc, cfg, l, xsrc, w_in, ident, opsc, shf, FM, VV, SM, MG):
    nc = sc.nc
    nseq, seq = cfg["nseq"], cfg["seq"]
    ntok = nseq * seq
    with ExitStack() as es:
        wb = sc.sbuf("w_in_bf", [128, 8, D_IN], BF16, es)
        for kc in range(8):
            sc.dma("pool", wb[:, kc, :], w_in[l, kc * 128:(kc + 1) * 128, :], reads=[w_in], writes=[wb], track=wb)
        xs = [sc.sbuf("xs", [128, 4, D], F32, es) for _ in range(2)]
        hT = [sc.sbuf("hT", [128, 8, 512], BF16, es) for _ in range(2)]
        fst = [sc.sbuf("fst", [128, 512], BF16, es) for _ in range(4)]
        vst = [sc.sbuf("vst", [128, VV_W], BF16, es) for _ in range(2)]
        sst = [sc.sbuf("sst", [128, 16], F32, es) for _ in range(2)]
        mst = [sc.sbuf("mst", [128, 3072], F32, es) for _ in range(2)]
        ps = [sc.psum("pps", [128, 512], F32, es) for _ in range(8)]
        pi = [0]
        def nextps():
            p = ps[pi[0] % 8]
            pi[0] += 1
            return p
        nblk = ntok // 512
        fi = 0
        for blk in range(nblk):
            b = (blk * 512) // seq
            x_t = xs[blk % 2]
            h_t = hT[blk % 2]
            sc.dma("sp", x_t[:], xsrc[blk * 512:(blk + 1) * 512, :].rearrange("(j p) d -> p j d", p=128),
                   reads=[xsrc], writes=[x_t], track=x_t, nowaw=False)
            for c in range(8):
                p = nextps()
                for j in range(4):
                    sc.op("pe", lambda: nc.tensor.transpose(p[:, j * 128:(j + 1) * 128], x_t[:, j, c * 128:(c + 1) * 128], ident[:]),
                          reads=[x_t, ident], writes=[p], nowaw=(j > 0))
                sc.op("act", lambda: nc.scalar.activation(out=h_t[:, c, :], in_=p[:, :], func=AF.Identity,
                                                          bias=shf[:, c, b:b + 1], scale=opsc[:, c, b:b + 1]),
                      reads=[p, shf, opsc], writes=[h_t], nowaw=(c > 0))
            for (nm, c0, wd) in FM_CHUNKS:
                p = nextps()
                for kc in range(8):
                    sc.op("pe", lambda: nc.tensor.matmul(p[:wd, :], lhsT=wb[:, kc, c0:c0 + wd], rhs=h_t[:, kc, :],
                                                         start=(kc == 0), stop=(kc == 7)),
                          reads=[wb, h_t], writes=[p], nowaw=(kc > 0))
                f = fst[fi % 4]
                ek = "dve" if fi % 2 == 0 else "act"
                if ek == "dve":
                    sc.op("dve", lambda: nc.vector.tensor_copy(f[:wd, :], p[:wd, :]), reads=[p], writes=[f])
                else:
                    sc.op("act", lambda: nc.scalar.copy(f[:wd, :], p[:wd, :]), reads=[p], writes=[f])
                r0 = FM_ROW[nm]
                sc.dma("sp", FM[r0:r0 + wd, blk * 512:(blk + 1) * 512], f[:wd, :], reads=[f], writes=[FM], track=f)
                fi += 1
            for j in range(4):
                tk = blk * 4 + j
                v_t, s_t, m_t = vst[tk % 2], sst[tk % 2], mst[tk % 2]
                tok0 = blk * 512 + j * 128
                def tm(c0, wd):
                    p = nextps()
                    for kc in range(8):
                        sc.op("pe", lambda: nc.tensor.matmul(p[:, :wd], lhsT=h_t[:, kc, j * 128:(j + 1) * 128],
                                                             rhs=wb[:, kc, c0:c0 + wd], start=(kc == 0), stop=(kc == 7)),
                              reads=[wb, h_t], writes=[p], nowaw=(kc > 0))
                    return p
                p = tm(C_AV, 64)
                sc.op("dve", lambda: nc.vector.tensor_copy(v_t[:, 0:64], p[:, 0:64]), reads=[p], writes=[v_t])
                p = tm(C_VS, 64)
                sc.op("dve", lambda: nc.vector.tensor_copy(v_t[:, 64:128], p[:, 0:64]), reads=[p], writes=[v_t], nowaw=True)
                p = tm(C_VW, 76)
                sc.op("dve", lambda: nc.vector.tensor_copy(v_t[:, 128:192], p[:, 0:64]), reads=[p], writes=[v_t], nowaw=True)
                sc.op("act", lambda: nc.scalar.activation(out=s_t[:, 4:16], in_=p[:, 64:76], func=AF.Sigmoid),
                      reads=[p], writes=[s_t])
                p = tm(C_IW, 4)
                sc.op("dve", lambda: nc.vector.tensor_copy(s_t[:, 0:4], p[:, 0:4]), reads=[p], writes=[s_t], nowaw=True)
                p = tm(C_CV, 512)
                sc.op("dve", lambda: nc.vector.tensor_copy(v_t[:, 192:704], p[:, :]), reads=[p], writes=[v_t], nowaw=True)
                sc.dma("sp", VV[tok0:tok0 + 128, :], v_t[:], reads=[v_t], writes=[VV], track=v_t)
                sc.dma("sp", SM[tok0:tok0 + 128, :], s_t[:], reads=[s_t], writes=[SM], track=s_t)
                for g in range(6):
                    p = tm(C_MG + g * 512, 512)
                    sc.op("act", lambda: nc.scalar.activation(out=m_t[:, g * 512:(g + 1) * 512], in_=p[:, :], func=AF.Sigmoid),
                          reads=[p], writes=[m_t], nowaw=(g > 0))
                sc.dma("sp", MG[tok0:tok0 + 128, :], m_t[:], reads=[m_t], writes=[MG], track=m_t)
        allb = [wb] + xs + hT + fst + vst + sst + mst + ps
        sc.barrier_on(allb)
        sched_release(sc, allb)


def rel_bucket_np(dist):
    n = np.maximum(dist, 0)
    nf = np.maximum(n, 1).astype(np.float32)
    large = 16 + (np.log(nf / 16) / np.float32(math.log(128 / 16)) * 16).astype(np.int32)
    large = np.minimum(large, 31)
    return np.where(n < 16, n, large)


def make_consts():
    import ml_dtypes
    c = {}
    r = np.arange(128)[:, None]
    s = np.arange(256)[None, :]
    dist = r + 128 - s
    bk = rel_bucket_np(dist)
    oh = np.zeros((128, 256, 32), np.float32)
    oh[np.arange(128)[:, None], np.arange(256)[None, :], bk] = 1.0
    c["c_oh"] = oh.reshape(128, 256 * 32).astype(ml_dtypes.bfloat16)
    c["c_cneg"] = np.where(dist >= 0, 0.0, NEG).astype(np.float32)
    cm = np.where(np.arange(128)[None, :] <= np.arange(128)[:, None], 0.0, -1e30).astype(np.float32)
    c["c_cm"] = cm
    cc = np.arange(384)[None, :]
    c["c_wneg"] = np.where(cc > r, 0.0, NEG).astype(np.float32)
    c["c_iota"] = np.broadcast_to(np.arange(512, dtype=np.float32)[None, :], (128, 512)).copy()
    qi = np.arange(32)[None, :]
    c["c_tc"] = ((r + qi * 128 - 31) / 16.0).astype(np.float32)
    n_cmp, n_slc = 255, 64
    start = np.arange(n_cmp) * 16
    end = start + 32
    bs = np.arange(n_slc) * 64
    ov = ((start[:, None] < bs[None, :] + 64) & (end[:, None] > bs[None, :])).astype(np.float32)
    ovp = np.zeros((256, 64), np.float32)
    ovp[:255] = ov
    c["c_ovl"] = np.ascontiguousarray(ovp.reshape(2, 128, 64).transpose(1, 0, 2)).astype(ml_dtypes.bfloat16)
    sel = np.zeros((32, 128, 192), np.float32)
    blk = np.arange(64)[None, :]
    for q in range(32):
        t = q * 128 + np.arange(128)[:, None]
        cur = t // 64
        forced = (blk == 0) | (blk == cur) | (blk == cur - 1)
        allowed = blk <= cur
        sel[q, :, 0:64] = (allowed & ~forced).astype(np.float32)
        sel[q, :, 64:128] = np.where(allowed, np.where(forced, 1e9 + 1024.0 * blk, 0.0), -1e30)
        sel[q, :, 128:192] = allowed.astype(np.float32)
    c["c_sel"] = sel
    c["c_pow2"] = np.broadcast_to((2.0 ** -np.arange(40, dtype=np.float32))[None, :], (128, 40)).copy()
    c["c_ident"] = np.eye(128, dtype=np.float32)
    c["c_ut"] = (np.arange(128)[:, None] <= np.arange(128)[None, :]).astype(np.float32)
    return c


CONST_SPECS = [("c_oh", [128, 8192], BF16), ("c_cneg", [128, 256], F32), ("c_cm", [128, 128], F32),
               ("c_wneg", [128, 384], F32), ("c_iota", [128, 512], F32), ("c_tc", [128, 32], F32),
               ("c_ovl", [128, 2, 64], BF16), ("c_sel", [32, 128, 192], F32), ("c_pow2", [128, 40], F32),
               ("c_ident", [128, 128], F32), ("c_ut", [128, 128], F32)]
NBIS = 29
TIE_EPS = 2.0 ** -24


def setup_attn_consts(sc, cd, rel_bias, es):
    nc = sc.nc
    K = {}
    def ld(name, shape, dt, src_ap, srcbuf):
        b = sc.sbuf(name, shape, dt, es)
        sc.dma("sp", b[:], src_ap, reads=[srcbuf], writes=[b], track=b)
        return b
    K["cneg"] = ld("cneg", [128, 256], F32, cd["c_cneg"][:, :], cd["c_cneg"])
    K["cm"] = ld("cm", [128, 128], F32, cd["c_cm"][:, :], cd["c_cm"])
    K["wneg"] = ld("wneg", [128, 384], F32, cd["c_wneg"][:, :], cd["c_wneg"])
    K["iota"] = ld("iota", [128, 512], F32, cd["c_iota"][:, :], cd["c_iota"])
    K["tc"] = ld("tc", [128, 32], F32, cd["c_tc"][:, :], cd["c_tc"])
    K["ovl"] = ld("ovl", [128, 2, 64], BF16, cd["c_ovl"][:, :, :], cd["c_ovl"])
    K["pow2"] = ld("pow2", [128, 40], F32, cd["c_pow2"][:, :], cd["c_pow2"])
    K["ident"] = ld("identf", [128, 128], F32, cd["c_ident"][:, :], cd["c_ident"])
    K["tabb"] = ld("tabb", [128, 384], F32,
                   rel_bias.t.rearrange("b h -> (b h)").unsqueeze(0).partition_broadcast(128), rel_bias)
    K["identb"] = sc.sbuf("identb", [128, 128], BF16, es)
    sc.op("dve", lambda: nc.vector.tensor_copy(K["identb"][:], K["ident"][:]), reads=[K["ident"]], writes=[K["identb"]])
    K["near"] = sc.sbuf("near", [128, 12, 256], F32, es)
    with ExitStack() as tes:
        oh = sc.sbuf("oh", [128, 8192], BF16, tes)
        tmp = sc.sbuf("ohtmp", [128, 8192], F32, tes)
        sc.dma("sp", oh[:], cd["c_oh"][:, :], reads=[cd["c_oh"]], writes=[oh], track=oh)
        for hd in range(12):
            tb = K["tabb"][:, :].rearrange("p (b h) -> p h b", h=12)[:, hd, :]
            sc.op("dve", lambda: nc.vector.tensor_tensor(out=tmp[:].rearrange("p (s b) -> p s b", b=32),
                                                        in0=oh[:].rearrange("p (s b) -> p s b", b=32),
                                                        in1=tb.unsqueeze(1).to_broadcast([128, 256, 32]), op=ALU.mult),
                  reads=[oh, K["tabb"]], writes=[tmp])
            sc.op("dve", lambda: nc.vector.tensor_reduce(out=K["near"][:, hd, :], in_=tmp[:].rearrange("p (s b) -> p s b", b=32),
                                                        axis=AX.X, op=ALU.add), reads=[tmp], writes=[K["near"]], nowaw=True)
            sc.op("dve", lambda: nc.vector.scalar_tensor_tensor(out=K["near"][:, hd, :], in0=K["near"][:, hd, :],
                                                               scalar=K["tabb"][:, 31 * 12 + hd:31 * 12 + hd + 1], in1=K["cneg"][:],
                                                               op0=ALU.subtract, op1=ALU.add), reads=[K["near"], K["cneg"], K["tabb"]], writes=[K["near"]])
        sc.barrier_on([oh, tmp])
        sched_release(sc, [oh, tmp])
    return K


def phase_compress(sc, cfg, l, FM, cmp_w1, cmp_w2, cposT, KC, VC):
    nc = sc.nc
    nseq, seq = cfg["nseq"], cfg["seq"]
    ncmp = (seq - 32) // 16 + 1
    with ExitStack() as es:
        w1 = sc.sbuf("cw1", [128, 32, 256], BF16, es)
        w2 = sc.sbuf("cw2", [128, 2, 2, 64], BF16, es)
        posT = sc.sbuf("cposT", [128, 32], F32, es)
        raw = sc.sbuf("craw", [128, seq], BF16, es)
        rawp = sc.sbuf("crawp", [128, 32, 256], BF16, es)
        hidT = [sc.sbuf("chid", [128, 2, 256], BF16, es) for _ in range(2)]
        t1 = sc.sbuf("ct1", [128, 256], F32, es)
        t2 = sc.sbuf("ct2", [128, 256], F32, es)
        ps = [sc.psum("cps", [128, 512], F32, es) for _ in range(2)]
        for j in range(2):
            sc.dma("pool", w1[j * 64:(j + 1) * 64, :, :], cmp_w1[l, j].rearrange("(p d) h -> d p h", d=64),
                   reads=[cmp_w1], writes=[w1], track=w1)
        sc.dma("pool", w2[:], cmp_w2[l].rearrange("j (hc p) d -> p j hc d", p=128), reads=[cmp_w2], writes=[w2], track=w2)
        sc.dma("sp", posT[:], cposT[l], reads=[cposT], writes=[posT], track=posT)
        sc.op("dve", lambda: nc.vector.memset(VC[:], 0.0), writes=[VC])
        sc.op("dve", lambda: nc.vector.memset(KC[:], 0.0), writes=[KC])
        r0 = FM_ROW["kvcr"]
        pi = 0
        for b in range(nseq):
            sc.dma("sp", raw[:], FM[r0:r0 + 128, b * seq:(b + 1) * seq], reads=[FM], writes=[raw], track=raw, nowaw=False)
            r3 = raw[:].rearrange("p (n s) -> p n s", s=16)
            for p in range(32):
                src = r3[:, 0:ncmp, p] if p < 16 else r3[:, 1:ncmp + 1, p - 16]
                sc.op("dve", lambda: nc.vector.tensor_scalar(out=rawp[:, p, :ncmp], in0=src, scalar1=posT[:, p:p + 1],
                                                            scalar2=None, op0=ALU.add),
                      reads=[raw, posT], writes=[rawp], nowaw=(p > 0))
            for j in range(2):
                for hc in range(2):
                    ph = ps[pi % 2]; pi += 1
                    for p in range(32):
                        sc.op("pe", lambda: nc.tensor.matmul(ph[:, :ncmp], lhsT=w1[j * 64:(j + 1) * 64, p, hc * 128:(hc + 1) * 128],
                                                             rhs=rawp[j * 64:(j + 1) * 64, p, :ncmp], start=(p == 0), stop=(p == 31)),
                              reads=[w1, rawp], writes=[ph], nowaw=(p > 0))
                    sc.op("act", lambda: nc.scalar.activation(out=t1[:, :ncmp], in_=ph[:, :ncmp], func=AF.Square), reads=[ph], writes=[t1])
                    sc.op("dve", lambda: nc.vector.tensor_scalar(out=t1[:, :ncmp], in0=t1[:, :ncmp], scalar1=0.044715, scalar2=1.0,
                                                                op0=ALU.mult, op1=ALU.add), reads=[t1], writes=[t1])
                    sc.op("dve", lambda: nc.vector.tensor_tensor(out=t1[:, :ncmp], in0=t1[:, :ncmp], in1=ph[:, :ncmp], op=ALU.mult),
                          reads=[t1, ph], writes=[t1])
                    sc.op("act", lambda: nc.scalar.activation(out=t2[:, :ncmp], in_=t1[:, :ncmp], func=AF.Sigmoid, scale=1.5957691216057308),
                          reads=[t1], writes=[t2])
                    sc.op("dve", lambda: nc.vector.tensor_tensor(out=hidT[j][:, hc, :ncmp], in0=t2[:, :ncmp], in1=ph[:, :ncmp], op=ALU.mult),
                          reads=[t2, ph], writes=[hidT[j]], nowaw=(hc > 0))
            pk = ps[pi % 2]; pi += 1
            for hc in range(2):
                sc.op("pe", lambda: nc.tensor.matmul(pk[:64, :ncmp], lhsT=w2[:, 0, hc, :], rhs=hidT[0][:, hc, :ncmp],
                                                     start=(hc == 0), stop=(hc == 1)), reads=[w2, hidT[0]], writes=[pk], nowaw=(hc > 0))
            sc.op("dve", lambda: nc.vector.tensor_copy(KC[:, b, :ncmp], pk[:64, :ncmp]), reads=[pk], writes=[KC], nowaw=True)
            for nch in range(2):
                n0 = nch * 128
                nsz = min(128, ncmp - n0)
                if nsz <= 0:
                    continue
                pv = ps[pi % 2]; pi += 1
                for hc in range(2):
                    sc.op("pe", lambda: nc.tensor.matmul(pv[:nsz, 0:64], lhsT=hidT[1][:, hc, n0:n0 + nsz], rhs=w2[:, 1, hc, :],
                                                         start=(hc == 0), stop=(hc == 1)), reads=[w2, hidT[1]], writes=[pv], nowaw=(hc > 0))
                sc.op("dve", lambda: nc.vector.tensor_copy(VC[:nsz, b, nch, :], pv[:nsz, 0:64]), reads=[pv], writes=[VC], nowaw=True)
        allb = [w1, w2, posT, raw, rawp, t1, t2] + hidT + ps
        sc.barrier_on(allb)
        sched_release(sc, allb)


def phase_attn(sc, cfg, l, K, FM, VV, SM, KC, VC, csel, diff_lambda, diff_norm_g, lam_init, OABC, qtiles=None):
    nc = sc.nc
    nseq, seq = cfg["nseq"], cfg["seq"]
    nqt = seq // 128
    IDXC = 0.5 * (32 ** -0.5)
    with ExitStack() as es:
        KT = sc.sbuf("KT", [128, 2, seq], BF16, es)
        CK = sc.sbuf("CK", [128, 4, seq], BF16, es)
        VT = sc.sbuf("VT", [128, nqt, VV_W], BF16, es)
        ISC = sc.sbuf("ISC", [128, seq], F32, es)
        JUNK = sc.sbuf("JUNK", [128, seq], BF16, es)
        QI = [sc.sbuf("QI", [128, 4, 128], BF16, es) for _ in range(2)]
        QA = [sc.sbuf("QA", [64, 4, 128], BF16, es) for _ in range(2)]
        QB = [sc.sbuf("QB", [128, 4, 128], BF16, es) for _ in range(2)]
        QC = [sc.sbuf("QC", [128, 4, 128], BF16, es) for _ in range(2)]
        SMt = [sc.sbuf("SMt", [128, 16], F32, es) for _ in range(2)]
        SELt = [sc.sbuf("SELt", [128, 192], F32, es) for _ in range(2)]
        OUT = [sc.sbuf("OUT", [128, 1024], F32, es) for _ in range(1)]
        AW = sc.sbuf("AW", [128, 4], F32, es)
        SGN = sc.sbuf("SGN", [128, 4], F32, es)
        RT = [sc.sbuf("RT", [128, 512], F32, es) for _ in range(2)]
        ST = sc.sbuf("STAT", [128, 64], F32, es)
        W = sc.sbuf("Wb", [128, 40], F32, es)
        CMK = sc.sbuf("CMK", [128, 256], F32, es)
        PCf = sc.sbuf("PCf", [128, 256], F32, es)
        PCm = sc.sbuf("PCm", [128, 4, 256], F32, es)
        PCn = sc.sbuf("PCn", [128, 5, 256], BF16, es)
        PCT = sc.sbuf("PCT", [128, 10, 128], BF16, es)
        CS = sc.sbuf("CS", [128, 8], F32, es)
        OCMP = sc.sbuf("OCMP", [128, 256], F32, es)
        OWIN = sc.sbuf("OWIN", [128, 256], F32, es)
        IMP = sc.sbuf("IMP", [128, 64], F32, es)
        RANK = sc.sbuf("RANK", [128, 64], F32, es)
        BM = sc.sbuf("BM", [128, 64], F32, es)
        PW = sc.sbuf("PW", [128, 640], BF16, es)
        TMPN = [sc.sbuf("TMPN", [128, 384], F32, es) for _ in range(2)]
        WS = sc.sbuf("WS", [128, 8], F32, es)
        P = [sc.sbuf("P", [128, 512], BF16, es) for _ in range(3)]
        PM = [sc.sbuf("PM", [128, 512], BF16, es) for _ in range(3)]
        MAc = sc.sbuf("MAc", [128, 512], BF16, es)
        PT = [sc.sbuf("PT", [128, 5, 128], BF16, es) for _ in range(3)]
        SUMS = sc.sbuf("SUMS", [128, 16, 16], F32, es)
        TOT = sc.sbuf("TOT", [128, 16], F32, es)
        RCP = sc.sbuf("RCP", [128, 16], F32, es)
        LAM = sc.sbuf("LAM", [128, 4], F32, es)
        DL = sc.sbuf("DL", [128, 256], F32, es)
        DNG = sc.sbuf("DNG", [128, 128], F32, es)
        OCt = sc.sbuf("OCt", [128, 128], F32, es)
        OCj = sc.sbuf("OCj", [128, 128], F32, es)
        TAB31 = sc.sbuf("TAB31", [128, 12], F32, es)
        psS = [sc.psum("psS", [128, 512], F32, es) for _ in range(3)]
        psT = [sc.psum("psT", [128, 8, 128], BF16, es) for _ in range(2)]
        psOab = sc.psum("psOab", [128, 512], F32, es)
        psOc = [sc.psum("psOc", [128, 512], F32, es) for _ in range(2)]
        cnt = {"s": 0, "t": 0, "p": 0, "pm": 0, "pt": 0, "rt": 0, "tn": 0, "ev": 0}
        def nxt(lst, key):
            b = lst[cnt[key] % len(lst)]
            cnt[key] += 1
            return b

        sc.op("dve", lambda: nc.vector.tensor_copy(TAB31[:], K["tabb"][:, 31 * 12:32 * 12]), reads=[K["tabb"]], writes=[TAB31])
        sc.dma("sp", DL[:], diff_lambda[l].rearrange("a d -> (a d)").unsqueeze(0).partition_broadcast(128),
               reads=[diff_lambda], writes=[DL], track=DL)
        sc.dma("sp", DNG[:], diff_norm_g[l:l + 1, :].partition_broadcast(128), reads=[diff_norm_g], writes=[DNG], track=DNG)
        sc.op("dve", lambda: nc.vector.tensor_scalar(out=DNG[:], in0=DNG[:], scalar1=float(1.0 - lam_init), scalar2=None, op0=ALU.mult),
              reads=[DNG], writes=[DNG])
        for a in range(2):
            sc.op("dve", lambda: nc.vector.scalar_tensor_tensor(out=OCt[:, 0:64], in0=DL[:, (2 * a) * 64:(2 * a + 1) * 64], scalar=1.0,
                                                               in1=DL[:, (2 * a + 1) * 64:(2 * a + 2) * 64], op0=ALU.mult, op1=ALU.mult,
                                                               accum_out=LAM[:, 2 + a:3 + a]), reads=[DL], writes=[OCt, LAM])
        sc.op("act", lambda: nc.scalar.activation(out=LAM[:, 2:4], in_=LAM[:, 2:4], func=AF.Exp), reads=[LAM], writes=[LAM])
        sc.op("dve", lambda: nc.vector.scalar_tensor_tensor(out=LAM[:, 0:1], in0=LAM[:, 2:3], scalar=float(lam_init), in1=LAM[:, 3:4],
                                                           op0=ALU.add, op1=ALU.subtract), reads=[LAM], writes=[LAM])
        sc.op("dve", lambda: nc.vector.tensor_scalar(out=LAM[:, 1:2], in0=LAM[:, 0:1], scalar1=-1.0, scalar2=None, op0=ALU.mult),
              reads=[LAM], writes=[LAM])
        sc.op("dve", lambda: nc.vector.memset(PCn[:], 0.0), writes=[PCn])

        qn = 0
        for b in range(nseq):
            c0 = b * seq
            sc.dma("sp", KT[64:96, 1, :], FM[FM_ROW["ik"]:FM_ROW["ik"] + 32, c0:c0 + seq], reads=[FM], writes=[KT], track=KT, nowaw=False)
            for nm, (p0, sl) in (("ak", (0, 0)), ("ks", (64, 0)), ("kw", (0, 1))):
                sc.dma("sp", KT[p0:p0 + 64, sl, :], FM[FM_ROW[nm]:FM_ROW[nm] + 64, c0:c0 + seq], reads=[FM], writes=[KT], track=KT)
            for h in range(4):
                r = FM_ROW["ck%d" % h]
                sc.dma("sp", CK[:, h, :], FM[r:r + 128, c0:c0 + seq], reads=[FM], writes=[CK], track=CK, nowaw=(h > 0))
            for n4 in range(0, nqt, 8):
                sc.dma("sp", VT[:, n4:n4 + 8, :], VV[c0 + n4 * 128:c0 + (n4 + 8) * 128, :].rearrange("(n p) c -> p n c", p=128),
                       reads=[VV], writes=[VT], track=VT, nowaw=(n4 > 0))
            for qi in (qtiles if qtiles is not None else range(nqt)):
                tok0 = c0 + qi * 128
                nk = qi + 1
                Nk = nk * 128
                qI, qA, qB, qC, smt, selt = (x[qn % 2] for x in (QI, QA, QB, QC, SMt, SELt))
                out = OUT[0]
                qn += 1
                r = FM_ROW["iq"]
                sc.dma("sp", qI[64:96, :, :], FM[r:r + 128, tok0:tok0 + 128].rearrange("(h d) q -> d h q", d=32), reads=[FM], writes=[qI], track=qI, nowaw=False)
                r = FM_ROW["aq0"]
                sc.dma("sp", qA[:], FM[r:r + 256, tok0:tok0 + 128].rearrange("(h d) q -> d h q", d=64), reads=[FM], writes=[qA], track=qA, nowaw=False)
                r = FM_ROW["bq0"]
                sc.dma("sp", qB[0:64, :, :], FM[r:r + 256, tok0:tok0 + 128].rearrange("(h d) q -> d h q", d=64), reads=[FM], writes=[qB], track=qB, nowaw=False)
                sc.dma("sp", qB[64:128, :, :], FM[r:r + 256, tok0:tok0 + 128].rearrange("(h d) q -> d h q", d=64), reads=[FM], writes=[qB], track=qB)
                r = FM_ROW["cq0"]
                sc.dma("sp", qC[:], FM[r:r + 512, tok0:tok0 + 128].rearrange("(h r) q -> r h q", r=128), reads=[FM], writes=[qC], track=qC, nowaw=False)
                sc.dma("sp", smt[:], SM[tok0:tok0 + 128, :], reads=[SM], writes=[smt], track=smt, nowaw=False)
                sc.dma("sp", selt[:], csel[qi], reads=[csel], writes=[selt], track=selt, nowaw=False)
                chunks = [(k0, min(512, Nk - k0)) for k0 in range(0, Nk, 512)]

                THR = ST[:, 7:8]
                sc.op("dve", lambda: nc.vector.memset(SUMS[:], 0.0), writes=[SUMS])
                firstO = {"ab": True, 0: True, 1: True}

                def genA():
                    sc.op("act", lambda: nc.scalar.activation(out=AW[:], in_=smt[:, 0:4], func=AF.Abs, scale=IDXC), reads=[smt], writes=[AW])
                    sc.op("act", lambda: nc.scalar.activation(out=SGN[:], in_=smt[:, 0:4], func=AF.Sign), reads=[smt], writes=[SGN])
                    for (k0, w) in chunks:
                        yield
                        for h in range(4):
                            ps = nxt(psS, "s")
                            sc.op("pe", lambda: nc.tensor.matmul(ps[:, :w], lhsT=qI[64:96, h, :], rhs=KT[64:96, 1, k0:k0 + w], start=True, stop=True),
                                  reads=[qI, KT], writes=[ps])
                            rt = nxt(RT, "rt")
                            sc.op("act", lambda: nc.scalar.activation(out=rt[:, :w], in_=ps[:, :w], func=AF.Relu, scale=AW[:, h:h + 1]),
                                  reads=[ps, AW], writes=[rt])
                            if h == 0:
                                sc.op("dve", lambda: nc.vector.tensor_scalar(out=ISC[:, k0:k0 + w], in0=rt[:, :w], scalar1=SGN[:, 0:1], scalar2=None,
                                                                            op0=ALU.mult), reads=[rt, SGN], writes=[ISC], nowaw=True)
                            else:
                                sc.op("dve", lambda: nc.vector.scalar_tensor_tensor(out=ISC[:, k0:k0 + w], in0=rt[:, :w], scalar=SGN[:, h:h + 1],
                                                                                   in1=ISC[:, k0:k0 + w], op0=ALU.mult, op1=ALU.add),
                                      reads=[rt, SGN, ISC], writes=[ISC])
                        rt = nxt(RT, "rt")
                        sc.op("dve", lambda: nc.vector.tensor_scalar(out=rt[:, :w], in0=K["iota"][:, :w], scalar1=float(k0), scalar2=-TIE_EPS,
                                                                    op0=ALU.add, op1=ALU.mult), reads=[K["iota"]], writes=[rt])
                        sc.op("dve", lambda: nc.vector.scalar_tensor_tensor(out=rt[:, :w], in0=ISC[:, k0:k0 + w], scalar=0.0, in1=rt[:, :w],
                                                                           op0=ALU.is_equal, op1=ALU.mult), reads=[ISC, rt], writes=[rt])
                        sc.op("dve", lambda: nc.vector.tensor_tensor(out=ISC[:, k0:k0 + w], in0=ISC[:, k0:k0 + w], in1=rt[:, :w], op=ALU.add),
                              reads=[ISC, rt], writes=[ISC])
                    sc.op("dve", lambda: nc.vector.tensor_tensor(out=ISC[:, Nk - 128:Nk], in0=ISC[:, Nk - 128:Nk], in1=K["cm"][:], op=ALU.add),
                          reads=[ISC, K["cm"]], writes=[ISC])
                    THR = ST[:, 7:8]
                    if qi >= 2:
                        sc.op("dve", lambda: nc.vector.tensor_reduce(out=ST[:, 0:1], in_=ISC[:, :Nk], axis=AX.X, op=ALU.max), reads=[ISC], writes=[ST])
                        sc.op("dve", lambda: nc.vector.tensor_reduce(out=ST[:, 1:2], in_=ISC[:, :Nk - 128], axis=AX.X, op=ALU.min), reads=[ISC], writes=[ST])
                        sc.op("dve", lambda: nc.vector.scalar_tensor_tensor(out=ST[:, 2:3], in0=ST[:, 0:1], scalar=1.0, in1=ST[:, 1:2],
                                                                           op0=ALU.add, op1=ALU.subtract), reads=[ST], writes=[ST])
                        sc.op("dve", lambda: nc.vector.tensor_scalar(out=W[:], in0=K["pow2"][:], scalar1=ST[:, 2:3], scalar2=None, op0=ALU.mult),
                              reads=[K["pow2"], ST], writes=[W])
                        sc.op("dve", lambda: nc.vector.tensor_tensor(out=ST[:, 3:4], in0=ST[:, 1:2], in1=W[:, 1:2], op=ALU.add), reads=[ST, W], writes=[ST])
                        for k in range(1, NBIS + 1):
                            yield
                            sc.op("dve", lambda: nc.vector.tensor_scalar(out=JUNK[:, :Nk], in0=ISC[:, :Nk], scalar1=ST[:, 3:4], scalar2=0.0,
                                                                        op0=ALU.is_ge, op1=ALU.add, accum_out=ST[:, 4:5]),
                                  reads=[ISC, ST], writes=[JUNK, ST])
                            sc.op("dve", lambda: nc.vector.tensor_scalar(out=ST[:, 5:6], in0=ST[:, 4:5], scalar1=255.5, scalar2=0.5,
                                                                        op0=ALU.is_ge, op1=ALU.subtract), reads=[ST], writes=[ST])
                            if k < NBIS:
                                sc.op("dve", lambda: nc.vector.scalar_tensor_tensor(out=ST[:, 3:4], in0=ST[:, 5:6], scalar=W[:, k:k + 1], in1=ST[:, 3:4],
                                                                                   op0=ALU.mult, op1=ALU.add), reads=[ST, W], writes=[ST])
                        sc.op("dve", lambda: nc.vector.tensor_scalar(out=ST[:, 6:7], in0=ST[:, 5:6], scalar1=0.5, scalar2=W[:, NBIS:NBIS + 1],
                                                                    op0=ALU.subtract, op1=ALU.mult), reads=[ST, W], writes=[ST])
                        sc.op("dve", lambda: nc.vector.tensor_tensor(out=THR, in0=ST[:, 3:4], in1=ST[:, 6:7], op=ALU.add), reads=[ST], writes=[ST])
                    else:
                        sc.op("dve", lambda: nc.vector.memset(THR, -1e29), reads=[], writes=[ST])

                    yield

                nch_ = len(chunks)

                def flush_map(pm_):
                    kd, hh, jj, sr, k0_, ntile_, last_ = pm_
                    pt = nxt(psT, "t")
                    for i in range(ntile_):
                        sc.op("pe", lambda: nc.tensor.transpose(pt[:, i, :], sr[:, i * 128:(i + 1) * 128], K["identb"][:]),
                              reads=[sr, K["identb"]], writes=[pt], nowaw=(i > 0))
                    ptb = nxt(PT, "pt")
                    ek = "act" if cnt["ev"] % 2 == 0 else "dve"
                    cnt["ev"] += 1
                    if ek == "act":
                        sc.op("act", lambda: nc.scalar.copy(ptb[:, 0:ntile_, :], pt[:, 0:ntile_, :]), reads=[pt], writes=[ptb])
                    else:
                        sc.op("dve", lambda: nc.vector.tensor_copy(ptb[:, 0:ntile_, :], pt[:, 0:ntile_, :]), reads=[pt], writes=[ptb])
                    for i in range(ntile_):
                        kt = k0_ // 128 + i
                        if kd == "a":
                            ob = psOab
                            o, rhsv, key = psOab[:, hh * 64:(hh + 1) * 64], VT[:, kt, 0:64], "ab"
                        elif kd == "s":
                            ob = psOab
                            o, rhsv, key = psOab[:, 256 + hh * 64:256 + (hh + 1) * 64], VT[:, kt, 64:128], "ab"
                        else:
                            m = hh * 2 + jj
                            ob = psOc[m // 4]
                            o, rhsv, key = ob[:, (m % 4) * 128:(m % 4 + 1) * 128], VT[:, kt, 192 + hh * 128:192 + (hh + 1) * 128], m // 4
                        fst = firstO[key]
                        firstO[key] = False
                        sc.op("pe", lambda: nc.tensor.matmul(o, lhsT=ptb[:, i, :], rhs=rhsv, start=fst, stop=(last_ and i == ntile_ - 1), skip_group_check=True),
                              reads=[ptb, VT], writes=[ob], nowaw=(not fst))

                def stageV(kinds_):
                  prev = None
                  for ci, (k0, w) in enumerate(chunks):
                      ntile = w // 128
                      wf = min(max(Nk - 256 - k0, 0), w)
                      wn = w - wf
                      no = (k0 + wf) - (Nk - 256)
                      if "a" in kinds_:
                          sc.op("dve", lambda: nc.vector.tensor_scalar(out=MAc[:, :w], in0=ISC[:, k0:k0 + w], scalar1=THR, scalar2=None, op0=ALU.is_ge),
                                reads=[ISC, ST], writes=[MAc])
                      maps = [("a", h, 0) for h in range(4)] + [("s", h, 0) for h in range(4)] + [("c", h, j) for h in range(4) for j in range(2)]
                      for mi, (kind, h, j) in enumerate(maps):
                          if kind not in kinds_:
                              continue
                          yield
                          ps = nxt(psS, "s")
                          if kind == "a":
                              lhsT, rhs, hd = qA[:, h, :], KT[0:64, 0, k0:k0 + w], h
                          elif kind == "s":
                              lhsT, rhs, hd = qB[64:128, h, :], KT[64:128, 0, k0:k0 + w], 4 + h
                          else:
                              lhsT, rhs, hd = qC[j * 64:(j + 1) * 64, h, :], CK[j * 64:(j + 1) * 64, h, k0:k0 + w], 8 + h
                          sc.op("pe", lambda: nc.tensor.matmul(ps[:, :w], lhsT=lhsT, rhs=rhs, start=True, stop=True),
                                reads=[qA, qB, qC, KT, CK], writes=[ps])
                          p = nxt(P, "p")
                          accf = SUMS[:, mi, 2 * ci:2 * ci + 1] if kind == "c" else None
                          accn = SUMS[:, mi, 2 * ci + 1:2 * ci + 2] if kind == "c" else None
                          if wf > 0 and not cfg.get("skipfar"):
                              if kind == "c":
                                  sc.op("act", lambda: nc.scalar.activation(out=p[:, :wf], in_=ps[:, :wf], func=AF.Exp, scale=0.125, accum_out=accf),
                                        reads=[ps], writes=[p, SUMS], nowaw=True)
                              else:
                                  sc.op("act", lambda: nc.scalar.activation(out=p[:, :wf], in_=ps[:, :wf], func=AF.Exp, scale=0.125),
                                        reads=[ps], writes=[p], nowaw=True)
                          if wn > 0 and not cfg.get("skipnear"):
                              tn = nxt(TMPN, "tn")
                              sc.op("dve", lambda: nc.vector.scalar_tensor_tensor(out=tn[:, :wn], in0=ps[:, wf:w], scalar=0.125, in1=K["near"][:, hd, no:no + wn],
                                                                                 op0=ALU.mult, op1=ALU.add), reads=[ps, K["near"]], writes=[tn])
                              if kind == "c":
                                  sc.op("act", lambda: nc.scalar.activation(out=p[:, wf:w], in_=tn[:, :wn], func=AF.Exp, accum_out=accn),
                                        reads=[tn], writes=[p, SUMS], nowaw=True)
                              else:
                                  sc.op("act", lambda: nc.scalar.activation(out=p[:, wf:w], in_=tn[:, :wn], func=AF.Exp),
                                        reads=[tn], writes=[p], nowaw=True)
                          if kind == "a":
                              pm = nxt(PM, "pm")
                              sc.op("dve", lambda: nc.vector.scalar_tensor_tensor(out=pm[:, :w], in0=p[:, :w], scalar=1.0, in1=MAc[:, :w], op0=ALU.mult, op1=ALU.mult,
                                                                                 accum_out=SUMS[:, mi, 2 * ci:2 * ci + 1]), reads=[p, MAc], writes=[pm, SUMS], nowaw=True)
                              src = pm
                          elif kind == "s":
                              pm = nxt(PM, "pm")
                              sc.op("dve", lambda: nc.vector.scalar_tensor_tensor(out=pm[:, :w].rearrange("p (j i) -> p j i", i=64),
                                                                                 in0=p[:, :w].rearrange("p (j i) -> p j i", i=64), scalar=1.0,
                                                                                 in1=BM[:, k0 // 64:(k0 + w) // 64].unsqueeze(2).to_broadcast([128, w // 64, 64]),
                                                                                 op0=ALU.mult, op1=ALU.mult, accum_out=SUMS[:, mi, 2 * ci:2 * ci + 1]),
                                    reads=[p, BM], writes=[pm, SUMS], nowaw=True)
                              src = pm
                          else:
                              src = p
                          if prev is not None and not cfg.get("nopv"):
                              flush_map(prev)
                          prev = (kind, h, j, src, k0, ntile, ci == nch_ - 1)
                  if prev is not None and not cfg.get("nopv"):
                      flush_map(prev)


                def genB():
                    sc.op("dve", lambda: nc.vector.tensor_scalar(out=CMK[:], in0=K["iota"][:, 0:256], scalar1=K["tc"][:, qi:qi + 1], scalar2=1.0, op0=ALU.is_gt, op1=ALU.subtract),
                          reads=[K["iota"], K["tc"]], writes=[CMK])
                    for h in range(4):
                        yield
                        ps = nxt(psS, "s")
                        sc.op("pe", lambda: nc.tensor.matmul(ps[:, :256], lhsT=qB[0:64, h, :], rhs=KC[:, b, :], start=True, stop=True),
                              reads=[qB, KC], writes=[ps])
                        sc.op("act", lambda: nc.scalar.activation(out=PCf[:, :255], in_=ps[:, :255], func=AF.Exp, scale=0.125), reads=[ps], writes=[PCf])
                        sc.op("dve", lambda: nc.vector.scalar_tensor_tensor(out=PCm[:, h, :255], in0=PCf[:, :255], scalar=-1.0, in1=CMK[:, :255],
                                                                           op0=ALU.mult, op1=ALU.mult, accum_out=CS[:, h:h + 1]),
                              reads=[PCf, CMK], writes=[PCm, CS], nowaw=True)
                    sc.op("dve", lambda: nc.vector.tensor_scalar(out=CS[:, 4:8], in0=CS[:, 0:4], scalar1=1e-30, scalar2=None, op0=ALU.max), reads=[CS], writes=[CS])
                    sc.op("dve", lambda: nc.vector.reciprocal(out=CS[:, 4:8], in_=CS[:, 4:8]), reads=[CS], writes=[CS])
                    for h in range(4):
                        sc.op("dve", lambda: nc.vector.tensor_scalar(out=PCn[:, h, :255], in0=PCm[:, h, :255], scalar1=CS[:, 4 + h:5 + h], scalar2=None,
                                                                    op0=ALU.mult), reads=[PCm, CS], writes=[PCn], nowaw=True)
                        if h == 0:
                            sc.op("dve", lambda: nc.vector.tensor_scalar(out=PCf[:, :255], in0=PCm[:, 0, :255], scalar1=CS[:, 4:5], scalar2=None,
                                                                        op0=ALU.mult), reads=[PCm, CS], writes=[PCf])
                        else:
                            sc.op("dve", lambda: nc.vector.scalar_tensor_tensor(out=PCf[:, :255], in0=PCm[:, h, :255], scalar=CS[:, 4 + h:5 + h],
                                                                               in1=PCf[:, :255], op0=ALU.mult, op1=ALU.add),
                                  reads=[PCm, CS, PCf], writes=[PCf])
                    sc.op("dve", lambda: nc.vector.tensor_copy(PCn[:, 4, :255], PCf[:, :255]), reads=[PCf], writes=[PCn], nowaw=True)
                    for g in range(2):
                        pt = nxt(psT, "t")
                        lo, hi = (0, 8) if g == 0 else (8, 10)
                        for i in range(lo, hi):
                            h, nch = i // 2, i % 2
                            sc.op("pe", lambda: nc.tensor.transpose(pt[:, i - lo, :], PCn[:, h, nch * 128:(nch + 1) * 128], K["identb"][:]),
                                  reads=[PCn, K["identb"]], writes=[pt], nowaw=(i > lo))
                        sc.op("act", lambda: nc.scalar.copy(PCT[:, lo:hi, :], pt[:, 0:hi - lo, :]), reads=[pt], writes=[PCT], nowaw=True)
                    first = True
                    for h in range(4):
                        for nch in range(2):
                            sc.op("pe", lambda: nc.tensor.matmul(psOab[:, h * 64:(h + 1) * 64], lhsT=PCT[:, h * 2 + nch, :], rhs=VC[:, b, nch, :],
                                                                 start=first, stop=(nch == 1), skip_group_check=True),
                                  reads=[PCT, VC], writes=[psOab], nowaw=(not first))
                            first = False
                    psI = nxt(psS, "s")
                    for nch in range(2):
                        sc.op("pe", lambda: nc.tensor.matmul(psI[:, 0:64], lhsT=PCT[:, 8 + nch, :], rhs=K["ovl"][:, nch, :], start=(nch == 0), stop=(nch == 1)),
                              reads=[PCT, K["ovl"]], writes=[psI], nowaw=(nch > 0))
                    sc.op("act", lambda: nc.scalar.copy(OCMP[:], psOab[:, 0:256]), reads=[psOab], writes=[OCMP])
                    sc.op("dve", lambda: nc.vector.tensor_tensor(out=IMP[:], in0=psI[:, 0:64], in1=selt[:, 0:64], op=ALU.mult), reads=[psI, selt], writes=[IMP])
                    sc.op("dve", lambda: nc.vector.tensor_tensor(out=IMP[:], in0=IMP[:], in1=selt[:, 64:128], op=ALU.add), reads=[IMP, selt], writes=[IMP])
                    J3 = JUNK[:, 0:4096].rearrange("p (j i) -> p j i", i=64)
                    sc.op("dve", lambda: nc.vector.tensor_tensor(out=J3, in0=IMP[:].unsqueeze(1).to_broadcast([128, 64, 64]),
                                                                in1=IMP[:].unsqueeze(2).to_broadcast([128, 64, 64]), op=ALU.is_gt),
                          reads=[IMP], writes=[JUNK])
                    sc.op("dve", lambda: nc.vector.tensor_reduce(out=RANK[:], in_=J3, axis=AX.X, op=ALU.add), reads=[JUNK], writes=[RANK])
                    sc.op("dve", lambda: nc.vector.scalar_tensor_tensor(out=BM[:], in0=RANK[:], scalar=15.5, in1=selt[:, 128:192], op0=ALU.is_lt, op1=ALU.mult),
                          reads=[RANK, selt], writes=[BM])

                    nwin = min(qi, 4) + 1
                    nnear = min(nwin, 2)
                    nfar = nwin - nnear
                    kt0 = qi - (nwin - 1)
                    for h in range(4):
                        yield
                        if nfar > 0:
                            ps = nxt(psS, "s")
                            wf = nfar * 128
                            sc.op("pe", lambda: nc.tensor.matmul(ps[:, :wf], lhsT=qB[0:64, h, :], rhs=KT[0:64, 1, kt0 * 128:kt0 * 128 + wf], start=True, stop=True),
                                  reads=[qB, KT], writes=[ps])
                            tn = nxt(TMPN, "tn")
                            sc.op("dve", lambda: nc.vector.scalar_tensor_tensor(out=tn[:, :wf], in0=ps[:, :wf], scalar=0.125, in1=K["wneg"][:, 384 - wf:384],
                                                                               op0=ALU.mult, op1=ALU.add), reads=[ps, K["wneg"]], writes=[tn])
                            sc.op("act", lambda: nc.scalar.activation(out=PW[:, 0:wf], in_=tn[:, :wf], func=AF.Exp,
                                                                      accum_out=WS[:, h:h + 1]), reads=[tn], writes=[PW, WS], nowaw=True)
                        else:
                            wf = 0
                            sc.op("dve", lambda: nc.vector.memset(WS[:, h:h + 1], 0.0), writes=[WS], nowaw=True)
                        ps = nxt(psS, "s")
                        wn = nnear * 128
                        kn0 = (qi - (nnear - 1)) * 128
                        sc.op("pe", lambda: nc.tensor.matmul(ps[:, :wn], lhsT=qB[0:64, h, :], rhs=KT[0:64, 1, kn0:kn0 + wn], start=True, stop=True),
                              reads=[qB, KT], writes=[ps])
                        tn = nxt(TMPN, "tn")
                        sc.op("dve", lambda: nc.vector.scalar_tensor_tensor(out=tn[:, :wn], in0=ps[:, :wn], scalar=0.125, in1=K["near"][:, 4 + h, 256 - wn:256],
                                                                           op0=ALU.mult, op1=ALU.add), reads=[ps, K["near"]], writes=[tn])
                        sc.op("act", lambda: nc.scalar.activation(out=PW[:, wf:wf + wn], in_=tn[:, :wn], func=AF.Exp, accum_out=WS[:, 4 + h:5 + h]),
                              reads=[tn], writes=[PW, WS], nowaw=True)
                        pt = nxt(psT, "t")
                        for i in range(nwin):
                            sc.op("pe", lambda: nc.tensor.transpose(pt[:, i, :], PW[:, i * 128:(i + 1) * 128], K["identb"][:]),
                                  reads=[PW, K["identb"]], writes=[pt], nowaw=(i > 0))
                        ptb = nxt(PT, "pt")
                        sc.op("act", lambda: nc.scalar.copy(ptb[:, 0:nwin, :], pt[:, 0:nwin, :]), reads=[pt], writes=[ptb])
                        for i in range(nwin):
                            fst = (h == 0 and i == 0)
                            sc.op("pe", lambda: nc.tensor.matmul(psOab[:, 256 + h * 64:256 + (h + 1) * 64], lhsT=ptb[:, i, :], rhs=VT[:, kt0 + i, 128:192],
                                                                 start=fst, stop=(i == nwin - 1), skip_group_check=True),
                                  reads=[ptb, VT], writes=[psOab], nowaw=(not fst))
                    sc.op("act", lambda: nc.scalar.copy(OWIN[:], psOab[:, 256:512]), reads=[psOab], writes=[OWIN])
                    sc.op("dve", lambda: nc.vector.tensor_tensor(out=WS[:, 0:4], in0=WS[:, 0:4], in1=WS[:, 4:8], op=ALU.add), reads=[WS], writes=[WS])

                    for _ in stageV("c"):
                        yield

                gens = [genA(), genB()]
                while gens:
                    for g_ in list(gens):
                        try:
                            next(g_)
                        except StopIteration:
                            gens.remove(g_)
                for _ in stageV("as"):
                    pass

                if cfg.get('prof_q') == (b, qi):
                    sc.mark('q_s6_final')
                sc.op("dve", lambda: nc.vector.tensor_reduce(out=TOT[:], in_=SUMS[:], axis=AX.X, op=ALU.add), reads=[SUMS], writes=[TOT])
                sc.op("dve", lambda: nc.vector.tensor_scalar(out=RCP[:], in0=TOT[:], scalar1=1e-30, scalar2=None, op0=ALU.max), reads=[TOT], writes=[RCP])
                sc.op("dve", lambda: nc.vector.reciprocal(out=RCP[:], in_=RCP[:]), reads=[RCP], writes=[RCP])
                sc.op("dve", lambda: nc.vector.reciprocal(out=WS[:, 4:8], in_=WS[:, 0:4]), reads=[WS], writes=[WS])
                for h in range(4):
                    sc.op("dve", lambda: nc.vector.tensor_scalar(out=out[:, h * 64:(h + 1) * 64], in0=psOab[:, h * 64:(h + 1) * 64], scalar1=RCP[:, h:h + 1],
                                                                scalar2=None, op0=ALU.mult), reads=[psOab, RCP], writes=[out], nowaw=(h > 0))
                for h in range(4):
                    g0, g1, g2 = (smt[:, 4 + 3 * h + k:5 + 3 * h + k] for k in range(3))
                    sc.op("dve", lambda: nc.vector.tensor_scalar(out=ST[:, 8:9], in0=RCP[:, 4 + h:5 + h], scalar1=g1, scalar2=None, op0=ALU.mult), reads=[RCP, smt], writes=[ST])
                    sc.op("dve", lambda: nc.vector.tensor_scalar(out=ST[:, 9:10], in0=WS[:, 4 + h:5 + h], scalar1=g2, scalar2=None, op0=ALU.mult), reads=[WS, smt], writes=[ST])
                    oslice = out[:, 256 + h * 64:256 + (h + 1) * 64]
                    sc.op("dve", lambda: nc.vector.tensor_scalar(out=oslice, in0=OCMP[:, h * 64:(h + 1) * 64], scalar1=g0, scalar2=None, op0=ALU.mult),
                          reads=[OCMP, smt], writes=[out], nowaw=True)
                    sc.op("dve", lambda: nc.vector.scalar_tensor_tensor(out=oslice, in0=psOab[:, 256 + h * 64:256 + (h + 1) * 64], scalar=ST[:, 8:9], in1=oslice,
                                                                       op0=ALU.mult, op1=ALU.add), reads=[psOab, ST, out], writes=[out])
                    sc.op("dve", lambda: nc.vector.scalar_tensor_tensor(out=oslice, in0=OWIN[:, h * 64:(h + 1) * 64], scalar=ST[:, 9:10], in1=oslice,
                                                                       op0=ALU.mult, op1=ALU.add), reads=[OWIN, ST, out], writes=[out])
                for h in range(4):
                    m0, m1 = 8 + h * 2, 9 + h * 2
                    ob0, ob1 = psOc[(h * 2) // 4], psOc[(h * 2 + 1) // 4]
                    o0 = ob0[:, ((h * 2) % 4) * 128:((h * 2) % 4 + 1) * 128]
                    o1 = ob1[:, ((h * 2 + 1) % 4) * 128:((h * 2 + 1) % 4 + 1) * 128]
                    sc.op("dve", lambda: nc.vector.tensor_scalar(out=ST[:, 10:11], in0=RCP[:, m1:m1 + 1], scalar1=LAM[:, 1:2], scalar2=None, op0=ALU.mult),
                          reads=[RCP, LAM], writes=[ST])
                    sc.op("dve", lambda: nc.vector.tensor_scalar(out=OCt[:], in0=o0, scalar1=RCP[:, m0:m0 + 1], scalar2=None, op0=ALU.mult),
                          reads=[ob0, RCP], writes=[OCt])
                    sc.op("dve", lambda: nc.vector.scalar_tensor_tensor(out=OCt[:], in0=o1, scalar=ST[:, 10:11], in1=OCt[:], op0=ALU.mult, op1=ALU.add),
                          reads=[ob1, ST, OCt], writes=[OCt])
                    sc.op("dve", lambda: nc.vector.scalar_tensor_tensor(out=OCj[:], in0=OCt[:], scalar=1.0, in1=OCt[:], op0=ALU.mult, op1=ALU.mult,
                                                                       accum_out=ST[:, 11:12]), reads=[OCt], writes=[OCj, ST])
                    sc.op("dve", lambda: nc.vector.tensor_scalar(out=ST[:, 12:13], in0=ST[:, 11:12], scalar1=1.0 / 128, scalar2=1e-5, op0=ALU.mult, op1=ALU.add),
                          reads=[ST], writes=[ST])
                    sc.op("act", lambda: nc.scalar.activation(out=ST[:, 13:14], in_=ST[:, 12:13], func=AF.Ln), reads=[ST], writes=[ST])
                    sc.op("act", lambda: nc.scalar.activation(out=ST[:, 14:15], in_=ST[:, 13:14], func=AF.Exp, scale=-0.5), reads=[ST], writes=[ST])
                    sc.op("dve", lambda: nc.vector.scalar_tensor_tensor(out=out[:, 512 + h * 128:512 + (h + 1) * 128], in0=OCt[:], scalar=ST[:, 14:15], in1=DNG[:],
                                                                       op0=ALU.mult, op1=ALU.mult), reads=[OCt, ST, DNG], writes=[out], nowaw=True)
                sc.dma("sp", OABC[tok0:tok0 + 128, :], out[:], reads=[out], writes=[OABC], track=out)
                if cfg.get('prof_q') == (b, qi):
                    sc.mark('L%d_phase_attn_rest' % l)
        allb = ([KT, CK, VT, ISC, JUNK, AW, SGN, ST, W, CMK, PCf, PCm, PCn, PCT, CS, OCMP, OWIN, IMP, RANK, BM, PW, WS, MAc, SUMS, TOT, RCP, LAM, DL, DNG,
                 OCt, OCj, TAB31, psOab] + QI + QA + QB + QC + SMt + SELt + OUT + RT + TMPN + P + PM + PT + psS + psT + psOc)
        sc.barrier_on(allb)
        sched_release(sc, allb)


def layer_norm_tile(sc, z, zc, junk, o, ST2, Gt, Bt):
    nc = sc.nc
    sc.op("dve", lambda: nc.vector.tensor_reduce(out=ST2[:, 0:1], in_=z[:], axis=AX.X, op=ALU.add), reads=[z], writes=[ST2])
    sc.op("dve", lambda: nc.vector.tensor_scalar(out=ST2[:, 1:2], in0=ST2[:, 0:1], scalar1=1.0 / D, scalar2=None, op0=ALU.mult), reads=[ST2], writes=[ST2])
    sc.op("dve", lambda: nc.vector.tensor_scalar(out=zc[:], in0=z[:], scalar1=ST2[:, 1:2], scalar2=None, op0=ALU.subtract), reads=[z, ST2], writes=[zc])
    sc.op("dve", lambda: nc.vector.scalar_tensor_tensor(out=junk[:], in0=zc[:], scalar=1.0, in1=zc[:], op0=ALU.mult, op1=ALU.mult,
                                                       accum_out=ST2[:, 2:3]), reads=[zc], writes=[junk, ST2])
    sc.op("dve", lambda: nc.vector.tensor_scalar(out=ST2[:, 3:4], in0=ST2[:, 2:3], scalar1=1.0 / D, scalar2=1e-5, op0=ALU.mult, op1=ALU.add),
          reads=[ST2], writes=[ST2])
    sc.op("act", lambda: nc.scalar.activation(out=ST2[:, 4:5], in_=ST2[:, 3:4], func=AF.Ln), reads=[ST2], writes=[ST2])
    sc.op("act", lambda: nc.scalar.activation(out=ST2[:, 5:6], in_=ST2[:, 4:5], func=AF.Exp, scale=-0.5), reads=[ST2], writes=[ST2])
    sc.op("dve", lambda: nc.vector.scalar_tensor_tensor(out=zc[:], in0=zc[:], scalar=ST2[:, 5:6], in1=Gt[:], op0=ALU.mult, op1=ALU.mult),
          reads=[zc, ST2, Gt], writes=[zc])
    sc.op("pool", lambda: nc.gpsimd.tensor_tensor(out=o[:], in0=zc[:], in1=Bt[:], op=ALU.add), reads=[zc, Bt], writes=[o])


def phase_tail(sc, cfg, l, K, OABC, MG, xsrc, wba_d, wbb_d, wbc_d, wo_d, ln_g, ln_b, G1P, X1):
    nc = sc.nc
    nseq, seq = cfg["nseq"], cfg["seq"]
    ntok = nseq * seq
    DN_ALPHA = (2 * DEPTH) ** 0.25
    with ExitStack() as es:
        wb = sc.sbuf("wbr", [128, 8, 1024], BF16, es)
        wo = sc.sbuf("wo", [128, 8, 1024], BF16, es)
        sc.dma("pool", wb[:, 0:2, :], wba_d[l].rearrange("(c p) n -> p c n", p=128), reads=[wba_d], writes=[wb], track=wb)
        sc.dma("pool", wb[:, 2:4, :], wbb_d[l].rearrange("(c p) n -> p c n", p=128), reads=[wbb_d], writes=[wb], track=wb)
        sc.dma("pool", wb[:, 4:8, :], wbc_d[l].rearrange("(c p) n -> p c n", p=128), reads=[wbc_d], writes=[wb], track=wb)
        sc.dma("pool", wo[:], wo_d[l].rearrange("(c p) n -> p c n", p=128), reads=[wo_d], writes=[wo], track=wo)
        Gt = sc.sbuf("lnG", [128, 1024], F32, es)
        Bt = sc.sbuf("lnB", [128, 1024], F32, es)
        sc.dma("sp", Gt[:], ln_g[l:l + 1, :].partition_broadcast(128), reads=[ln_g], writes=[Gt], track=Gt)
        sc.dma("sp", Bt[:], ln_b[l:l + 1, :].partition_broadcast(128), reads=[ln_b], writes=[Bt], track=Bt)
        OA = [sc.sbuf("tOA", [128, 1024], F32, es) for _ in range(2)]
        MGt = [sc.sbuf("tMG", [128, 3072], F32, es) for _ in range(2)]
        XT = [sc.sbuf("tX", [128, 1024], F32, es) for _ in range(2)]
        OB_2 = [sc.sbuf("tOB", [128, 1024], BF16, es) for _ in range(2)]
        oT_2 = [sc.sbuf("toT", [128, 8, 128], BF16, es) for _ in range(2)]
        M_2 = [sc.sbuf("tM", [128, 1024], F32, es) for _ in range(2)]
        TMP_2 = [sc.sbuf("tTMP", [128, 1024], F32, es) for _ in range(2)]
        MB_2 = [sc.sbuf("tMB", [128, 1024], BF16, es) for _ in range(2)]
        mT_2 = [sc.sbuf("tmT", [128, 8, 128], BF16, es) for _ in range(2)]
        Z_2 = [sc.sbuf("tZ", [128, 1024], F32, es) for _ in range(2)]
        ZC_2 = [sc.sbuf("tZC", [128, 1024], F32, es) for _ in range(2)]
        O = [sc.sbuf("tO", [128, 1024], F32, es) for _ in range(2)]
        ST2_2 = [sc.sbuf("tST", [128, 8], F32, es) for _ in range(2)]
        psT = [sc.psum("tpsT", [128, 8, 128], BF16, es) for _ in range(2)]
        psY = [sc.psum("tpsY", [128, 512], F32, es) for _ in range(6)]
        yi = 0
        for tt in range(ntok // 128):
            OB = OB_2[tt % 2]
            oT = oT_2[tt % 2]
            M = M_2[tt % 2]
            TMP = TMP_2[tt % 2]
            MB = MB_2[tt % 2]
            mT = mT_2[tt % 2]
            Z = Z_2[tt % 2]
            ZC = ZC_2[tt % 2]
            ST2 = ST2_2[tt % 2]
            tok0 = tt * 128
            b = tok0 // seq
            oa, mg, xt, o = OA[tt % 2], MGt[tt % 2], XT[tt % 2], O[tt % 2]
            sc.dma("sp", oa[:], OABC[tok0:tok0 + 128, :], reads=[OABC], writes=[oa], track=oa, nowaw=False)
            sc.dma("sp", mg[:], MG[tok0:tok0 + 128, :], reads=[MG], writes=[mg], track=mg, nowaw=False)
            sc.dma("sp", xt[:], xsrc[tok0:tok0 + 128, :], reads=[xsrc], writes=[xt], track=xt, nowaw=False)
            sc.op("act", lambda: nc.scalar.copy(OB[:], oa[:]), reads=[oa], writes=[OB])
            pt = psT[0]
            for c in range(8):
                sc.op("pe", lambda: nc.tensor.transpose(pt[:, c, :], OB[:, c * 128:(c + 1) * 128], K["identb"][:]), reads=[OB, K["identb"]], writes=[pt], nowaw=(c > 0))
            sc.op("act", lambda: nc.scalar.copy(oT[:], pt[:]), reads=[pt], writes=[oT])
            for hf in range(2):
                cs = slice(hf * 512, (hf + 1) * 512)
                ys = []
                for (k0, k1) in ((0, 2), (2, 4), (4, 8)):
                    py = psY[yi % 6]; yi += 1
                    for kc in range(k0, k1):
                        sc.op("pe", lambda: nc.tensor.matmul(py[:, :], lhsT=oT[:, kc, :], rhs=wb[:, kc, cs], start=(kc == k0), stop=(kc == k1 - 1)),
                              reads=[oT, wb], writes=[py], nowaw=(kc > k0))
                    ys.append(py)
                sc.op("dve", lambda: nc.vector.tensor_tensor(out=M[:, cs], in0=ys[0][:, :], in1=mg[:, hf * 512:(hf + 1) * 512], op=ALU.mult),
                      reads=[ys[0], mg], writes=[M], nowaw=(hf > 0))
                sc.op("dve", lambda: nc.vector.tensor_tensor(out=TMP[:, cs], in0=ys[1][:, :], in1=mg[:, 1024 + hf * 512:1024 + (hf + 1) * 512], op=ALU.mult),
                      reads=[ys[1], mg], writes=[TMP], nowaw=(hf > 0))
                sc.op("pool", lambda: nc.gpsimd.tensor_tensor(out=M[:, cs], in0=M[:, cs], in1=TMP[:, cs], op=ALU.add), reads=[M, TMP], writes=[M])
                sc.op("dve", lambda: nc.vector.tensor_tensor(out=TMP[:, cs], in0=ys[2][:, :], in1=mg[:, 2048 + hf * 512:2048 + (hf + 1) * 512], op=ALU.mult),
                      reads=[ys[2], mg], writes=[TMP])
                sc.op("pool", lambda: nc.gpsimd.tensor_tensor(out=MB[:, cs], in0=M[:, cs], in1=TMP[:, cs], op=ALU.add), reads=[M, TMP], writes=[MB], nowaw=(hf > 0))
            pt = psT[1]
            for c in range(8):
                sc.op("pe", lambda: nc.tensor.transpose(pt[:, c, :], MB[:, c * 128:(c + 1) * 128], K["identb"][:]), reads=[MB, K["identb"]], writes=[pt], nowaw=(c > 0))
            sc.op("act", lambda: nc.scalar.copy(mT[:], pt[:]), reads=[pt], writes=[mT])
            for hf in range(2):
                cs = slice(hf * 512, (hf + 1) * 512)
                py = psY[yi % 6]; yi += 1
                for kc in range(8):
                    sc.op("pe", lambda: nc.tensor.matmul(py[:, :], lhsT=mT[:, kc, :], rhs=wo[:, kc, cs], start=(kc == 0), stop=(kc == 7)),
                          reads=[mT, wo], writes=[py], nowaw=(kc > 0))
                sc.op("dve", lambda: nc.vector.tensor_tensor(out=TMP[:, cs], in0=py[:, :], in1=G1P[:, b, cs], op=ALU.mult), reads=[py, G1P], writes=[TMP])
                sc.op("dve", lambda: nc.vector.scalar_tensor_tensor(out=Z[:, cs], in0=xt[:, cs], scalar=DN_ALPHA, in1=TMP[:, cs], op0=ALU.mult, op1=ALU.add),
                      reads=[xt, TMP], writes=[Z], nowaw=(hf > 0))
            layer_norm_tile(sc, Z, ZC, TMP, o, ST2, Gt, Bt)
            sc.dma("sp", X1[tok0:tok0 + 128, :], o[:], reads=[o], writes=[X1], track=o)
        allb = [wb, wo, Gt, Bt, OB_2[0], OB_2[1], oT_2[0], oT_2[1], M_2[0], M_2[1], TMP_2[0], TMP_2[1], MB_2[0], MB_2[1], mT_2[0], mT_2[1], Z_2[0], Z_2[1], ZC_2[0], ZC_2[1], ST2_2[0], ST2_2[1]] + OA + MGt + XT + O + psT + psY
        sc.barrier_on(allb)
        sched_release(sc, allb)


def phase_moe_prep(sc, cfg, l, K, X1, opsc, shf, router_w, router_b, b_down, H2T, CWD, YACC):
    nc = sc.nc
    nseq, seq = cfg["nseq"], cfg["seq"]
    ntok = nseq * seq
    with ExitStack() as es:
        rw = sc.sbuf("rw", [128, 8, 32], F32, es)
        rb = sc.sbuf("rb", [128, 32], F32, es)
        bd = sc.sbuf("bd", [32, 1024], F32, es)
        sc.dma("sp", rw[:], router_w[l].rearrange("(c p) e -> p c e", p=128), reads=[router_w], writes=[rw], track=rw)
        sc.dma("sp", rb[:], router_b[l:l + 1, :].partition_broadcast(128), reads=[router_b], writes=[rb], track=rb)
        sc.dma("sp", bd[:], b_down[l], reads=[b_down], writes=[bd], track=bd)
        XT = [sc.sbuf("mX", [128, 1024], F32, es) for _ in range(2)]
        HF = sc.sbuf("mHF", [128, 8, 128], F32, es)
        HB = [sc.sbuf("mHB", [128, 8, 128], BF16, es) for _ in range(2)]
        Lg = sc.sbuf("mL", [128, 32], F32, es)
        J3 = sc.sbuf("mJ3", [128, 32, 32], F32, es)
        RK = sc.sbuf("mRK", [128, 32], F32, es)
        EX = sc.sbuf("mEX", [128, 32], F32, es)
        CW = [sc.sbuf("mCW", [128, 32], F32, es) for _ in range(2)]
        CWT = sc.sbuf("mCWT", [32, 128], F32, es)
        YB = [sc.sbuf("mYB", [128, 1024], F32, es) for _ in range(2)]
        ST = sc.sbuf("mST", [128, 8], F32, es)
        ps = [sc.psum("mps", [128, 512], F32, es) for _ in range(6)]
        pi = 0
        for tt in range(ntok // 128):
            tok0 = tt * 128
            b = tok0 // seq
            xt, hb, cw, yb = XT[tt % 2], HB[tt % 2], CW[tt % 2], YB[tt % 2]
            sc.dma("sp", xt[:], X1[tok0:tok0 + 128, :], reads=[X1], writes=[xt], track=xt, nowaw=False)
            for g in range(2):
                p = ps[pi % 6]; pi += 1
                for c4 in range(4):
                    c = g * 4 + c4
                    sc.op("pe", lambda: nc.tensor.transpose(p[:, c4 * 128:(c4 + 1) * 128], xt[:, c * 128:(c + 1) * 128], K["ident"][:]),
                          reads=[xt, K["ident"]], writes=[p], nowaw=(c4 > 0))
                for c4 in range(4):
                    c = g * 4 + c4
                    sc.op("act", lambda: nc.scalar.activation(out=HF[:, c, :], in_=p[:, c4 * 128:(c4 + 1) * 128], func=AF.Identity,
                                                              bias=shf[:, c, b:b + 1], scale=opsc[:, c, b:b + 1]),
                          reads=[p, shf, opsc], writes=[HF], nowaw=(c > 0))
            sc.op("dve", lambda: nc.vector.tensor_copy(hb[:], HF[:]), reads=[HF], writes=[hb])
            sc.dma("sp", H2T[:, tok0:tok0 + 128].rearrange("(c p) t -> p c t", p=128), hb[:], reads=[hb], writes=[H2T], track=hb)
            p = ps[pi % 6]; pi += 1
            for c in range(8):
                sc.op("pe", lambda: nc.tensor.matmul(p[:, 0:32], lhsT=HF[:, c, :], rhs=rw[:, c, :], start=(c == 0), stop=(c == 7)),
                      reads=[HF, rw], writes=[p], nowaw=(c > 0))
            sc.op("dve", lambda: nc.vector.tensor_tensor(out=Lg[:], in0=p[:, 0:32], in1=rb[:], op=ALU.add), reads=[p, rb], writes=[Lg])
            sc.op("dve", lambda: nc.vector.tensor_tensor(out=J3[:], in0=Lg[:].unsqueeze(1).to_broadcast([128, 32, 32]),
                                                        in1=Lg[:].unsqueeze(2).to_broadcast([128, 32, 32]), op=ALU.is_gt), reads=[Lg], writes=[J3])
            sc.op("dve", lambda: nc.vector.tensor_reduce(out=RK[:], in_=J3[:], axis=AX.X, op=ALU.add), reads=[J3], writes=[RK])
            sc.op("dve", lambda: nc.vector.tensor_reduce(out=ST[:, 0:1], in_=Lg[:], axis=AX.X, op=ALU.max), reads=[Lg], writes=[ST])
            sc.op("dve", lambda: nc.vector.tensor_scalar(out=ST[:, 1:2], in0=ST[:, 0:1], scalar1=-1.0, scalar2=None, op0=ALU.mult), reads=[ST], writes=[ST])
            sc.op("act", lambda: nc.scalar.activation(out=EX[:], in_=Lg[:], func=AF.Exp, bias=ST[:, 1:2]), reads=[Lg, ST], writes=[EX])
            sc.op("dve", lambda: nc.vector.tensor_scalar(out=RK[:], in0=RK[:], scalar1=3.5, scalar2=None, op0=ALU.is_lt), reads=[RK], writes=[RK])
            sc.op("dve", lambda: nc.vector.scalar_tensor_tensor(out=EX[:], in0=EX[:], scalar=1.0, in1=RK[:], op0=ALU.mult, op1=ALU.mult,
                                                               accum_out=ST[:, 2:3]), reads=[EX, RK], writes=[EX, ST])
            sc.op("dve", lambda: nc.vector.reciprocal(out=ST[:, 3:4], in_=ST[:, 2:3]), reads=[ST], writes=[ST])
            sc.op("dve", lambda: nc.vector.tensor_scalar(out=cw[:], in0=EX[:], scalar1=ST[:, 3:4], scalar2=None, op0=ALU.mult), reads=[EX, ST], writes=[cw])
            sc.dma("sp", CWD[tok0:tok0 + 128, :], cw[:], reads=[cw], writes=[CWD], track=cw)
            p = ps[pi % 6]; pi += 1
            sc.op("pe", lambda: nc.tensor.transpose(p[:32, 0:128], cw[:, :], K["ident"][:]), reads=[cw, K["ident"]], writes=[p])
            sc.op("act", lambda: nc.scalar.copy(CWT[:], p[:32, 0:128]), reads=[p], writes=[CWT])
            for hf in range(2):
                p = ps[pi % 6]; pi += 1
                sc.op("pe", lambda: nc.tensor.matmul(p[:, :], lhsT=CWT[:, :], rhs=bd[:, hf * 512:(hf + 1) * 512], start=True, stop=True),
                      reads=[CWT, bd], writes=[p])
                sc.op("act", lambda: nc.scalar.copy(yb[:, hf * 512:(hf + 1) * 512], p[:, :]), reads=[p], writes=[yb], nowaw=(hf > 0))
            sc.dma("sp", YACC[tok0:tok0 + 128, :], yb[:], reads=[yb], writes=[YACC], track=yb)
        allb = [rw, rb, bd, HF, Lg, J3, RK, EX, CWT, ST] + XT + HB + CW + YB + ps
        sc.barrier_on(allb)
        sched_release(sc, allb)


def phase_moe_experts(sc, cfg, l, H2T, CWD, YACC, w_gu, b_guT, w_down):
    nc = sc.nc
    nseq, seq = cfg["nseq"], cfg["seq"]
    ntok = nseq * seq
    CH = min(1024, ntok)
    ntile = CH // 128
    nsb = CH // 512
    ne = cfg.get("n_exp", 32)
    with ExitStack() as es:
        hT = sc.sbuf("eh", [128, 8, CH], BF16, es)
        ACC = sc.sbuf("eACC", [128, ntile, 1024], F32, es)
        CWc = sc.sbuf("eCW", [128, ntile, 32], F32, es)
        Wgu = [sc.sbuf("eWgu", [128, 8, 2048], BF16, es) for _ in range(2)]
        Wd = [sc.sbuf("eWd", [128, 8, 1024], BF16, es) for _ in range(2)]
        BG = [sc.sbuf("eBG", [128, 16], F32, es) for _ in range(2)]
        actT = [sc.sbuf("eact", [128, 8, 512], BF16, es) for _ in range(2)]
        G = [sc.sbuf("eG", [128, 512], F32, es) for _ in range(2)]
        Sg = [sc.sbuf("eS", [128, 512], F32, es) for _ in range(2)]
        U = [sc.sbuf("eU", [128, 512], F32, es) for _ in range(2)]
        psG = [sc.psum("epsG", [128, 512], F32, es) for _ in range(5)]
        psD = [sc.psum("epsD", [128, 512], F32, es) for _ in range(3)]
        gi = di = ai = ti = 0
        for ch in range(ntok // CH):
            t0 = ch * CH
            sc.dma("sp", hT[:], H2T[:, t0:t0 + CH].rearrange("(c p) t -> p c t", p=128), reads=[H2T], writes=[hT], track=hT, nowaw=False)
            sc.dma("sp", ACC[:], YACC[t0:t0 + CH, :].rearrange("(n p) d -> p n d", p=128), reads=[YACC], writes=[ACC], track=ACC, nowaw=False)
            sc.dma("sp", CWc[:], CWD[t0:t0 + CH, :].rearrange("(n p) e -> p n e", p=128), reads=[CWD], writes=[CWc], track=CWc, nowaw=False)
            for e in range(ne):
                k = (ch * ne + e) % 2
                wg, wd, bg = Wgu[k], Wd[k], BG[k]
                for c2 in range(0, 8, 2):
                    sc.dma("pool", wg[:, c2:c2 + 2, :], w_gu[l, e, c2 * 128:(c2 + 2) * 128, :].rearrange("(c p) n -> p c n", p=128),
                           reads=[w_gu], writes=[wg], track=wg, nowaw=(c2 > 0))
                for c4 in range(0, 8, 4):
                    sc.dma("pool", wd[:, c4:c4 + 4, :], w_down[l, e, c4 * 128:(c4 + 4) * 128, :].rearrange("(c p) n -> p c n", p=128),
                           reads=[w_down], writes=[wd], track=wd, nowaw=(c4 > 0))
                sc.dma("sp", bg[:], b_guT[l, e], reads=[b_guT], writes=[bg], track=bg, nowaw=False)
                for sb in range(nsb):
                    at = actT[ai % 2]; ai += 1
                    for fc in range(8):
                        pg = psG[gi % 5]; gi += 1
                        pu = psG[gi % 5]; gi += 1
                        for which, pp in ((0, pg), (1, pu)):
                            for kc in range(8):
                                lhsT = wg[:, kc, :].rearrange("p (f two) -> p two f", two=2)[:, which, fc * 128:(fc + 1) * 128]
                                sc.op("pe", lambda: nc.tensor.matmul(pp[:, :], lhsT=lhsT, rhs=hT[:, kc, sb * 512:(sb + 1) * 512],
                                                                     start=(kc == 0), stop=(kc == 7)), reads=[wg, hT], writes=[pp], nowaw=(kc > 0))
                        g_, s_, u_ = G[ti % 2], Sg[ti % 2], U[ti % 2]
                        ti += 1
                        sc.op("dve", lambda: nc.vector.tensor_scalar(out=g_[:], in0=pg[:, :], scalar1=bg[:, fc:fc + 1], scalar2=7.0, op0=ALU.add, op1=ALU.min),
                              reads=[pg, bg], writes=[g_])
                        sc.op("act", lambda: nc.scalar.activation(out=s_[:], in_=g_[:], func=AF.Sigmoid, scale=1.702), reads=[g_], writes=[s_])
                        sc.op("dve", lambda: nc.vector.tensor_scalar(out=u_[:], in0=pu[:, :], scalar1=bg[:, 8 + fc:9 + fc], scalar2=7.0, op0=ALU.add, op1=ALU.min),
                              reads=[pu, bg], writes=[u_])
                        sc.op("pool", lambda: nc.gpsimd.tensor_scalar(out=u_[:], in0=u_[:], scalar1=-7.0, scalar2=1.0, op0=ALU.max, op1=ALU.add),
                              reads=[u_], writes=[u_])
                        sc.op("pool", lambda: nc.gpsimd.tensor_tensor(out=g_[:], in0=g_[:], in1=s_[:], op=ALU.mult), reads=[g_, s_], writes=[g_])
                        sc.op("pool", lambda: nc.gpsimd.tensor_tensor(out=at[:, fc, :], in0=g_[:], in1=u_[:], op=ALU.mult), reads=[g_, u_], writes=[at], nowaw=(fc > 0))
                    for tl in range(4):
                        tix = sb * 4 + tl
                        for hf in range(2):
                            pd = psD[di % 3]; di += 1
                            for fc in range(8):
                                sc.op("pe", lambda: nc.tensor.matmul(pd[:, :], lhsT=at[:, fc, tl * 128:(tl + 1) * 128], rhs=wd[:, fc, hf * 512:(hf + 1) * 512],
                                                                     start=(fc == 0), stop=(fc == 7)), reads=[at, wd], writes=[pd], nowaw=(fc > 0))
                            sc.op("dve", lambda: nc.vector.scalar_tensor_tensor(out=ACC[:, tix, hf * 512:(hf + 1) * 512], in0=pd[:, :], scalar=CWc[:, tix, e:e + 1],
                                                                               in1=ACC[:, tix, hf * 512:(hf + 1) * 512], op0=ALU.mult, op1=ALU.add),
                                  reads=[pd, CWc, ACC], writes=[ACC])
            sc.dma("sp", YACC[t0:t0 + CH, :].rearrange("(n p) d -> p n d", p=128), ACC[:], reads=[ACC], writes=[YACC], track=ACC)
        allb = [hT, ACC, CWc] + Wgu + Wd + BG + actT + G + Sg + U + psG + psD
        sc.barrier_on(allb)
        sched_release(sc, allb)


def phase_moe_final(sc, cfg, l, X1, YACC, G1P, ln_g, ln_b, XOUT):
    nc = sc.nc
    nseq, seq = cfg["nseq"], cfg["seq"]
    ntok = nseq * seq
    DN_ALPHA = (2 * DEPTH) ** 0.25
    with ExitStack() as es:
        Gt = sc.sbuf("fG", [128, 1024], F32, es)
        Bt = sc.sbuf("fB", [128, 1024], F32, es)
        sc.dma("sp", Gt[:], ln_g[l:l + 1, :].partition_broadcast(128), reads=[ln_g], writes=[Gt], track=Gt)
        sc.dma("sp", Bt[:], ln_b[l:l + 1, :].partition_broadcast(128), reads=[ln_b], writes=[Bt], track=Bt)
        XT = [sc.sbuf("fX", [128, 1024], F32, es) for _ in range(2)]
        YT = [sc.sbuf("fY", [128, 1024], F32, es) for _ in range(2)]
        Z = sc.sbuf("fZ", [128, 1024], F32, es)
        ZC = sc.sbuf("fZC", [128, 1024], F32, es)
        TMP = sc.sbuf("fT", [128, 1024], F32, es)
        O = [sc.sbuf("fO", [128, 1024], F32, es) for _ in range(2)]
        ST2 = sc.sbuf("fST", [128, 8], F32, es)
        for tt in range(ntok // 128):
            tok0 = tt * 128
            b = tok0 // seq
            xt, yt, o = XT[tt % 2], YT[tt % 2], O[tt % 2]
            sc.dma("sp", xt[:], X1[tok0:tok0 + 128, :], reads=[X1], writes=[xt], track=xt, nowaw=False)
            sc.dma("sp", yt[:], YACC[tok0:tok0 + 128, :], reads=[YACC], writes=[yt], track=yt, nowaw=False)
            sc.op("pool", lambda: nc.gpsimd.tensor_tensor(out=TMP[:], in0=yt[:], in1=G1P[:, b, :], op=ALU.mult), reads=[yt, G1P], writes=[TMP])
            sc.op("dve", lambda: nc.vector.scalar_tensor_tensor(out=Z[:], in0=xt[:], scalar=DN_ALPHA, in1=TMP[:], op0=ALU.mult, op1=ALU.add),
                  reads=[xt, TMP], writes=[Z])
            layer_norm_tile(sc, Z, ZC, TMP, o, ST2, Gt, Bt)
            sc.dma("sp", XOUT[tok0:tok0 + 128, :], o[:], reads=[o], writes=[XOUT], track=o)
        allb = [Gt, Bt, Z, ZC, TMP, ST2] + XT + YT + O
        sc.barrier_on(allb)
        sched_release(sc, allb)


W_SPECS = [("rel_bias", [32, 12]), ("mod_attn_w", [DEPTH, D, 3 * D]), ("mod_attn_b", [DEPTH, 3 * D]), ("w_in", [DEPTH, D, D_IN]),
           ("cmp_w1", [DEPTH, 2, 2048, 256]), ("cmp_w2", [DEPTH, 2, 256, 64]), ("diff_lambda", [DEPTH, 4, 64]),
           ("diff_norm_g", [DEPTH, 128]), ("w_branch_a", [DEPTH, 256, D]), ("w_branch_b", [DEPTH, 256, D]),
           ("w_branch_c", [DEPTH, 512, D]), ("w_out", [DEPTH, D, D]), ("ln1_g", [DEPTH, D]), ("ln1_b", [DEPTH, D]),
           ("mod_ffn_w", [DEPTH, D, 3 * D]), ("mod_ffn_b", [DEPTH, 3 * D]), ("router_w", [DEPTH, D, 32]), ("router_b", [DEPTH, 32]),
           ("exp_w_gu", [DEPTH, 32, D, 2 * D]), ("exp_w_down", [DEPTH, 32, D, D]), ("exp_b_down", [DEPTH, 32, D]),
           ("ln2_g", [DEPTH, D]), ("ln2_b", [DEPTH, D]),
           ("modbT", [DEPTH, 2, 128, 24]), ("cposT", [DEPTH, 128, 32]), ("b_guT", [DEPTH, 32, 128, 16])]


def build_program(cfg):
    nc = bass.Bass("TRN2", target_bir_lowering=False)
    nseq, seq = cfg["nseq"], cfg["seq"]
    ntok = nseq * seq
    depth = cfg.get("depth", DEPTH)
    with ExitStack() as es:
        sc = Sched(nc, es)
        sc.prof = bool(cfg.get("prof"))
        x = sc.dram("x", [ntok, D], F32, kind="ExternalInput")
        cT = sc.dram("cT", [128, 8, nseq], F32, kind="ExternalInput")
        Wd_ = {n: sc.dram(n, s, F32, kind="ExternalInput") for n, s in W_SPECS}
        cd = {n: sc.dram(n, s, d, kind="ExternalInput") for n, s, d in CONST_SPECS}
        y = sc.dram("y", [ntok, D], F32, kind="ExternalOutput")
        FM = sc.dram("FM", [FM_ROWS, ntok], BF16)
        VV = sc.dram("VV", [ntok, VV_W], BF16)
        SM = sc.dram("SM", [ntok, 16], F32)
        MG = sc.dram("MG", [ntok, 3072], F32)
        OABC = sc.dram("OABC", [ntok, 1024], F32)
        X1 = sc.dram("X1", [ntok, D], F32)
        XL = sc.dram("XL", [ntok, D], F32)
        H2T = sc.dram("H2T", [D, ntok], BF16)
        CWD = sc.dram("CWD", [ntok, 32], F32)
        YACC = sc.dram("YACC", [ntok, D], F32)
        Cap = moe_capacity(ntok)
        Xg = sc.dram("Xg", [32 * Cap + 128, D], BF16)
        Yg = sc.dram("Yg", [32 * Cap + 128, D], F32)
        DSTD = sc.dram("DSTD", [ntok, 4], I32)
        CWKD = sc.dram("CWKD", [ntok, 4], F32)
        K = setup_attn_consts(sc, cd, Wd_["rel_bias"], es)
        siluT = sc.sbuf("siluT", [128, 8, nseq], F32)
        sc.dma("sp", siluT[:], cT[:, :, :], reads=[cT], writes=[siluT], track=siluT)
        sc.op("act", lambda: nc.scalar.activation(out=siluT[:], in_=siluT[:], func=AF.Silu), reads=[siluT], writes=[siluT])
        opsc = sc.sbuf("opsc", [128, 8, nseq], F32)
        shf = sc.sbuf("shf", [128, 8, nseq], F32)
        G1P = sc.sbuf("G1P", [128, nseq, 1024], F32)
        KC = sc.sbuf("KC", [64, nseq, 256], BF16)
        VC = sc.sbuf("VC", [128, nseq, 2, 64], BF16)
        xin = x
        for l in range(depth):
            lam_init = 0.8 - 0.6 * math.exp(-0.3 * l)
            xout = y if l == depth - 1 else XL
            sc.mark("L%d_phase_adaln" % l)
            phase_adaln(sc, cfg, l, Wd_["mod_attn_w"], Wd_["modbT"], Wd_["mod_attn_b"], 0, siluT, opsc, shf, None, "fm")
            sc.mark("L%d_phase_proj" % l)
            phase_proj(sc, cfg, l, xin, Wd_["w_in"], K["ident"], opsc, shf, FM, VV, SM, MG)
            sc.mark("L%d_phase_compress" % l)
            phase_compress(sc, cfg, l, FM, Wd_["cmp_w1"], Wd_["cmp_w2"], Wd_["cposT"], KC, VC)
            sc.mark("L%d_phase_attn" % l)
            phase_attn(sc, cfg, l, K, FM, VV, SM, KC, VC, cd["c_sel"], Wd_["diff_lambda"], Wd_["diff_norm_g"], lam_init, OABC)
            sc.mark("L%d_phase_adaln" % l)
            phase_adaln(sc, cfg, l, Wd_["mod_attn_w"], Wd_["modbT"], Wd_["mod_attn_b"], 0, siluT, None, None, G1P, "gate")
            sc.mark("L%d_phase_tail" % l)
            phase_tail(sc, cfg, l, K, OABC, MG, xin, Wd_["w_branch_a"], Wd_["w_branch_b"], Wd_["w_branch_c"], Wd_["w_out"],
                       Wd_["ln1_g"], Wd_["ln1_b"], G1P, X1)
            sc.mark("L%d_phase_adaln" % l)
            phase_adaln(sc, cfg, l, Wd_["mod_ffn_w"], Wd_["modbT"], Wd_["mod_ffn_b"], 1, siluT, opsc, shf, None, "fm")
            if cfg.get("dense_moe"):
                sc.mark("L%d_phase_moe_prep" % l)
                phase_moe_prep(sc, cfg, l, K, X1, opsc, shf, Wd_["router_w"], Wd_["router_b"], Wd_["exp_b_down"], H2T, CWD, YACC)
                sc.mark("L%d_phase_moe_experts" % l)
                phase_moe_experts(sc, cfg, l, H2T, CWD, YACC, Wd_["exp_w_gu"], Wd_["b_guT"], Wd_["exp_w_down"])
                sc.mark("L%d_phase_adaln" % l)
                phase_adaln(sc, cfg, l, Wd_["mod_ffn_w"], Wd_["modbT"], Wd_["mod_ffn_b"], 1, siluT, None, None, G1P, "gate")
                sc.mark("L%d_phase_moe_final" % l)
                phase_moe_final(sc, cfg, l, X1, YACC, G1P, Wd_["ln2_g"], Wd_["ln2_b"], xout)
            else:
                with ExitStack() as mes:
                    OPTM = sc.sbuf("OPTM", [128, nseq, 1024], F32, mes)
                    SHTM = sc.sbuf("SHTM", [128, nseq, 1024], F32, mes)
                    sc.mark("L%d_phase_adaln" % l)
                    phase_adaln(sc, cfg, l, Wd_["mod_ffn_w"], Wd_["modbT"], Wd_["mod_ffn_b"], 1, siluT, None, None, OPTM, "gate", tm_third=1, add_one=1.0)
                    sc.mark("L%d_phase_adaln" % l)
                    phase_adaln(sc, cfg, l, Wd_["mod_ffn_w"], Wd_["modbT"], Wd_["mod_ffn_b"], 1, siluT, None, None, SHTM, "gate", tm_third=0, add_one=0.0)
                    sc.mark("L%d_phase_moe_prep2" % l)
                    phase_moe_prep2(sc, cfg, l, K, X1, opsc, shf, OPTM, SHTM, Wd_["router_w"], Wd_["router_b"], Wd_["exp_b_down"],
                                    Xg, DSTD, CWKD, YACC, cd)
                    sc.barrier_on([OPTM, SHTM])
                sc.mark("L%d_phase_moe_experts2" % l)
                phase_moe_experts2(sc, cfg, l, K, Xg, Yg, Wd_["exp_w_gu"], Wd_["b_guT"], Wd_["exp_w_down"])
                sc.mark("L%d_phase_adaln" % l)
                phase_adaln(sc, cfg, l, Wd_["mod_ffn_w"], Wd_["modbT"], Wd_["mod_ffn_b"], 1, siluT, None, None, G1P, "gate")
                sc.mark("L%d_phase_moe_final2" % l)
                phase_moe_final2(sc, cfg, l, X1, YACC, Yg, DSTD, CWKD, G1P, Wd_["ln2_g"], Wd_["ln2_b"], xout)
            xin = xout
        sc.mark("end")
        sc.finish([y])
        cfg["_n_inst"] = sc.n_inst
    return nc


def host_layouts(inp):
    L = DEPTH
    out = {}
    out["modbT"] = np.ascontiguousarray(np.stack([np.stack([inp["mod_attn_b"][l].reshape(24, 128).T,
                                                            inp["mod_ffn_b"][l].reshape(24, 128).T]) for l in range(L)]), dtype=np.float32)
    out["cposT"] = np.ascontiguousarray(np.stack([np.concatenate([inp["cmp_pos"][l, 0].T, inp["cmp_pos"][l, 1].T], 0) for l in range(L)]), dtype=np.float32)
    bg = inp["exp_b_gu"].reshape(L, 32, 8, 128, 2)
    out["b_guT"] = np.ascontiguousarray(bg.transpose(0, 1, 3, 4, 2).reshape(L, 32, 128, 16), dtype=np.float32)
    return out


def run_module(inp, cfg, n_cores):
    nseq, seq = cfg["nseq"], cfg["seq"]
    nc = build_program(cfg)
    shared = {n: np.ascontiguousarray(inp[n], dtype=np.float32) for n, _ in W_SPECS if n in inp}
    shared.update(host_layouts(inp))
    shared.update(make_consts())
    x = np.asarray(inp["x"], dtype=np.float32)
    c = np.asarray(inp["c"], dtype=np.float32)
    in_maps = []
    for core in range(n_cores):
        xs = x[core * nseq:(core + 1) * nseq].reshape(nseq * seq, D)
        cs = c[core * nseq:(core + 1) * nseq]
        cT = np.ascontiguousarray(cs.reshape(nseq, 8, 128).transpose(2, 1, 0))
        m = dict(shared)
        m["x"] = np.ascontiguousarray(xs)
        m["cT"] = cT
        in_maps.append(m)
    res = run_bass_kernel_spmd(nc, in_maps, core_ids=list(range(n_cores)))
    outs = [r["y"].reshape(nseq, seq, D) for r in res.results]
    return np.concatenate(outs, axis=0).astype(np.float32)


def kernel(**inputs):
    cfg = dict(nseq=NSEQ, seq=S)
    return run_module(inputs, cfg, 8)


def moe_capacity(ntok):
    c = (15 * ntok) // 64
    return ((c + 127) // 128) * 128


def phase_moe_prep2(sc, cfg, l, K, X1, opsc, shf, OPTM, SHTM, router_w, router_b, b_down, Xg, DSTD, CWKD, YACC, cd):
    nc = sc.nc
    nseq, seq = cfg["nseq"], cfg["seq"]
    ntok = nseq * seq
    C = moe_capacity(ntok)
    with ExitStack() as es:
        rw = sc.sbuf("rw", [128, 8, 32], F32, es)
        rb = sc.sbuf("rb", [128, 32], F32, es)
        bd = sc.sbuf("bd", [32, 1024], F32, es)
        UT = sc.sbuf("UT", [128, 128], BF16, es)
        ONES = sc.sbuf("ONES", [128, 128], BF16, es)
        EOFF = sc.sbuf("EOFF", [128, 32], F32, es)
        BASE = sc.sbuf("BASE", [128, 32], F32, es)
        sc.dma("sp", rw[:], router_w[l].rearrange("(c p) e -> p c e", p=128), reads=[router_w], writes=[rw], track=rw)
        sc.dma("sp", rb[:], router_b[l:l + 1, :].partition_broadcast(128), reads=[router_b], writes=[rb], track=rb)
        sc.dma("sp", bd[:], b_down[l], reads=[b_down], writes=[bd], track=bd)
        sc.dma("pool", UT[:], cd["c_ut"][:, :], reads=[cd["c_ut"]], writes=[UT], track=UT)
        sc.op("dve", lambda: nc.vector.memset(ONES[:], 1.0), writes=[ONES])
        sc.op("dve", lambda: nc.vector.memset(BASE[:], 0.0), writes=[BASE])
        sc.op("dve", lambda: nc.vector.tensor_scalar(out=EOFF[:], in0=K["iota"][:, 0:32], scalar1=float(C), scalar2=-1.0, op0=ALU.mult, op1=ALU.add),
              reads=[K["iota"]], writes=[EOFF])
        XT = [sc.sbuf("mX", [128, 1024], F32, es) for _ in range(2)]
        HF_2 = [sc.sbuf("mHF", [128, 8, 128], F32, es) for _ in range(2)]
        HT_2 = [sc.sbuf("mHT", [128, 1024], F32, es) for _ in range(2)]
        HBt = [sc.sbuf("mHBt", [128, 1024], BF16, es) for _ in range(2)]
        Lg_2 = [sc.sbuf("mL", [128, 32], F32, es) for _ in range(2)]
        J3_2 = [sc.sbuf("mJ3", [128, 32, 32], F32, es) for _ in range(2)]
        RK_2 = [sc.sbuf("mRK", [128, 32], F32, es) for _ in range(2)]
        Mk_2 = [sc.sbuf("mMk", [128, 32], F32, es) for _ in range(2)]
        Mb_2 = [sc.sbuf("mMb", [128, 32], BF16, es) for _ in range(2)]
        EX_2 = [sc.sbuf("mEX", [128, 32], F32, es) for _ in range(2)]
        CW_2 = [sc.sbuf("mCW", [128, 32], F32, es) for _ in range(2)]
        ROW_2 = [sc.sbuf("mROW", [128, 32], F32, es) for _ in range(2)]
        POS_2 = [sc.sbuf("mPOS", [128, 32], F32, es) for _ in range(2)]
        JK_2 = [sc.sbuf("mJK", [128, 32], F32, es) for _ in range(2)]
        DSTf_2 = [sc.sbuf("mDSTf", [128, 4], F32, es) for _ in range(2)]
        DSTi = [sc.sbuf("mDSTi", [128, 4], I32, es) for _ in range(2)]
        CWK = [sc.sbuf("mCWK", [128, 4], F32, es) for _ in range(2)]
        CWT_2 = [sc.sbuf("mCWT", [32, 128], F32, es) for _ in range(2)]
        YB = [sc.sbuf("mYB", [128, 1024], F32, es) for _ in range(2)]
        ST_2 = [sc.sbuf("mST", [128, 8], F32, es) for _ in range(2)]
        ps = [sc.psum("mps", [128, 512], F32, es) for _ in range(6)]
        pi = 0
        for tt in range(ntok // 128):
            HF = HF_2[tt % 2]
            HT = HT_2[tt % 2]
            Lg = Lg_2[tt % 2]
            J3 = J3_2[tt % 2]
            RK = RK_2[tt % 2]
            Mk = Mk_2[tt % 2]
            Mb = Mb_2[tt % 2]
            EX = EX_2[tt % 2]
            CW = CW_2[tt % 2]
            ROW = ROW_2[tt % 2]
            POS = POS_2[tt % 2]
            JK = JK_2[tt % 2]
            DSTf = DSTf_2[tt % 2]
            CWT = CWT_2[tt % 2]
            ST = ST_2[tt % 2]
            tok0 = tt * 128
            b = tok0 // seq
            xt, hbt, dsti, cwk, yb = XT[tt % 2], HBt[tt % 2], DSTi[tt % 2], CWK[tt % 2], YB[tt % 2]
            sc.dma("sp", xt[:], X1[tok0:tok0 + 128, :], reads=[X1], writes=[xt], track=xt, nowaw=False)
            sc.op("pool", lambda: nc.gpsimd.tensor_tensor(out=HT[:], in0=xt[:], in1=OPTM[:, b, :], op=ALU.mult), reads=[xt, OPTM], writes=[HT])
            sc.op("pool", lambda: nc.gpsimd.tensor_tensor(out=hbt[:], in0=HT[:], in1=SHTM[:, b, :], op=ALU.add), reads=[HT, SHTM], writes=[hbt])
            for g in range(2):
                p = ps[pi % 6]; pi += 1
                for c4 in range(4):
                    c = g * 4 + c4
                    sc.op("pe", lambda: nc.tensor.transpose(p[:, c4 * 128:(c4 + 1) * 128], xt[:, c * 128:(c + 1) * 128], K["ident"][:]),
                          reads=[xt, K["ident"]], writes=[p], nowaw=(c4 > 0))
                for c4 in range(4):
                    c = g * 4 + c4
                    sc.op("act", lambda: nc.scalar.activation(out=HF[:, c, :], in_=p[:, c4 * 128:(c4 + 1) * 128], func=AF.Identity,
                                                              bias=shf[:, c, b:b + 1], scale=opsc[:, c, b:b + 1]),
                          reads=[p, shf, opsc], writes=[HF], nowaw=(c > 0))
            p = ps[pi % 6]; pi += 1
            for c in range(8):
                sc.op("pe", lambda: nc.tensor.matmul(p[:, 0:32], lhsT=HF[:, c, :], rhs=rw[:, c, :], start=(c == 0), stop=(c == 7)),
                      reads=[HF, rw], writes=[p], nowaw=(c > 0))
            sc.op("dve", lambda: nc.vector.tensor_tensor(out=Lg[:], in0=p[:, 0:32], in1=rb[:], op=ALU.add), reads=[p, rb], writes=[Lg])
            sc.op("dve", lambda: nc.vector.tensor_tensor(out=J3[:], in0=Lg[:].unsqueeze(1).to_broadcast([128, 32, 32]),
                                                        in1=Lg[:].unsqueeze(2).to_broadcast([128, 32, 32]), op=ALU.is_gt), reads=[Lg], writes=[J3])
            sc.op("dve", lambda: nc.vector.tensor_reduce(out=RK[:], in_=J3[:], axis=AX.X, op=ALU.add), reads=[J3], writes=[RK])
            sc.op("dve", lambda: nc.vector.tensor_reduce(out=ST[:, 0:1], in_=Lg[:], axis=AX.X, op=ALU.max), reads=[Lg], writes=[ST])
            sc.op("dve", lambda: nc.vector.tensor_scalar(out=ST[:, 1:2], in0=ST[:, 0:1], scalar1=-1.0, scalar2=None, op0=ALU.mult), reads=[ST], writes=[ST])
            sc.op("act", lambda: nc.scalar.activation(out=EX[:], in_=Lg[:], func=AF.Exp, bias=ST[:, 1:2]), reads=[Lg, ST], writes=[EX])
            sc.op("dve", lambda: nc.vector.tensor_scalar(out=Mk[:], in0=RK[:], scalar1=3.5, scalar2=None, op0=ALU.is_lt), reads=[RK], writes=[Mk])
            sc.op("dve", lambda: nc.vector.scalar_tensor_tensor(out=EX[:], in0=EX[:], scalar=1.0, in1=Mk[:], op0=ALU.mult, op1=ALU.mult,
                                                               accum_out=ST[:, 2:3]), reads=[EX, Mk], writes=[EX, ST])
            sc.op("dve", lambda: nc.vector.reciprocal(out=ST[:, 3:4], in_=ST[:, 2:3]), reads=[ST], writes=[ST])
            sc.op("dve", lambda: nc.vector.tensor_scalar(out=CW[:], in0=EX[:], scalar1=ST[:, 3:4], scalar2=None, op0=ALU.mult), reads=[EX, ST], writes=[CW])
            sc.op("dve", lambda: nc.vector.tensor_copy(Mb[:], Mk[:]), reads=[Mk], writes=[Mb])
            pc = ps[pi % 6]; pi += 1
            sc.op("pe", lambda: nc.tensor.matmul(pc[:, 0:32], lhsT=UT[:], rhs=Mb[:], start=True, stop=True), reads=[UT, Mb], writes=[pc])
            ptot = ps[pi % 6]; pi += 1
            sc.op("pe", lambda: nc.tensor.matmul(ptot[:, 0:32], lhsT=ONES[:], rhs=Mb[:], start=True, stop=True), reads=[ONES, Mb], writes=[ptot])
            sc.op("dve", lambda: nc.vector.tensor_tensor(out=POS[:], in0=pc[:, 0:32], in1=BASE[:], op=ALU.add), reads=[pc, BASE], writes=[POS])
            sc.op("dve", lambda: nc.vector.tensor_tensor(out=ROW[:], in0=POS[:], in1=EOFF[:], op=ALU.add), reads=[POS, EOFF], writes=[ROW])
            sc.op("dve", lambda: nc.vector.tensor_scalar(out=POS[:], in0=POS[:], scalar1=float(C) + 0.5, scalar2=1e9, op0=ALU.is_gt, op1=ALU.mult),
                  reads=[POS], writes=[POS])
            sc.op("dve", lambda: nc.vector.tensor_tensor(out=ROW[:], in0=ROW[:], in1=POS[:], op=ALU.add), reads=[ROW, POS], writes=[ROW])
            sc.op("dve", lambda: nc.vector.tensor_scalar(out=ROW[:], in0=ROW[:], scalar1=float(32 * C), scalar2=None, op0=ALU.min), reads=[ROW], writes=[ROW])
            sc.op("dve", lambda: nc.vector.tensor_tensor(out=BASE[:], in0=BASE[:], in1=ptot[:, 0:32], op=ALU.add), reads=[BASE, ptot], writes=[BASE])
            for k in range(4):
                sc.op("dve", lambda: nc.vector.scalar_tensor_tensor(out=JK[:], in0=RK[:], scalar=float(k), in1=ROW[:], op0=ALU.is_equal, op1=ALU.mult,
                                                                   accum_out=DSTf[:, k:k + 1]), reads=[RK, ROW], writes=[JK, DSTf])
                sc.op("dve", lambda: nc.vector.scalar_tensor_tensor(out=JK[:], in0=RK[:], scalar=float(k), in1=CW[:], op0=ALU.is_equal, op1=ALU.mult,
                                                                   accum_out=cwk[:, k:k + 1]), reads=[RK, CW], writes=[JK, cwk])
            sc.op("dve", lambda: nc.vector.tensor_copy(dsti[:], DSTf[:]), reads=[DSTf], writes=[dsti])
            sc.dma("sp", DSTD[tok0:tok0 + 128, :], dsti[:], reads=[dsti], writes=[DSTD], track=dsti)
            sc.dma("sp", CWKD[tok0:tok0 + 128, :], cwk[:], reads=[cwk], writes=[CWKD], track=cwk)
            for k in range(4):
                deps_r, deps_w = [hbt, dsti], [Xg]
                d = sc._collect(deps_r, deps_w, True)
                sc._wait("pool", d)
                if hbt.dsem is None:
                    if sc.sempool:
                        hbt.dsem, hbt.dcount = sc.sempool.pop()
                    else:
                        hbt.dsem = sc.es.enter_context(nc.semaphore("d_" + hbt.name))
                ins = nc.gpsimd.indirect_dma_start(out=Xg[:, :], out_offset=bass.IndirectOffsetOnAxis(ap=dsti[:, k:k + 1], axis=0),
                                                   in_=hbt[:, :], in_offset=None)
                ins.then_inc(hbt.dsem, 16)
                hbt.dcount += 1
                for bb in deps_r:
                    bb.rd[hbt] = hbt.dcount
                Xg.wr[hbt] = hbt.dcount
            p = ps[pi % 6]; pi += 1
            sc.op("pe", lambda: nc.tensor.transpose(p[:32, 0:128], CW[:, :], K["ident"][:]), reads=[CW, K["ident"]], writes=[p])
            sc.op("act", lambda: nc.scalar.copy(CWT[:], p[:32, 0:128]), reads=[p], writes=[CWT])
            for hf in range(2):
                p = ps[pi % 6]; pi += 1
                sc.op("pe", lambda: nc.tensor.matmul(p[:, :], lhsT=CWT[:, :], rhs=bd[:, hf * 512:(hf + 1) * 512], start=True, stop=True),
                      reads=[CWT, bd], writes=[p])
                sc.op("act", lambda: nc.scalar.copy(yb[:, hf * 512:(hf + 1) * 512], p[:, :]), reads=[p], writes=[yb], nowaw=(hf > 0))
            sc.dma("sp", YACC[tok0:tok0 + 128, :], yb[:], reads=[yb], writes=[YACC], track=yb)
        allb = [rw, rb, bd, UT, ONES, EOFF, BASE, HF_2[0], HF_2[1], HT_2[0], HT_2[1], Lg_2[0], Lg_2[1], J3_2[0], J3_2[1], RK_2[0], RK_2[1], Mk_2[0], Mk_2[1], Mb_2[0], Mb_2[1], EX_2[0], EX_2[1], CW_2[0], CW_2[1], ROW_2[0], ROW_2[1], POS_2[0], POS_2[1], JK_2[0], JK_2[1], DSTf_2[0], DSTf_2[1], CWT_2[0], CWT_2[1], ST_2[0], ST_2[1]] + XT + HBt + DSTi + CWK + YB + ps
        sc.barrier_on(allb)
        sched_release(sc, allb)


def phase_moe_experts2(sc, cfg, l, K, Xg, Yg, w_gu, b_guT, w_down):
    nc = sc.nc
    nseq, seq = cfg["nseq"], cfg["seq"]
    ntok = nseq * seq
    C = moe_capacity(ntok)
    sbs = [(o, min(4, C // 128 - o)) for o in range(0, C // 128, 4)]
    ne = cfg.get("n_exp", 32)
    with ExitStack() as es:
        Wgu = [sc.sbuf("eWgu", [128, 8, 2048], BF16, es) for _ in range(2)]
        Wd = [sc.sbuf("eWd", [128, 8, 1024], BF16, es) for _ in range(2)]
        BG = [sc.sbuf("eBG", [128, 16], F32, es) for _ in range(2)]
        XR = [sc.sbuf("eXR", [128, 4, 1024], BF16, es) for _ in range(2)]
        XT = [sc.sbuf("eXT", [128, 8, 512], BF16, es) for _ in range(2)]
        actT = [sc.sbuf("eact", [128, 8, 512], BF16, es) for _ in range(2)]
        G = [sc.sbuf("eG", [128, 512], F32, es) for _ in range(2)]
        Sg = [sc.sbuf("eS", [128, 512], F32, es) for _ in range(2)]
        U = [sc.sbuf("eU", [128, 512], F32, es) for _ in range(2)]
        YO = [sc.sbuf("eYO", [128, 1024], F32, es) for _ in range(3)]
        psG = [sc.psum("epsG", [128, 512], F32, es) for _ in range(4)]
        psD = [sc.psum("epsD", [128, 512], F32, es) for _ in range(2)]
        psT = [sc.psum("epsT", [128, 8, 128], BF16, es) for _ in range(2)]
        gi = di = ai = ti = xi = yi = pti = 0
        def load_w(e):
            wg, wd, bg = Wgu[e % 2], Wd[e % 2], BG[e % 2]
            for c2 in range(0, 8, 2):
                sc.dma("pool", wg[:, c2:c2 + 2, :], w_gu[l, e, c2 * 128:(c2 + 2) * 128, :].rearrange("(c p) n -> p c n", p=128),
                       reads=[w_gu], writes=[wg], track=wg, nowaw=(c2 > 0))
            for c4 in range(0, 8, 4):
                sc.dma("pool", wd[:, c4:c4 + 4, :], w_down[l, e, c4 * 128:(c4 + 4) * 128, :].rearrange("(c p) n -> p c n", p=128),
                       reads=[w_down], writes=[wd], track=wd, nowaw=(c4 > 0))
            sc.dma("sp", bg[:], b_guT[l, e], reads=[b_guT], writes=[bg], track=bg, nowaw=False)
        load_w(0)
        for e in range(ne):
            wg, wd, bg = Wgu[e % 2], Wd[e % 2], BG[e % 2]
            if e + 1 < ne:
                load_w(e + 1)
            for (tl0, ntl) in sbs:
                r0 = e * C + tl0 * 128
                wdt = ntl * 128
                xr, xt_, at = XR[xi % 2], XT[xi % 2], actT[xi % 2]
                xi += 1
                sc.dma("sp", xr[:, 0:ntl, :], Xg[r0:r0 + wdt, :].rearrange("(n p) d -> p n d", p=128), reads=[Xg], writes=[xr], track=xr, nowaw=False)
                for tl in range(ntl):
                    pt = psT[pti % 2]; pti += 1
                    for c in range(8):
                        sc.op("pe", lambda: nc.tensor.transpose(pt[:, c, :], xr[:, tl, c * 128:(c + 1) * 128], K["identb"][:]),
                              reads=[xr, K["identb"]], writes=[pt], nowaw=(c > 0))
                    if tl % 2 == 0:
                        sc.op("act", lambda: nc.scalar.copy(xt_[:, :, tl * 128:(tl + 1) * 128], pt[:]), reads=[pt], writes=[xt_], nowaw=(tl > 0))
                    else:
                        sc.op("dve", lambda: nc.vector.tensor_copy(xt_[:, :, tl * 128:(tl + 1) * 128], pt[:]), reads=[pt], writes=[xt_], nowaw=True)
                for fc in range(8):
                    pg = psG[gi % 4]; gi += 1
                    pu = psG[gi % 4]; gi += 1
                    for which, pp in ((0, pg), (1, pu)):
                        for kc in range(8):
                            lhsT = wg[:, kc, :].rearrange("p (f two) -> p two f", two=2)[:, which, fc * 128:(fc + 1) * 128]
                            sc.op("pe", lambda: nc.tensor.matmul(pp[:, :wdt], lhsT=lhsT, rhs=xt_[:, kc, :wdt], start=(kc == 0), stop=(kc == 7)),
                                  reads=[wg, xt_], writes=[pp], nowaw=(kc > 0))
                    g_, s_, u_ = G[ti % 2], Sg[ti % 2], U[ti % 2]
                    ti += 1
                    sc.op("dve", lambda: nc.vector.tensor_scalar(out=g_[:, :wdt], in0=pg[:, :wdt], scalar1=bg[:, fc:fc + 1], scalar2=7.0, op0=ALU.add, op1=ALU.min),
                          reads=[pg, bg], writes=[g_])
                    sc.op("act", lambda: nc.scalar.activation(out=s_[:, :wdt], in_=g_[:, :wdt], func=AF.Sigmoid, scale=1.702), reads=[g_], writes=[s_])
                    sc.op("dve", lambda: nc.vector.tensor_scalar(out=u_[:, :wdt], in0=pu[:, :wdt], scalar1=bg[:, 8 + fc:9 + fc], scalar2=7.0, op0=ALU.add, op1=ALU.min),
                          reads=[pu, bg], writes=[u_])
                    sc.op("dve", lambda: nc.vector.tensor_scalar(out=u_[:, :wdt], in0=u_[:, :wdt], scalar1=-7.0, scalar2=1.0, op0=ALU.max, op1=ALU.add),
                          reads=[u_], writes=[u_])
                    sc.op("dve", lambda: nc.vector.tensor_tensor(out=g_[:, :wdt], in0=g_[:, :wdt], in1=s_[:, :wdt], op=ALU.mult), reads=[g_, s_], writes=[g_])
                    sc.op("dve", lambda: nc.vector.tensor_tensor(out=at[:, fc, :wdt], in0=g_[:, :wdt], in1=u_[:, :wdt], op=ALU.mult), reads=[g_, u_], writes=[at], nowaw=(fc > 0))
                for tl in range(ntl):
                    yo = YO[yi % 3]; yi += 1
                    for hf in range(2):
                        pd = psD[di % 2]; di += 1
                        for fc in range(8):
                            sc.op("pe", lambda: nc.tensor.matmul(pd[:, :], lhsT=at[:, fc, tl * 128:(tl + 1) * 128], rhs=wd[:, fc, hf * 512:(hf + 1) * 512],
                                                                 start=(fc == 0), stop=(fc == 7)), reads=[at, wd], writes=[pd], nowaw=(fc > 0))
                        sc.op("act", lambda: nc.scalar.copy(yo[:, hf * 512:(hf + 1) * 512], pd[:, :]), reads=[pd], writes=[yo], nowaw=(hf > 0))
                    sc.dma("sp", Yg[r0 + tl * 128:r0 + (tl + 1) * 128, :], yo[:], reads=[yo], writes=[Yg], track=yo)
        allb = Wgu + Wd + BG + XR + XT + actT + G + Sg + U + YO + psG + psD + psT
        sc.barrier_on(allb)
        sched_release(sc, allb)


def phase_moe_final2(sc, cfg, l, X1, YACC, Yg, DSTD, CWKD, G1P, ln_g, ln_b, XOUT):
    nc = sc.nc
    nseq, seq = cfg["nseq"], cfg["seq"]
    ntok = nseq * seq
    C = moe_capacity(ntok)
    DN_ALPHA = (2 * DEPTH) ** 0.25
    with ExitStack() as es:
        Gt = sc.sbuf("fG", [128, 1024], F32, es)
        Bt = sc.sbuf("fB", [128, 1024], F32, es)
        sc.dma("sp", Gt[:], ln_g[l:l + 1, :].partition_broadcast(128), reads=[ln_g], writes=[Gt], track=Gt)
        sc.dma("sp", Bt[:], ln_b[l:l + 1, :].partition_broadcast(128), reads=[ln_b], writes=[Bt], track=Bt)
        XT = [sc.sbuf("fX", [128, 1024], F32, es) for _ in range(2)]
        YT = [sc.sbuf("fY", [128, 1024], F32, es) for _ in range(2)]
        YK = [sc.sbuf("fYK", [128, 4, 1024], F32, es) for _ in range(2)]
        DSTi = [sc.sbuf("fDST", [128, 4], I32, es) for _ in range(2)]
        CWK = [sc.sbuf("fCWK", [128, 4], F32, es) for _ in range(2)]
        Z_2 = [sc.sbuf("fZ", [128, 1024], F32, es) for _ in range(2)]
        ZC_2 = [sc.sbuf("fZC", [128, 1024], F32, es) for _ in range(2)]
        TMP_2 = [sc.sbuf("fT", [128, 1024], F32, es) for _ in range(2)]
        O = [sc.sbuf("fO", [128, 1024], F32, es) for _ in range(2)]
        ST2_2 = [sc.sbuf("fST", [128, 8], F32, es) for _ in range(2)]
        for tt in range(ntok // 128):
            Z = Z_2[tt % 2]
            ZC = ZC_2[tt % 2]
            TMP = TMP_2[tt % 2]
            ST2 = ST2_2[tt % 2]
            tok0 = tt * 128
            b = tok0 // seq
            xt, yt, yk, dsti, cwk, o = XT[tt % 2], YT[tt % 2], YK[tt % 2], DSTi[tt % 2], CWK[tt % 2], O[tt % 2]
            sc.dma("sp", xt[:], X1[tok0:tok0 + 128, :], reads=[X1], writes=[xt], track=xt, nowaw=False)
            sc.dma("sp", yt[:], YACC[tok0:tok0 + 128, :], reads=[YACC], writes=[yt], track=yt, nowaw=False)
            sc.dma("sp", dsti[:], DSTD[tok0:tok0 + 128, :], reads=[DSTD], writes=[dsti], track=dsti, nowaw=False)
            sc.dma("sp", cwk[:], CWKD[tok0:tok0 + 128, :], reads=[CWKD], writes=[cwk], track=cwk, nowaw=False)
            for k in range(4):
                d = sc._collect([Yg, dsti], [yk], k > 0)
                sc._wait("pool", d)
                if yk.dsem is None:
                    if sc.sempool:
                        yk.dsem, yk.dcount = sc.sempool.pop()
                    else:
                        yk.dsem = sc.es.enter_context(nc.semaphore("d_" + yk.name))
                ins = nc.gpsimd.indirect_dma_start(out=yk[:, k, :], out_offset=None, in_=Yg[:, :],
                                                   in_offset=bass.IndirectOffsetOnAxis(ap=dsti[:, k:k + 1], axis=0))
                ins.then_inc(yk.dsem, 16)
                yk.dcount += 1
                for bb in (Yg, dsti):
                    bb.rd[yk] = yk.dcount
                if k == 0:
                    yk.wr = {yk: yk.dcount}
                    yk.rd = {}
                else:
                    yk.wr[yk] = yk.dcount
            for k in range(4):
                sc.op("dve", lambda: nc.vector.scalar_tensor_tensor(out=yt[:], in0=yk[:, k, :], scalar=cwk[:, k:k + 1], in1=yt[:], op0=ALU.mult, op1=ALU.add),
                      reads=[yk, cwk, yt], writes=[yt])
            sc.op("pool", lambda: nc.gpsimd.tensor_tensor(out=TMP[:], in0=yt[:], in1=G1P[:, b, :], op=ALU.mult), reads=[yt, G1P], writes=[TMP])
            sc.op("dve", lambda: nc.vector.scalar_tensor_tensor(out=Z[:], in0=xt[:], scalar=DN_ALPHA, in1=TMP[:], op0=ALU.mult, op1=ALU.add),
                  reads=[xt, TMP], writes=[Z])
            layer_norm_tile(sc, Z, ZC, TMP, o, ST2, Gt, Bt)
            sc.dma("sp", XOUT[tok0:tok0 + 128, :], o[:], reads=[o], writes=[XOUT], track=o)
        allb = [Gt, Bt, Z_2[0], Z_2[1], ZC_2[0], ZC_2[1], TMP_2[0], TMP_2[1], ST2_2[0], ST2_2[1]] + XT + YT + YK + DSTi + CWK + O
        sc.barrier_on(allb)
        sched_release(sc, allb)
```

```python
import math
from contextlib import ExitStack
import numpy as np
import concourse.bass as bass
import concourse.mybir as mybir
from concourse.bass_utils import run_bass_kernel_spmd

F32 = mybir.dt.float32
BF16 = mybir.dt.bfloat16
I32 = mybir.dt.int32
AF = mybir.ActivationFunctionType
ALU = mybir.AluOpType
AX = mybir.AxisListType

D = 1024
S = 4096
NSEQ = 2
T = NSEQ * S
DEPTH = 2
D_IN = 5808
NEG = -30000.0


class Buf:
    def __init__(self, name, t):
        self.name = name
        self.t = t
        self.wr = {}
        self.rd = {}
        self.dsem = None
        self.dcount = 0

    def __getitem__(self, idx):
        return self.t[idx]


class Sched:
    def __init__(self, nc, es):
        self.nc = nc
        self.es = es
        self.eng = {"pe": nc.tensor, "dve": nc.vector, "act": nc.scalar,
                    "pool": nc.gpsimd, "sp": nc.sync}
        self.sem = {}
        self.cnt = {}
        for k in ("pe", "dve", "act", "pool"):
            self.sem[k] = es.enter_context(nc.semaphore("s_" + k))
            self.cnt[k] = 0
        self.known = {k: {} for k in self.eng}
        self.nbuf = 0
        self.n_inst = 0
        self.sempool = []

    def sbuf(self, name, shape, dt, es=None):
        es = es or self.es
        self.nbuf += 1
        name = "%s_%d" % (name, self.nbuf)
        t = es.enter_context(self.nc.sbuf_tensor(name, list(shape), dt))
        b = Buf(name, t)
        b.es = es
        return b

    def psum(self, name, shape, dt=F32, es=None):
        es = es or self.es
        self.nbuf += 1
        name = "%s_%d" % (name, self.nbuf)
        t = es.enter_context(self.nc.psum_tensor(name, list(shape), dt))
        b = Buf(name, t)
        b.is_psum = True
        b.es = es
        return b

    def dram(self, name, shape, dt, kind=None):
        if kind is None:
            t = self.nc.dram_tensor(name, list(shape), dt)
            b = Buf(name, t.ap())
            b.es = self.es
            return b
        else:
            t = self.nc.dram_tensor(name, list(shape), dt, kind=kind)
        b = Buf(name, t.ap())
        b.es = self.es
        return b

    def _semobj(self, key):
        if isinstance(key, str):
            return self.sem[key]
        return key.dsem

    def _wait(self, ekey, deps):
        eng = self.eng[ekey]
        kn = self.known[ekey]
        for key, val in deps.items():
            if isinstance(key, str):
                if key == "pe" and ekey == "pe":
                    continue
                v = val
            else:
                if key.dsem is None:
                    continue
                v = key.dcount * 16
            if kn.get(key, 0) >= v:
                continue
            eng.wait_ge(self._semobj(key), v)
            kn[key] = v

    def _collect(self, reads, writes, nowaw):
        deps = {}
        def add(d):
            for k, v in d.items():
                if deps.get(k, 0) < v:
                    deps[k] = v
        for b in reads:
            add(b.wr)
            if getattr(b, "is_psum", False):
                add(b.rd)
        for b in writes:
            if not nowaw:
                add(b.wr)
            add(b.rd)
        return deps

    def op(self, ekey, fn, reads=(), writes=(), nowaw=False):
        deps = self._collect(reads, writes, nowaw)
        self._wait(ekey, deps)
        ins = fn()
        self.cnt[ekey] += 1
        ins.then_inc(self.sem[ekey], 1)
        tk = self.cnt[ekey]
        self.n_inst += 1
        for b in reads:
            if b.rd.get(ekey, 0) < tk:
                b.rd[ekey] = tk
        for b in writes:
            if nowaw:
                b.wr[ekey] = tk
            else:
                b.wr = {ekey: tk}
                b.rd = {}
        return ins

    def dma(self, qkey, out_ap, in_ap, reads=(), writes=(), track=None, nowaw=True, **kw):
        assert track is not None
        if track.dsem is None:
            if self.sempool:
                track.dsem, track.dcount = self.sempool.pop()
            else:
                track.dsem = self.es.enter_context(self.nc.semaphore("d_" + track.name))
        deps = self._collect(reads, writes, nowaw)
        self._wait(qkey, deps)
        ins = self.eng[qkey].dma_start(out=out_ap, in_=in_ap, **kw)
        ins.then_inc(track.dsem, 16)
        track.dcount += 1
        self.n_inst += 1
        for b in reads:
            b.rd[track] = track.dcount
        for b in writes:
            if nowaw:
                b.wr[track] = track.dcount
            else:
                b.wr = {track: track.dcount}
                b.rd = {}
        return ins

    def mark(self, name):
        if not getattr(self, "prof", False):
            return
        cur = getattr(self, "_cur_scope", None)
        if cur is not None:
            self.nc.leave_named_scope(cur[0], cur[1], False)
        sid, _ = self.nc.enter_named_scope(name, False)
        self._cur_scope = (name, sid)

    def barrier_on(self, bufs):
        deps = {}
        for b in bufs:
            for d in (b.wr, b.rd):
                for k, v in d.items():
                    if deps.get(k, 0) < v:
                        deps[k] = v
        for e in self.eng:
            self._wait(e, deps)

    def finish(self, bufs):
        deps = {}
        for b in bufs:
            for k, v in b.wr.items():
                if deps.get(k, 0) < v:
                    deps[k] = v
        self._wait("sp", deps)


C_AQ, C_AK, C_AV, C_IQ, C_IK, C_IW = 0, 256, 320, 384, 512, 544
C_BQ, C_KCR, C_VCR, C_KS, C_VS, C_KW, C_VW, C_BG = 548, 804, 868, 932, 996, 1060, 1124, 1188
C_CQ, C_CK, C_CV, C_MG = 1200, 1712, 2224, 2736
FM_CHUNKS = [("aq0", 0, 128), ("aq1", 128, 128), ("ak", 256, 64), ("iq", 384, 128), ("ik", 512, 32),
             ("bq0", 548, 128), ("bq1", 676, 128), ("kvcr", 804, 128), ("ks", 932, 64), ("kw", 1060, 64),
             ("cq0", 1200, 128), ("cq1", 1328, 128), ("cq2", 1456, 128), ("cq3", 1584, 128),
             ("ck0", 1712, 128), ("ck1", 1840, 128), ("ck2", 1968, 128), ("ck3", 2096, 128)]
FM_ROW = {}
_r = 0
for _n, _c, _w in FM_CHUNKS:
    FM_ROW[_n] = _r
    _r += 128
FM_ROWS = _r
VV_W = 704


def sched_release(sc, bufs):
    for b in bufs:
        if b.dsem is not None:
            sc.sempool.append((b.dsem, b.dcount))
            b.dsem = None


def load_cast(sc, es, dst, dst_ap, src, src_ap):
    sc.dma("pool", dst_ap, src_ap, reads=[src], writes=[dst], track=dst)


def phase_adaln(sc, cfg, l, modw, modbT, modb, which, siluT, out_opsc, out_shf, out_gate, want, tm_third=2, add_one=1.0):
    nc = sc.nc
    nseq = cfg["nseq"]
    with ExitStack() as es:
        wk = [sc.sbuf("adw", [128, 3072], F32, es) for _ in range(2)]
        bT = sc.sbuf("adbT", [128, 24], F32, es)
        gb = sc.sbuf("adgb", [128, 1024], F32, es)
        psA = sc.psum("adpsA", [128, 512], F32, es)
        psG = [[sc.psum("adpsG", [128, 512], F32, es) for _ in range(2)] for _ in range(nseq)]
        silu_bc = []
        if want == "gate":
            for b in range(nseq):
                t = sc.sbuf("silubc", [128, 8, 128], F32, es)
                sc.op("dve", lambda: nc.vector.tensor_copy(t[:], siluT[:, :, b:b + 1].to_broadcast([128, 8, 128])), reads=[siluT], writes=[t])
                silu_bc.append(t)
        sc.dma("sp", bT[:], modbT[l, which], reads=[modbT], writes=[bT], track=bT)
        sc.dma("sp", gb[:], modb[l:l + 1, tm_third * 1024:(tm_third + 1) * 1024].partition_broadcast(128), reads=[modb], writes=[gb], track=gb)
        for kc in range(8):
            w = wk[kc % 2]
            sc.dma("sp", w[:], modw[l, kc * 128:(kc + 1) * 128, :], reads=[modw], writes=[w], track=w, nowaw=False)
            for j in (range(16) if want == "fm" else []):
                sc.op("pe", lambda: nc.tensor.matmul(psA[:, j * nseq:(j + 1) * nseq], lhsT=w[:, j * 128:(j + 1) * 128],
                                                     rhs=siluT[:, kc, :], start=(kc == 0 and j == 0), stop=(kc == 7),
                                                     skip_group_check=True),
                      reads=[w, siluT], writes=[psA], nowaw=True)
            for b in (range(nseq) if want == "gate" else []):
                for hf in range(2):
                    sc.op("pe", lambda: nc.tensor.matmul(psG[b][hf][:, :], lhsT=silu_bc[b][:, kc, :],
                                                         rhs=w[:, tm_third * 1024 + hf * 512:tm_third * 1024 + (hf + 1) * 512],
                                                         start=(kc == 0), stop=(kc == 7)),
                          reads=[w, silu_bc[b]], writes=[psG[b][hf]], nowaw=True)
        for j in (range(8) if want == "fm" else []):
            sc.op("dve", lambda: nc.vector.tensor_scalar(out=out_shf[:, j, :], in0=psA[:, j * nseq:(j + 1) * nseq],
                                                        scalar1=bT[:, j:j + 1], scalar2=None, op0=ALU.add),
                  reads=[psA, bT], writes=[out_shf], nowaw=True)
            sc.op("dve", lambda: nc.vector.tensor_scalar(out=out_opsc[:, j, :], in0=psA[:, (j + 8) * nseq:(j + 9) * nseq],
                                                        scalar1=bT[:, j + 8:j + 9], scalar2=1.0, op0=ALU.add, op1=ALU.add),
                  reads=[psA, bT], writes=[out_opsc], nowaw=True)
        for b in (range(nseq) if want == "gate" else []):
            for hf in range(2):
                sc.op("dve", lambda: nc.vector.scalar_tensor_tensor(out=out_gate[:, b, hf * 512:(hf + 1) * 512],
                                                                   in0=psG[b][hf][:, :], scalar=float(add_one),
                                                                   in1=gb[:, hf * 512:(hf + 1) * 512],
                                                                   op0=ALU.add, op1=ALU.add),
                      reads=[psG[b][hf], gb], writes=[out_gate], nowaw=True)
        sc.barrier_on([wk[0], wk[1], bT, gb, psA] + [p for q in psG for p in q] + silu_bc)
        sched_release(sc, [wk[0], wk[1], bT, gb])


def phase_proj(sc, cfg, l, xsrc, w_in, ident, opsc, shf, FM, VV, SM, MG):
    nc = sc.nc
    nseq, seq = cfg["nseq"], cfg["seq"]
    ntok = nseq * seq
    with ExitStack() as es:
        wb = sc.sbuf("w_in_bf", [128, 8, D_IN], BF16, es)
        for kc in range(8):
            sc.dma("pool", wb[:, kc, :], w_in[l, kc * 128:(kc + 1) * 128, :], reads=[w_in], writes=[wb], track=wb)
        xs = [sc.sbuf("xs", [128, 4, D], F32, es) for _ in range(2)]
        hT = [sc.sbuf("hT", [128, 8, 512], BF16, es) for _ in range(2)]
        fst = [sc.sbuf("fst", [128, 512], BF16, es) for _ in range(4)]
        vst = [sc.sbuf("vst", [128, VV_W], BF16, es) for _ in range(2)]
        sst = [sc.sbuf("sst", [128, 16], F32, es) for _ in range(2)]
        mst = [sc.sbuf("mst", [128, 3072], F32, es) for _ in range(2)]
        ps = [sc.psum("pps", [128, 512], F32, es) for _ in range(8)]
        pi = [0]
        def nextps():
            p = ps[pi[0] % 8]
            pi[0] += 1
            return p
        nblk = ntok // 512
        fi = 0
        for blk in range(nblk):
            b = (blk * 512) // seq
            x_t = xs[blk % 2]
            h_t = hT[blk % 2]
            sc.dma("sp", x_t[:], xsrc[blk * 512:(blk + 1) * 512, :].rearrange("(j p) d -> p j d", p=128),
                   reads=[xsrc], writes=[x_t], track=x_t, nowaw=False)
            for c in range(8):
                p = nextps()
                for j in range(4):
                    sc.op("pe", lambda: nc.tensor.transpose(p[:, j * 128:(j + 1) * 128], x_t[:, j, c * 128:(c + 1) * 128], ident[:]),
                          reads=[x_t, ident], writes=[p], nowaw=(j > 0))
                sc.op("act", lambda: nc.scalar.activation(out=h_t[:, c, :], in_=p[:, :], func=AF.Identity,
                                                          bias=shf[:, c, b:b + 1], scale=opsc[:, c, b:b + 1]),
                      reads=[p, shf, opsc], writes=[h_t], nowaw=(c > 0))
            for (nm, c0, wd) in FM_CHUNKS:
                p = nextps()
                for kc in range(8):
                    sc.op("pe", lambda: nc.tensor.matmul(p[:wd, :], lhsT=wb[:, kc, c0:c0 + wd], rhs=h_t[:, kc, :],
                                                         start=(kc == 0), stop=(kc == 7)),
                          reads=[wb, h_t], writes=[p], nowaw=(kc > 0))
                f = fst[fi % 4]
                ek = "dve" if fi % 2 == 0 else "act"
                if ek == "dve":
                    sc.op("dve", lambda: nc.vector.tensor_copy(f[:wd, :], p[:wd, :]), reads=[p], writes=[f])
                else:
                    sc.op("act", lambda: nc.scalar.copy(f[:wd, :], p[:wd, :]), reads=[p], writes=[f])
                r0 = FM_ROW[nm]
                sc.dma("sp", FM[r0:r0 + wd, blk * 512:(blk + 1) * 512], f[:wd, :], reads=[f], writes=[FM], track=f)
                fi += 1
            for j in range(4):
                tk = blk * 4 + j
                v_t, s_t, m_t = vst[tk % 2], sst[tk % 2], mst[tk % 2]
                tok0 = blk * 512 + j * 128
                def tm(c0, wd):
                    p = nextps()
                    for kc in range(8):
                        sc.op("pe", lambda: nc.tensor.matmul(p[:, :wd], lhsT=h_t[:, kc, j * 128:(j + 1) * 128],
                                                             rhs=wb[:, kc, c0:c0 + wd], start=(kc == 0), stop=(kc == 7)),
                              reads=[wb, h_t], writes=[p], nowaw=(kc > 0))
                    return p
                p = tm(C_AV, 64)
                sc.op("dve", lambda: nc.vector.tensor_copy(v_t[:, 0:64], p[:, 0:64]), reads=[p], writes=[v_t])
                p = tm(C_VS, 64)
                sc.op("dve", lambda: nc.vector.tensor_copy(v_t[:, 64:128], p[:, 0:64]), reads=[p], writes=[v_t], nowaw=True)
                p = tm(C_VW, 76)
                sc.op("dve", lambda: nc.vector.tensor_copy(v_t[:, 128:192], p[:, 0:64]), reads=[p], writes=[v_t], nowaw=True)
                sc.op("act", lambda: nc.scalar.activation(out=s_t[:, 4:16], in_=p[:, 64:76], func=AF.Sigmoid),
                      reads=[p], writes=[s_t])
                p = tm(C_IW, 4)
                sc.op("dve", lambda: nc.vector.tensor_copy(s_t[:, 0:4], p[:, 0:4]), reads=[p], writes=[s_t], nowaw=True)
                p = tm(C_CV, 512)
                sc.op("dve", lambda: nc.vector.tensor_copy(v_t[:, 192:704], p[:, :]), reads=[p], writes=[v_t], nowaw=True)
                sc.dma("sp", VV[tok0:tok0 + 128, :], v_t[:], reads=[v_t], writes=[VV], track=v_t)
                sc.dma("sp", SM[tok0:tok0 + 128, :], s_t[:], reads=[s_t], writes=[SM], track=s_t)
                for g in range(6):
                    p = tm(C_MG + g * 512, 512)
                    sc.op("act", lambda: nc.scalar.activation(out=m_t[:, g * 512:(g + 1) * 512], in_=p[:, :], func=AF.Sigmoid),
                          reads=[p], writes=[m_t], nowaw=(g > 0))
                sc.dma("sp", MG[tok0:tok0 + 128, :], m_t[:], reads=[m_t], writes=[MG], track=m_t)
        allb = [wb] + xs + hT + fst + vst + sst + mst + ps
        sc.barrier_on(allb)
        sched_release(sc, allb)


def rel_bucket_np(dist):
    n = np.maximum(dist, 0)
    nf = np.maximum(n, 1).astype(np.float32)
    large = 16 + (np.log(nf / 16) / np.float32(math.log(128 / 16)) * 16).astype(np.int32)
    large = np.minimum(large, 31)
    return np.where(n < 16, n, large)


def make_consts():
    import ml_dtypes
    c = {}
    r = np.arange(128)[:, None]
    s = np.arange(256)[None, :]
    dist = r + 128 - s
    bk = rel_bucket_np(dist)
    oh = np.zeros((128, 256, 32), np.float32)
    oh[np.arange(128)[:, None], np.arange(256)[None, :], bk] = 1.0
    c["c_oh"] = oh.reshape(128, 256 * 32).astype(ml_dtypes.bfloat16)
    c["c_cneg"] = np.where(dist >= 0, 0.0, NEG).astype(np.float32)
    cm = np.where(np.arange(128)[None, :] <= np.arange(128)[:, None], 0.0, -1e30).astype(np.float32)
    c["c_cm"] = cm
    cc = np.arange(384)[None, :]
    c["c_wneg"] = np.where(cc > r, 0.0, NEG).astype(np.float32)
    c["c_iota"] = np.broadcast_to(np.arange(512, dtype=np.float32)[None, :], (128, 512)).copy()
    qi = np.arange(32)[None, :]
    c["c_tc"] = ((r + qi * 128 - 31) / 16.0).astype(np.float32)
    n_cmp, n_slc = 255, 64
    start = np.arange(n_cmp) * 16
    end = start + 32
    bs = np.arange(n_slc) * 64
    ov = ((start[:, None] < bs[None, :] + 64) & (end[:, None] > bs[None, :])).astype(np.float32)
    ovp = np.zeros((256, 64), np.float32)
    ovp[:255] = ov
    c["c_ovl"] = np.ascontiguousarray(ovp.reshape(2, 128, 64).transpose(1, 0, 2)).astype(ml_dtypes.bfloat16)
    sel = np.zeros((32, 128, 192), np.float32)
    blk = np.arange(64)[None, :]
    for q in range(32):
        t = q * 128 + np.arange(128)[:, None]
        cur = t // 64
        forced = (blk == 0) | (blk == cur) | (blk == cur - 1)
        allowed = blk <= cur
        sel[q, :, 0:64] = (allowed & ~forced).astype(np.float32)
        sel[q, :, 64:128] = np.where(allowed, np.where(forced, 1e9 + 1024.0 * blk, 0.0), -1e30)
        sel[q, :, 128:192] = allowed.astype(np.float32)
    c["c_sel"] = sel
    c["c_pow2"] = np.broadcast_to((2.0 ** -np.arange(40, dtype=np.float32))[None, :], (128, 40)).copy()
    c["c_ident"] = np.eye(128, dtype=np.float32)
    c["c_ut"] = (np.arange(128)[:, None] <= np.arange(128)[None, :]).astype(np.float32)
    return c


CONST_SPECS = [("c_oh", [128, 8192], BF16), ("c_cneg", [128, 256], F32), ("c_cm", [128, 128], F32),
               ("c_wneg", [128, 384], F32), ("c_iota", [128, 512], F32), ("c_tc", [128, 32], F32),
               ("c_ovl", [128, 2, 64], BF16), ("c_sel", [32, 128, 192], F32), ("c_pow2", [128, 40], F32),
               ("c_ident", [128, 128], F32), ("c_ut", [128, 128], F32)]
NBIS = 29
TIE_EPS = 2.0 ** -24


def setup_attn_consts(sc, cd, rel_bias, es):
    nc = sc.nc
    K = {}
    def ld(name, shape, dt, src_ap, srcbuf):
        b = sc.sbuf(name, shape, dt, es)
        sc.dma("sp", b[:], src_ap, reads=[srcbuf], writes=[b], track=b)
        return b
    K["cneg"] = ld("cneg", [128, 256], F32, cd["c_cneg"][:, :], cd["c_cneg"])
    K["cm"] = ld("cm", [128, 128], F32, cd["c_cm"][:, :], cd["c_cm"])
    K["wneg"] = ld("wneg", [128, 384], F32, cd["c_wneg"][:, :], cd["c_wneg"])
    K["iota"] = ld("iota", [128, 512], F32, cd["c_iota"][:, :], cd["c_iota"])
    K["tc"] = ld("tc", [128, 32], F32, cd["c_tc"][:, :], cd["c_tc"])
    K["ovl"] = ld("ovl", [128, 2, 64], BF16, cd["c_ovl"][:, :, :], cd["c_ovl"])
    K["pow2"] = ld("pow2", [128, 40], F32, cd["c_pow2"][:, :], cd["c_pow2"])
    K["ident"] = ld("identf", [128, 128], F32, cd["c_ident"][:, :], cd["c_ident"])
    K["tabb"] = ld("tabb", [128, 384], F32,
                   rel_bias.t.rearrange("b h -> (b h)").unsqueeze(0).partition_broadcast(128), rel_bias)
    K["identb"] = sc.sbuf("identb", [128, 128], BF16, es)
    sc.op("dve", lambda: nc.vector.tensor_copy(K["identb"][:], K["ident"][:]), reads=[K["ident"]], writes=[K["identb"]])
    K["near"] = sc.sbuf("near", [128, 12, 256], F32, es)
    with ExitStack() as tes:
        oh = sc.sbuf("oh", [128, 8192], BF16, tes)
        tmp = sc.sbuf("ohtmp", [128, 8192], F32, tes)
        sc.dma("sp", oh[:], cd["c_oh"][:, :], reads=[cd["c_oh"]], writes=[oh], track=oh)
        for hd in range(12):
            tb = K["tabb"][:, :].rearrange("p (b h) -> p h b", h=12)[:, hd, :]
            sc.op("dve", lambda: nc.vector.tensor_tensor(out=tmp[:].rearrange("p (s b) -> p s b", b=32),
                                                        in0=oh[:].rearrange("p (s b) -> p s b", b=32),
                                                        in1=tb.unsqueeze(1).to_broadcast([128, 256, 32]), op=ALU.mult),
                  reads=[oh, K["tabb"]], writes=[tmp])
            sc.op("dve", lambda: nc.vector.tensor_reduce(out=K["near"][:, hd, :], in_=tmp[:].rearrange("p (s b) -> p s b", b=32),
                                                        axis=AX.X, op=ALU.add), reads=[tmp], writes=[K["near"]], nowaw=True)
            sc.op("dve", lambda: nc.vector.scalar_tensor_tensor(out=K["near"][:, hd, :], in0=K["near"][:, hd, :],
                                                               scalar=K["tabb"][:, 31 * 12 + hd:31 * 12 + hd + 1], in1=K["cneg"][:],
                                                               op0=ALU.subtract, op1=ALU.add), reads=[K["near"], K["cneg"], K["tabb"]], writes=[K["near"]])
        sc.barrier_on([oh, tmp])
        sched_release(sc, [oh, tmp])
    return K


def phase_compress(sc, cfg, l, FM, cmp_w1, cmp_w2, cposT, KC, VC):
    nc = sc.nc
    nseq, seq = cfg["nseq"], cfg["seq"]
    ncmp = (seq - 32) // 16 + 1
    with ExitStack() as es:
        w1 = sc.sbuf("cw1", [128, 32, 256], BF16, es)
        w2 = sc.sbuf("cw2", [128, 2, 2, 64], BF16, es)
        posT = sc.sbuf("cposT", [128, 32], F32, es)
        raw = sc.sbuf("craw", [128, seq], BF16, es)
        rawp = sc.sbuf("crawp", [128, 32, 256], BF16, es)
        hidT = [sc.sbuf("chid", [128, 2, 256], BF16, es) for _ in range(2)]
        t1 = sc.sbuf("ct1", [128, 256], F32, es)
        t2 = sc.sbuf("ct2", [128, 256], F32, es)
        ps = [sc.psum("cps", [128, 512], F32, es) for _ in range(2)]
        for j in range(2):
            sc.dma("pool", w1[j * 64:(j + 1) * 64, :, :], cmp_w1[l, j].rearrange("(p d) h -> d p h", d=64),
                   reads=[cmp_w1], writes=[w1], track=w1)
        sc.dma("pool", w2[:], cmp_w2[l].rearrange("j (hc p) d -> p j hc d", p=128), reads=[cmp_w2], writes=[w2], track=w2)
        sc.dma("sp", posT[:], cposT[l], reads=[cposT], writes=[posT], track=posT)
        sc.op("dve", lambda: nc.vector.memset(VC[:], 0.0), writes=[VC])
        sc.op("dve", lambda: nc.vector.memset(KC[:], 0.0), writes=[KC])
        r0 = FM_ROW["kvcr"]
        pi = 0
        for b in range(nseq):
            sc.dma("sp", raw[:], FM[r0:r0 + 128, b * seq:(b + 1) * seq], reads=[FM], writes=[raw], track=raw, nowaw=False)
            r3 = raw[:].rearrange("p (n s) -> p n s", s=16)
            for p in range(32):
                src = r3[:, 0:ncmp, p] if p < 16 else r3[:, 1:ncmp + 1, p - 16]
                sc.op("dve", lambda: nc.vector.tensor_scalar(out=rawp[:, p, :ncmp], in0=src, scalar1=posT[:, p:p + 1],
                                                            scalar2=None, op0=ALU.add),
                      reads=[raw, posT], writes=[rawp], nowaw=(p > 0))
            for j in range(2):
                for hc in range(2):
                    ph = ps[pi % 2]; pi += 1
                    for p in range(32):
                        sc.op("pe", lambda: nc.tensor.matmul(ph[:, :ncmp], lhsT=w1[j * 64:(j + 1) * 64, p, hc * 128:(hc + 1) * 128],
                                                             rhs=rawp[j * 64:(j + 1) * 64, p, :ncmp], start=(p == 0), stop=(p == 31)),
                              reads=[w1, rawp], writes=[ph], nowaw=(p > 0))
                    sc.op("act", lambda: nc.scalar.activation(out=t1[:, :ncmp], in_=ph[:, :ncmp], func=AF.Square), reads=[ph], writes=[t1])
                    sc.op("dve", lambda: nc.vector.tensor_scalar(out=t1[:, :ncmp], in0=t1[:, :ncmp], scalar1=0.044715, scalar2=1.0,
                                                                op0=ALU.mult, op1=ALU.add), reads=[t1], writes=[t1])
                    sc.op("dve", lambda: nc.vector.tensor_tensor(out=t1[:, :ncmp], in0=t1[:, :ncmp], in1=ph[:, :ncmp], op=ALU.mult),
                          reads=[t1, ph], writes=[t1])
                    sc.op("act", lambda: nc.scalar.activation(out=t2[:, :ncmp], in_=t1[:, :ncmp], func=AF.Sigmoid, scale=1.5957691216057308),
                          reads=[t1], writes=[t2])
                    sc.op("dve", lambda: nc.vector.tensor_tensor(out=hidT[j][:, hc, :ncmp], in0=t2[:, :ncmp], in1=ph[:, :ncmp], op=ALU.mult),
                          reads=[t2, ph], writes=[hidT[j]], nowaw=(hc > 0))
            pk = ps[pi % 2]; pi += 1
            for hc in range(2):
                sc.op("pe", lambda: nc.tensor.matmul(pk[:64, :ncmp], lhsT=w2[:, 0, hc, :], rhs=hidT[0][:, hc, :ncmp],
                                                     start=(hc == 0), stop=(hc == 1)), reads=[w2, hidT[0]], writes=[pk], nowaw=(hc > 0))
            sc.op("dve", lambda: nc.vector.tensor_copy(KC[:, b, :ncmp], pk[:64, :ncmp]), reads=[pk], writes=[KC], nowaw=True)
            for nch in range(2):
                n0 = nch * 128
                nsz = min(128, ncmp - n0)
                if nsz <= 0:
                    continue
                pv = ps[pi % 2]; pi += 1
                for hc in range(2):
                    sc.op("pe", lambda: nc.tensor.matmul(pv[:nsz, 0:64], lhsT=hidT[1][:, hc, n0:n0 + nsz], rhs=w2[:, 1, hc, :],
                                                         start=(hc == 0), stop=(hc == 1)), reads=[w2, hidT[1]], writes=[pv], nowaw=(hc > 0))
                sc.op("dve", lambda: nc.vector.tensor_copy(VC[:nsz, b, nch, :], pv[:nsz, 0:64]), reads=[pv], writes=[VC], nowaw=True)
        allb = [w1, w2, posT, raw, rawp, t1, t2] + hidT + ps
        sc.barrier_on(allb)
        sched_release(sc, allb)


def phase_attn(sc, cfg, l, K, FM, VV, SM, KC, VC, csel, diff_lambda, diff_norm_g, lam_init, OABC, qtiles=None):
    nc = sc.nc
    nseq, seq = cfg["nseq"], cfg["seq"]
    nqt = seq // 128
    IDXC = 0.5 * (32 ** -0.5)
    with ExitStack() as es:
        KT = sc.sbuf("KT", [128, 2, seq], BF16, es)
        CK = sc.sbuf("CK", [128, 4, seq], BF16, es)
        VT = sc.sbuf("VT", [128, nqt, VV_W], BF16, es)
        ISC = sc.sbuf("ISC", [128, seq], F32, es)
        JUNK = sc.sbuf("JUNK", [128, seq], BF16, es)
        QI = [sc.sbuf("QI", [128, 4, 128], BF16, es) for _ in range(2)]
        QA = [sc.sbuf("QA", [64, 4, 128], BF16, es) for _ in range(2)]
        QB = [sc.sbuf("QB", [128, 4, 128], BF16, es) for _ in range(2)]
        QC = [sc.sbuf("QC", [128, 4, 128], BF16, es) for _ in range(2)]
        SMt = [sc.sbuf("SMt", [128, 16], F32, es) for _ in range(2)]
        SELt = [sc.sbuf("SELt", [128, 192], F32, es) for _ in range(2)]
        OUT = [sc.sbuf("OUT", [128, 1024], F32, es) for _ in range(1)]
        AW = sc.sbuf("AW", [128, 4], F32, es)
        SGN = sc.sbuf("SGN", [128, 4], F32, es)
        RT = [sc.sbuf("RT", [128, 512], F32, es) for _ in range(2)]
        ST = sc.sbuf("STAT", [128, 64], F32, es)
        W = sc.sbuf("Wb", [128, 40], F32, es)
        CMK = sc.sbuf("CMK", [128, 256], F32, es)
        PCf = sc.sbuf("PCf", [128, 256], F32, es)
        PCm = sc.sbuf("PCm", [128, 4, 256], F32, es)
        PCn = sc.sbuf("PCn", [128, 5, 256], BF16, es)
        PCT = sc.sbuf("PCT", [128, 10, 128], BF16, es)
        CS = sc.sbuf("CS", [128, 8], F32, es)
        OCMP = sc.sbuf("OCMP", [128, 256], F32, es)
        OWIN = sc.sbuf("OWIN", [128, 256], F32, es)
        IMP = sc.sbuf("IMP", [128, 64], F32, es)
        RANK = sc.sbuf("RANK", [128, 64], F32, es)
        BM = sc.sbuf("BM", [128, 64], F32, es)
        PW = sc.sbuf("PW", [128, 640], BF16, es)
        TMPN = [sc.sbuf("TMPN", [128, 384], F32, es) for _ in range(2)]
        WS = sc.sbuf("WS", [128, 8], F32, es)
        P = [sc.sbuf("P", [128, 512], BF16, es) for _ in range(4)]
        PM = [sc.sbuf("PM", [128, 512], BF16, es) for _ in range(4)]
        MAc = sc.sbuf("MAc", [128, 512], BF16, es)
        PT = [sc.sbuf("PT", [128, 5, 128], BF16, es) for _ in range(3)]
        SUMS = sc.sbuf("SUMS", [128, 16, 16], F32, es)
        TOT = sc.sbuf("TOT", [128, 16], F32, es)
        RCP = sc.sbuf("RCP", [128, 16], F32, es)
        LAM = sc.sbuf("LAM", [128, 4], F32, es)
        DL = sc.sbuf("DL", [128, 256], F32, es)
        DNG = sc.sbuf("DNG", [128, 128], F32, es)
        OCt = sc.sbuf("OCt", [128, 128], F32, es)
        OCj = sc.sbuf("OCj", [128, 128], F32, es)
        TAB31 = sc.sbuf("TAB31", [128, 12], F32, es)
        psS = [sc.psum("psS", [128, 512], F32, es) for _ in range(3)]
        psT = [sc.psum("psT", [128, 8, 128], BF16, es) for _ in range(2)]
        psOab = sc.psum("psOab", [128, 512], F32, es)
        psOc = [sc.psum("psOc", [128, 512], F32, es) for _ in range(2)]
        cnt = {"s": 0, "t": 0, "p": 0, "pm": 0, "pt": 0, "rt": 0, "tn": 0, "ev": 0}
        def nxt(lst, key):
            b = lst[cnt[key] % len(lst)]
            cnt[key] += 1
            return b

        sc.op("dve", lambda: nc.vector.tensor_copy(TAB31[:], K["tabb"][:, 31 * 12:32 * 12]), reads=[K["tabb"]], writes=[TAB31])
        sc.dma("sp", DL[:], diff_lambda[l].rearrange("a d -> (a d)").unsqueeze(0).partition_broadcast(128),
               reads=[diff_lambda], writes=[DL], track=DL)
        sc.dma("sp", DNG[:], diff_norm_g[l:l + 1, :].partition_broadcast(128), reads=[diff_norm_g], writes=[DNG], track=DNG)
        sc.op("dve", lambda: nc.vector.tensor_scalar(out=DNG[:], in0=DNG[:], scalar1=float(1.0 - lam_init), scalar2=None, op0=ALU.mult),
              reads=[DNG], writes=[DNG])
        for a in range(2):
            sc.op("dve", lambda: nc.vector.scalar_tensor_tensor(out=OCt[:, 0:64], in0=DL[:, (2 * a) * 64:(2 * a + 1) * 64], scalar=1.0,
                                                               in1=DL[:, (2 * a + 1) * 64:(2 * a + 2) * 64], op0=ALU.mult, op1=ALU.mult,
                                                               accum_out=LAM[:, 2 + a:3 + a]), reads=[DL], writes=[OCt, LAM])
        sc.op("act", lambda: nc.scalar.activation(out=LAM[:, 2:4], in_=LAM[:, 2:4], func=AF.Exp), reads=[LAM], writes=[LAM])
        sc.op("dve", lambda: nc.vector.scalar_tensor_tensor(out=LAM[:, 0:1], in0=LAM[:, 2:3], scalar=float(lam_init), in1=LAM[:, 3:4],
                                                           op0=ALU.add, op1=ALU.subtract), reads=[LAM], writes=[LAM])
        sc.op("dve", lambda: nc.vector.tensor_scalar(out=LAM[:, 1:2], in0=LAM[:, 0:1], scalar1=-1.0, scalar2=None, op0=ALU.mult),
              reads=[LAM], writes=[LAM])
        sc.op("dve", lambda: nc.vector.memset(PCn[:], 0.0), writes=[PCn])

        qn = 0
        for b in range(nseq):
            c0 = b * seq
            sc.dma("sp", KT[64:96, 1, :], FM[FM_ROW["ik"]:FM_ROW["ik"] + 32, c0:c0 + seq], reads=[FM], writes=[KT], track=KT, nowaw=False)
            for nm, (p0, sl) in (("ak", (0, 0)), ("ks", (64, 0)), ("kw", (0, 1))):
                sc.dma("sp", KT[p0:p0 + 64, sl, :], FM[FM_ROW[nm]:FM_ROW[nm] + 64, c0:c0 + seq], reads=[FM], writes=[KT], track=KT)
            for h in range(4):
                r = FM_ROW["ck%d" % h]
                sc.dma("sp", CK[:, h, :], FM[r:r + 128, c0:c0 + seq], reads=[FM], writes=[CK], track=CK, nowaw=(h > 0))
            for n4 in range(0, nqt, 8):
                sc.dma("sp", VT[:, n4:n4 + 8, :], VV[c0 + n4 * 128:c0 + (n4 + 8) * 128, :].rearrange("(n p) c -> p n c", p=128),
                       reads=[VV], writes=[VT], track=VT, nowaw=(n4 > 0))
            for qi in (qtiles if qtiles is not None else range(nqt)):
                tok0 = c0 + qi * 128
                nk = qi + 1
                Nk = nk * 128
                qI, qA, qB, qC, smt, selt = (x[qn % 2] for x in (QI, QA, QB, QC, SMt, SELt))
                out = OUT[0]
                qn += 1
                r = FM_ROW["iq"]
                sc.dma("sp", qI[64:96, :, :], FM[r:r + 128, tok0:tok0 + 128].rearrange("(h d) q -> d h q", d=32), reads=[FM], writes=[qI], track=qI, nowaw=False)
                r = FM_ROW["aq0"]
                sc.dma("sp", qA[:], FM[r:r + 256, tok0:tok0 + 128].rearrange("(h d) q -> d h q", d=64), reads=[FM], writes=[qA], track=qA, nowaw=False)
                r = FM_ROW["bq0"]
                sc.dma("sp", qB[0:64, :, :], FM[r:r + 256, tok0:tok0 + 128].rearrange("(h d) q -> d h q", d=64), reads=[FM], writes=[qB], track=qB, nowaw=False)
                sc.dma("sp", qB[64:128, :, :], FM[r:r + 256, tok0:tok0 + 128].rearrange("(h d) q -> d h q", d=64), reads=[FM], writes=[qB], track=qB)
                r = FM_ROW["cq0"]
                sc.dma("sp", qC[:], FM[r:r + 512, tok0:tok0 + 128].rearrange("(h r) q -> r h q", r=128), reads=[FM], writes=[qC], track=qC, nowaw=False)
                sc.dma("sp", smt[:], SM[tok0:tok0 + 128, :], reads=[SM], writes=[smt], track=smt, nowaw=False)
                sc.dma("sp", selt[:], csel[qi], reads=[csel], writes=[selt], track=selt, nowaw=False)
                chunks = [(k0, min(512, Nk - k0)) for k0 in range(0, Nk, 512)]

                THR = ST[:, 7:8]
                sc.op("dve", lambda: nc.vector.memset(SUMS[:], 0.0), writes=[SUMS])
                firstO = {"ab": True, 0: True, 1: True}

                def genA():
                    sc.op("act", lambda: nc.scalar.activation(out=AW[:], in_=smt[:, 0:4], func=AF.Abs, scale=IDXC), reads=[smt], writes=[AW])
                    sc.op("act", lambda: nc.scalar.activation(out=SGN[:], in_=smt[:, 0:4], func=AF.Sign), reads=[smt], writes=[SGN])
                    for (k0, w) in chunks:
                        yield
                        for h in range(4):
                            ps = nxt(psS, "s")
                            sc.op("pe", lambda: nc.tensor.matmul(ps[:, :w], lhsT=qI[64:96, h, :], rhs=KT[64:96, 1, k0:k0 + w], start=True, stop=True),
                                  reads=[qI, KT], writes=[ps])
                            rt = nxt(RT, "rt")
                            sc.op("act", lambda: nc.scalar.activation(out=rt[:, :w], in_=ps[:, :w], func=AF.Relu, scale=AW[:, h:h + 1]),
                                  reads=[ps, AW], writes=[rt])
                            if h == 0:
                                sc.op("dve", lambda: nc.vector.tensor_scalar(out=ISC[:, k0:k0 + w], in0=rt[:, :w], scalar1=SGN[:, 0:1], scalar2=None,
                                                                            op0=ALU.mult), reads=[rt, SGN], writes=[ISC], nowaw=True)
                            else:
                                sc.op("dve", lambda: nc.vector.scalar_tensor_tensor(out=ISC[:, k0:k0 + w], in0=rt[:, :w], scalar=SGN[:, h:h + 1],
                                                                                   in1=ISC[:, k0:k0 + w], op0=ALU.mult, op1=ALU.add),
                                      reads=[rt, SGN, ISC], writes=[ISC])
                        rt = nxt(RT, "rt")
                        sc.op("dve", lambda: nc.vector.tensor_scalar(out=rt[:, :w], in0=K["iota"][:, :w], scalar1=float(k0), scalar2=-TIE_EPS,
                                                                    op0=ALU.add, op1=ALU.mult), reads=[K["iota"]], writes=[rt])
                        sc.op("dve", lambda: nc.vector.scalar_tensor_tensor(out=rt[:, :w], in0=ISC[:, k0:k0 + w], scalar=0.0, in1=rt[:, :w],
                                                                           op0=ALU.is_equal, op1=ALU.mult), reads=[ISC, rt], writes=[rt])
                        sc.op("dve", lambda: nc.vector.tensor_tensor(out=ISC[:, k0:k0 + w], in0=ISC[:, k0:k0 + w], in1=rt[:, :w], op=ALU.add),
                              reads=[ISC, rt], writes=[ISC])
                    sc.op("dve", lambda: nc.vector.tensor_tensor(out=ISC[:, Nk - 128:Nk], in0=ISC[:, Nk - 128:Nk], in1=K["cm"][:], op=ALU.add),
                          reads=[ISC, K["cm"]], writes=[ISC])
                    THR = ST[:, 7:8]
                    if qi >= 2:
                        sc.op("dve", lambda: nc.vector.tensor_reduce(out=ST[:, 0:1], in_=ISC[:, :Nk], axis=AX.X, op=ALU.max), reads=[ISC], writes=[ST])
                        sc.op("dve", lambda: nc.vector.tensor_reduce(out=ST[:, 1:2], in_=ISC[:, :Nk - 128], axis=AX.X, op=ALU.min), reads=[ISC], writes=[ST])
                        sc.op("dve", lambda: nc.vector.scalar_tensor_tensor(out=ST[:, 2:3], in0=ST[:, 0:1], scalar=1.0, in1=ST[:, 1:2],
                                                                           op0=ALU.add, op1=ALU.subtract), reads=[ST], writes=[ST])
                        sc.op("dve", lambda: nc.vector.tensor_scalar(out=W[:], in0=K["pow2"][:], scalar1=ST[:, 2:3], scalar2=None, op0=ALU.mult),
                              reads=[K["pow2"], ST], writes=[W])
                        sc.op("dve", lambda: nc.vector.tensor_tensor(out=ST[:, 3:4], in0=ST[:, 1:2], in1=W[:, 1:2], op=ALU.add), reads=[ST, W], writes=[ST])
                        for k in range(1, NBIS + 1):
                            yield
                            sc.op("dve", lambda: nc.vector.tensor_scalar(out=JUNK[:, :Nk], in0=ISC[:, :Nk], scalar1=ST[:, 3:4], scalar2=0.0,
                                                                        op0=ALU.is_ge, op1=ALU.add, accum_out=ST[:, 4:5]),
                                  reads=[ISC, ST], writes=[JUNK, ST])
                            sc.op("dve", lambda: nc.vector.tensor_scalar(out=ST[:, 5:6], in0=ST[:, 4:5], scalar1=255.5, scalar2=0.5,
                                                                        op0=ALU.is_ge, op1=ALU.subtract), reads=[ST], writes=[ST])
                            if k < NBIS:
                                sc.op("dve", lambda: nc.vector.scalar_tensor_tensor(out=ST[:, 3:4], in0=ST[:, 5:6], scalar=W[:, k:k + 1], in1=ST[:, 3:4],
                                                                                   op0=ALU.mult, op1=ALU.add), reads=[ST, W], writes=[ST])
                        sc.op("dve", lambda: nc.vector.tensor_scalar(out=ST[:, 6:7], in0=ST[:, 5:6], scalar1=0.5, scalar2=W[:, NBIS:NBIS + 1],
                                                                    op0=ALU.subtract, op1=ALU.mult), reads=[ST, W], writes=[ST])
                        sc.op("dve", lambda: nc.vector.tensor_tensor(out=THR, in0=ST[:, 3:4], in1=ST[:, 6:7], op=ALU.add), reads=[ST], writes=[ST])
                    else:
                        sc.op("dve", lambda: nc.vector.memset(THR, -1e29), reads=[], writes=[ST])

                    yield

                nch_ = len(chunks)

                def flush_map(pm_):
                    kd, hh, jj, sr, k0_, ntile_, last_ = pm_
                    pt = nxt(psT, "t")
                    for i in range(ntile_):
                        sc.op("pe", lambda: nc.tensor.transpose(pt[:, i, :], sr[:, i * 128:(i + 1) * 128], K["identb"][:]),
                              reads=[sr, K["identb"]], writes=[pt], nowaw=(i > 0))
                    ptb = nxt(PT, "pt")
                    ek = "act" if cnt["ev"] % 2 == 0 else "dve"
                    cnt["ev"] += 1
                    if ek == "act":
                        sc.op("act", lambda: nc.scalar.copy(ptb[:, 0:ntile_, :], pt[:, 0:ntile_, :]), reads=[pt], writes=[ptb])
                    else:
                        sc.op("dve", lambda: nc.vector.tensor_copy(ptb[:, 0:ntile_, :], pt[:, 0:ntile_, :]), reads=[pt], writes=[ptb])
                    for i in range(ntile_):
                        kt = k0_ // 128 + i
                        if kd == "a":
                            ob = psOab
                            o, rhsv, key = psOab[:, hh * 64:(hh + 1) * 64], VT[:, kt, 0:64], "ab"
                        elif kd == "s":
                            ob = psOab
                            o, rhsv, key = psOab[:, 256 + hh * 64:256 + (hh + 1) * 64], VT[:, kt, 64:128], "ab"
                        else:
                            m = hh * 2 + jj
                            ob = psOc[m // 4]
                            o, rhsv, key = ob[:, (m % 4) * 128:(m % 4 + 1) * 128], VT[:, kt, 192 + hh * 128:192 + (hh + 1) * 128], m // 4
                        fst = firstO[key]
                        firstO[key] = False
                        sc.op("pe", lambda: nc.tensor.matmul(o, lhsT=ptb[:, i, :], rhs=rhsv, start=fst, stop=(last_ and i == ntile_ - 1), skip_group_check=True),
                              reads=[ptb, VT], writes=[ob], nowaw=(not fst))

                def stageV(kinds_):
                  pending = []
                  for ci, (k0, w) in enumerate(chunks):
                      ntile = w // 128
                      wf = min(max(Nk - 256 - k0, 0), w)
                      wn = w - wf
                      no = (k0 + wf) - (Nk - 256)
                      if "a" in kinds_:
                          sc.op("dve", lambda: nc.vector.tensor_scalar(out=MAc[:, :w], in0=ISC[:, k0:k0 + w], scalar1=THR, scalar2=None, op0=ALU.is_ge),
                                reads=[ISC, ST], writes=[MAc])
                      maps = [("a", h, 0) for h in range(4)] + [("s", h, 0) for h in range(4)] + [("c", h, j) for h in range(4) for j in range(2)]
                      for mi, (kind, h, j) in enumerate(maps):
                          if kind not in kinds_:
                              continue
                          yield
                          ps = nxt(psS, "s")
                          if kind == "a":
                              lhsT, rhs, hd = qA[:, h, :], KT[0:64, 0, k0:k0 + w], h
                          elif kind == "s":
                              lhsT, rhs, hd = qB[64:128, h, :], KT[64:128, 0, k0:k0 + w], 4 + h
                          else:
                              lhsT, rhs, hd = qC[j * 64:(j + 1) * 64, h, :], CK[j * 64:(j + 1) * 64, h, k0:k0 + w], 8 + h
                          sc.op("pe", lambda: nc.tensor.matmul(ps[:, :w], lhsT=lhsT, rhs=rhs, start=True, stop=True),
                                reads=[qA, qB, qC, KT, CK], writes=[ps])
                          p = nxt(P, "p")
                          accf = SUMS[:, mi, 2 * ci:2 * ci + 1] if kind == "c" else None
                          accn = SUMS[:, mi, 2 * ci + 1:2 * ci + 2] if kind == "c" else None
                          if wf > 0 and not cfg.get("skipfar"):
                              if kind == "c":
                                  sc.op("act", lambda: nc.scalar.activation(out=p[:, :wf], in_=ps[:, :wf], func=AF.Exp, scale=0.125, accum_out=accf),
                                        reads=[ps], writes=[p, SUMS], nowaw=True)
                              else:
                                  sc.op("act", lambda: nc.scalar.activation(out=p[:, :wf], in_=ps[:, :wf], func=AF.Exp, scale=0.125),
                                        reads=[ps], writes=[p], nowaw=True)
                          if wn > 0 and not cfg.get("skipnear"):
                              tn = nxt(TMPN, "tn")
                              sc.op("dve", lambda: nc.vector.scalar_tensor_tensor(out=tn[:, :wn], in0=ps[:, wf:w], scalar=0.125, in1=K["near"][:, hd, no:no + wn],
                                                                                 op0=ALU.mult, op1=ALU.add), reads=[ps, K["near"]], writes=[tn])
                              if kind == "c":
                                  sc.op("act", lambda: nc.scalar.activation(out=p[:, wf:w], in_=tn[:, :wn], func=AF.Exp, accum_out=accn),
                                        reads=[tn], writes=[p, SUMS], nowaw=True)
                              else:
                                  sc.op("act", lambda: nc.scalar.activation(out=p[:, wf:w], in_=tn[:, :wn], func=AF.Exp),
                                        reads=[tn], writes=[p], nowaw=True)
                          if kind == "a":
                              pm = nxt(PM, "pm")
                              sc.op("dve", lambda: nc.vector.scalar_tensor_tensor(out=pm[:, :w], in0=p[:, :w], scalar=1.0, in1=MAc[:, :w], op0=ALU.mult, op1=ALU.mult,
                                                                                 accum_out=SUMS[:, mi, 2 * ci:2 * ci + 1]), reads=[p, MAc], writes=[pm, SUMS], nowaw=True)
                              src = pm
                          elif kind == "s":
                              pm = nxt(PM, "pm")
                              sc.op("dve", lambda: nc.vector.scalar_tensor_tensor(out=pm[:, :w].rearrange("p (j i) -> p j i", i=64),
                                                                                 in0=p[:, :w].rearrange("p (j i) -> p j i", i=64), scalar=1.0,
                                                                                 in1=BM[:, k0 // 64:(k0 + w) // 64].unsqueeze(2).to_broadcast([128, w // 64, 64]),
                                                                                 op0=ALU.mult, op1=ALU.mult, accum_out=SUMS[:, mi, 2 * ci:2 * ci + 1]),
                                    reads=[p, BM], writes=[pm, SUMS], nowaw=True)
                              src = pm
                          else:
                              src = p
                          pending.append((kind, h, j, src, k0, ntile, ci == nch_ - 1))
                          if len(pending) > 2:
                              flush_map(pending.pop(0))
                  while pending:
                      flush_map(pending.pop(0))


                def genB():
                    sc.op("dve", lambda: nc.vector.tensor_scalar(out=CMK[:], in0=K["iota"][:, 0:256], scalar1=K["tc"][:, qi:qi + 1], scalar2=1.0, op0=ALU.is_gt, op1=ALU.subtract),
                          reads=[K["iota"], K["tc"]], writes=[CMK])
                    for h in range(4):
                        yield
                        ps = nxt(psS, "s")
                        sc.op("pe", lambda: nc.tensor.matmul(ps[:, :256], lhsT=qB[0:64, h, :], rhs=KC[:, b, :], start=True, stop=True),
                              reads=[qB, KC], writes=[ps])
                        sc.op("act", lambda: nc.scalar.activation(out=PCf[:, :255], in_=ps[:, :255], func=AF.Exp, scale=0.125), reads=[ps], writes=[PCf])
                        sc.op("dve", lambda: nc.vector.scalar_tensor_tensor(out=PCm[:, h, :255], in0=PCf[:, :255], scalar=-1.0, in1=CMK[:, :255],
                                                                           op0=ALU.mult, op1=ALU.mult, accum_out=CS[:, h:h + 1]),
                              reads=[PCf, CMK], writes=[PCm, CS], nowaw=True)
                    sc.op("dve", lambda: nc.vector.tensor_scalar(out=CS[:, 4:8], in0=CS[:, 0:4], scalar1=1e-30, scalar2=None, op0=ALU.max), reads=[CS], writes=[CS])
                    sc.op("dve", lambda: nc.vector.reciprocal(out=CS[:, 4:8], in_=CS[:, 4:8]), reads=[CS], writes=[CS])
                    for h in range(4):
                        sc.op("dve", lambda: nc.vector.tensor_scalar(out=PCn[:, h, :255], in0=PCm[:, h, :255], scalar1=CS[:, 4 + h:5 + h], scalar2=None,
                                                                    op0=ALU.mult), reads=[PCm, CS], writes=[PCn], nowaw=True)
                        if h == 0:
                            sc.op("dve", lambda: nc.vector.tensor_scalar(out=PCf[:, :255], in0=PCm[:, 0, :255], scalar1=CS[:, 4:5], scalar2=None,
                                                                        op0=ALU.mult), reads=[PCm, CS], writes=[PCf])
                        else:
                            sc.op("dve", lambda: nc.vector.scalar_tensor_tensor(out=PCf[:, :255], in0=PCm[:, h, :255], scalar=CS[:, 4 + h:5 + h],
                                                                               in1=PCf[:, :255], op0=ALU.mult, op1=ALU.add),
                                  reads=[PCm, CS, PCf], writes=[PCf])
                    sc.op("dve", lambda: nc.vector.tensor_copy(PCn[:, 4, :255], PCf[:, :255]), reads=[PCf], writes=[PCn], nowaw=True)
                    for g in range(2):
                        pt = nxt(psT, "t")
                        lo, hi = (0, 8) if g == 0 else (8, 10)
                        for i in range(lo, hi):
                            h, nch = i // 2, i % 2
                            sc.op("pe", lambda: nc.tensor.transpose(pt[:, i - lo, :], PCn[:, h, nch * 128:(nch + 1) * 128], K["identb"][:]),
                                  reads=[PCn, K["identb"]], writes=[pt], nowaw=(i > lo))
                        sc.op("act", lambda: nc.scalar.copy(PCT[:, lo:hi, :], pt[:, 0:hi - lo, :]), reads=[pt], writes=[PCT], nowaw=True)
                    first = True
                    for h in range(4):
                        for nch in range(2):
                            sc.op("pe", lambda: nc.tensor.matmul(psOab[:, h * 64:(h + 1) * 64], lhsT=PCT[:, h * 2 + nch, :], rhs=VC[:, b, nch, :],
                                                                 start=first, stop=(nch == 1), skip_group_check=True),
                                  reads=[PCT, VC], writes=[psOab], nowaw=(not first))
                            first = False
                    psI = nxt(psS, "s")
                    for nch in range(2):
                        sc.op("pe", lambda: nc.tensor.matmul(psI[:, 0:64], lhsT=PCT[:, 8 + nch, :], rhs=K["ovl"][:, nch, :], start=(nch == 0), stop=(nch == 1)),
                              reads=[PCT, K["ovl"]], writes=[psI], nowaw=(nch > 0))
                    sc.op("act", lambda: nc.scalar.copy(OCMP[:], psOab[:, 0:256]), reads=[psOab], writes=[OCMP])
                    sc.op("dve", lambda: nc.vector.tensor_tensor(out=IMP[:], in0=psI[:, 0:64], in1=selt[:, 0:64], op=ALU.mult), reads=[psI, selt], writes=[IMP])
                    sc.op("dve", lambda: nc.vector.tensor_tensor(out=IMP[:], in0=IMP[:], in1=selt[:, 64:128], op=ALU.add), reads=[IMP, selt], writes=[IMP])
                    J3 = JUNK[:, 0:4096].rearrange("p (j i) -> p j i", i=64)
                    sc.op("dve", lambda: nc.vector.tensor_tensor(out=J3, in0=IMP[:].unsqueeze(1).to_broadcast([128, 64, 64]),
                                                                in1=IMP[:].unsqueeze(2).to_broadcast([128, 64, 64]), op=ALU.is_gt),
                          reads=[IMP], writes=[JUNK])
                    sc.op("dve", lambda: nc.vector.tensor_reduce(out=RANK[:], in_=J3, axis=AX.X, op=ALU.add), reads=[JUNK], writes=[RANK])
                    sc.op("dve", lambda: nc.vector.scalar_tensor_tensor(out=BM[:], in0=RANK[:], scalar=15.5, in1=selt[:, 128:192], op0=ALU.is_lt, op1=ALU.mult),
                          reads=[RANK, selt], writes=[BM])

                    nwin = min(qi, 4) + 1
                    nnear = min(nwin, 2)
                    nfar = nwin - nnear
                    kt0 = qi - (nwin - 1)
                    for h in range(4):
                        yield
                        if nfar > 0:
                            ps = nxt(psS, "s")
                            wf = nfar * 128
                            sc.op("pe", lambda: nc.tensor.matmul(ps[:, :wf], lhsT=qB[0:64, h, :], rhs=KT[0:64, 1, kt0 * 128:kt0 * 128 + wf], start=True, stop=True),
                                  reads=[qB, KT], writes=[ps])
                            tn = nxt(TMPN, "tn")
                            sc.op("dve", lambda: nc.vector.scalar_tensor_tensor(out=tn[:, :wf], in0=ps[:, :wf], scalar=0.125, in1=K["wneg"][:, 384 - wf:384],
                                                                               op0=ALU.mult, op1=ALU.add), reads=[ps, K["wneg"]], writes=[tn])
                            sc.op("act", lambda: nc.scalar.activation(out=PW[:, 0:wf], in_=tn[:, :wf], func=AF.Exp,
                                                                      accum_out=WS[:, h:h + 1]), reads=[tn], writes=[PW, WS], nowaw=True)
                        else:
                            wf = 0
                            sc.op("dve", lambda: nc.vector.memset(WS[:, h:h + 1], 0.0), writes=[WS], nowaw=True)
                        ps = nxt(psS, "s")
                        wn = nnear * 128
                        kn0 = (qi - (nnear - 1)) * 128
                        sc.op("pe", lambda: nc.tensor.matmul(ps[:, :wn], lhsT=qB[0:64, h, :], rhs=KT[0:64, 1, kn0:kn0 + wn], start=True, stop=True),
                              reads=[qB, KT], writes=[ps])
                        tn = nxt(TMPN, "tn")
                        sc.op("dve", lambda: nc.vector.scalar_tensor_tensor(out=tn[:, :wn], in0=ps[:, :wn], scalar=0.125, in1=K["near"][:, 4 + h, 256 - wn:256],
                                                                           op0=ALU.mult, op1=ALU.add), reads=[ps, K["near"]], writes=[tn])
                        sc.op("act", lambda: nc.scalar.activation(out=PW[:, wf:wf + wn], in_=tn[:, :wn], func=AF.Exp, accum_out=WS[:, 4 + h:5 + h]),
                              reads=[tn], writes=[PW, WS], nowaw=True)
                        pt = nxt(psT, "t")
                        for i in range(nwin):
                            sc.op("pe", lambda: nc.tensor.transpose(pt[:, i, :], PW[:, i * 128:(i + 1) * 128], K["identb"][:]),
                                  reads=[PW, K["identb"]], writes=[pt], nowaw=(i > 0))
                        ptb = nxt(PT, "pt")
                        sc.op("act", lambda: nc.scalar.copy(ptb[:, 0:nwin, :], pt[:, 0:nwin, :]), reads=[pt], writes=[ptb])
                        for i in range(nwin):
                            fst = (h == 0 and i == 0)
                            sc.op("pe", lambda: nc.tensor.matmul(psOab[:, 256 + h * 64:256 + (h + 1) * 64], lhsT=ptb[:, i, :], rhs=VT[:, kt0 + i, 128:192],
                                                                 start=fst, stop=(i == nwin - 1), skip_group_check=True),
                                  reads=[ptb, VT], writes=[psOab], nowaw=(not fst))
                    sc.op("act", lambda: nc.scalar.copy(OWIN[:], psOab[:, 256:512]), reads=[psOab], writes=[OWIN])
                    sc.op("dve", lambda: nc.vector.tensor_tensor(out=WS[:, 0:4], in0=WS[:, 0:4], in1=WS[:, 4:8], op=ALU.add), reads=[WS], writes=[WS])

                    for _ in stageV("sc"):
                        yield

                gens = [genA(), genB()]
                while gens:
                    for g_ in list(gens):
                        try:
                            next(g_)
                        except StopIteration:
                            gens.remove(g_)
                for _ in stageV("a"):
                    pass

                if cfg.get('prof_q') == (b, qi):
                    sc.mark('q_s6_final')
                sc.op("dve", lambda: nc.vector.tensor_reduce(out=TOT[:], in_=SUMS[:], axis=AX.X, op=ALU.add), reads=[SUMS], writes=[TOT])
                sc.op("dve", lambda: nc.vector.tensor_scalar(out=RCP[:], in0=TOT[:], scalar1=1e-30, scalar2=None, op0=ALU.max), reads=[TOT], writes=[RCP])
                sc.op("dve", lambda: nc.vector.reciprocal(out=RCP[:], in_=RCP[:]), reads=[RCP], writes=[RCP])
                sc.op("dve", lambda: nc.vector.reciprocal(out=WS[:, 4:8], in_=WS[:, 0:4]), reads=[WS], writes=[WS])
                for h in range(4):
                    sc.op("dve", lambda: nc.vector.tensor_scalar(out=out[:, h * 64:(h + 1) * 64], in0=psOab[:, h * 64:(h + 1) * 64], scalar1=RCP[:, h:h + 1],
                                                                scalar2=None, op0=ALU.mult), reads=[psOab, RCP], writes=[out], nowaw=(h > 0))
                for h in range(4):
                    g0, g1, g2 = (smt[:, 4 + 3 * h + k:5 + 3 * h + k] for k in range(3))
                    sc.op("dve", lambda: nc.vector.tensor_scalar(out=ST[:, 8:9], in0=RCP[:, 4 + h:5 + h], scalar1=g1, scalar2=None, op0=ALU.mult), reads=[RCP, smt], writes=[ST])
                    sc.op("dve", lambda: nc.vector.tensor_scalar(out=ST[:, 9:10], in0=WS[:, 4 + h:5 + h], scalar1=g2, scalar2=None, op0=ALU.mult), reads=[WS, smt], writes=[ST])
                    oslice = out[:, 256 + h * 64:256 + (h + 1) * 64]
                    sc.op("dve", lambda: nc.vector.tensor_scalar(out=oslice, in0=OCMP[:, h * 64:(h + 1) * 64], scalar1=g0, scalar2=None, op0=ALU.mult),
                          reads=[OCMP, smt], writes=[out], nowaw=True)
                    sc.op("dve", lambda: nc.vector.scalar_tensor_tensor(out=oslice, in0=psOab[:, 256 + h * 64:256 + (h + 1) * 64], scalar=ST[:, 8:9], in1=oslice,
                                                                       op0=ALU.mult, op1=ALU.add), reads=[psOab, ST, out], writes=[out])
                    sc.op("dve", lambda: nc.vector.scalar_tensor_tensor(out=oslice, in0=OWIN[:, h * 64:(h + 1) * 64], scalar=ST[:, 9:10], in1=oslice,
                                                                       op0=ALU.mult, op1=ALU.add), reads=[OWIN, ST, out], writes=[out])
                for h in range(4):
                    m0, m1 = 8 + h * 2, 9 + h * 2
                    ob0, ob1 = psOc[(h * 2) // 4], psOc[(h * 2 + 1) // 4]
                    o0 = ob0[:, ((h * 2) % 4) * 128:((h * 2) % 4 + 1) * 128]
                    o1 = ob1[:, ((h * 2 + 1) % 4) * 128:((h * 2 + 1) % 4 + 1) * 128]
                    sc.op("dve", lambda: nc.vector.tensor_scalar(out=ST[:, 10:11], in0=RCP[:, m1:m1 + 1], scalar1=LAM[:, 1:2], scalar2=None, op0=ALU.mult),
                          reads=[RCP, LAM], writes=[ST])
                    sc.op("dve", lambda: nc.vector.tensor_scalar(out=OCt[:], in0=o0, scalar1=RCP[:, m0:m0 + 1], scalar2=None, op0=ALU.mult),
                          reads=[ob0, RCP], writes=[OCt])
                    sc.op("dve", lambda: nc.vector.scalar_tensor_tensor(out=OCt[:], in0=o1, scalar=ST[:, 10:11], in1=OCt[:], op0=ALU.mult, op1=ALU.add),
                          reads=[ob1, ST, OCt], writes=[OCt])
                    sc.op("dve", lambda: nc.vector.scalar_tensor_tensor(out=OCj[:], in0=OCt[:], scalar=1.0, in1=OCt[:], op0=ALU.mult, op1=ALU.mult,
                                                                       accum_out=ST[:, 11:12]), reads=[OCt], writes=[OCj, ST])
                    sc.op("dve", lambda: nc.vector.tensor_scalar(out=ST[:, 12:13], in0=ST[:, 11:12], scalar1=1.0 / 128, scalar2=1e-5, op0=ALU.mult, op1=ALU.add),
                          reads=[ST], writes=[ST])
                    sc.op("act", lambda: nc.scalar.activation(out=ST[:, 13:14], in_=ST[:, 12:13], func=AF.Ln), reads=[ST], writes=[ST])
                    sc.op("act", lambda: nc.scalar.activation(out=ST[:, 14:15], in_=ST[:, 13:14], func=AF.Exp, scale=-0.5), reads=[ST], writes=[ST])
                    sc.op("dve", lambda: nc.vector.scalar_tensor_tensor(out=out[:, 512 + h * 128:512 + (h + 1) * 128], in0=OCt[:], scalar=ST[:, 14:15], in1=DNG[:],
                                                                       op0=ALU.mult, op1=ALU.mult), reads=[OCt, ST, DNG], writes=[out], nowaw=True)
                sc.dma("sp", OABC[tok0:tok0 + 128, :], out[:], reads=[out], writes=[OABC], track=out)
                if cfg.get('prof_q') == (b, qi):
                    sc.mark('L%d_phase_attn_rest' % l)
        allb = ([KT, CK, VT, ISC, JUNK, AW, SGN, ST, W, CMK, PCf, PCm, PCn, PCT, CS, OCMP, OWIN, IMP, RANK, BM, PW, WS, MAc, SUMS, TOT, RCP, LAM, DL, DNG,
                 OCt, OCj, TAB31, psOab] + QI + QA + QB + QC + SMt + SELt + OUT + RT + TMPN + P + PM + PT + psS + psT + psOc)
        sc.barrier_on(allb)
        sched_release(sc, allb)


def layer_norm_tile(sc, z, zc, junk, o, ST2, Gt, Bt):
    nc = sc.nc
    sc.op("dve", lambda: nc.vector.tensor_reduce(out=ST2[:, 0:1], in_=z[:], axis=AX.X, op=ALU.add), reads=[z], writes=[ST2])
    sc.op("dve", lambda: nc.vector.tensor_scalar(out=ST2[:, 1:2], in0=ST2[:, 0:1], scalar1=1.0 / D, scalar2=None, op0=ALU.mult), reads=[ST2], writes=[ST2])
    sc.op("dve", lambda: nc.vector.tensor_scalar(out=zc[:], in0=z[:], scalar1=ST2[:, 1:2], scalar2=None, op0=ALU.subtract), reads=[z, ST2], writes=[zc])
    sc.op("dve", lambda: nc.vector.scalar_tensor_tensor(out=junk[:], in0=zc[:], scalar=1.0, in1=zc[:], op0=ALU.mult, op1=ALU.mult,
                                                       accum_out=ST2[:, 2:3]), reads=[zc], writes=[junk, ST2])
    sc.op("dve", lambda: nc.vector.tensor_scalar(out=ST2[:, 3:4], in0=ST2[:, 2:3], scalar1=1.0 / D, scalar2=1e-5, op0=ALU.mult, op1=ALU.add),
          reads=[ST2], writes=[ST2])
    sc.op("act", lambda: nc.scalar.activation(out=ST2[:, 4:5], in_=ST2[:, 3:4], func=AF.Ln), reads=[ST2], writes=[ST2])
    sc.op("act", lambda: nc.scalar.activation(out=ST2[:, 5:6], in_=ST2[:, 4:5], func=AF.Exp, scale=-0.5), reads=[ST2], writes=[ST2])
    sc.op("dve", lambda: nc.vector.scalar_tensor_tensor(out=zc[:], in0=zc[:], scalar=ST2[:, 5:6], in1=Gt[:], op0=ALU.mult, op1=ALU.mult),
          reads=[zc, ST2, Gt], writes=[zc])
    sc.op("pool", lambda: nc.gpsimd.tensor_tensor(out=o[:], in0=zc[:], in1=Bt[:], op=ALU.add), reads=[zc, Bt], writes=[o])


def phase_tail(sc, cfg, l, K, OABC, MG, xsrc, wba_d, wbb_d, wbc_d, wo_d, ln_g, ln_b, G1P, X1):
    nc = sc.nc
    nseq, seq = cfg["nseq"], cfg["seq"]
    ntok = nseq * seq
    DN_ALPHA = (2 * DEPTH) ** 0.25
    with ExitStack() as es:
        wb = sc.sbuf("wbr", [128, 8, 1024], BF16, es)
        wo = sc.sbuf("wo", [128, 8, 1024], BF16, es)
        sc.dma("pool", wb[:, 0:2, :], wba_d[l].rearrange("(c p) n -> p c n", p=128), reads=[wba_d], writes=[wb], track=wb)
        sc.dma("pool", wb[:, 2:4, :], wbb_d[l].rearrange("(c p) n -> p c n", p=128), reads=[wbb_d], writes=[wb], track=wb)
        sc.dma("pool", wb[:, 4:8, :], wbc_d[l].rearrange("(c p) n -> p c n", p=128), reads=[wbc_d], writes=[wb], track=wb)
        sc.dma("pool", wo[:], wo_d[l].rearrange("(c p) n -> p c n", p=128), reads=[wo_d], writes=[wo], track=wo)
        Gt = sc.sbuf("lnG", [128, 1024], F32, es)
        Bt = sc.sbuf("lnB", [128, 1024], F32, es)
        sc.dma("sp", Gt[:], ln_g[l:l + 1, :].partition_broadcast(128), reads=[ln_g], writes=[Gt], track=Gt)
        sc.dma("sp", Bt[:], ln_b[l:l + 1, :].partition_broadcast(128), reads=[ln_b], writes=[Bt], track=Bt)
        OA = [sc.sbuf("tOA", [128, 1024], F32, es) for _ in range(2)]
        MGt = [sc.sbuf("tMG", [128, 3072], F32, es) for _ in range(2)]
        XT = [sc.sbuf("tX", [128, 1024], F32, es) for _ in range(2)]
        OB_2 = [sc.sbuf("tOB", [128, 1024], BF16, es) for _ in range(2)]
        oT_2 = [sc.sbuf("toT", [128, 8, 128], BF16, es) for _ in range(2)]
        M_2 = [sc.sbuf("tM", [128, 1024], F32, es) for _ in range(2)]
        TMP_2 = [sc.sbuf("tTMP", [128, 1024], F32, es) for _ in range(2)]
        MB_2 = [sc.sbuf("tMB", [128, 1024], BF16, es) for _ in range(2)]
        mT_2 = [sc.sbuf("tmT", [128, 8, 128], BF16, es) for _ in range(2)]
        Z_2 = [sc.sbuf("tZ", [128, 1024], F32, es) for _ in range(2)]
        ZC_2 = [sc.sbuf("tZC", [128, 1024], F32, es) for _ in range(2)]
        O = [sc.sbuf("tO", [128, 1024], F32, es) for _ in range(2)]
        ST2_2 = [sc.sbuf("tST", [128, 8], F32, es) for _ in range(2)]
        psT = [sc.psum("tpsT", [128, 8, 128], BF16, es) for _ in range(2)]
        psY = [sc.psum("tpsY", [128, 512], F32, es) for _ in range(6)]
        yi = 0

        def tail_loads(t_):
            k0_ = t_ * 128
            sc.dma("sp", OA[t_ % 2][:], OABC[k0_:k0_ + 128, :], reads=[OABC], writes=[OA[t_ % 2]], track=OA[t_ % 2], nowaw=False)
            sc.dma("sp", MGt[t_ % 2][:], MG[k0_:k0_ + 128, :], reads=[MG], writes=[MGt[t_ % 2]], track=MGt[t_ % 2], nowaw=False)
            sc.dma("sp", XT[t_ % 2][:], xsrc[k0_:k0_ + 128, :], reads=[xsrc], writes=[XT[t_ % 2]], track=XT[t_ % 2], nowaw=False)

        for tt in range(ntok // 128):
            OB = OB_2[tt % 2]
            oT = oT_2[tt % 2]
            M = M_2[tt % 2]
            TMP = TMP_2[tt % 2]
            MB = MB_2[tt % 2]
            mT = mT_2[tt % 2]
            Z = Z_2[tt % 2]
            ZC = ZC_2[tt % 2]
            ST2 = ST2_2[tt % 2]
            tok0 = tt * 128
            b = tok0 // seq
            oa, mg, xt, o = OA[tt % 2], MGt[tt % 2], XT[tt % 2], O[tt % 2]
            if tt == 0:
                tail_loads(0)
            if tt + 1 < ntok // 128:
                tail_loads(tt + 1)
            sc.op("act", lambda: nc.scalar.copy(OB[:], oa[:]), reads=[oa], writes=[OB])
            pt = psT[0]
            for c in range(8):
                sc.op("pe", lambda: nc.tensor.transpose(pt[:, c, :], OB[:, c * 128:(c + 1) * 128], K["identb"][:]), reads=[OB, K["identb"]], writes=[pt], nowaw=(c > 0))
            sc.op("act", lambda: nc.scalar.copy(oT[:], pt[:]), reads=[pt], writes=[oT])
            for hf in range(2):
                cs = slice(hf * 512, (hf + 1) * 512)
                ys = []
                for (k0, k1) in ((0, 2), (2, 4), (4, 8)):
                    py = psY[yi % 6]; yi += 1
                    for kc in range(k0, k1):
                        sc.op("pe", lambda: nc.tensor.matmul(py[:, :], lhsT=oT[:, kc, :], rhs=wb[:, kc, cs], start=(kc == k0), stop=(kc == k1 - 1)),
                              reads=[oT, wb], writes=[py], nowaw=(kc > k0))
                    ys.append(py)
                sc.op("dve", lambda: nc.vector.tensor_tensor(out=M[:, cs], in0=ys[0][:, :], in1=mg[:, hf * 512:(hf + 1) * 512], op=ALU.mult),
                      reads=[ys[0], mg], writes=[M], nowaw=(hf > 0))
                sc.op("dve", lambda: nc.vector.tensor_tensor(out=TMP[:, cs], in0=ys[1][:, :], in1=mg[:, 1024 + hf * 512:1024 + (hf + 1) * 512], op=ALU.mult),
                      reads=[ys[1], mg], writes=[TMP], nowaw=(hf > 0))
                sc.op("pool", lambda: nc.gpsimd.tensor_tensor(out=M[:, cs], in0=M[:, cs], in1=TMP[:, cs], op=ALU.add), reads=[M, TMP], writes=[M])
                sc.op("dve", lambda: nc.vector.tensor_tensor(out=TMP[:, cs], in0=ys[2][:, :], in1=mg[:, 2048 + hf * 512:2048 + (hf + 1) * 512], op=ALU.mult),
                      reads=[ys[2], mg], writes=[TMP])
                sc.op("pool", lambda: nc.gpsimd.tensor_tensor(out=MB[:, cs], in0=M[:, cs], in1=TMP[:, cs], op=ALU.add), reads=[M, TMP], writes=[MB], nowaw=(hf > 0))
            pt = psT[1]
            for c in range(8):
                sc.op("pe", lambda: nc.tensor.transpose(pt[:, c, :], MB[:, c * 128:(c + 1) * 128], K["identb"][:]), reads=[MB, K["identb"]], writes=[pt], nowaw=(c > 0))
            sc.op("act", lambda: nc.scalar.copy(mT[:], pt[:]), reads=[pt], writes=[mT])
            for hf in range(2):
                cs = slice(hf * 512, (hf + 1) * 512)
                py = psY[yi % 6]; yi += 1
                for kc in range(8):
                    sc.op("pe", lambda: nc.tensor.matmul(py[:, :], lhsT=mT[:, kc, :], rhs=wo[:, kc, cs], start=(kc == 0), stop=(kc == 7)),
                          reads=[mT, wo], writes=[py], nowaw=(kc > 0))
                sc.op("dve", lambda: nc.vector.tensor_tensor(out=TMP[:, cs], in0=py[:, :], in1=G1P[:, b, cs], op=ALU.mult), reads=[py, G1P], writes=[TMP])
                sc.op("dve", lambda: nc.vector.scalar_tensor_tensor(out=Z[:, cs], in0=xt[:, cs], scalar=DN_ALPHA, in1=TMP[:, cs], op0=ALU.mult, op1=ALU.add),
                      reads=[xt, TMP], writes=[Z], nowaw=(hf > 0))
            layer_norm_tile(sc, Z, ZC, TMP, o, ST2, Gt, Bt)
            sc.dma("pool", X1[tok0:tok0 + 128, :], o[:], reads=[o], writes=[X1], track=o)
        allb = [wb, wo, Gt, Bt, OB_2[0], OB_2[1], oT_2[0], oT_2[1], M_2[0], M_2[1], TMP_2[0], TMP_2[1], MB_2[0], MB_2[1], mT_2[0], mT_2[1], Z_2[0], Z_2[1], ZC_2[0], ZC_2[1], ST2_2[0], ST2_2[1]] + OA + MGt + XT + O + psT + psY
        sc.barrier_on(allb)
        sched_release(sc, allb)


def phase_moe_prep(sc, cfg, l, K, X1, opsc, shf, router_w, router_b, b_down, H2T, CWD, YACC):
    nc = sc.nc
    nseq, seq = cfg["nseq"], cfg["seq"]
    ntok = nseq * seq
    with ExitStack() as es:
        rw = sc.sbuf("rw", [128, 8, 32], F32, es)
        rb = sc.sbuf("rb", [128, 32], F32, es)
        bd = sc.sbuf("bd", [32, 1024], F32, es)
        sc.dma("sp", rw[:], router_w[l].rearrange("(c p) e -> p c e", p=128), reads=[router_w], writes=[rw], track=rw)
        sc.dma("sp", rb[:], router_b[l:l + 1, :].partition_broadcast(128), reads=[router_b], writes=[rb], track=rb)
        sc.dma("sp", bd[:], b_down[l], reads=[b_down], writes=[bd], track=bd)
        XT = [sc.sbuf("mX", [128, 1024], F32, es) for _ in range(2)]
        HF = sc.sbuf("mHF", [128, 8, 128], F32, es)
        HB = [sc.sbuf("mHB", [128, 8, 128], BF16, es) for _ in range(2)]
        Lg = sc.sbuf("mL", [128, 32], F32, es)
        J3 = sc.sbuf("mJ3", [128, 32, 32], F32, es)
        RK = sc.sbuf("mRK", [128, 32], F32, es)
        EX = sc.sbuf("mEX", [128, 32], F32, es)
        CW = [sc.sbuf("mCW", [128, 32], F32, es) for _ in range(2)]
        CWT = sc.sbuf("mCWT", [32, 128], F32, es)
        YB = [sc.sbuf("mYB", [128, 1024], F32, es) for _ in range(2)]
        ST = sc.sbuf("mST", [128, 8], F32, es)
        ps = [sc.psum("mps", [128, 512], F32, es) for _ in range(6)]
        pi = 0
        for tt in range(ntok // 128):
            tok0 = tt * 128
            b = tok0 // seq
            xt, hb, cw, yb = XT[tt % 2], HB[tt % 2], CW[tt % 2], YB[tt % 2]
            sc.dma("sp", xt[:], X1[tok0:tok0 + 128, :], reads=[X1], writes=[xt], track=xt, nowaw=False)
            for g in range(2):
                p = ps[pi % 6]; pi += 1
                for c4 in range(4):
                    c = g * 4 + c4
                    sc.op("pe", lambda: nc.tensor.transpose(p[:, c4 * 128:(c4 + 1) * 128], xt[:, c * 128:(c + 1) * 128], K["ident"][:]),
                          reads=[xt, K["ident"]], writes=[p], nowaw=(c4 > 0))
                for c4 in range(4):
                    c = g * 4 + c4
                    sc.op("act", lambda: nc.scalar.activation(out=HF[:, c, :], in_=p[:, c4 * 128:(c4 + 1) * 128], func=AF.Identity,
                                                              bias=shf[:, c, b:b + 1], scale=opsc[:, c, b:b + 1]),
                          reads=[p, shf, opsc], writes=[HF], nowaw=(c > 0))
            sc.op("dve", lambda: nc.vector.tensor_copy(hb[:], HF[:]), reads=[HF], writes=[hb])
            sc.dma("sp", H2T[:, tok0:tok0 + 128].rearrange("(c p) t -> p c t", p=128), hb[:], reads=[hb], writes=[H2T], track=hb)
            p = ps[pi % 6]; pi += 1
            for c in range(8):
                sc.op("pe", lambda: nc.tensor.matmul(p[:, 0:32], lhsT=HF[:, c, :], rhs=rw[:, c, :], start=(c == 0), stop=(c == 7)),
                      reads=[HF, rw], writes=[p], nowaw=(c > 0))
            sc.op("dve", lambda: nc.vector.tensor_tensor(out=Lg[:], in0=p[:, 0:32], in1=rb[:], op=ALU.add), reads=[p, rb], writes=[Lg])
            sc.op("dve", lambda: nc.vector.tensor_tensor(out=J3[:], in0=Lg[:].unsqueeze(1).to_broadcast([128, 32, 32]),
                                                        in1=Lg[:].unsqueeze(2).to_broadcast([128, 32, 32]), op=ALU.is_gt), reads=[Lg], writes=[J3])
            sc.op("dve", lambda: nc.vector.tensor_reduce(out=RK[:], in_=J3[:], axis=AX.X, op=ALU.add), reads=[J3], writes=[RK])
            sc.op("dve", lambda: nc.vector.tensor_reduce(out=ST[:, 0:1], in_=Lg[:], axis=AX.X, op=ALU.max), reads=[Lg], writes=[ST])
            sc.op("dve", lambda: nc.vector.tensor_scalar(out=ST[:, 1:2], in0=ST[:, 0:1], scalar1=-1.0, scalar2=None, op0=ALU.mult), reads=[ST], writes=[ST])
            sc.op("act", lambda: nc.scalar.activation(out=EX[:], in_=Lg[:], func=AF.Exp, bias=ST[:, 1:2]), reads=[Lg, ST], writes=[EX])
            sc.op("dve", lambda: nc.vector.tensor_scalar(out=RK[:], in0=RK[:], scalar1=3.5, scalar2=None, op0=ALU.is_lt), reads=[RK], writes=[RK])
            sc.op("dve", lambda: nc.vector.scalar_tensor_tensor(out=EX[:], in0=EX[:], scalar=1.0, in1=RK[:], op0=ALU.mult, op1=ALU.mult,
                                                               accum_out=ST[:, 2:3]), reads=[EX, RK], writes=[EX, ST])
            sc.op("dve", lambda: nc.vector.reciprocal(out=ST[:, 3:4], in_=ST[:, 2:3]), reads=[ST], writes=[ST])
            sc.op("dve", lambda: nc.vector.tensor_scalar(out=cw[:], in0=EX[:], scalar1=ST[:, 3:4], scalar2=None, op0=ALU.mult), reads=[EX, ST], writes=[cw])
            sc.dma("sp", CWD[tok0:tok0 + 128, :], cw[:], reads=[cw], writes=[CWD], track=cw)
            p = ps[pi % 6]; pi += 1
            sc.op("pe", lambda: nc.tensor.transpose(p[:32, 0:128], cw[:, :], K["ident"][:]), reads=[cw, K["ident"]], writes=[p])
            sc.op("act", lambda: nc.scalar.copy(CWT[:], p[:32, 0:128]), reads=[p], writes=[CWT])
            for hf in range(2):
                p = ps[pi % 6]; pi += 1
                sc.op("pe", lambda: nc.tensor.matmul(p[:, :], lhsT=CWT[:, :], rhs=bd[:, hf * 512:(hf + 1) * 512], start=True, stop=True),
                      reads=[CWT, bd], writes=[p])
                sc.op("act", lambda: nc.scalar.copy(yb[:, hf * 512:(hf + 1) * 512], p[:, :]), reads=[p], writes=[yb], nowaw=(hf > 0))
            sc.dma("sp", YACC[tok0:tok0 + 128, :], yb[:], reads=[yb], writes=[YACC], track=yb)
        allb = [rw, rb, bd, HF, Lg, J3, RK, EX, CWT, ST] + XT + HB + CW + YB + ps
        sc.barrier_on(allb)
        sched_release(sc, allb)


def phase_moe_experts(sc, cfg, l, H2T, CWD, YACC, w_gu, b_guT, w_down):
    nc = sc.nc
    nseq, seq = cfg["nseq"], cfg["seq"]
    ntok = nseq * seq
    CH = min(1024, ntok)
    ntile = CH // 128
    nsb = CH // 512
    ne = cfg.get("n_exp", 32)
    with ExitStack() as es:
        hT = sc.sbuf("eh", [128, 8, CH], BF16, es)
        ACC = sc.sbuf("eACC", [128, ntile, 1024], F32, es)
        CWc = sc.sbuf("eCW", [128, ntile, 32], F32, es)
        Wgu = [sc.sbuf("eWgu", [128, 8, 2048], BF16, es) for _ in range(2)]
        Wd = [sc.sbuf("eWd", [128, 8, 1024], BF16, es) for _ in range(2)]
        BG = [sc.sbuf("eBG", [128, 16], F32, es) for _ in range(2)]
        actT = [sc.sbuf("eact", [128, 8, 512], BF16, es) for _ in range(2)]
        G = [sc.sbuf("eG", [128, 512], F32, es) for _ in range(2)]
        Sg = [sc.sbuf("eS", [128, 512], F32, es) for _ in range(2)]
        U = [sc.sbuf("eU", [128, 512], F32, es) for _ in range(2)]
        psG = [sc.psum("epsG", [128, 512], F32, es) for _ in range(5)]
        psD = [sc.psum("epsD", [128, 512], F32, es) for _ in range(3)]
        gi = di = ai = ti = 0
        for ch in range(ntok // CH):
            t0 = ch * CH
            sc.dma("sp", hT[:], H2T[:, t0:t0 + CH].rearrange("(c p) t -> p c t", p=128), reads=[H2T], writes=[hT], track=hT, nowaw=False)
            sc.dma("sp", ACC[:], YACC[t0:t0 + CH, :].rearrange("(n p) d -> p n d", p=128), reads=[YACC], writes=[ACC], track=ACC, nowaw=False)
            sc.dma("sp", CWc[:], CWD[t0:t0 + CH, :].rearrange("(n p) e -> p n e", p=128), reads=[CWD], writes=[CWc], track=CWc, nowaw=False)
            for e in range(ne):
                k = (ch * ne + e) % 2
                wg, wd, bg = Wgu[k], Wd[k], BG[k]
                for c2 in range(0, 8, 2):
                    sc.dma("pool", wg[:, c2:c2 + 2, :], w_gu[l, e, c2 * 128:(c2 + 2) * 128, :].rearrange("(c p) n -> p c n", p=128),
                           reads=[w_gu], writes=[wg], track=wg, nowaw=(c2 > 0))
                for c4 in range(0, 8, 4):
                    sc.dma("pool", wd[:, c4:c4 + 4, :], w_down[l, e, c4 * 128:(c4 + 4) * 128, :].rearrange("(c p) n -> p c n", p=128),
                           reads=[w_down], writes=[wd], track=wd, nowaw=(c4 > 0))
                sc.dma("sp", bg[:], b_guT[l, e], reads=[b_guT], writes=[bg], track=bg, nowaw=False)
                for sb in range(nsb):
                    at = actT[ai % 2]; ai += 1
                    for fc in range(8):
                        pg = psG[gi % 5]; gi += 1
                        pu = psG[gi % 5]; gi += 1
                        for which, pp in ((0, pg), (1, pu)):
                            for kc in range(8):
                                lhsT = wg[:, kc, :].rearrange("p (f two) -> p two f", two=2)[:, which, fc * 128:(fc + 1) * 128]
                                sc.op("pe", lambda: nc.tensor.matmul(pp[:, :], lhsT=lhsT, rhs=hT[:, kc, sb * 512:(sb + 1) * 512],
                                                                     start=(kc == 0), stop=(kc == 7)), reads=[wg, hT], writes=[pp], nowaw=(kc > 0))
                        g_, s_, u_ = G[ti % 2], Sg[ti % 2], U[ti % 2]
                        ti += 1
                        sc.op("dve", lambda: nc.vector.tensor_scalar(out=g_[:], in0=pg[:, :], scalar1=bg[:, fc:fc + 1], scalar2=7.0, op0=ALU.add, op1=ALU.min),
                              reads=[pg, bg], writes=[g_])
                        sc.op("act", lambda: nc.scalar.activation(out=s_[:], in_=g_[:], func=AF.Sigmoid, scale=1.702), reads=[g_], writes=[s_])
                        sc.op("dve", lambda: nc.vector.tensor_scalar(out=u_[:], in0=pu[:, :], scalar1=bg[:, 8 + fc:9 + fc], scalar2=7.0, op0=ALU.add, op1=ALU.min),
                              reads=[pu, bg], writes=[u_])
                        sc.op("pool", lambda: nc.gpsimd.tensor_scalar(out=u_[:], in0=u_[:], scalar1=-7.0, scalar2=1.0, op0=ALU.max, op1=ALU.add),
                              reads=[u_], writes=[u_])
                        sc.op("pool", lambda: nc.gpsimd.tensor_tensor(out=g_[:], in0=g_[:], in1=s_[:], op=ALU.mult), reads=[g_, s_], writes=[g_])
                        sc.op("pool", lambda: nc.gpsimd.tensor_tensor(out=at[:, fc, :], in0=g_[:], in1=u_[:], op=ALU.mult), reads=[g_, u_], writes=[at], nowaw=(fc > 0))
                    for tl in range(4):
                        tix = sb * 4 + tl
                        for hf in range(2):
                            pd = psD[di % 3]; di += 1
                            for fc in range(8):
                                sc.op("pe", lambda: nc.tensor.matmul(pd[:, :], lhsT=at[:, fc, tl * 128:(tl + 1) * 128], rhs=wd[:, fc, hf * 512:(hf + 1) * 512],
                                                                     start=(fc == 0), stop=(fc == 7)), reads=[at, wd], writes=[pd], nowaw=(fc > 0))
                            sc.op("dve", lambda: nc.vector.scalar_tensor_tensor(out=ACC[:, tix, hf * 512:(hf + 1) * 512], in0=pd[:, :], scalar=CWc[:, tix, e:e + 1],
                                                                               in1=ACC[:, tix, hf * 512:(hf + 1) * 512], op0=ALU.mult, op1=ALU.add),
                                  reads=[pd, CWc, ACC], writes=[ACC])
            sc.dma("sp", YACC[t0:t0 + CH, :].rearrange("(n p) d -> p n d", p=128), ACC[:], reads=[ACC], writes=[YACC], track=ACC)
        allb = [hT, ACC, CWc] + Wgu + Wd + BG + actT + G + Sg + U + psG + psD
        sc.barrier_on(allb)
        sched_release(sc, allb)


def phase_moe_final(sc, cfg, l, X1, YACC, G1P, ln_g, ln_b, XOUT):
    nc = sc.nc
    nseq, seq = cfg["nseq"], cfg["seq"]
    ntok = nseq * seq
    DN_ALPHA = (2 * DEPTH) ** 0.25
    with ExitStack() as es:
        Gt = sc.sbuf("fG", [128, 1024], F32, es)
        Bt = sc.sbuf("fB", [128, 1024], F32, es)
        sc.dma("sp", Gt[:], ln_g[l:l + 1, :].partition_broadcast(128), reads=[ln_g], writes=[Gt], track=Gt)
        sc.dma("sp", Bt[:], ln_b[l:l + 1, :].partition_broadcast(128), reads=[ln_b], writes=[Bt], track=Bt)
        XT = [sc.sbuf("fX", [128, 1024], F32, es) for _ in range(2)]
        YT = [sc.sbuf("fY", [128, 1024], F32, es) for _ in range(2)]
        Z = sc.sbuf("fZ", [128, 1024], F32, es)
        ZC = sc.sbuf("fZC", [128, 1024], F32, es)
        TMP = sc.sbuf("fT", [128, 1024], F32, es)
        O = [sc.sbuf("fO", [128, 1024], F32, es) for _ in range(2)]
        ST2 = sc.sbuf("fST", [128, 8], F32, es)
        for tt in range(ntok // 128):
            tok0 = tt * 128
            b = tok0 // seq
            xt, yt, o = XT[tt % 2], YT[tt % 2], O[tt % 2]
            sc.dma("sp", xt[:], X1[tok0:tok0 + 128, :], reads=[X1], writes=[xt], track=xt, nowaw=False)
            sc.dma("sp", yt[:], YACC[tok0:tok0 + 128, :], reads=[YACC], writes=[yt], track=yt, nowaw=False)
            sc.op("pool", lambda: nc.gpsimd.tensor_tensor(out=TMP[:], in0=yt[:], in1=G1P[:, b, :], op=ALU.mult), reads=[yt, G1P], writes=[TMP])
            sc.op("dve", lambda: nc.vector.scalar_tensor_tensor(out=Z[:], in0=xt[:], scalar=DN_ALPHA, in1=TMP[:], op0=ALU.mult, op1=ALU.add),
                  reads=[xt, TMP], writes=[Z])
            layer_norm_tile(sc, Z, ZC, TMP, o, ST2, Gt, Bt)
            sc.dma("sp", XOUT[tok0:tok0 + 128, :], o[:], reads=[o], writes=[XOUT], track=o)
        allb = [Gt, Bt, Z, ZC, TMP, ST2] + XT + YT + O
        sc.barrier_on(allb)
        sched_release(sc, allb)


W_SPECS = [("rel_bias", [32, 12]), ("mod_attn_w", [DEPTH, D, 3 * D]), ("mod_attn_b", [DEPTH, 3 * D]), ("w_in", [DEPTH, D, D_IN]),
           ("cmp_w1", [DEPTH, 2, 2048, 256]), ("cmp_w2", [DEPTH, 2, 256, 64]), ("diff_lambda", [DEPTH, 4, 64]),
           ("diff_norm_g", [DEPTH, 128]), ("w_branch_a", [DEPTH, 256, D]), ("w_branch_b", [DEPTH, 256, D]),
           ("w_branch_c", [DEPTH, 512, D]), ("w_out", [DEPTH, D, D]), ("ln1_g", [DEPTH, D]), ("ln1_b", [DEPTH, D]),
           ("mod_ffn_w", [DEPTH, D, 3 * D]), ("mod_ffn_b", [DEPTH, 3 * D]), ("router_w", [DEPTH, D, 32]), ("router_b", [DEPTH, 32]),
           ("exp_w_gu", [DEPTH, 32, D, 2 * D]), ("exp_w_down", [DEPTH, 32, D, D]), ("exp_b_down", [DEPTH, 32, D]),
           ("ln2_g", [DEPTH, D]), ("ln2_b", [DEPTH, D]),
           ("modbT", [DEPTH, 2, 128, 24]), ("cposT", [DEPTH, 128, 32]), ("b_guT", [DEPTH, 32, 128, 16])]


def build_program(cfg):
    nc = bass.Bass("TRN2", target_bir_lowering=False)
    nseq, seq = cfg["nseq"], cfg["seq"]
    ntok = nseq * seq
    depth = cfg.get("depth", DEPTH)
    with ExitStack() as es:
        sc = Sched(nc, es)
        sc.prof = bool(cfg.get("prof"))
        x = sc.dram("x", [ntok, D], F32, kind="ExternalInput")
        cT = sc.dram("cT", [128, 8, nseq], F32, kind="ExternalInput")
        Wd_ = {n: sc.dram(n, s, F32, kind="ExternalInput") for n, s in W_SPECS}
        cd = {n: sc.dram(n, s, d, kind="ExternalInput") for n, s, d in CONST_SPECS}
        y = sc.dram("y", [ntok, D], F32, kind="ExternalOutput")
        FM = sc.dram("FM", [FM_ROWS, ntok], BF16)
        VV = sc.dram("VV", [ntok, VV_W], BF16)
        SM = sc.dram("SM", [ntok, 16], F32)
        MG = sc.dram("MG", [ntok, 3072], F32)
        OABC = sc.dram("OABC", [ntok, 1024], F32)
        X1 = sc.dram("X1", [ntok, D], F32)
        XL = sc.dram("XL", [ntok, D], F32)
        H2T = sc.dram("H2T", [D, ntok], BF16)
        CWD = sc.dram("CWD", [ntok, 32], F32)
        YACC = sc.dram("YACC", [ntok, D], F32)
        Cap = moe_capacity(ntok)
        Xg = sc.dram("Xg", [32 * Cap + 128, D], BF16)
        Yg = sc.dram("Yg", [32 * Cap + 128, D], F32)
        DSTD = sc.dram("DSTD", [ntok, 4], I32)
        CWKD = sc.dram("CWKD", [ntok, 4], F32)
        K = setup_attn_consts(sc, cd, Wd_["rel_bias"], es)
        siluT = sc.sbuf("siluT", [128, 8, nseq], F32)
        sc.dma("sp", siluT[:], cT[:, :, :], reads=[cT], writes=[siluT], track=siluT)
        sc.op("act", lambda: nc.scalar.activation(out=siluT[:], in_=siluT[:], func=AF.Silu), reads=[siluT], writes=[siluT])
        opsc = sc.sbuf("opsc", [128, 8, nseq], F32)
        shf = sc.sbuf("shf", [128, 8, nseq], F32)
        G1P = sc.sbuf("G1P", [128, nseq, 1024], F32)
        KC = sc.sbuf("KC", [64, nseq, 256], BF16)
        VC = sc.sbuf("VC", [128, nseq, 2, 64], BF16)
        xin = x
        for l in range(depth):
            lam_init = 0.8 - 0.6 * math.exp(-0.3 * l)
            xout = y if l == depth - 1 else XL
            sc.mark("L%d_phase_adaln" % l)
            phase_adaln(sc, cfg, l, Wd_["mod_attn_w"], Wd_["modbT"], Wd_["mod_attn_b"], 0, siluT, opsc, shf, None, "fm")
            sc.mark("L%d_phase_proj" % l)
            phase_proj(sc, cfg, l, xin, Wd_["w_in"], K["ident"], opsc, shf, FM, VV, SM, MG)
            sc.mark("L%d_phase_compress" % l)
            phase_compress(sc, cfg, l, FM, Wd_["cmp_w1"], Wd_["cmp_w2"], Wd_["cposT"], KC, VC)
            sc.mark("L%d_phase_attn" % l)
            phase_attn(sc, cfg, l, K, FM, VV, SM, KC, VC, cd["c_sel"], Wd_["diff_lambda"], Wd_["diff_norm_g"], lam_init, OABC)
            sc.mark("L%d_phase_adaln" % l)
            phase_adaln(sc, cfg, l, Wd_["mod_attn_w"], Wd_["modbT"], Wd_["mod_attn_b"], 0, siluT, None, None, G1P, "gate")
            sc.mark("L%d_phase_tail" % l)
            phase_tail(sc, cfg, l, K, OABC, MG, xin, Wd_["w_branch_a"], Wd_["w_branch_b"], Wd_["w_branch_c"], Wd_["w_out"],
                       Wd_["ln1_g"], Wd_["ln1_b"], G1P, X1)
            sc.mark("L%d_phase_adaln" % l)
            phase_adaln(sc, cfg, l, Wd_["mod_ffn_w"], Wd_["modbT"], Wd_["mod_ffn_b"], 1, siluT, opsc, shf, None, "fm")
            if cfg.get("dense_moe"):
                sc.mark("L%d_phase_moe_prep" % l)
                phase_moe_prep(sc, cfg, l, K, X1, opsc, shf, Wd_["router_w"], Wd_["router_b"], Wd_["exp_b_down"], H2T, CWD, YACC)
                sc.mark("L%d_phase_moe_experts" % l)
                phase_moe_experts(sc, cfg, l, H2T, CWD, YACC, Wd_["exp_w_gu"], Wd_["b_guT"], Wd_["exp_w_down"])
                sc.mark("L%d_phase_adaln" % l)
                phase_adaln(sc, cfg, l, Wd_["mod_ffn_w"], Wd_["modbT"], Wd_["mod_ffn_b"], 1, siluT, None, None, G1P, "gate")
                sc.mark("L%d_phase_moe_final" % l)
                phase_moe_final(sc, cfg, l, X1, YACC, G1P, Wd_["ln2_g"], Wd_["ln2_b"], xout)
            else:
                with ExitStack() as mes:
                    OPTM = sc.sbuf("OPTM", [128, nseq, 1024], F32, mes)
                    SHTM = sc.sbuf("SHTM", [128, nseq, 1024], F32, mes)
                    sc.mark("L%d_phase_adaln" % l)
                    phase_adaln(sc, cfg, l, Wd_["mod_ffn_w"], Wd_["modbT"], Wd_["mod_ffn_b"], 1, siluT, None, None, OPTM, "gate", tm_third=1, add_one=1.0)
                    sc.mark("L%d_phase_adaln" % l)
                    phase_adaln(sc, cfg, l, Wd_["mod_ffn_w"], Wd_["modbT"], Wd_["mod_ffn_b"], 1, siluT, None, None, SHTM, "gate", tm_third=0, add_one=0.0)
                    sc.mark("L%d_phase_moe_prep2" % l)
                    phase_moe_prep2(sc, cfg, l, K, X1, opsc, shf, OPTM, SHTM, Wd_["router_w"], Wd_["router_b"], Wd_["exp_b_down"],
                                    Xg, DSTD, CWKD, YACC, cd)
                    sc.barrier_on([OPTM, SHTM])
                sc.mark("L%d_phase_moe_experts2" % l)
                phase_moe_experts2(sc, cfg, l, K, Xg, Yg, Wd_["exp_w_gu"], Wd_["b_guT"], Wd_["exp_w_down"])
                sc.mark("L%d_phase_adaln" % l)
                phase_adaln(sc, cfg, l, Wd_["mod_ffn_w"], Wd_["modbT"], Wd_["mod_ffn_b"], 1, siluT, None, None, G1P, "gate")
                sc.mark("L%d_phase_moe_final2" % l)
                phase_moe_final2(sc, cfg, l, X1, YACC, Yg, DSTD, CWKD, G1P, Wd_["ln2_g"], Wd_["ln2_b"], xout)
            xin = xout
        sc.mark("end")
        sc.finish([y])
        cfg["_n_inst"] = sc.n_inst
    return nc


def host_layouts(inp):
    L = DEPTH
    out = {}
    out["modbT"] = np.ascontiguousarray(np.stack([np.stack([inp["mod_attn_b"][l].reshape(24, 128).T,
                                                            inp["mod_ffn_b"][l].reshape(24, 128).T]) for l in range(L)]), dtype=np.float32)
    out["cposT"] = np.ascontiguousarray(np.stack([np.concatenate([inp["cmp_pos"][l, 0].T, inp["cmp_pos"][l, 1].T], 0) for l in range(L)]), dtype=np.float32)
    bg = inp["exp_b_gu"].reshape(L, 32, 8, 128, 2)
    out["b_guT"] = np.ascontiguousarray(bg.transpose(0, 1, 3, 4, 2).reshape(L, 32, 128, 16), dtype=np.float32)
    return out


def run_module(inp, cfg, n_cores):
    nseq, seq = cfg["nseq"], cfg["seq"]
    nc = build_program(cfg)
    shared = {n: np.ascontiguousarray(inp[n], dtype=np.float32) for n, _ in W_SPECS if n in inp}
    shared.update(host_layouts(inp))
    shared.update(make_consts())
    x = np.asarray(inp["x"], dtype=np.float32)
    c = np.asarray(inp["c"], dtype=np.float32)
    in_maps = []
    for core in range(n_cores):
        xs = x[core * nseq:(core + 1) * nseq].reshape(nseq * seq, D)
        cs = c[core * nseq:(core + 1) * nseq]
        cT = np.ascontiguousarray(cs.reshape(nseq, 8, 128).transpose(2, 1, 0))
        m = dict(shared)
        m["x"] = np.ascontiguousarray(xs)
        m["cT"] = cT
        in_maps.append(m)
    res = run_bass_kernel_spmd(nc, in_maps, core_ids=list(range(n_cores)))
    outs = [r["y"].reshape(nseq, seq, D) for r in res.results]
    return np.concatenate(outs, axis=0).astype(np.float32)


def kernel(**inputs):
    cfg = dict(nseq=NSEQ, seq=S)
    return run_module(inputs, cfg, 8)


def moe_capacity(ntok):
    c = (15 * ntok) // 64
    return ((c + 127) // 128) * 128


def phase_moe_prep2(sc, cfg, l, K, X1, opsc, shf, OPTM, SHTM, router_w, router_b, b_down, Xg, DSTD, CWKD, YACC, cd):
    nc = sc.nc
    nseq, seq = cfg["nseq"], cfg["seq"]
    ntok = nseq * seq
    C = moe_capacity(ntok)
    with ExitStack() as es:
        rw = sc.sbuf("rw", [128, 8, 32], F32, es)
        rb = sc.sbuf("rb", [128, 32], F32, es)
        bd = sc.sbuf("bd", [32, 1024], F32, es)
        UT = sc.sbuf("UT", [128, 128], BF16, es)
        ONES = sc.sbuf("ONES", [128, 128], BF16, es)
        EOFF = sc.sbuf("EOFF", [128, 32], F32, es)
        BASE = sc.sbuf("BASE", [128, 32], F32, es)
        sc.dma("sp", rw[:], router_w[l].rearrange("(c p) e -> p c e", p=128), reads=[router_w], writes=[rw], track=rw)
        sc.dma("sp", rb[:], router_b[l:l + 1, :].partition_broadcast(128), reads=[router_b], writes=[rb], track=rb)
        sc.dma("sp", bd[:], b_down[l], reads=[b_down], writes=[bd], track=bd)
        sc.dma("pool", UT[:], cd["c_ut"][:, :], reads=[cd["c_ut"]], writes=[UT], track=UT)
        sc.op("dve", lambda: nc.vector.memset(ONES[:], 1.0), writes=[ONES])
        sc.op("dve", lambda: nc.vector.memset(BASE[:], 0.0), writes=[BASE])
        sc.op("dve", lambda: nc.vector.tensor_scalar(out=EOFF[:], in0=K["iota"][:, 0:32], scalar1=float(C), scalar2=-1.0, op0=ALU.mult, op1=ALU.add),
              reads=[K["iota"]], writes=[EOFF])
        XT = [sc.sbuf("mX", [128, 1024], F32, es) for _ in range(2)]
        HF_2 = [sc.sbuf("mHF", [128, 8, 128], F32, es) for _ in range(2)]
        HT_2 = [sc.sbuf("mHT", [128, 1024], F32, es) for _ in range(2)]
        HBt = [sc.sbuf("mHBt", [128, 1024], BF16, es) for _ in range(2)]
        Lg_2 = [sc.sbuf("mL", [128, 32], F32, es) for _ in range(2)]
        J3_2 = [sc.sbuf("mJ3", [128, 32, 32], F32, es) for _ in range(2)]
        RK_2 = [sc.sbuf("mRK", [128, 32], F32, es) for _ in range(2)]
        Mk_2 = [sc.sbuf("mMk", [128, 32], F32, es) for _ in range(2)]
        Mb_2 = [sc.sbuf("mMb", [128, 32], BF16, es) for _ in range(2)]
        EX_2 = [sc.sbuf("mEX", [128, 32], F32, es) for _ in range(2)]
        CW_2 = [sc.sbuf("mCW", [128, 32], F32, es) for _ in range(2)]
        ROW_2 = [sc.sbuf("mROW", [128, 32], F32, es) for _ in range(2)]
        POS_2 = [sc.sbuf("mPOS", [128, 32], F32, es) for _ in range(2)]
        JK_2 = [sc.sbuf("mJK", [128, 32], F32, es) for _ in range(2)]
        DSTf_2 = [sc.sbuf("mDSTf", [128, 4], F32, es) for _ in range(2)]
        DSTi = [sc.sbuf("mDSTi", [128, 4], I32, es) for _ in range(2)]
        CWK = [sc.sbuf("mCWK", [128, 4], F32, es) for _ in range(2)]
        CWT_2 = [sc.sbuf("mCWT", [32, 128], F32, es) for _ in range(2)]
        YB = [sc.sbuf("mYB", [128, 1024], F32, es) for _ in range(2)]
        ST_2 = [sc.sbuf("mST", [128, 8], F32, es) for _ in range(2)]
        ps = [sc.psum("mps", [128, 512], F32, es) for _ in range(6)]
        pi = 0
        for tt in range(ntok // 128):
            HF = HF_2[tt % 2]
            HT = HT_2[tt % 2]
            Lg = Lg_2[tt % 2]
            J3 = J3_2[tt % 2]
            RK = RK_2[tt % 2]
            Mk = Mk_2[tt % 2]
            Mb = Mb_2[tt % 2]
            EX = EX_2[tt % 2]
            CW = CW_2[tt % 2]
            ROW = ROW_2[tt % 2]
            POS = POS_2[tt % 2]
            JK = JK_2[tt % 2]
            DSTf = DSTf_2[tt % 2]
            CWT = CWT_2[tt % 2]
            ST = ST_2[tt % 2]
            tok0 = tt * 128
            b = tok0 // seq
            xt, hbt, dsti, cwk, yb = XT[tt % 2], HBt[tt % 2], DSTi[tt % 2], CWK[tt % 2], YB[tt % 2]
            if tt == 0:
                sc.dma("sp", XT[0][:], X1[0:128, :], reads=[X1], writes=[XT[0]], track=XT[0], nowaw=False)
            if tt + 1 < ntok // 128:
                sc.dma("sp", XT[(tt + 1) % 2][:], X1[tok0 + 128:tok0 + 256, :], reads=[X1], writes=[XT[(tt + 1) % 2]], track=XT[(tt + 1) % 2], nowaw=False)
            sc.op("pool", lambda: nc.gpsimd.tensor_tensor(out=HT[:], in0=xt[:], in1=OPTM[:, b, :], op=ALU.mult), reads=[xt, OPTM], writes=[HT])
            sc.op("pool", lambda: nc.gpsimd.tensor_tensor(out=hbt[:], in0=HT[:], in1=SHTM[:, b, :], op=ALU.add), reads=[HT, SHTM], writes=[hbt])
            for g in range(2):
                p = ps[pi % 6]; pi += 1
                for c4 in range(4):
                    c = g * 4 + c4
                    sc.op("pe", lambda: nc.tensor.transpose(p[:, c4 * 128:(c4 + 1) * 128], xt[:, c * 128:(c + 1) * 128], K["ident"][:]),
                          reads=[xt, K["ident"]], writes=[p], nowaw=(c4 > 0))
                for c4 in range(4):
                    c = g * 4 + c4
                    sc.op("act", lambda: nc.scalar.activation(out=HF[:, c, :], in_=p[:, c4 * 128:(c4 + 1) * 128], func=AF.Identity,
                                                              bias=shf[:, c, b:b + 1], scale=opsc[:, c, b:b + 1]),
                          reads=[p, shf, opsc], writes=[HF], nowaw=(c > 0))
            p = ps[pi % 6]; pi += 1
            for c in range(8):
                sc.op("pe", lambda: nc.tensor.matmul(p[:, 0:32], lhsT=HF[:, c, :], rhs=rw[:, c, :], start=(c == 0), stop=(c == 7)),
                      reads=[HF, rw], writes=[p], nowaw=(c > 0))
            sc.op("dve", lambda: nc.vector.tensor_tensor(out=Lg[:], in0=p[:, 0:32], in1=rb[:], op=ALU.add), reads=[p, rb], writes=[Lg])
            sc.op("dve", lambda: nc.vector.tensor_tensor(out=J3[:], in0=Lg[:].unsqueeze(1).to_broadcast([128, 32, 32]),
                                                        in1=Lg[:].unsqueeze(2).to_broadcast([128, 32, 32]), op=ALU.is_gt), reads=[Lg], writes=[J3])
            sc.op("dve", lambda: nc.vector.tensor_reduce(out=RK[:], in_=J3[:], axis=AX.X, op=ALU.add), reads=[J3], writes=[RK])
            sc.op("dve", lambda: nc.vector.tensor_reduce(out=ST[:, 0:1], in_=Lg[:], axis=AX.X, op=ALU.max), reads=[Lg], writes=[ST])
            sc.op("dve", lambda: nc.vector.tensor_scalar(out=ST[:, 1:2], in0=ST[:, 0:1], scalar1=-1.0, scalar2=None, op0=ALU.mult), reads=[ST], writes=[ST])
            sc.op("act", lambda: nc.scalar.activation(out=EX[:], in_=Lg[:], func=AF.Exp, bias=ST[:, 1:2]), reads=[Lg, ST], writes=[EX])
            sc.op("dve", lambda: nc.vector.tensor_scalar(out=Mk[:], in0=RK[:], scalar1=3.5, scalar2=None, op0=ALU.is_lt), reads=[RK], writes=[Mk])
            sc.op("dve", lambda: nc.vector.scalar_tensor_tensor(out=EX[:], in0=EX[:], scalar=1.0, in1=Mk[:], op0=ALU.mult, op1=ALU.mult,
                                                               accum_out=ST[:, 2:3]), reads=[EX, Mk], writes=[EX, ST])
            sc.op("dve", lambda: nc.vector.reciprocal(out=ST[:, 3:4], in_=ST[:, 2:3]), reads=[ST], writes=[ST])
            sc.op("dve", lambda: nc.vector.tensor_scalar(out=CW[:], in0=EX[:], scalar1=ST[:, 3:4], scalar2=None, op0=ALU.mult), reads=[EX, ST], writes=[CW])
            sc.op("dve", lambda: nc.vector.tensor_copy(Mb[:], Mk[:]), reads=[Mk], writes=[Mb])
            pc = ps[pi % 6]; pi += 1
            sc.op("pe", lambda: nc.tensor.matmul(pc[:, 0:32], lhsT=UT[:], rhs=Mb[:], start=True, stop=True), reads=[UT, Mb], writes=[pc])
            ptot = ps[pi % 6]; pi += 1
            sc.op("pe", lambda: nc.tensor.matmul(ptot[:, 0:32], lhsT=ONES[:], rhs=Mb[:], start=True, stop=True), reads=[ONES, Mb], writes=[ptot])
            sc.op("dve", lambda: nc.vector.tensor_tensor(out=POS[:], in0=pc[:, 0:32], in1=BASE[:], op=ALU.add), reads=[pc, BASE], writes=[POS])
            sc.op("dve", lambda: nc.vector.tensor_tensor(out=ROW[:], in0=POS[:], in1=EOFF[:], op=ALU.add), reads=[POS, EOFF], writes=[ROW])
            sc.op("dve", lambda: nc.vector.tensor_scalar(out=POS[:], in0=POS[:], scalar1=float(C) + 0.5, scalar2=1e9, op0=ALU.is_gt, op1=ALU.mult),
                  reads=[POS], writes=[POS])
            sc.op("dve", lambda: nc.vector.tensor_tensor(out=ROW[:], in0=ROW[:], in1=POS[:], op=ALU.add), reads=[ROW, POS], writes=[ROW])
            sc.op("dve", lambda: nc.vector.tensor_scalar(out=ROW[:], in0=ROW[:], scalar1=float(32 * C), scalar2=None, op0=ALU.min), reads=[ROW], writes=[ROW])
            sc.op("dve", lambda: nc.vector.tensor_tensor(out=BASE[:], in0=BASE[:], in1=ptot[:, 0:32], op=ALU.add), reads=[BASE, ptot], writes=[BASE])
            for k in range(4):
                sc.op("dve", lambda: nc.vector.scalar_tensor_tensor(out=JK[:], in0=RK[:], scalar=float(k), in1=ROW[:], op0=ALU.is_equal, op1=ALU.mult,
                                                                   accum_out=DSTf[:, k:k + 1]), reads=[RK, ROW], writes=[JK, DSTf])
                sc.op("dve", lambda: nc.vector.scalar_tensor_tensor(out=JK[:], in0=RK[:], scalar=float(k), in1=CW[:], op0=ALU.is_equal, op1=ALU.mult,
                                                                   accum_out=cwk[:, k:k + 1]), reads=[RK, CW], writes=[JK, cwk])
            sc.op("dve", lambda: nc.vector.tensor_copy(dsti[:], DSTf[:]), reads=[DSTf], writes=[dsti])
            sc.dma("sp", DSTD[tok0:tok0 + 128, :], dsti[:], reads=[dsti], writes=[DSTD], track=dsti)
            sc.dma("sp", CWKD[tok0:tok0 + 128, :], cwk[:], reads=[cwk], writes=[CWKD], track=cwk)
            for k in range(4):
                deps_r, deps_w = [hbt, dsti], [Xg]
                d = sc._collect(deps_r, deps_w, True)
                sc._wait("pool", d)
                if hbt.dsem is None:
                    if sc.sempool:
                        hbt.dsem, hbt.dcount = sc.sempool.pop()
                    else:
                        hbt.dsem = sc.es.enter_context(nc.semaphore("d_" + hbt.name))
                ins = nc.gpsimd.indirect_dma_start(out=Xg[:, :], out_offset=bass.IndirectOffsetOnAxis(ap=dsti[:, k:k + 1], axis=0),
                                                   in_=hbt[:, :], in_offset=None)
                ins.then_inc(hbt.dsem, 16)
                hbt.dcount += 1
                for bb in deps_r:
                    bb.rd[hbt] = hbt.dcount
                Xg.wr[hbt] = hbt.dcount
            p = ps[pi % 6]; pi += 1
            sc.op("pe", lambda: nc.tensor.transpose(p[:32, 0:128], CW[:, :], K["ident"][:]), reads=[CW, K["ident"]], writes=[p])
            sc.op("act", lambda: nc.scalar.copy(CWT[:], p[:32, 0:128]), reads=[p], writes=[CWT])
            for hf in range(2):
                p = ps[pi % 6]; pi += 1
                sc.op("pe", lambda: nc.tensor.matmul(p[:, :], lhsT=CWT[:, :], rhs=bd[:, hf * 512:(hf + 1) * 512], start=True, stop=True),
                      reads=[CWT, bd], writes=[p])
                sc.op("act", lambda: nc.scalar.copy(yb[:, hf * 512:(hf + 1) * 512], p[:, :]), reads=[p], writes=[yb], nowaw=(hf > 0))
            sc.dma("sp", YACC[tok0:tok0 + 128, :], yb[:], reads=[yb], writes=[YACC], track=yb)
        allb = [rw, rb, bd, UT, ONES, EOFF, BASE, HF_2[0], HF_2[1], HT_2[0], HT_2[1], Lg_2[0], Lg_2[1], J3_2[0], J3_2[1], RK_2[0], RK_2[1], Mk_2[0], Mk_2[1], Mb_2[0], Mb_2[1], EX_2[0], EX_2[1], CW_2[0], CW_2[1], ROW_2[0], ROW_2[1], POS_2[0], POS_2[1], JK_2[0], JK_2[1], DSTf_2[0], DSTf_2[1], CWT_2[0], CWT_2[1], ST_2[0], ST_2[1]] + XT + HBt + DSTi + CWK + YB + ps
        sc.barrier_on(allb)
        sched_release(sc, allb)


def phase_moe_experts2(sc, cfg, l, K, Xg, Yg, w_gu, b_guT, w_down):
    nc = sc.nc
    nseq, seq = cfg["nseq"], cfg["seq"]
    ntok = nseq * seq
    C = moe_capacity(ntok)
    sbs = [(o, min(4, C // 128 - o)) for o in range(0, C // 128, 4)]
    ne = cfg.get("n_exp", 32)
    with ExitStack() as es:
        Wgu = [sc.sbuf("eWgu", [128, 8, 2048], BF16, es) for _ in range(2)]
        Wd = [sc.sbuf("eWd", [128, 8, 1024], BF16, es) for _ in range(2)]
        BG = [sc.sbuf("eBG", [128, 16], F32, es) for _ in range(2)]
        XR = [sc.sbuf("eXR", [128, 4, 1024], BF16, es) for _ in range(2)]
        XT = [sc.sbuf("eXT", [128, 8, 512], BF16, es) for _ in range(2)]
        actT = [sc.sbuf("eact", [128, 8, 512], BF16, es) for _ in range(2)]
        G = [sc.sbuf("eG", [128, 512], F32, es) for _ in range(2)]
        Sg = [sc.sbuf("eS", [128, 512], F32, es) for _ in range(2)]
        U = [sc.sbuf("eU", [128, 512], F32, es) for _ in range(2)]
        YO = [sc.sbuf("eYO", [128, 1024], F32, es) for _ in range(3)]
        psG = [sc.psum("epsG", [128, 512], F32, es) for _ in range(4)]
        psD = [sc.psum("epsD", [128, 512], F32, es) for _ in range(2)]
        psT = [sc.psum("epsT", [128, 8, 128], BF16, es) for _ in range(2)]
        gi = di = ai = ti = xi = yi = pti = 0
        def load_w(e):
            wg, wd, bg = Wgu[e % 2], Wd[e % 2], BG[e % 2]
            for c2 in range(0, 8, 2):
                sc.dma("pool", wg[:, c2:c2 + 2, :], w_gu[l, e, c2 * 128:(c2 + 2) * 128, :].rearrange("(c p) n -> p c n", p=128),
                       reads=[w_gu], writes=[wg], track=wg, nowaw=(c2 > 0))
            for c4 in range(0, 8, 4):
                sc.dma("pool", wd[:, c4:c4 + 4, :], w_down[l, e, c4 * 128:(c4 + 4) * 128, :].rearrange("(c p) n -> p c n", p=128),
                       reads=[w_down], writes=[wd], track=wd, nowaw=(c4 > 0))
            sc.dma("sp", bg[:], b_guT[l, e], reads=[b_guT], writes=[bg], track=bg, nowaw=False)
        load_w(0)
        for e in range(ne):
            wg, wd, bg = Wgu[e % 2], Wd[e % 2], BG[e % 2]
            if e + 1 < ne:
                load_w(e + 1)
            for (tl0, ntl) in sbs:
                r0 = e * C + tl0 * 128
                wdt = ntl * 128
                xr, xt_, at = XR[xi % 2], XT[xi % 2], actT[xi % 2]
                xi += 1
                sc.dma("sp", xr[:, 0:ntl, :], Xg[r0:r0 + wdt, :].rearrange("(n p) d -> p n d", p=128), reads=[Xg], writes=[xr], track=xr, nowaw=False)
                for tl in range(ntl):
                    pt = psT[pti % 2]; pti += 1
                    for c in range(8):
                        sc.op("pe", lambda: nc.tensor.transpose(pt[:, c, :], xr[:, tl, c * 128:(c + 1) * 128], K["identb"][:]),
                              reads=[xr, K["identb"]], writes=[pt], nowaw=(c > 0))
                    if tl % 2 == 0:
                        sc.op("act", lambda: nc.scalar.copy(xt_[:, :, tl * 128:(tl + 1) * 128], pt[:]), reads=[pt], writes=[xt_], nowaw=(tl > 0))
                    else:
                        sc.op("dve", lambda: nc.vector.tensor_copy(xt_[:, :, tl * 128:(tl + 1) * 128], pt[:]), reads=[pt], writes=[xt_], nowaw=True)
                for fc in range(8):
                    pg = psG[gi % 4]; gi += 1
                    pu = psG[gi % 4]; gi += 1
                    for which, pp in ((0, pg), (1, pu)):
                        for kc in range(8):
                            lhsT = wg[:, kc, :].rearrange("p (f two) -> p two f", two=2)[:, which, fc * 128:(fc + 1) * 128]
                            sc.op("pe", lambda: nc.tensor.matmul(pp[:, :wdt], lhsT=lhsT, rhs=xt_[:, kc, :wdt], start=(kc == 0), stop=(kc == 7)),
                                  reads=[wg, xt_], writes=[pp], nowaw=(kc > 0))
                    g_, s_, u_ = G[ti % 2], Sg[ti % 2], U[ti % 2]
                    ti += 1
                    sc.op("dve", lambda: nc.vector.tensor_scalar(out=g_[:, :wdt], in0=pg[:, :wdt], scalar1=bg[:, fc:fc + 1], scalar2=7.0, op0=ALU.add, op1=ALU.min),
                          reads=[pg, bg], writes=[g_])
                    sc.op("act", lambda: nc.scalar.activation(out=s_[:, :wdt], in_=g_[:, :wdt], func=AF.Sigmoid, scale=1.702), reads=[g_], writes=[s_])
                    sc.op("dve", lambda: nc.vector.tensor_scalar(out=u_[:, :wdt], in0=pu[:, :wdt], scalar1=bg[:, 8 + fc:9 + fc], scalar2=7.0, op0=ALU.add, op1=ALU.min),
                          reads=[pu, bg], writes=[u_])
                    sc.op("dve", lambda: nc.vector.tensor_scalar(out=u_[:, :wdt], in0=u_[:, :wdt], scalar1=-7.0, scalar2=1.0, op0=ALU.max, op1=ALU.add),
                          reads=[u_], writes=[u_])
                    sc.op("dve", lambda: nc.vector.tensor_tensor(out=g_[:, :wdt], in0=g_[:, :wdt], in1=s_[:, :wdt], op=ALU.mult), reads=[g_, s_], writes=[g_])
                    sc.op("dve", lambda: nc.vector.tensor_tensor(out=at[:, fc, :wdt], in0=g_[:, :wdt], in1=u_[:, :wdt], op=ALU.mult), reads=[g_, u_], writes=[at], nowaw=(fc > 0))
                for tl in range(ntl):
                    yo = YO[yi % 3]; yi += 1
                    for hf in range(2):
                        pd = psD[di % 2]; di += 1
                        for fc in range(8):
                            sc.op("pe", lambda: nc.tensor.matmul(pd[:, :], lhsT=at[:, fc, tl * 128:(tl + 1) * 128], rhs=wd[:, fc, hf * 512:(hf + 1) * 512],
                                                                 start=(fc == 0), stop=(fc == 7)), reads=[at, wd], writes=[pd], nowaw=(fc > 0))
                        sc.op("act", lambda: nc.scalar.copy(yo[:, hf * 512:(hf + 1) * 512], pd[:, :]), reads=[pd], writes=[yo], nowaw=(hf > 0))
                    sc.dma("sp", Yg[r0 + tl * 128:r0 + (tl + 1) * 128, :], yo[:], reads=[yo], writes=[Yg], track=yo)
        allb = Wgu + Wd + BG + XR + XT + actT + G + Sg + U + YO + psG + psD + psT
        sc.barrier_on(allb)
        sched_release(sc, allb)


def phase_moe_final2(sc, cfg, l, X1, YACC, Yg, DSTD, CWKD, G1P, ln_g, ln_b, XOUT):
    nc = sc.nc
    nseq, seq = cfg["nseq"], cfg["seq"]
    ntok = nseq * seq
    C = moe_capacity(ntok)
    DN_ALPHA = (2 * DEPTH) ** 0.25
    with ExitStack() as es:
        Gt = sc.sbuf("fG", [128, 1024], F32, es)
        Bt = sc.sbuf("fB", [128, 1024], F32, es)
        sc.dma("sp", Gt[:], ln_g[l:l + 1, :].partition_broadcast(128), reads=[ln_g], writes=[Gt], track=Gt)
        sc.dma("sp", Bt[:], ln_b[l:l + 1, :].partition_broadcast(128), reads=[ln_b], writes=[Bt], track=Bt)
        XT = [sc.sbuf("fX", [128, 1024], F32, es) for _ in range(2)]
        YT = [sc.sbuf("fY", [128, 1024], F32, es) for _ in range(2)]
        YK = [sc.sbuf("fYK", [128, 4, 1024], F32, es) for _ in range(2)]
        DSTi = [sc.sbuf("fDST", [128, 4], I32, es) for _ in range(2)]
        CWK = [sc.sbuf("fCWK", [128, 4], F32, es) for _ in range(2)]
        Z_2 = [sc.sbuf("fZ", [128, 1024], F32, es) for _ in range(2)]
        ZC_2 = [sc.sbuf("fZC", [128, 1024], F32, es) for _ in range(2)]
        TMP_2 = [sc.sbuf("fT", [128, 1024], F32, es) for _ in range(2)]
        O = [sc.sbuf("fO", [128, 1024], F32, es) for _ in range(2)]
        ST2_2 = [sc.sbuf("fST", [128, 8], F32, es) for _ in range(2)]
        for tt in range(ntok // 128):
            Z = Z_2[tt % 2]
            ZC = ZC_2[tt % 2]
            TMP = TMP_2[tt % 2]
            ST2 = ST2_2[tt % 2]
            tok0 = tt * 128
            b = tok0 // seq
            xt, yt, yk, dsti, cwk, o = XT[tt % 2], YT[tt % 2], YK[tt % 2], DSTi[tt % 2], CWK[tt % 2], O[tt % 2]

            def fin_loads(t_):
                k0_ = t_ * 128
                sc.dma("sp", XT[t_ % 2][:], X1[k0_:k0_ + 128, :], reads=[X1], writes=[XT[t_ % 2]], track=XT[t_ % 2], nowaw=False)
                sc.dma("sp", YT[t_ % 2][:], YACC[k0_:k0_ + 128, :], reads=[YACC], writes=[YT[t_ % 2]], track=YT[t_ % 2], nowaw=False)
                sc.dma("sp", DSTi[t_ % 2][:], DSTD[k0_:k0_ + 128, :], reads=[DSTD], writes=[DSTi[t_ % 2]], track=DSTi[t_ % 2], nowaw=False)
                sc.dma("sp", CWK[t_ % 2][:], CWKD[k0_:k0_ + 128, :], reads=[CWKD], writes=[CWK[t_ % 2]], track=CWK[t_ % 2], nowaw=False)
            if tt == 0:
                fin_loads(0)
            if tt + 1 < ntok // 128:
                fin_loads(tt + 1)
            for k in range(4):
                d = sc._collect([Yg, dsti], [yk], k > 0)
                sc._wait("pool", d)
                if yk.dsem is None:
                    if sc.sempool:
                        yk.dsem, yk.dcount = sc.sempool.pop()
                    else:
                        yk.dsem = sc.es.enter_context(nc.semaphore("d_" + yk.name))
                ins = nc.gpsimd.indirect_dma_start(out=yk[:, k, :], out_offset=None, in_=Yg[:, :],
                                                   in_offset=bass.IndirectOffsetOnAxis(ap=dsti[:, k:k + 1], axis=0))
                ins.then_inc(yk.dsem, 16)
                yk.dcount += 1
                for bb in (Yg, dsti):
                    bb.rd[yk] = yk.dcount
                if k == 0:
                    yk.wr = {yk: yk.dcount}
                    yk.rd = {}
                else:
                    yk.wr[yk] = yk.dcount
            for k in range(4):
                sc.op("dve", lambda: nc.vector.scalar_tensor_tensor(out=yt[:], in0=yk[:, k, :], scalar=cwk[:, k:k + 1], in1=yt[:], op0=ALU.mult, op1=ALU.add),
                      reads=[yk, cwk, yt], writes=[yt])
            sc.op("pool", lambda: nc.gpsimd.tensor_tensor(out=TMP[:], in0=yt[:], in1=G1P[:, b, :], op=ALU.mult), reads=[yt, G1P], writes=[TMP])
            sc.op("dve", lambda: nc.vector.scalar_tensor_tensor(out=Z[:], in0=xt[:], scalar=DN_ALPHA, in1=TMP[:], op0=ALU.mult, op1=ALU.add),
                  reads=[xt, TMP], writes=[Z])
            layer_norm_tile(sc, Z, ZC, TMP, o, ST2, Gt, Bt)
            sc.dma("pool", XOUT[tok0:tok0 + 128, :], o[:], reads=[o], writes=[XOUT], track=o)
        allb = [Gt, Bt, Z_2[0], Z_2[1], ZC_2[0], ZC_2[1], TMP_2[0], TMP_2[1], ST2_2[0], ST2_2[1]] + XT + YT + YK + DSTi + CWK + O
        sc.barrier_on(allb)
        sched_release(sc, allb)
```
